# Optimizing a Trainium2 kernel written in Bass

```python
import math
import jax, jax.numpy as jnp
from jax import lax
import numpy as np

D_MODEL = 1024
BATCH = 4
SEQ = 8192
DEPTH = 4

CTX_LEN = 256
GRID_W = 64
ROPE_BASE = 10000.0
EPS = 1e-6

RET_HEADS = 8
RET_DK = 64
RET_DV = 128
RET_CHUNK = 128
ATT_HEADS = 8
ATT_KV_HEADS = 2
ATT_HD = 64
Q_BLOCK = 128
DN_HEADS = 8
DN_DK = 128
DN_DV = 128
DN_CHUNK = 64
DN_CONV = 3
N_GROUPS = 4
EXPERTS_PER_GROUP = 8
N_EXPERTS = N_GROUPS * EXPERTS_PER_GROUP
TOP_K = 2
D_EXPERT = 512

EVEN_SPLITS = (RET_HEADS * RET_DK, RET_HEADS * RET_DK, RET_HEADS * RET_DV, RET_HEADS * RET_DV,
               ATT_HEADS * ATT_HD, ATT_KV_HEADS * ATT_HD, ATT_KV_HEADS * ATT_HD)
EVEN_IN = sum(EVEN_SPLITS)
EVEN_MIX = RET_HEADS * RET_DV + ATT_HEADS * ATT_HD
DN_CONV_CH = 2 * DN_HEADS * DN_DK + DN_HEADS * DN_DV
ODD_SPLITS = (DN_CONV_CH, DN_HEADS * DN_DV, DN_HEADS, DN_HEADS, DN_HEADS, DN_HEADS)
ODD_IN = sum(ODD_SPLITS)

kernel_name = "hybrid_retention_gqa_gdn_hmoe_dit"


def _split(p, sizes):
    idx = np.cumsum(sizes)[:-1].tolist()
    return jnp.split(p, idx, axis=-1)


def _flip(t):
    return jnp.flip(t, axis=2)


def rmsnorm(x, gain):
    xf = x.astype(jnp.float32)
    y = xf * lax.rsqrt(jnp.mean(xf * xf, axis=-1, keepdims=True) + EPS)
    return (y * gain.astype(jnp.float32)).astype(x.dtype)


def l2norm(x):
    xf = x.astype(jnp.float32)
    return xf * lax.rsqrt(jnp.sum(xf * xf, axis=-1, keepdims=True) + EPS)


def modulate(h, shift, scale):
    return h * (1.0 + scale) + shift


def axial_rope(n_tok, head_dim):
    n_rows = n_tok // GRID_W
    rows = jnp.repeat(jnp.arange(n_rows), GRID_W).astype(jnp.float32)
    cols = jnp.tile(jnp.arange(GRID_W), n_rows).astype(jnp.float32)
    nf = head_dim // 4
    inv = ROPE_BASE ** (-jnp.arange(nf, dtype=jnp.float32) / nf)
    ang = jnp.stack([rows[:, None] * inv, cols[:, None] * inv], axis=1)
    return jnp.cos(ang), jnp.sin(ang)


def apply_rope(x, cos, sin):
    B, L, H, hd = x.shape
    nf = hd // 4
    xr = x.astype(jnp.float32).reshape(B, L, H, 2, 2, nf)
    x1, x2 = xr[..., 0, :], xr[..., 1, :]
    c, s = cos[None, :, None], sin[None, :, None]
    out = jnp.stack([x1 * c - x2 * s, x2 * c + x1 * s], axis=-2)
    return out.reshape(B, L, H, hd).astype(x.dtype)


def retention_chunked(q, k, v, log_gamma, s0):
    B, H, L, dk = q.shape
    dv = v.shape[-1]
    C = RET_CHUNK
    n = L // C
    qc = q.reshape(B, H, n, C, dk)
    kc = k.reshape(B, H, n, C, dk)
    vc = v.reshape(B, H, n, C, dv)
    pos = jnp.arange(C, dtype=jnp.float32)
    lg = log_gamma[:, None]
    diff = pos[:, None] - pos[None, :]
    dmask = jnp.where(diff >= 0, jnp.exp(lg[:, :, None] * jnp.maximum(diff, 0.0)), 0.0)
    scores = jnp.einsum('bhnid,bhnjd->bhnij', qc, kc) * dmask[None, :, None]
    intra = jnp.einsum('bhnij,bhnje->bhnie', scores, vc)
    kv = jnp.einsum('bhnjd,bhnje->nbhde', kc * jnp.exp(lg * (C - 1 - pos))[None, :, None, :, None], vc)
    g_chunk = jnp.exp(log_gamma * C)[None, :, None, None]

    def step(S, kv_n):
        return g_chunk * S + kv_n, S

    s_final, s_prev = lax.scan(step, s0, kv)
    inter = jnp.einsum('bhnid,nbhde->bhnie', qc * jnp.exp(lg * (pos + 1.0))[None, :, None, :, None], s_prev)
    return (intra + inter).reshape(B, H, L, dv), s_final


def gated_delta_chunked(q, k, v, g, beta, s0):
    B, H, L, dk = q.shape
    dv = v.shape[-1]
    C = DN_CHUNK
    n = L // C
    qc = (q * dk ** -0.5).reshape(B, H, n, C, dk)
    kc = k.reshape(B, H, n, C, dk)
    vc = v.reshape(B, H, n, C, dv)
    gc = jnp.cumsum(g.reshape(B, H, n, C), axis=-1)
    bc = beta.reshape(B, H, n, C)[..., None]
    incl = jnp.tril(jnp.ones((C, C), dtype=bool))
    strict = jnp.tril(jnp.ones((C, C), dtype=bool), -1)
    gdiff = gc[..., :, None] - gc[..., None, :]
    decay = jnp.where(incl, jnp.exp(jnp.where(incl, gdiff, 0.0)), 0.0)
    kb = kc * bc
    lower = jnp.where(strict, jnp.einsum('bhnid,bhnjd->bhnij', kb, kc) * decay, 0.0)
    a_mat = lower + jnp.eye(C, dtype=jnp.float32)
    rhs = jnp.concatenate([vc * bc, kb * jnp.exp(gc)[..., None]], axis=-1)
    sol = lax.linalg.triangular_solve(a_mat, rhs, left_side=True, lower=True, unit_diagonal=True)
    u, w = sol[..., :dv], sol[..., dv:]
    attn = jnp.where(incl, jnp.einsum('bhnid,bhnjd->bhnij', qc, kc) * decay, 0.0)
    qg = qc * jnp.exp(gc)[..., None]
    kd = kc * jnp.exp(gc[..., -1:] - gc)[..., None]
    gl = jnp.exp(gc[..., -1])[..., None, None]
    xs = tuple(jnp.moveaxis(t, 2, 0) for t in (u, w, qg, attn, kd, gl))

    def step(S, inp):
        u_i, w_i, qg_i, attn_i, kd_i, gl_i = inp
        v_new = u_i - jnp.einsum('bhcd,bhde->bhce', w_i, S)
        o = jnp.einsum('bhcd,bhde->bhce', qg_i, S) + jnp.einsum('bhij,bhje->bhie', attn_i, v_new)
        S = S * gl_i + jnp.einsum('bhcd,bhce->bhde', kd_i, v_new)
        return S, o

    s_final, o = lax.scan(step, s0, xs)
    return jnp.moveaxis(o, 0, 2).reshape(B, H, L, dv), s_final


def gqa_block(qg, k, v):
    s = jnp.einsum('bqgrd,bkgd->bgrqk', qg, k, preferred_element_type=jnp.float32) * ATT_HD ** -0.5
    p = jax.nn.softmax(s, axis=-1).astype(v.dtype)
    return jnp.einsum('bgrqk,bkgd->bqgrd', p, v)


def attend_blocked(q, k, v):
    B, L, H, d = q.shape
    R = H // ATT_KV_HEADS
    nb = L // Q_BLOCK
    qb = jnp.moveaxis(q.reshape(B, nb, Q_BLOCK, ATT_KV_HEADS, R, d), 1, 0)
    o = lax.map(lambda qblk: gqa_block(qblk, k, v), qb)
    return jnp.moveaxis(o, 0, 1).reshape(B, L, H * d)


def retention_readout(o, gate):
    o = o * lax.rsqrt(jnp.mean(o * o, axis=-1, keepdims=True) + EPS)
    B, H, L, dv = o.shape
    o = jnp.swapaxes(o, 1, 2).reshape(B, L, H * dv).astype(gate.dtype)
    return jax.nn.silu(gate) * o


def mix_retention_attention(h_lat, h_ctx, w_in, q_gain, k_gain, decay_f, decay_b, w_out, cos, sin, with_ctx_out):
    B = h_lat.shape[0]

    def project(h, rotate):
        L = h.shape[1]
        rq, rk, rv, rg, aq, ak, av = _split(h @ w_in, EVEN_SPLITS)
        rq = rq.reshape(B, L, RET_HEADS, RET_DK)
        rk = rk.reshape(B, L, RET_HEADS, RET_DK)
        aq = rmsnorm(aq.reshape(B, L, ATT_HEADS, ATT_HD), q_gain)
        ak = rmsnorm(ak.reshape(B, L, ATT_KV_HEADS, ATT_HD), k_gain)
        if rotate:
            rq, rk = apply_rope(rq, cos, sin), apply_rope(rk, cos, sin)
            aq, ak = apply_rope(aq, cos, sin), apply_rope(ak, cos, sin)
        hf = lambda t: jnp.swapaxes(t, 1, 2).astype(jnp.float32)
        ret = (hf(rq), hf(rk) * RET_DK ** -0.5, hf(rv.reshape(B, L, RET_HEADS, RET_DV)))
        return ret, rg, aq, ak, av.reshape(B, L, ATT_KV_HEADS, ATT_HD)

    (lq, lk, lv), l_gate, laq, lak, lav = project(h_lat, True)
    (cq, ck, cv), c_gate, caq, cak, cav = project(h_ctx, False)

    ld_f = -jnp.exp(decay_f.astype(jnp.float32))
    ld_b = -jnp.exp(decay_b.astype(jnp.float32))
    s0 = jnp.zeros((B, RET_HEADS, RET_DK, RET_DV), jnp.float32)
    oc_f, sc_f = retention_chunked(cq, ck, cv, ld_f, s0)
    oc_b, sc_b = retention_chunked(_flip(cq), _flip(ck), _flip(cv), ld_b, s0)
    ol_f, _ = retention_chunked(lq, lk, lv, ld_f, sc_f)
    ol_b, _ = retention_chunked(_flip(lq), _flip(lk), _flip(lv), ld_b, sc_b)
    ret_lat = retention_readout(ol_f + _flip(ol_b), l_gate)

    k_all = jnp.concatenate([lak, cak], axis=1)
    v_all = jnp.concatenate([lav, cav], axis=1)
    att_lat = attend_blocked(laq, k_all, v_all)
    y_lat = jnp.concatenate([ret_lat, att_lat], axis=-1) @ w_out
    if not with_ctx_out:
        return y_lat, None

    Lc = h_ctx.shape[1]
    ret_ctx = retention_readout(oc_f + _flip(oc_b), c_gate)
    R = ATT_HEADS // ATT_KV_HEADS
    att_ctx = gqa_block(caq.reshape(B, Lc, ATT_KV_HEADS, R, ATT_HD), cak, cav).reshape(B, Lc, ATT_HEADS * ATT_HD)
    y_ctx = jnp.concatenate([ret_ctx, att_ctx], axis=-1) @ w_out
    return y_lat, y_ctx


def short_conv(x, w):
    y = lax.conv_general_dilated(x, w[:, None, :], window_strides=(1,),
                                 padding=[(DN_CONV // 2, DN_CONV // 2)],
                                 dimension_numbers=('NWC', 'WIO', 'NWC'),
                                 feature_group_count=x.shape[-1])
    return jax.nn.silu(y)


def decay_and_beta(a, b, a_log, dt_bias):
    g = -jnp.exp(a_log.astype(jnp.float32)) * jax.nn.softplus(a.astype(jnp.float32) + dt_bias.astype(jnp.float32))
    beta = jax.nn.sigmoid(b.astype(jnp.float32))
    return jnp.swapaxes(g, 1, 2), jnp.swapaxes(beta, 1, 2)


def deltanet_readout(o, z, gain):
    B, H, L, dv = o.shape
    o = rmsnorm(jnp.swapaxes(o, 1, 2), gain)
    o = o * jax.nn.silu(z.reshape(B, L, H, dv).astype(jnp.float32))
    return o.reshape(B, L, H * dv).astype(z.dtype)


def mix_gated_deltanet(h_lat, h_ctx, w_in, conv_w, a_log_f, a_log_b, dt_bias_f, dt_bias_b, out_gain, w_out, with_ctx_out):
    B = h_lat.shape[0]

    def project(h):
        L = h.shape[1]
        qkv, z, a_f, a_b, b_f, b_b = _split(h @ w_in, ODD_SPLITS)
        qkv = short_conv(qkv, conv_w)
        q, k, v = _split(qkv, (DN_HEADS * DN_DK, DN_HEADS * DN_DK, DN_HEADS * DN_DV))
        hf = lambda t: jnp.swapaxes(t, 1, 2)
        q = hf(l2norm(q.reshape(B, L, DN_HEADS, DN_DK)))
        k = hf(l2norm(k.reshape(B, L, DN_HEADS, DN_DK)))
        v = hf(v.reshape(B, L, DN_HEADS, DN_DV).astype(jnp.float32))
        return q, k, v, decay_and_beta(a_f, b_f, a_log_f, dt_bias_f), decay_and_beta(a_b, b_b, a_log_b, dt_bias_b), z

    lq, lk, lv, (lgf, lbf), (lgb, lbb), lz = project(h_lat)
    cq, ck, cv, (cgf, cbf), (cgb, cbb), cz = project(h_ctx)
    s0 = jnp.zeros((B, DN_HEADS, DN_DK, DN_DV), jnp.float32)
    oc_f, sc_f = gated_delta_chunked(cq, ck, cv, cgf, cbf, s0)
    oc_b, sc_b = gated_delta_chunked(_flip(cq), _flip(ck), _flip(cv), _flip(cgb), _flip(cbb), s0)
    ol_f, _ = gated_delta_chunked(lq, lk, lv, lgf, lbf, sc_f)
    ol_b, _ = gated_delta_chunked(_flip(lq), _flip(lk), _flip(lv), _flip(lgb), _flip(lbb), sc_b)
    y_lat = deltanet_readout(ol_f + _flip(ol_b), lz, out_gain) @ w_out
    if not with_ctx_out:
        return y_lat, None
    y_ctx = deltanet_readout(oc_f + _flip(oc_b), cz, out_gain) @ w_out
    return y_lat, y_ctx


def hier_moe(h, w_group, b_group, w_expert, b_expert, w_gate_up, w_down):
    T = h.shape[0]
    rows = jnp.arange(T)
    g_logits = jnp.dot(h, w_group, preferred_element_type=jnp.float32) + b_group.astype(jnp.float32)
    g_prob = jax.nn.softmax(g_logits, axis=-1)
    grp = jnp.argmax(g_logits, axis=-1)
    p_grp = g_prob[rows, grp][:, None]
    e_logits = (jnp.dot(h, w_expert, preferred_element_type=jnp.float32) + b_expert.astype(jnp.float32)).reshape(T, N_GROUPS, EXPERTS_PER_GROUP)
    e_sel = e_logits[rows, grp]
    top_val, top_idx = lax.top_k(e_sel, TOP_K)
    weights = jax.nn.softmax(top_val, axis=-1) * p_grp
    expert = grp[:, None] * EXPERTS_PER_GROUP + top_idx
    flat_e = expert.reshape(-1)
    flat_w = weights.reshape(-1)
    flat_tok = jnp.repeat(rows, TOP_K)
    order = jnp.argsort(flat_e)
    tok_s = flat_tok[order]
    w_s = flat_w[order]
    sizes = jnp.bincount(flat_e, length=N_EXPERTS).astype(jnp.int32)
    xs = h[tok_s]
    gate, up = jnp.split(lax.ragged_dot(xs, w_gate_up, sizes), 2, axis=-1)
    y = lax.ragged_dot(jax.nn.silu(gate) * up, w_down, sizes)
    y = y * w_s[:, None].astype(y.dtype)
    return jnp.zeros_like(h).at[tok_s].add(y)


def setup_inputs(seed: int = 0) -> dict:
    key = jax.random.key(seed)
    ks = list(jax.random.split(key, 40))
    nxt = ks.pop
    f32 = jnp.float32
    D = D_MODEL
    n_even = (DEPTH + 1) // 2
    n_odd = DEPTH // 2

    def w(shape, fan_in, scale=1.0):
        return jax.random.normal(nxt(), shape, f32) * (scale * fan_in ** -0.5)

    def gain(shape):
        return 1.0 + 0.05 * jax.random.normal(nxt(), shape, f32)

    def small(shape, s):
        return s * jax.random.normal(nxt(), shape, f32)

    ret_base = jnp.log(-jnp.log1p(-(2.0 ** (-5.0 - jnp.arange(RET_HEADS, dtype=f32)))))

    def dt_bias(shape):
        dt = jnp.exp(jax.random.uniform(nxt(), shape, f32, math.log(1e-3), math.log(1e-1)))
        return dt + jnp.log(-jnp.expm1(-dt))

    def a_log(shape):
        return jnp.log(jax.random.uniform(nxt(), shape, f32, 1.0, 16.0))

    inputs = {
        "x": jax.random.normal(nxt(), (BATCH, SEQ, D), f32),
        "c": jax.random.normal(nxt(), (BATCH, D), f32),
        "ctx": jax.random.normal(nxt(), (BATCH, CTX_LEN, D), f32),
        "c_ctx": jax.random.normal(nxt(), (D,), f32),
        "w_ada": w((DEPTH, D, 6 * D), D, 0.5),
        "b_ada": small((DEPTH, 6 * D), 0.02),
        "norm_mix": gain((DEPTH, D)),
        "norm_ffn": gain((DEPTH, D)),
        "ev_w_in": w((n_even, D, EVEN_IN), D),
        "ev_q_gain": gain((n_even, ATT_HD)),
        "ev_k_gain": gain((n_even, ATT_HD)),
        "ev_decay_f": ret_base + small((n_even, RET_HEADS), 0.05),
        "ev_decay_b": ret_base + small((n_even, RET_HEADS), 0.05),
        "ev_w_out": w((n_even, EVEN_MIX, D), EVEN_MIX),
        "od_w_in": jnp.concatenate([w((n_odd, D, DN_CONV_CH + DN_HEADS * DN_DV), D),
                                    w((n_odd, D, 4 * DN_HEADS), D, 0.1)], axis=-1),
        "od_conv": w((n_odd, DN_CONV, DN_CONV_CH), DN_CONV),
        "od_a_log_f": a_log((n_odd, DN_HEADS)),
        "od_a_log_b": a_log((n_odd, DN_HEADS)),
        "od_dt_bias_f": dt_bias((n_odd, DN_HEADS)),
        "od_dt_bias_b": dt_bias((n_odd, DN_HEADS)),
        "od_out_gain": gain((n_odd, DN_DV)),
        "od_w_out": w((n_odd, DN_HEADS * DN_DV, D), DN_HEADS * DN_DV),
        "moe_w_group": w((DEPTH, D, N_GROUPS), D),
        "moe_b_group": small((DEPTH, N_GROUPS), 0.01),
        "moe_w_expert": w((DEPTH, D, N_EXPERTS), D),
        "moe_b_expert": small((DEPTH, N_EXPERTS), 0.01),
        "moe_w_gate_up": w((DEPTH, N_EXPERTS, D, 2 * D_EXPERT), D),
        "moe_w_down": w((DEPTH, N_EXPERTS, D_EXPERT, D), D_EXPERT),
        "final_norm": gain((D,)),
    }
    return inputs


def reference(x, c, ctx, c_ctx, w_ada, b_ada, norm_mix, norm_ffn,
              ev_w_in, ev_q_gain, ev_k_gain, ev_decay_f, ev_decay_b, ev_w_out,
              od_w_in, od_conv, od_a_log_f, od_a_log_b, od_dt_bias_f, od_dt_bias_b, od_out_gain, od_w_out,
              moe_w_group, moe_b_group, moe_w_expert, moe_b_expert, moe_w_gate_up, moe_w_down,
              final_norm):
    B, L, D = x.shape
    Lc = ctx.shape[1]
    cos, sin = axial_rope(L, ATT_HD)
    silu_c = jax.nn.silu(c)
    silu_cc = jax.nn.silu(c_ctx)
    for layer in range(DEPTH):
        last = layer == DEPTH - 1
        mod_l = (silu_c @ w_ada[layer] + b_ada[layer])[:, None, :]
        mod_c = silu_cc @ w_ada[layer] + b_ada[layer]
        sh1, sc1, g1, sh2, sc2, g2 = jnp.split(mod_l, 6, axis=-1)
        csh1, csc1, cg1, csh2, csc2, cg2 = jnp.split(mod_c, 6, axis=-1)
        h_lat = modulate(rmsnorm(x, norm_mix[layer]), sh1, sc1)
        h_ctx = modulate(rmsnorm(ctx, norm_mix[layer]), csh1, csc1)
        i = layer // 2
        if layer % 2 == 0:
            y_lat, y_ctx = mix_retention_attention(h_lat, h_ctx, ev_w_in[i], ev_q_gain[i], ev_k_gain[i],
                                                   ev_decay_f[i], ev_decay_b[i], ev_w_out[i], cos, sin, not last)
        else:
            y_lat, y_ctx = mix_gated_deltanet(h_lat, h_ctx, od_w_in[i], od_conv[i], od_a_log_f[i], od_a_log_b[i],
                                              od_dt_bias_f[i], od_dt_bias_b[i], od_out_gain[i], od_w_out[i], not last)
        x = x + g1 * y_lat
        f_lat = modulate(rmsnorm(x, norm_ffn[layer]), sh2, sc2).reshape(B * L, D)
        moe_args = (moe_w_group[layer], moe_b_group[layer], moe_w_expert[layer], moe_b_expert[layer],
                    moe_w_gate_up[layer], moe_w_down[layer])
        if last:
            y = hier_moe(f_lat, *moe_args)
            x = x + g2 * y.reshape(B, L, D)
        else:
            ctx = ctx + cg1 * y_ctx
            f_ctx = modulate(rmsnorm(ctx, norm_ffn[layer]), csh2, csc2).reshape(B * Lc, D)
            y = hier_moe(jnp.concatenate([f_lat, f_ctx], axis=0), *moe_args)
            x = x + g2 * y[:B * L].reshape(B, L, D)
            ctx = ctx + cg2 * y[B * L:].reshape(B, Lc, D)
    return rmsnorm(x, final_norm)
```

```python
import numpy as np
from contextlib import ExitStack
import concourse.bass as bass
import concourse.mybir as mybir
from concourse.bass_utils import run_bass_kernel_spmd

F32 = mybir.dt.float32
BF16 = mybir.dt.bfloat16
I32 = mybir.dt.int32
AF = mybir.ActivationFunctionType
ALU = mybir.AluOpType
AX = mybir.AxisListType

D = 1024
LC = 256
L = 8192
T = LC + L
NT = T // 128
DEPTH = 4
EPS = 1e-6
EVEN_IN = 3840
ODD_IN = 4128


class Buf:
    __slots__ = ("w", "r")

    def __init__(self):
        self.w = None
        self.r = {}


class Tl:
    __slots__ = ("t", "b", "psum")

    def __init__(self, t, psum=False):
        self.t = t
        self.b = Buf()
        self.psum = psum

    def __getitem__(self, k):
        return self.t[k]


class KB:
    NDMA = {"sp": 12, "pool": 12, "act": 4}

    def __init__(self, nc):
        self.nc = nc
        self.es = ExitStack()
        self.eng = {"pe": nc.tensor, "act": nc.scalar, "dve": nc.vector, "pool": nc.gpsimd, "sp": nc.sync}
        self.sem = {}
        self.cnt = {}
        self.seen = {e: {} for e in self.eng}
        for e in self.eng:
            self.sem[e] = self.es.enter_context(nc.semaphore("s_" + e))
            self.cnt[e] = 0
        self.dsem = {}
        self.dval = {}
        self.dnext = {}
        for q, n in self.NDMA.items():
            self.dsem[q] = [self.es.enter_context(nc.semaphore(f"d_{q}{i}")) for i in range(n)]
            self.dval[q] = [0] * n
            self.dnext[q] = 0
        self.tags = {}
        self.ninst = 0
        self.cur = self.es

    def semobj(self, key):
        if isinstance(key, tuple):
            return self.dsem[key[0]][key[1]]
        return self.sem[key]

    def tag(self, *key):
        b = self.tags.get(key)
        if b is None:
            b = Tl(None)
            self.tags[key] = b
        return b

    def _deps(self, eng, r, w):
        deps = {}

        def add(ev, raw):
            if ev is None:
                return
            k, v = ev
            if k == eng and (not raw or eng in ("pe", "sp")):
                return
            if deps.get(k, 0) < v:
                deps[k] = v

        for x in r:
            add(x.b.w, True)
        for x in w:
            add(x.b.w, False)
            for k, v in x.b.r.items():
                add((k, v), False)
        return deps

    def _wait(self, eng, deps):
        seen = self.seen[eng]
        e = self.eng[eng]
        for k, v in deps.items():
            if seen.get(k, 0) >= v:
                continue
            e.wait_ge(self.semobj(k), v)
            seen[k] = v
            self.ninst += 1

    def _mark(self, ev, r, w):
        k, v = ev
        for x in r:
            if x.b.r.get(k, 0) < v:
                x.b.r[k] = v
        for x in w:
            x.b.w = ev
            x.b.r = {}

    def op(self, eng, fn, r=(), w=()):
        if any(x.psum for x in r):
            w = list(w) + [x for x in r if x.psum]
            r = [x for x in r if not x.psum]
        self._wait(eng, self._deps(eng, r, w))
        ins = fn(self.eng[eng])
        self.cnt[eng] += 1
        ins.then_inc(self.sem[eng], 1)
        self._mark((eng, self.cnt[eng]), r, w)
        self.ninst += 1
        return ins

    def dma(self, q, out, in_, r=(), w=(), indirect=None, **kw):
        deps = self._deps("dma", r, w)
        i = self.dnext[q]
        self.dnext[q] = (i + 1) % len(self.dsem[q])
        key = (q, i)
        if self.dval[q][i] > 0:
            deps[key] = max(deps.get(key, 0), self.dval[q][i])
        self._wait(q, deps)
        e = self.eng[q]
        if indirect is not None:
            ins = e.indirect_dma_start(out=out, in_=in_, **indirect, **kw)
        else:
            ins = e.dma_start(out=out, in_=in_, **kw)
        self.dval[q][i] += 16
        ins.then_inc(self.dsem[q][i], 16)
        self._mark((key, self.dval[q][i]), r, w)
        self.ninst += 1
        return ins

    def barrier(self, engines=("pe", "act", "dve", "pool", "sp")):
        deps = {e: self.cnt[e] for e in self.eng if self.cnt[e] > 0}
        for q in self.dsem:
            for i, v in enumerate(self.dval[q]):
                if v > 0:
                    deps[(q, i)] = v
        for e in engines:
            d = {k: v for k, v in deps.items() if k != e}
            self._wait(e, d)

    def sb(self, name, shape, dtype):
        self.nalloc = getattr(self, "nalloc", 0) + 1
        return Tl(self.cur.enter_context(self.nc.sbuf_tensor(f"{name}_{self.nalloc}", shape, dtype)))

    def phase(self):
        kb = self

        class _P:
            def __enter__(self_):
                self_.prev = getattr(kb, "cur", kb.es)
                self_.st = ExitStack()
                kb.cur = self_.st
                return self_

            def __exit__(self_, *a):
                if a[0] is None:
                    kb.barrier()
                self_.st.close()
                kb.cur = self_.prev
                return False

        return _P()


class Ctx:
    pass


def declare_io(nc, C):
    def din(name, shape, dt=F32):
        return nc.dram_tensor(name, shape, dt, kind="ExternalInput").ap()
    C.xin = din("xin", [T, D])
    C.cT = din("cT", [128, 16])
    C.normT = din("normT", [128, 72])
    C.badaT = din("badaT", [128, 192])
    C.b_ada = din("b_ada", [DEPTH, 6 * D])
    C.rope = din("rope", [L, 128])
    C.identb = din("identb", [128, 128], BF16)
    C.identf = din("identf", [128, 128])
    C.w_ada = din("w_ada", [DEPTH, D, 6 * D])
    C.ev_w_in = din("ev_w_in", [2, D, EVEN_IN])
    C.ev_qk_gain = din("ev_qk_gain", [2, 128])
    C.ev_decay = din("ev_decay", [2, 16])
    C.ev_w_out = din("ev_w_out", [2, 1536, D])
    C.rconst = din("rconst", [128, 770])
    C.norms = din("norms", [9, D])
    C.od_w_in = din("od_w_in", [2, D, ODD_IN])
    C.od_prm = din("od_prm", [2, 32])
    C.od_conv = din("od_conv", [2, 3, 3072])
    C.od_gain = din("od_gain", [2, 128])
    C.od_w_out = din("od_w_out", [2, D, D])
    C.dconst = din("dconst", [128, 512])
    C.wr = din("wr", [DEPTH, D, 36])
    C.br = din("br", [DEPTH, 36])
    C.mconst = din("mconst", [128, 33 + NSLOT])
    C.triones = din("triones", [128, 256], BF16)
    C.srcidx = din("srcidx", [128, NT], I32)
    C.listinit = din("listinit", [128, (NSLOT + 1) * 4], I32)
    C.wgu = din("wgu", [DEPTH, 32 * 128, 8 * 1024])
    C.wd = din("wd", [DEPTH, 32 * 128, 4 * 1024])


def setup_globals(C):
    kb, nc = C.kb, C.nc
    C.ps = [Tl(kb.es.enter_context(nc.psum_tensor(f"ps{i}", [128, 512], F32)), psum=True) for i in range(8)]
    C.modT = kb.sb("modT", [128, DEPTH, 48, 2], F32)
    C.normTs = kb.sb("normTs", [128, 72], F32)
    C.identb_s = kb.sb("identb_s", [128, 128], BF16)
    C.identf_s = kb.sb("identf_s", [128, 128], F32)
    kb.dma("sp", C.normTs[:, :], C.normT, w=[C.normTs])
    kb.dma("sp", C.identb_s[:, :], C.identb, w=[C.identb_s])
    kb.dma("sp", C.identf_s[:, :], C.identf, w=[C.identf_s])
    def dscr(name, shape, dt=F32):
        kind = "ExternalOutput" if name in C.dbg_out else "Internal"
        return nc.dram_tensor(name, shape, dt, kind=kind).ap()
    C.G = dscr("G", [DEPTH, 128, 8192])
    C.X = dscr("X", [T, D])
    C.PA = dscr("PA", [T, ODD_IN], BF16)
    C.OF = dscr("OF", [T, D])
    C.MIX = dscr("MIX", [T, D], BF16)
    C.AT = dscr("AT", [512, T], BF16)
    C.F = dscr("F", [T, D], BF16)
    C.ZP = dscr("ZP", [TZ, 3072], BF16)
    C.GB = dscr("GB", [T, 32])
    C.OB = dscr("OB", [T, D])
    C.LIST = dscr("LIST", [(NSLOT + 1) * 128, 4], I32)
    C.YB = dscr("YB", [2 * TPAD, D])
    C.widx = kb.sb("widx", [128, NSLOT], I32)


def phase0(C):
    kb = C.kb
    ps0, ps1 = C.ps[0], C.ps[1]
    with kb.phase():
        cT = kb.sb("cT", [128, 16], F32)
        sc = kb.sb("sc", [128, 16], F32)
        screp = kb.sb("screp", [128, 16, 128], F32)
        badaT = kb.sb("badaT", [128, 192], F32)
        brep = kb.sb("brep", [128, 4096], F32)
        gout = kb.sb("gout", [128, 8192], F32)
        wblk = [kb.sb("wada", [128, 8, 512], F32) for _ in range(2)]
        kb.dma("sp", cT[:, :], C.cT, w=[cT])
        kb.dma("sp", badaT[:, :], C.badaT, w=[badaT])
        kb.op("act", lambda e: e.activation(out=sc[:, :], in_=cT[:, :], func=AF.Silu), r=[cT], w=[sc])
        kb.op("dve", lambda e: e.tensor_copy(out=screp[:, :, :], in_=sc[:, :].unsqueeze(2).to_broadcast([128, 16, 128])),
              r=[sc], w=[screp])
        for l in range(DEPTH):
            for gi, c0 in enumerate((2048, 5120, 3072, 4096)):
                kb.dma("sp", brep[:, gi * 1024:(gi + 1) * 1024], C.b_ada[l, c0:c0 + 1024].partition_broadcast(128), w=[brep])
            wv = C.w_ada[l].rearrange("(k p) c -> p k c", p=128)
            for j in range(12):
                wb = wblk[j % 2]
                kb.dma("sp", wb[:, :, :], wv[:, :, j * 512:(j + 1) * 512], w=[wb])
                for c4 in range(4):
                    for k in range(8):
                        kb.op("pe", lambda e: e.matmul(ps0[:, c4 * 2:(c4 + 1) * 2], lhsT=wb[:, k, c4 * 128:(c4 + 1) * 128],
                                                       rhs=sc[:, 2 * k:2 * k + 2], start=(k == 0), stop=(k == 7)),
                              r=[wb, sc], w=[ps0])
                kb.op("dve", lambda e: e.tensor_tensor(
                    out=C.modT[:, l, j * 4:(j + 1) * 4, :], in0=ps0[:, 0:8].rearrange("p (c r) -> p c r", r=2),
                    in1=badaT[:, l * 48 + j * 4:l * 48 + j * 4 + 4].unsqueeze(2).to_broadcast([128, 4, 2]), op=ALU.add),
                    r=[ps0, badaT], w=[C.modT])
                if j in (4, 5, 10, 11, 6, 7, 8, 9):
                    gi = {4: 0, 5: 0, 10: 1, 11: 1, 6: 2, 7: 2, 8: 3, 9: 3}[j]
                    half = j % 2
                    for r_ in range(2):
                        for k in range(8):
                            kb.op("pe", lambda e: e.matmul(ps1[:, :], lhsT=screp[:, 2 * k + r_, :], rhs=wb[:, k, :],
                                                           start=(k == 0), stop=(k == 7)), r=[screp, wb], w=[ps1])
                        o0 = ((r_ * 2 + gi) if gi < 2 else (4 + r_ * 2 + gi - 2)) * 1024 + half * 512
                        b0 = gi * 1024 + half * 512
                        kb.op("dve", lambda e: e.tensor_tensor(out=gout[:, o0:o0 + 512], in0=ps1[:, :], in1=brep[:, b0:b0 + 512],
                                                               op=ALU.add), r=[ps1, brep], w=[gout])
            kb.dma("sp", C.G[l], gout[:, :], r=[gout], w=[kb.tag("G", l)])


def setup_consts(C):
    kb = C.kb
    C.cneg = kb.sb("cneg", [128, 64], F32)
    kb.op("pool", lambda e: e.memset(C.cneg[:, :], -0.5), w=[C.cneg])


def rsqrt_mean(C, dst, src, n, inv_n, tmp):
    kb = C.kb
    kb.op("dve", lambda e: e.tensor_scalar(out=tmp[:, 0:n], in0=src[:, 0:n], scalar1=inv_n, scalar2=EPS,
                                           op0=ALU.mult, op1=ALU.add), r=[src], w=[tmp])
    kb.op("pool", lambda e: e.tensor_tensor(out=dst[:, 0:n], in0=tmp[:, 0:n], in1=C.cneg[:, 0:n], op=ALU.pow),
          r=[tmp, C.cneg], w=[dst])


def layer_mod_tables(C, l, which):
    kb = C.kb
    gs = kb.sb("gs", [128, 8, 2], F32)
    sc_blk = 8 if which == 0 else 32
    sh_blk = 0 if which == 0 else 24
    nrm = (l if which == 0 else DEPTH + l) * 8
    kb.op("dve", lambda e: e.tensor_scalar(out=gs[:, :, :], in0=C.modT[:, l, sc_blk:sc_blk + 8, :], scalar1=1.0, scalar2=None,
                                           op0=ALU.add), r=[C.modT], w=[gs])
    kb.op("dve", lambda e: e.tensor_tensor(out=gs[:, :, :], in0=gs[:, :, :],
                                           in1=C.normTs[:, nrm:nrm + 8].unsqueeze(2).to_broadcast([128, 8, 2]), op=ALU.mult),
          r=[gs, C.normTs], w=[gs])
    return gs, sh_blk


def rope_apply(C, dst_ap, dst_tl, src_ap, src_tl, nh, rp, t1, t2):
    kb = C.kb
    n = nh * 64
    kb.op("dve", lambda e: e.tensor_tensor(out=t1[:, 0:n].rearrange("p (h d) -> p h d", h=nh),
                                           in0=src_ap.rearrange("p (h d) -> p h d", h=nh),
                                           in1=rp[:, 0:64].unsqueeze(1).to_broadcast([128, nh, 64]), op=ALU.mult),
          r=[src_tl, rp], w=[t1])
    sv = src_ap.rearrange("p (h a b f) -> p h a b f", h=nh, a=2, b=2, f=16)
    tv = t2[:, 0:n].rearrange("p (h a b f) -> p h a b f", h=nh, a=2, b=2, f=16)
    sn = rp[:, 64:128].rearrange("p (a b f) -> p a b f", a=2, b=2, f=16)
    for b_ in range(2):
        kb.op("dve", lambda e: e.tensor_tensor(out=tv[:, :, :, b_, :], in0=sv[:, :, :, 1 - b_, :],
                                               in1=sn[:, :, b_, :].unsqueeze(1).to_broadcast([128, nh, 2, 16]), op=ALU.mult),
              r=[src_tl, rp], w=[t2])
    kb.op("pool", lambda e: e.tensor_tensor(out=dst_ap, in0=t1[:, 0:n], in1=t2[:, 0:n], op=ALU.add),
          r=[t1, t2], w=[dst_tl])


def norm_tile(C, xt, ss, rstd, tmp1, junk, xn):
    kb = C.kb
    kb.op("act", lambda e: e.activation(out=junk[:, :], in_=xt[:, :], func=AF.Square, accum_out=ss[:, 0:1]),
          r=[xt], w=[junk, ss])
    rsqrt_mean(C, rstd, ss, 1, 1.0 / D, tmp1)
    kb.op("act", lambda e: e.activation(out=xn[:, :], in_=xt[:, :], func=AF.Copy, scale=rstd[:, 0:1]),
          r=[xt, rstd], w=[xn])


def phaseA_even(C, l, tiles, x_src):
    kb = C.kb
    i = l // 2
    ps = C.ps
    with kb.phase():
        W = kb.sb("Win", [128, 8, EVEN_IN], BF16)
        wv = C.ev_w_in[i].rearrange("(k p) c -> p k c", p=128)
        for k in range(8):
            kb.dma("pool", W[:, k, :], wv[:, k, :], w=[W])
        gs1, sh_blk = layer_mod_tables(C, l, 0)
        gain = kb.sb("qkgain", [128, 128], F32)
        kb.dma("sp", gain[:, :], C.ev_qk_gain[i].partition_broadcast(128), w=[gain])
        kb.op("dve", lambda e: e.tensor_scalar(out=gain[:, 0:64], in0=gain[:, 0:64], scalar1=0.125, scalar2=None, op0=ALU.mult),
              r=[gain], w=[gain])
        xb = [kb.sb("xt", [128, D], F32) for _ in range(2)]
        rpb = [kb.sb("rp", [128, 128], F32) for _ in range(2)]
        junk = kb.sb("junk", [128, D], BF16)
        xn = kb.sb("xn", [128, D], BF16)
        hT = [kb.sb("hT", [128, 8, 128], BF16) for _ in range(2)]
        pa = [kb.sb("pa", [128, EVEN_IN], BF16) for _ in range(2)]
        ss = kb.sb("ss", [128, 1], F32)
        rstd = kb.sb("rstd", [128, 1], F32)
        tmp1 = kb.sb("tmp1", [128, 8], F32)
        ssh = kb.sb("ssh", [128, 8], F32)
        rs8 = kb.sb("rs8", [128, 8], F32)
        tA = kb.sb("tA", [128, 512], F32)
        tB = kb.sb("tB", [128, 512], F32)
        t1 = kb.sb("t1", [128, 512], F32)
        t2 = kb.sb("t2", [128, 512], F32)
        for n_, t in enumerate(tiles):
            lat = t >= 2
            r_ = 0 if lat else 1
            xt = xb[n_ % 2]
            rp = rpb[n_ % 2]
            h = hT[n_ % 2]
            po = pa[n_ % 2]
            x_src(t, xt)
            if lat:
                kb.dma("sp", rp[:, :], C.rope[(t - 2) * 128:(t - 1) * 128, :], w=[rp])
            norm_tile(C, xt, ss, rstd, tmp1, junk, xn)
            psT = ps[0]
            pv = psT[:, :].bitcast(BF16)
            for k in range(8):
                kb.op("pe", lambda e: e.transpose(pv[:, k * 128:(k + 1) * 128], xn[:, k * 128:(k + 1) * 128], C.identb_s[:, :]),
                      r=[xn, C.identb_s], w=[psT])
            for k in range(8):
                kb.op("act", lambda e: e.activation(out=h[:, k, :], in_=pv[:, k * 128:(k + 1) * 128], func=AF.Identity,
                                                    scale=gs1[:, k, r_:r_ + 1], bias=C.modT[:, l, sh_blk + k, r_:r_ + 1]),
                      r=[psT, gs1, C.modT], w=[h])
            for j in range(8):
                c0 = j * 512
                nc_ = 512 if j < 7 else 256
                pj = ps[1 + (j % 6)]
                for k in range(8):
                    kb.op("pe", lambda e: e.matmul(pj[:, 0:nc_], lhsT=h[:, k, :], rhs=W[:, k, c0:c0 + nc_],
                                                   start=(k == 0), stop=(k == 7)), r=[h, W], w=[pj])
                if j == 0:
                    if lat:
                        rope_apply(C, po[:, c0:c0 + 512], po, pj[:, :], pj, 8, rp, t1, t2)
                    else:
                        kb.op("act", lambda e: e.copy(out=po[:, c0:c0 + 512], in_=pj[:, :]), r=[pj], w=[po])
                elif j == 1:
                    if lat:
                        kb.op("act", lambda e: e.mul(out=tA[:, :], in_=pj[:, :], mul=0.125), r=[pj], w=[tA])
                        rope_apply(C, po[:, c0:c0 + 512], po, tA[:, :], tA, 8, rp, t1, t2)
                    else:
                        kb.op("act", lambda e: e.mul(out=po[:, c0:c0 + 512], in_=pj[:, :], mul=0.125), r=[pj], w=[po])
                elif j in (2, 3):
                    kb.op("act", lambda e: e.copy(out=po[:, c0:c0 + 512], in_=pj[:, :]), r=[pj], w=[po])
                elif j in (4, 5):
                    kb.op("act", lambda e: e.activation(out=po[:, c0:c0 + 512], in_=pj[:, :], func=AF.Silu), r=[pj], w=[po])
                else:
                    nh = 8 if j == 6 else 2
                    n = nh * 64
                    g0 = 0 if j == 6 else 64
                    kb.op("act", lambda e: e.activation(out=tA[:, 0:n], in_=pj[:, 0:n], func=AF.Square), r=[pj], w=[tA])
                    kb.op("dve", lambda e: e.reduce_sum(out=ssh[:, 0:nh], in_=tA[:, 0:n].rearrange("p (h d) -> p h d", h=nh),
                                                        axis=AX.X), r=[tA], w=[ssh])
                    rsqrt_mean(C, rs8, ssh, nh, 1.0 / 64, tmp1)
                    kb.op("dve", lambda e: e.tensor_tensor(out=tB[:, 0:n].rearrange("p (h d) -> p h d", h=nh),
                                                           in0=pj[:, 0:n].rearrange("p (h d) -> p h d", h=nh),
                                                           in1=rs8[:, 0:nh].unsqueeze(2).to_broadcast([128, nh, 64]), op=ALU.mult),
                          r=[pj, rs8], w=[tB])
                    if lat:
                        kb.op("dve", lambda e: e.tensor_tensor(out=tB[:, 0:n].rearrange("p (h d) -> p h d", h=nh),
                                                               in0=tB[:, 0:n].rearrange("p (h d) -> p h d", h=nh),
                                                               in1=gain[:, g0:g0 + 64].unsqueeze(1).to_broadcast([128, nh, 64]),
                                                               op=ALU.mult), r=[tB, gain], w=[tB])
                        rope_apply(C, po[:, c0:c0 + n], po, tB[:, 0:n], tB, nh, rp, t1, t2)
                    else:
                        kb.op("dve", lambda e: e.tensor_tensor(out=po[:, c0:c0 + n].rearrange("p (h d) -> p h d", h=nh),
                                                               in0=tB[:, 0:n].rearrange("p (h d) -> p h d", h=nh),
                                                               in1=gain[:, g0:g0 + 64].unsqueeze(1).to_broadcast([128, nh, 64]),
                                                               op=ALU.mult), r=[tB, gain], w=[po])
                    if j == 7:
                        kb.op("act", lambda e: e.copy(out=po[:, c0 + 128:c0 + 256], in_=pj[:, 128:256]), r=[pj], w=[po])
            kb.dma("sp", C.PA[t * 128:(t + 1) * 128, 0:EVEN_IN], po[:, :], r=[po], w=[kb.tag("PA", t)])


def rope_table():
    t = np.arange(L)
    rows = (t // 64).astype(np.float32)
    cols = (t % 64).astype(np.float32)
    inv = (10000.0 ** (-np.arange(16, dtype=np.float32) / 16)).astype(np.float32)
    ang = np.stack([rows[:, None] * inv, cols[:, None] * inv], axis=1).astype(np.float32)
    c, s = np.cos(ang), np.sin(ang)
    C64 = np.stack([c, c], axis=2)
    S64 = np.stack([-s, s], axis=2)
    return np.concatenate([C64.reshape(L, 64), S64.reshape(L, 64)], axis=1).astype(np.float32)


def fm(v):
    v = np.asarray(v, np.float32).reshape(-1)
    return np.ascontiguousarray(v.reshape(-1, 128).T)


def host_prep(inp):
    import ml_dtypes
    shared = {}
    shared["normT"] = np.concatenate([fm(inp["norm_mix"][l]) for l in range(DEPTH)] + [fm(inp["norm_ffn"][l]) for l in range(DEPTH)]
                                     + [fm(inp["final_norm"])], axis=1)
    shared["badaT"] = np.concatenate([fm(inp["b_ada"][l]) for l in range(DEPTH)], axis=1)
    shared["b_ada"] = np.ascontiguousarray(inp["b_ada"], np.float32)
    shared["rope"] = rope_table()
    shared["identb"] = np.eye(128, dtype=np.float32).astype(ml_dtypes.bfloat16)
    shared["identf"] = np.eye(128, dtype=np.float32)
    shared["rconst"] = ret_consts()
    shared["od_w_in"] = np.ascontiguousarray(inp["od_w_in"], np.float32)
    shared["od_prm"] = np.ascontiguousarray(np.concatenate([inp["od_a_log_f"], inp["od_a_log_b"], inp["od_dt_bias_f"], inp["od_dt_bias_b"]], axis=1), np.float32)
    shared["od_conv"] = np.ascontiguousarray(inp["od_conv"], np.float32)
    shared["od_gain"] = np.ascontiguousarray(inp["od_out_gain"], np.float32)
    shared["od_w_out"] = np.ascontiguousarray(inp["od_w_out"], np.float32)
    shared["dconst"] = dn_consts()
    shared["norms"] = np.ascontiguousarray(np.concatenate([inp["norm_mix"], inp["norm_ffn"], np.asarray(inp["final_norm"])[None, :]], axis=0), np.float32)
    shared["wr"] = np.ascontiguousarray(np.concatenate([inp["moe_w_group"], inp["moe_w_expert"]], axis=2), np.float32)
    shared["br"] = np.ascontiguousarray(np.concatenate([inp["moe_b_group"], inp["moe_b_expert"]], axis=1), np.float32)
    shared["triones"], shared["mconst"], shared["srcidx"], shared["listinit"] = moe_consts()
    shared["wgu"] = np.ascontiguousarray(np.asarray(inp["moe_w_gate_up"], np.float32).reshape(DEPTH, 32, 8, 128, 1024).transpose(0, 1, 3, 2, 4)).reshape(DEPTH, 32 * 128, 8 * 1024)
    shared["wd"] = np.ascontiguousarray(np.asarray(inp["moe_w_down"], np.float32).reshape(DEPTH, 32, 4, 128, 1024).transpose(0, 1, 3, 2, 4)).reshape(DEPTH, 32 * 128, 4 * 1024)
    shared["w_ada"] = np.ascontiguousarray(inp["w_ada"], np.float32)
    shared["ev_w_in"] = np.ascontiguousarray(inp["ev_w_in"], np.float32)
    shared["ev_qk_gain"] = np.ascontiguousarray(np.concatenate([inp["ev_q_gain"], inp["ev_k_gain"]], axis=1), np.float32)
    shared["ev_decay"] = np.ascontiguousarray(np.concatenate([inp["ev_decay_f"], inp["ev_decay_b"]], axis=1), np.float32)
    shared["ev_w_out"] = np.ascontiguousarray(inp["ev_w_out"], np.float32)
    maps = []
    for core in range(8):
        b = core % 4
        m = dict(shared)
        m["xin"] = np.ascontiguousarray(np.concatenate([inp["ctx"][b], inp["x"][b]], axis=0), np.float32)
        cv = np.stack([np.asarray(inp["c"][b], np.float32), np.asarray(inp["c_ctx"], np.float32)], axis=0)
        m["cT"] = np.ascontiguousarray(cv.reshape(2, 8, 128).transpose(2, 1, 0).reshape(128, 16))
        maps.append(m)
    return maps


def ret_consts():
    p = np.arange(128, dtype=np.float32)
    diff = p[None, :] - p[:, None]
    dpos = np.maximum(diff, 0)
    dneg = np.maximum(-diff, 0)
    mge = (diff >= 0).astype(np.float32)
    mle = (diff <= 0).astype(np.float32)
    pos1 = np.tile(p[None, :] + 1, (128, 1))
    rpos1 = np.tile(128 - p[None, :], (128, 1))
    pcol = np.stack([127 - p, p], axis=1)
    return np.ascontiguousarray(np.concatenate([dpos, dneg, mge, mle, pos1, rpos1, pcol], axis=1), np.float32)


def phaseB_ret(C, l, n_ctx_tiles=2, lat_tiles=None):
    kb = C.kb
    i = l // 2
    ps = C.ps
    if lat_tiles is None:
        lat_tiles = list(range(2, NT))
    with kb.phase():
        rc = kb.sb("rc", [128, 770], F32)
        kb.dma("sp", rc[:, :], C.rconst, w=[rc])
        ld = kb.sb("ld", [128, 16], F32)
        kb.dma("sp", ld[:, :], C.ev_decay[i].partition_broadcast(128), w=[ld])
        kb.op("act", lambda e: e.activation(out=ld[:, :], in_=ld[:, :], func=AF.Exp), r=[ld], w=[ld])
        kb.op("dve", lambda e: e.tensor_scalar(out=ld[:, :], in0=ld[:, :], scalar1=-1.0, scalar2=None, op0=ALU.mult), r=[ld], w=[ld])
        maskT = kb.sb("maskT", [128, 16, 128], BF16)
        DQ = kb.sb("DQ", [64, 16, 128], F32)
        DK = kb.sb("DK", [128, 16], F32)
        GC = kb.sb("GC", [64, 16], F32)
        tmpm = kb.sb("tmpm", [128, 128], F32)
        for d_ in range(2):
            for h in range(8):
                c = d_ * 8 + h
                kb.op("act", lambda e: e.activation(out=tmpm[:, :], in_=rc[:, d_ * 128:(d_ + 1) * 128], func=AF.Exp,
                                                    scale=ld[:, c:c + 1]), r=[rc, ld], w=[tmpm])
                kb.op("dve", lambda e: e.tensor_tensor(out=maskT[:, c, :], in0=tmpm[:, :], in1=rc[:, (2 + d_) * 128:(3 + d_) * 128],
                                                       op=ALU.mult), r=[tmpm, rc], w=[maskT])
                kb.op("act", lambda e: e.activation(out=DQ[:, c, :], in_=rc[0:64, (4 + d_) * 128:(5 + d_) * 128], func=AF.Exp,
                                                    scale=ld[0:64, c:c + 1]), r=[rc, ld], w=[DQ])
            kb.op("dve", lambda e: e.tensor_scalar(out=DK[:, d_ * 8:(d_ + 1) * 8], in0=ld[:, d_ * 8:(d_ + 1) * 8],
                                                   scalar1=rc[:, 768 + d_:769 + d_], scalar2=None, op0=ALU.mult), r=[ld, rc], w=[DK])
        kb.op("act", lambda e: e.activation(out=DK[:, :], in_=DK[:, :], func=AF.Exp), r=[DK], w=[DK])
        kb.op("act", lambda e: e.activation(out=GC[:, :], in_=ld[0:64, :], func=AF.Exp, scale=128.0), r=[ld], w=[GC])
        qkvb = [kb.sb("qkv", [128, 2048], BF16) for _ in range(2)]
        gateb = [kb.sb("gate", [128, 1024], BF16) for _ in range(2)]
        ofb = [kb.sb("of", [128, 1024], F32) for _ in range(2)]
        qT = kb.sb("qT", [64, 8, 128], BF16)
        qTd = kb.sb("qTd", [64, 8, 128], BF16)
        kT = kb.sb("kT", [64, 8, 128], BF16)
        kdec = kb.sb("kdec", [128, 8, 64], BF16)
        smb = [kb.sb("sm", [128, 128], BF16) for _ in range(2)]
        S = kb.sb("S", [64, 8, 128], F32)
        Sb = kb.sb("Sb", [64, 8, 128], BF16)
        osum = kb.sb("osum", [128, 1024], F32)
        junk = kb.sb("junkr", [128, 1024], F32)
        ssh = kb.sb("sshr", [128, 8], F32)
        rs8 = kb.sb("rs8r", [128, 8], F32)
        tmp8 = kb.sb("tmp8r", [128, 8], F32)
        mixb = [kb.sb("mixr", [128, 1024], BF16) for _ in range(2)]
        psQ, psK = ps[0], ps[1]
        psS = [ps[2], ps[3]]
        psO = [ps[4], ps[5]]
        psKV = [ps[6], ps[7]]
        pq = psQ[:, :].bitcast(BF16)
        pk = psK[:, :].bitcast(BF16)
        n_ = 0
        for d_ in range(2):
            order = list(range(n_ctx_tiles)) + list(lat_tiles)
            if d_ == 1:
                order = list(range(n_ctx_tiles))[::-1] + list(lat_tiles)[::-1]
            kb.op("dve", lambda e: e.memset(S[:, :, :], 0.0), w=[S])
            kb.op("dve", lambda e: e.memset(Sb[:, :, :], 0.0), w=[Sb])
            for t in order:
                if getattr(C, "dbgB", 9) < 1:
                    break
                qkv = qkvb[n_ % 2]
                gate = gateb[n_ % 2]
                of = ofb[n_ % 2]
                mix = mixb[n_ % 2]
                n_ += 1
                kb.dma("sp", qkv[:, :], C.PA[t * 128:(t + 1) * 128, 0:2048], r=[kb.tag("PA", t)], w=[qkv])
                if d_ == 1:
                    kb.dma("sp", gate[:, :], C.PA[t * 128:(t + 1) * 128, 2048:3072], r=[kb.tag("PA", t)], w=[gate])
                    kb.dma("sp", of[:, :], C.OF[t * 128:(t + 1) * 128, :], r=[kb.tag("OF", t)], w=[of])
                for h in range(8):
                    kb.op("pe", lambda e: e.transpose(pq[0:64, h * 128:(h + 1) * 128], qkv[:, h * 64:(h + 1) * 64], C.identb_s[:, :]),
                          r=[qkv, C.identb_s], w=[psQ])
                for h in range(8):
                    kb.op("pe", lambda e: e.transpose(pk[0:64, h * 128:(h + 1) * 128], qkv[:, 512 + h * 64:512 + (h + 1) * 64],
                                                      C.identb_s[:, :]), r=[qkv, C.identb_s], w=[psK])
                kb.op("act", lambda e: e.copy(out=qT[:, :, :].rearrange("p h t -> p (h t)"), in_=pq[0:64, :]), r=[psQ], w=[qT])
                kb.op("dve", lambda e: e.tensor_tensor(out=qTd[:, :, :], in0=pq[0:64, :].rearrange("p (h t) -> p h t", h=8),
                                                       in1=DQ[:, d_ * 8:(d_ + 1) * 8, :], op=ALU.mult), r=[psQ, DQ], w=[qTd])
                kb.op("act", lambda e: e.copy(out=kT[:, :, :].rearrange("p h t -> p (h t)"), in_=pk[0:64, :]), r=[psK], w=[kT])
                kb.op("dve", lambda e: e.tensor_tensor(out=kdec[:, :, :], in0=qkv[:, 512:1024].rearrange("p (h d) -> p h d", h=8),
                                                        in1=DK[:, d_ * 8:(d_ + 1) * 8].unsqueeze(2).to_broadcast([128, 8, 64]),
                                                        op=ALU.mult), r=[qkv, DK], w=[kdec])
                if getattr(C, "dbgB", 9) < 2:
                    continue
                for h in range(8):
                    pS = psS[h % 2]
                    sm = smb[h % 2]
                    pO = psO[h // 4]
                    pKV = psKV[h // 4]
                    oc = (h % 4) * 128
                    vh = qkv[:, 1024 + h * 128:1024 + (h + 1) * 128]
                    kb.op("pe", lambda e: e.matmul(pS[:, 0:128], lhsT=kT[:, h, :], rhs=qT[:, h, :], start=True, stop=True),
                          r=[kT, qT], w=[pS])
                    kb.op("dve", lambda e: e.tensor_tensor(out=sm[:, :], in0=pS[:, 0:128], in1=maskT[:, d_ * 8 + h, :], op=ALU.mult),
                          r=[pS, maskT], w=[sm])
                    kb.op("pe", lambda e: e.matmul(pO[:, oc:oc + 128], lhsT=sm[:, :], rhs=vh, start=True, stop=False),
                          r=[sm, qkv], w=[pO])
                    kb.op("pe", lambda e: e.matmul(pO[:, oc:oc + 128], lhsT=qTd[:, h, :], rhs=Sb[:, h, :], start=False, stop=True),
                          r=[qTd, Sb], w=[pO])
                    kb.op("pe", lambda e: e.matmul(pKV[0:64, oc:oc + 128], lhsT=kdec[:, h, :], rhs=vh, start=True, stop=True),
                          r=[kdec, qkv], w=[pKV])
                if getattr(C, "dbgB", 9) < 3:
                    continue
                kb.op("dve", lambda e: e.tensor_tensor(out=S[:, :, :], in0=S[:, :, :],
                                                       in1=GC[:, d_ * 8:(d_ + 1) * 8].unsqueeze(2).to_broadcast([64, 8, 128]), op=ALU.mult),
                      r=[S, GC], w=[S])
                for hb in range(2):
                    kb.op("dve", lambda e: e.tensor_tensor(out=S[:, hb * 4:(hb + 1) * 4, :], in0=S[:, hb * 4:(hb + 1) * 4, :],
                                                           in1=psKV[hb][0:64, :].rearrange("p (h t) -> p h t", h=4), op=ALU.add),
                          r=[S, psKV[hb]], w=[S])
                kb.op("act", lambda e: e.copy(out=Sb[:, :, :].rearrange("p h t -> p (h t)"), in_=S[:, :, :].rearrange("p h t -> p (h t)")), r=[S], w=[Sb])
                if d_ == 0:
                    for hb in range(2):
                        kb.op("act", lambda e: e.copy(out=of[:, hb * 512:(hb + 1) * 512], in_=psO[hb][:, :]), r=[psO[hb]], w=[of])
                    kb.dma("sp", C.OF[t * 128:(t + 1) * 128, :], of[:, :], r=[of], w=[kb.tag("OF", t)])
                else:
                    for hb in range(2):
                        kb.op("dve", lambda e: e.tensor_tensor(out=osum[:, hb * 512:(hb + 1) * 512], in0=psO[hb][:, :],
                                                               in1=of[:, hb * 512:(hb + 1) * 512], op=ALU.add),
                              r=[psO[hb], of], w=[osum])
                    kb.op("act", lambda e: e.activation(out=junk[:, :], in_=osum[:, :], func=AF.Square), r=[osum], w=[junk])
                    kb.op("dve", lambda e: e.reduce_sum(out=ssh[:, :], in_=junk[:, :].rearrange("p (h d) -> p h d", h=8), axis=AX.X),
                          r=[junk], w=[ssh])
                    rsqrt_mean(C, rs8, ssh, 8, 1.0 / 128, tmp8)
                    kb.op("dve", lambda e: e.tensor_tensor(out=osum[:, :].rearrange("p (h d) -> p h d", h=8),
                                                           in0=osum[:, :].rearrange("p (h d) -> p h d", h=8),
                                                           in1=rs8[:, :].unsqueeze(2).to_broadcast([128, 8, 128]), op=ALU.mult),
                          r=[osum, rs8], w=[osum])
                    kb.op("pool", lambda e: e.tensor_tensor(out=mix[:, :], in0=osum[:, :], in1=gate[:, :], op=ALU.mult),
                          r=[osum, gate], w=[mix])
                    kb.dma("sp", C.MIX[t * 128:(t + 1) * 128, 0:1024], mix[:, :], r=[mix], w=[kb.tag("MIXr", t)])


def phaseC_att(C, l, qblocks=None, key_tiles=None):
    kb = C.kb
    ps = C.ps
    if key_tiles is None:
        key_tiles = list(range(NT))
    if qblocks is None:
        qblocks = [([0, 1], [0, 1])] + [([2 + 4 * b + j for j in range(4)], key_tiles) for b in range(16)]
    with kb.phase():
        kTa = kb.sb("kTa", [64, 2, T], BF16)
        Va = kb.sb("Va", [128, NT, 2, 128], BF16)
        kb.op("pool", lambda e: e.memset(Va[:, :, :, :], 1.0), w=[Va])
        kvb = [kb.sb("kvld", [128, 256], BF16) for _ in range(2)]
        pT0 = ps[0]
        pt0 = pT0[:, :].bitcast(BF16)
        for n_, t in enumerate(key_tiles):
            kv = kvb[n_ % 2]
            kb.dma("sp", kv[:, :], C.PA[t * 128:(t + 1) * 128, 3584:3840], r=[kb.tag("PA", t)], w=[kv])
            for g in range(2):
                kb.op("pe", lambda e: e.transpose(pt0[0:64, g * 128:(g + 1) * 128], kv[:, g * 64:(g + 1) * 64], C.identb_s[:, :]),
                      r=[kv, C.identb_s], w=[pT0])
            for g in range(2):
                kb.op("act", lambda e: e.copy(out=kTa[:, g, t * 128:(t + 1) * 128], in_=pt0[0:64, g * 128:(g + 1) * 128]),
                      r=[pT0], w=[kTa])
            kb.op("pool", lambda e: e.tensor_copy(out=Va[:, t, :, 0:64], in_=kv[:, 128:256].rearrange("p (g d) -> p g d", g=2)),
                  r=[kv], w=[Va])
        aqb = [kb.sb("aq", [128, 4, 512], BF16) for _ in range(2)]
        qTb = kb.sb("qTb", [64, 8, 512], BF16)
        pTb = [kb.sb("pT", [128, 512], BF16) for _ in range(3)]
        rsb = kb.sb("rsb", [128, 512], F32)
        atb = [kb.sb("attT", [64, 512], BF16) for _ in range(2)]
        psS = [ps[2], ps[3], ps[4]]
        psO = [ps[5], ps[6]]
        psQ = [ps[0], ps[1]]
        for bi, (qt, keys) in enumerate(qblocks):
            nq = len(qt) * 128
            aq = aqb[bi % 2]
            for j, t in enumerate(qt):
                kb.dma("sp", aq[:, j, :], C.PA[t * 128:(t + 1) * 128, 3072:3584], r=[kb.tag("PA", t)], w=[aq])
            for hp in range(4):
                pQ = psQ[hp % 2]
                pqv = pQ[:, :].bitcast(BF16)
                for hh in range(2):
                    h = hp * 2 + hh
                    for j in range(len(qt)):
                        kb.op("pe", lambda e: e.transpose(pqv[0:64, hh * 512 + j * 128:hh * 512 + (j + 1) * 128],
                                                          aq[:, j, h * 64:(h + 1) * 64], C.identb_s[:, :]),
                              r=[aq, C.identb_s], w=[pQ])
                kb.op("dve", lambda e: e.tensor_copy(out=qTb[:, hp * 2:hp * 2 + 2, 0:nq],
                                                     in_=pqv[0:64, :].rearrange("p (h q) -> p h q", h=2)[:, :, 0:nq]),
                      r=[pQ], w=[qTb])
            tok0 = qt[0] * 128
            for h in range(8):
                g = h // 4
                pO = psO[h % 2]
                at = atb[h % 2]
                nk = len(keys)

                def s_mm(ki):
                    kt = keys[ki]
                    pS = psS[ki % 3]
                    kb.op("pe", lambda e: e.matmul(pS[:, 0:nq], lhsT=kTa[:, g, kt * 128:(kt + 1) * 128], rhs=qTb[:, h, 0:nq],
                                                   start=True, stop=True), r=[kTa, qTb], w=[pS])
                s_mm(0)
                if nk > 1:
                    s_mm(1)
                for ki in range(nk):
                    kt = keys[ki]
                    pS = psS[ki % 3]
                    pT = pTb[ki % 3]
                    kb.op("act", lambda e: e.activation(out=pT[:, 0:nq], in_=pS[:, 0:nq], func=AF.Exp), r=[pS], w=[pT])
                    if ki + 2 < nk:
                        s_mm(ki + 2)
                    kb.op("pe", lambda e: e.matmul(pO[:, 0:nq], lhsT=Va[:, kt, g, :], rhs=pT[:, 0:nq], start=(ki == 0), stop=(ki == nk - 1)),
                          r=[Va, pT], w=[pO])
                kb.op("dve", lambda e: e.reciprocal(out=rsb[64:128, 0:nq], in_=pO[64:128, 0:nq]), r=[pO], w=[rsb])
                kb.op("dve", lambda e: e.tensor_tensor(out=at[:, 0:nq], in0=pO[0:64, 0:nq], in1=rsb[64:128, 0:nq], op=ALU.mult),
                      r=[pO, rsb], w=[at])
                kb.dma("sp", C.AT[h * 64:(h + 1) * 64, tok0:tok0 + nq], at[:, 0:nq], r=[at], w=[kb.tag("AT", bi)])


def phaseD_out(C, l, tiles, w_out_ap, nk, mix_src, post):
    kb = C.kb
    ps = C.ps
    with kb.phase():
        Wo = kb.sb("Wo", [128, nk, D], BF16)
        wv = w_out_ap.rearrange("(k p) c -> p k c", p=128)
        for k in range(nk):
            kb.dma("pool", Wo[:, k, :], wv[:, k, :], w=[Wo])
        Gt = kb.sb("Gt", [128, 4096], F32)
        kb.dma("sp", Gt[:, :], C.G[l], r=[kb.tag("G", l)], w=[Gt])
        mTb = [kb.sb("mT", [128, nk, 128], BF16) for _ in range(2)]
        xb = [kb.sb("xd", [128, D], F32) for _ in range(2)]
        yb = kb.sb("yb", [128, D], F32)
        st = post(None, None, None, setup=True)
        for n_, t in enumerate(tiles):
            lat = t >= 2
            mT = mTb[n_ % 2]
            xt = xb[n_ % 2]
            kb.dma("sp", xt[:, :], C.X[t * 128:(t + 1) * 128, :], r=[kb.tag("X", t)], w=[xt])
            mix_src(t, mT, n_)
            psY = [ps[6], ps[7]]
            for nb in range(2):
                for k in range(nk):
                    kb.op("pe", lambda e: e.matmul(psY[nb][:, :], lhsT=mT[:, k, :], rhs=Wo[:, k, nb * 512:(nb + 1) * 512],
                                                   start=(k == 0), stop=(k == nk - 1)), r=[mT, Wo], w=[psY[nb]])
            g0 = 0 if lat else 2048
            for nb in range(2):
                kb.op("dve", lambda e: e.tensor_tensor(out=yb[:, nb * 512:(nb + 1) * 512], in0=psY[nb][:, :],
                                                       in1=Gt[:, g0 + nb * 512:g0 + (nb + 1) * 512], op=ALU.mult),
                      r=[psY[nb], Gt], w=[yb])
            kb.op("pool", lambda e: e.tensor_tensor(out=xt[:, :], in0=xt[:, :], in1=yb[:, :], op=ALU.add), r=[xt, yb], w=[xt])
            kb.dma("sp", C.X[t * 128:(t + 1) * 128, :], xt[:, :], r=[xt], w=[kb.tag("X", t)])
            post(t, xt, Gt, st=st)
        post(None, None, None, st=st, finish=True)


def even_mix_src(C):
    kb = C.kb
    mrb = [kb.sb("mr", [128, D], BF16) for _ in range(2)]
    ATv = C.AT.rearrange("(c p) t -> p c t", p=128)

    def src(t, mT, n_):
        mr = mrb[n_ % 2]
        bi = 0 if t < 2 else 1 + (t - 2) // 4
        kb.dma("sp", mr[:, :], C.MIX[t * 128:(t + 1) * 128, 0:1024], r=[kb.tag("MIXr", t)], w=[mr])
        kb.dma("sp", mT[:, 8:12, :], ATv[:, :, t * 128:(t + 1) * 128], r=[kb.tag("AT", bi)], w=[mT])
        pT = C.ps[0]
        pv = pT[:, :].bitcast(BF16)
        for k in range(8):
            kb.op("pe", lambda e: e.transpose(pv[:, k * 128:(k + 1) * 128], mr[:, k * 128:(k + 1) * 128], C.identb_s[:, :]),
                  r=[mr, C.identb_s], w=[pT])
        kb.op("act", lambda e: e.copy(out=mT[:, 0:8, :].rearrange("p k t -> p (k t)"), in_=pv[:, :]), r=[pT], w=[mT])
    return src


NSLOT = 164
TPAD = T + 128
BIGI = 1.0e6
NEG = -1.0e30


def moe_consts():
    import ml_dtypes
    p = np.arange(128)
    tri = (p[:, None] < p[None, :]).astype(np.float32).astype(ml_dtypes.bfloat16)
    ones = np.ones((128, 128), np.float32).astype(ml_dtypes.bfloat16)
    eidx = np.tile(np.arange(32, dtype=np.float32)[None, :], (128, 1))
    pidx = p.astype(np.float32)[:, None]
    sidx = np.tile(np.arange(NSLOT, dtype=np.float32)[None, :], (128, 1))
    mconst = np.ascontiguousarray(np.concatenate([eidx, pidx, sidx], axis=1), np.float32)
    src = (np.arange(NT)[None, :] * 128 + p[:, None]).astype(np.int32)
    li = np.zeros((NSLOT + 1, 128, 4), np.int32)
    li[:, :, 1] = T + p[None, :]
    li = np.ascontiguousarray(li.transpose(1, 0, 2).reshape(128, (NSLOT + 1) * 4))
    return np.ascontiguousarray(np.concatenate([tri, ones], axis=1)), mconst, src, li


def phaseD(C, l, tiles, w_out_ap, nk, mix_src_factory, route_tiles):
    kb = C.kb
    ps = C.ps
    with kb.phase():
        Wo = kb.sb("Wo", [128, nk, D], BF16)
        wv = w_out_ap.rearrange("(k p) c -> p k c", p=128)
        for k in range(nk):
            kb.dma("pool", Wo[:, k, :], wv[:, k, :], w=[Wo])
        Gt = kb.sb("Gt", [128, 8192], F32)
        kb.dma("sp", Gt[:, :], C.G[l], r=[kb.tag("G", l)], w=[Gt])
        nrep = kb.sb("nrep", [128, D], F32)
        kb.dma("sp", nrep[:, :], C.norms[DEPTH + l].partition_broadcast(128), w=[nrep])
        for v_ in range(2):
            o0 = (5 + 2 * v_) * 1024
            kb.op("dve", lambda e: e.scalar_tensor_tensor(out=Gt[:, o0:o0 + 1024], in0=Gt[:, o0:o0 + 1024], scalar=1.0, in1=nrep[:, :],
                                                          op0=ALU.add, op1=ALU.mult), r=[Gt, nrep], w=[Gt])
        Wr = kb.sb("Wr", [128, 8, 36], F32)
        kb.dma("sp", Wr[:, :, :], C.wr[l].rearrange("(k p) c -> p k c", p=128), w=[Wr])
        brr = kb.sb("brr", [128, 36], F32)
        kb.dma("sp", brr[:, :], C.br[l].partition_broadcast(128), w=[brr])
        mc = kb.sb("mc", [128, 33 + NSLOT], F32)
        kb.dma("sp", mc[:, :], C.mconst, w=[mc])
        tro = kb.sb("tro", [128, 256], BF16)
        kb.dma("sp", tro[:, :], C.triones, w=[tro])
        srci = kb.sb("srci", [128, NT], I32)
        kb.dma("sp", srci[:, :], C.srcidx, w=[srci])
        linit = kb.sb("linit", [128, (NSLOT + 1) * 4], I32)
        kb.dma("sp", linit[:, :], C.listinit, w=[linit])
        kb.dma("sp", C.LIST.rearrange("(s p) c -> p s c", p=128), linit[:, :].rearrange("p (s c) -> p s c", c=4), r=[linit],
               w=[kb.tag("LISTinit")])
        base = kb.sb("base", [128, 32], F32)
        kb.op("dve", lambda e: e.memset(base[:, :], 0.0), w=[base])
        RT = kb.sb("RT", [128, NT, 8], F32)
        kb.op("dve", lambda e: e.memset(RT[:, :, :], 0.0), w=[RT])
        mTb = [kb.sb("mT", [128, nk, 128], BF16) for _ in range(2)]
        xb = [kb.sb("xd", [128, D], F32) for _ in range(2)]
        yb = kb.sb("yb", [128, D], F32)
        junk = kb.sb("junkd", [128, D], BF16)
        xn2 = kb.sb("xn2", [128, D], F32)
        fb = [kb.sb("fb", [128, D], BF16) for _ in range(2)]
        fT = kb.sb("fT", [128, 8, 128], F32)
        ss = kb.sb("ssd", [128, 1], F32)
        rstd = kb.sb("rstdd", [128, 1], F32)
        tmp1 = kb.sb("tmp1d", [128, 8], F32)
        lg = kb.sb("lg", [128, 36], F32)
        sm = kb.sb("smalls", [128, 16], F32)
        geq = kb.sb("geq", [128, 4], F32)
        gex = kb.sb("gex", [128, 4], F32)
        ml = kb.sb("ml", [128, 32], F32)
        ml2 = kb.sb("ml2", [128, 32], F32)
        oh1 = kb.sb("oh1", [128, 32], F32)
        oh2 = kb.sb("oh2", [128, 32], F32)
        ind = kb.sb("ind", [128, 32], BF16)
        pos = kb.sb("pos", [128, 32], F32)
        t32 = kb.sb("t32", [128, 32], F32)
        mix_src = mix_src_factory()
        EIDX = mc[:, 0:32]
        for n_, t in enumerate(tiles):
            lat = t >= 2
            mT = mTb[n_ % 2]
            xt = xb[n_ % 2]
            kb.dma("sp", xt[:, :], C.X[t * 128:(t + 1) * 128, :], r=[kb.tag("X", t)], w=[xt])
            mix_src(t, mT, n_)
            psY = [ps[6], ps[7]]
            for nb in range(2):
                for k in range(nk):
                    kb.op("pe", lambda e: e.matmul(psY[nb][:, :], lhsT=mT[:, k, :], rhs=Wo[:, k, nb * 512:(nb + 1) * 512],
                                                   start=(k == 0), stop=(k == nk - 1)), r=[mT, Wo], w=[psY[nb]])
            g0 = 0 if lat else 2048
            for nb in range(2):
                kb.op("dve", lambda e: e.tensor_tensor(out=yb[:, nb * 512:(nb + 1) * 512], in0=psY[nb][:, :],
                                                       in1=Gt[:, g0 + nb * 512:g0 + (nb + 1) * 512], op=ALU.mult),
                      r=[psY[nb], Gt], w=[yb])
            kb.op("pool", lambda e: e.tensor_tensor(out=xt[:, :], in0=xt[:, :], in1=yb[:, :], op=ALU.add), r=[xt, yb], w=[xt])
            kb.dma("sp", C.X[t * 128:(t + 1) * 128, :], xt[:, :], r=[xt], w=[kb.tag("X", t)])
            if t not in route_tiles:
                continue
            f = fb[n_ % 2]
            norm_tile(C, xt, ss, rstd, tmp1, junk, xn2)
            v0 = 4096 if lat else 6144
            kb.op("dve", lambda e: e.tensor_tensor(out=xn2[:, :], in0=xn2[:, :], in1=Gt[:, v0 + 1024:v0 + 2048], op=ALU.mult),
                  r=[xn2, Gt], w=[xn2])
            kb.op("pool", lambda e: e.tensor_tensor(out=xn2[:, :], in0=xn2[:, :], in1=Gt[:, v0:v0 + 1024], op=ALU.add),
                  r=[xn2, Gt], w=[xn2])
            kb.op("act", lambda e: e.copy(out=f[:, :], in_=xn2[:, :]), r=[xn2], w=[f])
            kb.dma("sp", C.F[t * 128:(t + 1) * 128, :], f[:, :], r=[f], w=[kb.tag("F", t)])
            psT = [ps[0], ps[1]]
            for k in range(8):
                kb.op("pe", lambda e: e.transpose(psT[k // 4][:, (k % 4) * 128:(k % 4 + 1) * 128], xn2[:, k * 128:(k + 1) * 128],
                                                  C.identf_s[:, :]), r=[xn2, C.identf_s], w=[psT[k // 4]])
            for hb in range(2):
                kb.op("act", lambda e: e.copy(out=fT[:, hb * 4:(hb + 1) * 4, :].rearrange("p k t -> p (k t)"), in_=psT[hb][:, :]),
                      r=[psT[hb]], w=[fT])
            pL = ps[2]
            for k in range(8):
                kb.op("pe", lambda e: e.matmul(pL[:, 0:36], lhsT=fT[:, k, :], rhs=Wr[:, k, :], start=(k == 0), stop=(k == 7)),
                      r=[fT, Wr], w=[pL])
            kb.op("dve", lambda e: e.tensor_tensor(out=lg[:, :], in0=pL[:, 0:36], in1=brr[:, :], op=ALU.add), r=[pL, brr], w=[lg])
            gmax, ngmax, gsum, pg, m1, m2, dd, ed, w1, w2 = [sm[:, i:i + 1] for i in range(10)]
            kb.op("dve", lambda e: e.reduce_max(out=gmax, in_=lg[:, 0:4], axis=AX.X), r=[lg], w=[sm])
            kb.op("dve", lambda e: e.tensor_scalar(out=ngmax, in0=gmax, scalar1=-1.0, scalar2=None, op0=ALU.mult), r=[sm], w=[sm])
            kb.op("dve", lambda e: e.tensor_scalar(out=geq[:, :], in0=lg[:, 0:4], scalar1=gmax, scalar2=None, op0=ALU.is_equal),
                  r=[lg, sm], w=[geq])
            kb.op("act", lambda e: e.activation(out=gex[:, :], in_=lg[:, 0:4], func=AF.Exp, bias=ngmax, scale=1.0, accum_out=gsum),
                  r=[lg, sm], w=[gex, sm])
            kb.op("dve", lambda e: e.reciprocal(out=pg, in_=gsum), r=[sm], w=[sm])
            kb.op("dve", lambda e: e.tensor_scalar(out=geq[:, :], in0=geq[:, :], scalar1=-NEG, scalar2=NEG, op0=ALU.mult, op1=ALU.add),
                  r=[geq], w=[geq])
            kb.op("dve", lambda e: e.tensor_tensor(out=ml[:, :].rearrange("p (g e) -> p g e", g=4),
                                                   in0=lg[:, 4:36].rearrange("p (g e) -> p g e", g=4),
                                                   in1=geq[:, :].unsqueeze(2).to_broadcast([128, 4, 8]), op=ALU.add), r=[lg, geq], w=[ml])
            kb.op("dve", lambda e: e.reduce_max(out=m1, in_=ml[:, :], axis=AX.X), r=[ml], w=[sm])
            kb.op("dve", lambda e: e.tensor_scalar(out=oh1[:, :], in0=ml[:, :], scalar1=m1, scalar2=None, op0=ALU.is_equal),
                  r=[ml, sm], w=[oh1])
            kb.op("dve", lambda e: e.scalar_tensor_tensor(out=ml2[:, :], in0=oh1[:, :], scalar=NEG, in1=ml[:, :], op0=ALU.mult, op1=ALU.add),
                  r=[oh1, ml], w=[ml2])
            kb.op("dve", lambda e: e.reduce_max(out=m2, in_=ml2[:, :], axis=AX.X), r=[ml2], w=[sm])
            kb.op("dve", lambda e: e.tensor_scalar(out=oh2[:, :], in0=ml2[:, :], scalar1=m2, scalar2=None, op0=ALU.is_equal),
                  r=[ml2, sm], w=[oh2])
            kb.op("dve", lambda e: e.tensor_tensor(out=dd, in0=m2, in1=m1, op=ALU.subtract), r=[sm], w=[sm])
            kb.op("act", lambda e: e.activation(out=ed, in_=dd, func=AF.Exp), r=[sm], w=[sm])
            kb.op("dve", lambda e: e.tensor_scalar(out=w1, in0=ed, scalar1=1.0, scalar2=None, op0=ALU.add), r=[sm], w=[sm])
            kb.op("dve", lambda e: e.reciprocal(out=w1, in_=w1), r=[sm], w=[sm])
            kb.op("dve", lambda e: e.tensor_tensor(out=w2, in0=ed, in1=w1, op=ALU.mult), r=[sm], w=[sm])
            kb.op("dve", lambda e: e.tensor_tensor(out=RT[:, t, 2:3], in0=w1, in1=pg, op=ALU.mult), r=[sm], w=[RT])
            kb.op("dve", lambda e: e.tensor_tensor(out=RT[:, t, 5:6], in0=w2, in1=pg, op=ALU.mult), r=[sm], w=[RT])
            kb.op("dve", lambda e: e.tensor_tensor(out=ind[:, :], in0=oh1[:, :], in1=oh2[:, :], op=ALU.add), r=[oh1, oh2], w=[ind])
            pR = ps[3]
            kb.op("pe", lambda e: e.matmul(pR[:, 0:32], lhsT=tro[:, 0:128], rhs=ind[:, :], start=True, stop=True), r=[tro, ind], w=[pR])
            kb.op("pe", lambda e: e.matmul(pR[:, 32:64], lhsT=tro[:, 128:256], rhs=ind[:, :], start=True, stop=True), r=[tro, ind], w=[pR])
            kb.op("dve", lambda e: e.tensor_tensor(out=pos[:, :], in0=pR[:, 0:32], in1=base[:, :], op=ALU.add), r=[pR, base], w=[pos])
            kb.op("dve", lambda e: e.tensor_tensor(out=base[:, :], in0=pR[:, 32:64], in1=base[:, :], op=ALU.add), r=[pR, base], w=[base])
            for k_, oh in enumerate((oh1, oh2)):
                kb.op("dve", lambda e: e.tensor_tensor(out=t32[:, :], in0=oh[:, :], in1=EIDX, op=ALU.mult), r=[oh, mc], w=[t32])
                kb.op("dve", lambda e: e.reduce_sum(out=RT[:, t, 3 * k_:3 * k_ + 1], in_=t32[:, :], axis=AX.X), r=[t32], w=[RT])
                kb.op("dve", lambda e: e.tensor_tensor(out=t32[:, :], in0=oh[:, :], in1=pos[:, :], op=ALU.mult), r=[oh, pos], w=[t32])
                kb.op("dve", lambda e: e.reduce_sum(out=RT[:, t, 3 * k_ + 1:3 * k_ + 2], in_=t32[:, :], axis=AX.X), r=[t32], w=[RT])
        ni = kb.sb("ni", [128, 32], I32)
        ca = kb.sb("ca", [128, 32], F32)
        cb = kb.sb("cb", [128, 32], F32)
        tl_ = kb.sb("tl", [128, 32], F32)
        kb.op("dve", lambda e: e.tensor_scalar(out=t32[:, :], in0=base[:, :], scalar1=127.0, scalar2=None, op0=ALU.add), r=[base], w=[t32])
        kb.op("dve", lambda e: e.tensor_copy(out=ni[:, :], in_=t32[:, :]), r=[t32], w=[ni])
        kb.op("dve", lambda e: e.tensor_scalar(out=ni[:, :], in0=ni[:, :], scalar1=7, scalar2=None, op0=ALU.arith_shift_right), r=[ni], w=[ni])
        kb.op("dve", lambda e: e.tensor_copy(out=tl_[:, :], in_=ni[:, :]), r=[ni], w=[tl_])
        kb.op("dve", lambda e: e.tensor_copy(out=ca[:, :], in_=tl_[:, :]), r=[tl_], w=[ca])
        a_, b_ = ca, cb
        for d_ in (1, 2, 4, 8, 16):
            kb.op("dve", lambda e: e.tensor_copy(out=b_[:, 0:d_], in_=a_[:, 0:d_]), r=[a_], w=[b_])
            kb.op("dve", lambda e: e.tensor_tensor(out=b_[:, d_:32], in0=a_[:, d_:32], in1=a_[:, 0:32 - d_], op=ALU.add), r=[a_], w=[b_])
            a_, b_ = b_, a_
        cum = a_
        ss128 = b_
        kb.op("dve", lambda e: e.tensor_tensor(out=ss128[:, :], in0=cum[:, :], in1=tl_[:, :], op=ALU.subtract), r=[cum, tl_], w=[ss128])
        kb.op("dve", lambda e: e.tensor_scalar(out=ss128[:, :], in0=ss128[:, :], scalar1=128.0, scalar2=None, op0=ALU.mult),
              r=[ss128], w=[ss128])
        big = kb.sb("big", [128, NT, 32], F32)
        offf = kb.sb("offf", [128, 2, NT], F32)
        offi = kb.sb("offi", [128, 2, NT], I32)
        ent = kb.sb("ent", [128, 2, NT, 4], I32)
        kb.op("dve", lambda e: e.memset(ent[:, :, :, :], 0), w=[ent])
        for k_ in range(2):
            kb.op("dve", lambda e: e.tensor_tensor(out=big[:, :, :], in0=mc[:, 0:32].unsqueeze(1).to_broadcast([128, NT, 32]),
                                                   in1=RT[:, :, 3 * k_:3 * k_ + 1].to_broadcast([128, NT, 32]), op=ALU.is_equal),
                  r=[mc, RT], w=[big])
            kb.op("dve", lambda e: e.tensor_tensor(out=big[:, :, :], in0=big[:, :, :],
                                                   in1=ss128[:, :].unsqueeze(1).to_broadcast([128, NT, 32]), op=ALU.mult),
                  r=[big, ss128], w=[big])
            kb.op("dve", lambda e: e.reduce_sum(out=offf[:, k_, :], in_=big[:, :, :], axis=AX.X), r=[big], w=[offf])
            kb.op("dve", lambda e: e.tensor_tensor(out=offf[:, k_, :], in0=offf[:, k_, :], in1=RT[:, :, 3 * k_ + 1], op=ALU.add),
                  r=[offf, RT], w=[offf])
            kb.op("dve", lambda e: e.tensor_copy(out=offi[:, k_, :], in_=offf[:, k_, :]), r=[offf], w=[offi])
            kb.op("dve", lambda e: e.tensor_copy(out=ent[:, k_, :, 0], in_=srci[:, :]), r=[srci], w=[ent])
            kb.op("dve", lambda e: e.tensor_scalar(out=ent[:, k_, :, 1], in0=srci[:, :], scalar1=k_ * TPAD, scalar2=None, op0=ALU.add),
                  r=[srci], w=[ent])
            kb.op("dve", lambda e: e.tensor_copy(out=ent[:, k_, :, 2], in_=RT[:, :, 3 * k_ + 2].bitcast(I32)), r=[RT], w=[ent])
        if hasattr(C, "dbgD"):
            kb.dma("sp", C.dbgD["offi"], offi[:, :, :].rearrange("p k t -> p (k t)"), r=[offi], w=[kb.tag("dbg1")])
            kb.dma("sp", C.dbgD["RT"], RT[:, :, :].rearrange("p t c -> p (t c)"), r=[RT], w=[kb.tag("dbg2")])
            kb.dma("sp", C.dbgD["base"], base[:, :], r=[base], w=[kb.tag("dbg4")])
            kb.dma("sp", C.dbgD["ent"], ent[:, :, :, :].rearrange("p k t c -> p (k t c)"), r=[ent], w=[kb.tag("dbg5")])
        for t in (route_tiles if not getattr(C, "skip_scatter", False) else []):
            for k_ in range(2):
                kb.dma("pool", C.LIST, ent[:, k_, t, :], r=[ent, offi, kb.tag("LISTinit")], w=[kb.tag("LISTs", t, k_)],
                       indirect=dict(out_offset=bass.IndirectOffsetOnAxis(ap=offi[:, k_, t:t + 1], axis=0), in_offset=None))
        SIDX = mc[:, 33:33 + NSLOT]
        cmp_ = kb.sb("cmp", [128, NSLOT, 32], F32)
        eid = kb.sb("eid", [128, NSLOT + 2], F32)
        kb.op("dve", lambda e: e.memset(eid[:, 0:2], -1.0), w=[eid])
        kb.op("dve", lambda e: e.tensor_tensor(out=cmp_[:, :, :], in0=cum[:, :].unsqueeze(1).to_broadcast([128, NSLOT, 32]),
                                               in1=SIDX.unsqueeze(2).to_broadcast([128, NSLOT, 32]), op=ALU.is_le), r=[cum, mc], w=[cmp_])
        kb.op("dve", lambda e: e.reduce_sum(out=eid[:, 2:2 + NSLOT], in_=cmp_[:, :, :], axis=AX.X), r=[cmp_], w=[eid])
        ldf = kb.sb("ldf", [128, NSLOT], F32)
        vld = kb.sb("vld", [128, NSLOT], F32)
        wix = kb.sb("wix", [128, NSLOT], F32)
        kb.op("dve", lambda e: e.tensor_tensor(out=ldf[:, :], in0=eid[:, 2:2 + NSLOT], in1=eid[:, 0:NSLOT], op=ALU.not_equal), r=[eid], w=[ldf])
        kb.op("dve", lambda e: e.tensor_scalar(out=vld[:, :], in0=eid[:, 2:2 + NSLOT], scalar1=31.5, scalar2=None, op0=ALU.is_lt), r=[eid], w=[vld])
        kb.op("dve", lambda e: e.tensor_tensor(out=ldf[:, :], in0=ldf[:, :], in1=vld[:, :], op=ALU.mult), r=[ldf, vld], w=[ldf])
        kb.op("dve", lambda e: e.tensor_scalar(out=wix[:, :], in0=eid[:, 2:2 + NSLOT], scalar1=31.0, scalar2=128.0,
                                               op0=ALU.min, op1=ALU.mult), r=[eid], w=[wix])
        kb.op("dve", lambda e: e.tensor_scalar(out=wix[:, :], in0=wix[:, :], scalar1=mc[:, 32:33], scalar2=float(l * 4096), op0=ALU.add,
                                               op1=ALU.add), r=[wix, mc], w=[wix])
        if getattr(C, "moe_skip", False):
            kb.op("dve", lambda e: e.tensor_scalar(out=wix[:, :], in0=wix[:, :], scalar1=-BIGI, scalar2=None, op0=ALU.add), r=[wix], w=[wix])
            kb.op("dve", lambda e: e.tensor_tensor(out=wix[:, :], in0=wix[:, :], in1=ldf[:, :], op=ALU.mult), r=[wix, ldf], w=[wix])
            kb.op("dve", lambda e: e.tensor_scalar(out=wix[:, :], in0=wix[:, :], scalar1=BIGI, scalar2=None, op0=ALU.add), r=[wix], w=[wix])
        kb.op("dve", lambda e: e.tensor_copy(out=C.widx[:, :], in_=wix[:, :]), r=[wix], w=[C.widx])
        if hasattr(C, "dbgD"):
            kb.dma("sp", C.dbgD["widx"], C.widx[:, :], r=[C.widx], w=[kb.tag("dbg6")])
            kb.dma("sp", C.dbgD["eid"], eid[:, :], r=[eid], w=[kb.tag("dbg3")])


def phaseE(C, l, nslot=None, lvl=9):
    nslot = NSLOT if nslot is None else nslot
    kb = C.kb
    ps = C.ps
    with kb.phase():
        Wgu = [kb.sb("Wgu", [128, 8 * 1024], BF16) for _ in range(2)]
        Wd = [kb.sb("Wd", [128, 4 * 1024], BF16) for _ in range(2)]
        for a in range(2):
            kb.op("pool", lambda e: e.memset(Wgu[a][:, :], 0.0), w=[Wgu[a]])
            kb.op("pool", lambda e: e.memset(Wd[a][:, :], 0.0), w=[Wd[a]])
        entb = [kb.sb("ente", [128, 4], I32) for _ in range(2)]
        xsb = [kb.sb("xs", [128, D], BF16) for _ in range(2)]
        xsT = kb.sb("xsT", [128, 8, 128], BF16)
        actf = kb.sb("actf", [128, 512], F32)
        actb = kb.sb("actb", [128, 512], BF16)
        actT = kb.sb("actT", [128, 4, 128], BF16)
        ywb = [kb.sb("yw", [128, D], F32) for _ in range(2)]
        if not hasattr(C, "bc_reg"):
            C.bc_reg = C.nc.gpsimd.to_reg(DEPTH * 4096 - 1)
        wgu_v = C.wgu[l].rearrange("r (a c) -> r a c", c=2048)
        wd_v = C.wd[l].rearrange("r (a c) -> r a c", c=2048)

        def loads(s):
            A = s % 2
            ent = entb[A]
            msk = getattr(C, "ldmask", 15)
            kb.dma("sp", ent[:, :], C.LIST[s * 128:(s + 1) * 128, :], w=[ent])
            extra = dict(bounds_check=C.bc_reg, oob_is_err=False) if (msk & 8) else {}
            if msk & 1:
                kb.dma("pool", Wgu[A][:, :], C.wgu.rearrange("l r c -> (l r) c"), r=[C.widx], w=[Wgu[A]],
                       indirect=dict(out_offset=None, in_offset=bass.IndirectOffsetOnAxis(ap=C.widx[:, s:s + 1], axis=0), **extra))
            if msk & 2:
                kb.dma("pool", Wd[A][:, :], C.wd.rearrange("l r c -> (l r) c"), r=[C.widx], w=[Wd[A]],
                       indirect=dict(out_offset=None, in_offset=bass.IndirectOffsetOnAxis(ap=C.widx[:, s:s + 1], axis=0), **extra))
            if msk & 4:
                kb.dma("pool", xsb[A][:, :], C.F, r=[ent], w=[xsb[A]],
                       indirect=dict(out_offset=None, in_offset=bass.IndirectOffsetOnAxis(ap=ent[:, 0:1], axis=0)))

        loads(0)
        for s in range(nslot):
            A = s % 2
            ent, xs, yw = entb[A], xsb[A], ywb[A]
            if s + 1 < nslot:
                loads(s + 1)
            if lvl < 1:
                continue
            pT = ps[0]
            pv = pT[:, :].bitcast(BF16)
            for k in range(8):
                kb.op("pe", lambda e: e.transpose(pv[:, k * 128:(k + 1) * 128], xs[:, k * 128:(k + 1) * 128], C.identb_s[:, :]),
                      r=[xs, C.identb_s], w=[pT])
            kb.op("dve", lambda e: e.tensor_copy(out=xsT[:, :, :].rearrange("p k t -> p (k t)"), in_=pv[:, :]), r=[pT], w=[xsT])
            pG, pU = ps[1], ps[2]
            for nb, pp in enumerate((pG, pU)):
                for k in range(8):
                    kb.op("pe", lambda e: e.matmul(pp[:, :], lhsT=xsT[:, k, :], rhs=Wgu[A][:, k * 1024 + nb * 512:k * 1024 + (nb + 1) * 512],
                                                   start=(k == 0), stop=(k == 7)), r=[xsT, Wgu[A]], w=[pp])
            kb.op("act", lambda e: e.activation(out=actf[:, :], in_=pG[:, :], func=AF.Silu), r=[pG], w=[actf])
            kb.op("dve", lambda e: e.tensor_tensor(out=actb[:, :], in0=pU[:, :], in1=actf[:, :], op=ALU.mult), r=[pU, actf], w=[actb])
            pA = ps[3]
            pav = pA[:, :].bitcast(BF16)
            for c in range(4):
                kb.op("pe", lambda e: e.transpose(pav[:, c * 128:(c + 1) * 128], actb[:, c * 128:(c + 1) * 128], C.identb_s[:, :]),
                      r=[actb, C.identb_s], w=[pA])
            kb.op("act", lambda e: e.copy(out=actT[:, :, :].rearrange("p k t -> p (k t)"), in_=pav[:, 0:512]), r=[pA], w=[actT])
            pY = [ps[4 + 2 * (s % 2)], ps[5 + 2 * (s % 2)]]
            for nb in range(2):
                for c in range(4):
                    kb.op("pe", lambda e: e.matmul(pY[nb][:, :], lhsT=actT[:, c, :], rhs=Wd[A][:, c * 1024 + nb * 512:c * 1024 + (nb + 1) * 512],
                                                   start=(c == 0), stop=(c == 3)), r=[actT, Wd[A]], w=[pY[nb]])
            if lvl < 2:
                continue
            wcol = ent[:, 2:3].bitcast(F32)
            kb.op("dve", lambda e: e.tensor_scalar(out=yw[:, 0:512], in0=pY[0][:, :], scalar1=wcol, scalar2=None, op0=ALU.mult),
                  r=[pY[0], ent], w=[yw])
            kb.op("act", lambda e: e.activation(out=yw[:, 512:1024], in_=pY[1][:, :], func=AF.Copy, scale=wcol), r=[pY[1], ent], w=[yw])
            if lvl < 3:
                continue
            kb.dma("pool", C.YB, yw[:, :], r=[yw, ent], w=[kb.tag("YBs", s)],
                   indirect=dict(out_offset=bass.IndirectOffsetOnAxis(ap=ent[:, 1:2], axis=0), in_offset=None))


def combine_src(C, l_prev, store=True):
    kb = C.kb
    Gt = kb.sb("Gc", [128, 2048], F32)
    kb.dma("sp", Gt[:, 0:1024], C.G[l_prev][:, 1024:2048], r=[kb.tag("G", l_prev)], w=[Gt])
    kb.dma("sp", Gt[:, 1024:2048], C.G[l_prev][:, 3072:4096], r=[kb.tag("G", l_prev)], w=[Gt])
    y1b = [kb.sb("y1", [128, D], F32) for _ in range(2)]
    y2b = [kb.sb("y2", [128, D], F32) for _ in range(2)]
    cnt = [0]

    def src(t, xt):
        n_ = cnt[0]
        cnt[0] += 1
        y1, y2 = y1b[n_ % 2], y2b[n_ % 2]
        kb.dma("sp", xt[:, :], C.X[t * 128:(t + 1) * 128, :], r=[kb.tag("X", t)], w=[xt])
        kb.dma("sp", y1[:, :], C.YB[t * 128:(t + 1) * 128, :], w=[y1])
        kb.dma("sp", y2[:, :], C.YB[TPAD + t * 128:TPAD + (t + 1) * 128, :], w=[y2])
        g0 = 0 if t >= 2 else 1024
        kb.op("pool", lambda e: e.tensor_tensor(out=y1[:, :], in0=y1[:, :], in1=y2[:, :], op=ALU.add), r=[y1, y2], w=[y1])
        kb.op("dve", lambda e: e.tensor_tensor(out=y1[:, :], in0=y1[:, :], in1=Gt[:, g0:g0 + 1024], op=ALU.mult), r=[y1, Gt], w=[y1])
        kb.op("pool", lambda e: e.tensor_tensor(out=xt[:, :], in0=xt[:, :], in1=y1[:, :], op=ALU.add), r=[xt, y1], w=[xt])
        if store:
            kb.dma("sp", C.X[t * 128:(t + 1) * 128, :], xt[:, :], r=[xt], w=[kb.tag("X", t)])
    return src


TZ = T + 4


def zbase(t):
    return 1 + t * 128 if t < 2 else 259 + (t - 2) * 128


def dn_consts():
    p = np.arange(128)
    same = (p[:, None] // 64) == (p[None, :] // 64)
    mge = (same & (p[None, :] >= p[:, None])).astype(np.float32)
    mle = (same & (p[None, :] <= p[:, None])).astype(np.float32)
    slt = (same & (p[None, :] < p[:, None])).astype(np.float32)
    sgt = (same & (p[None, :] > p[:, None])).astype(np.float32)
    return np.ascontiguousarray(np.concatenate([mge, mle, slt, sgt], axis=1), np.float32)


def phaseA_odd(C, l, tiles, x_src):
    kb = C.kb
    i = l // 2
    ps = C.ps
    with kb.phase():
        W = kb.sb("Wino", [128, 8, ODD_IN], BF16)
        wv = C.od_w_in[i].rearrange("(k p) c -> p k c", p=128)
        for k in range(8):
            kb.dma("pool", W[:, k, :], wv[:, k, :], w=[W])
        gs1, sh_blk = layer_mod_tables(C, l, 0)
        prm = kb.sb("dnprm", [128, 32], F32)
        kb.dma("sp", prm[:, :], C.od_prm[i].partition_broadcast(128), w=[prm])
        kb.op("act", lambda e: e.activation(out=prm[:, 0:16], in_=prm[:, 0:16], func=AF.Exp), r=[prm], w=[prm])
        kb.op("dve", lambda e: e.tensor_scalar(out=prm[:, 0:16], in0=prm[:, 0:16], scalar1=-1.0, scalar2=None, op0=ALU.mult), r=[prm], w=[prm])
        zrow = kb.sb("zrow", [4, 3072], BF16)
        kb.op("dve", lambda e: e.memset(zrow[:, :], 0.0), w=[zrow])
        for n_, r0 in enumerate((0, 257, 258, TZ - 1)):
            kb.dma("sp", C.ZP[r0:r0 + 1, :], zrow[0:1, :], r=[zrow], w=[kb.tag("ZPz", n_)])
        xb = [kb.sb("xt", [128, D], F32) for _ in range(2)]
        junk = kb.sb("junk", [128, D], BF16)
        xn = kb.sb("xn", [128, D], BF16)
        hT = [kb.sb("hT", [128, 8, 128], BF16) for _ in range(2)]
        zt = [kb.sb("zt", [128, 4096], BF16) for _ in range(2)]
        gb = [kb.sb("gb", [128, 32], F32) for _ in range(2)]
        ss = kb.sb("ss", [128, 1], F32)
        rstd = kb.sb("rstd", [128, 1], F32)
        tmp1 = kb.sb("tmp1", [128, 8], F32)
        t16 = kb.sb("t16", [128, 16], F32)
        for n_, t in enumerate(tiles):
            r_ = 0 if t >= 2 else 1
            xt = xb[n_ % 2]
            h = hT[n_ % 2]
            z = zt[n_ % 2]
            g = gb[n_ % 2]
            x_src(t, xt)
            norm_tile(C, xt, ss, rstd, tmp1, junk, xn)
            psT = ps[0]
            pv = psT[:, :].bitcast(BF16)
            for k in range(8):
                kb.op("pe", lambda e: e.transpose(pv[:, k * 128:(k + 1) * 128], xn[:, k * 128:(k + 1) * 128], C.identb_s[:, :]),
                      r=[xn, C.identb_s], w=[psT])
            for k in range(8):
                kb.op("act", lambda e: e.activation(out=h[:, k, :], in_=pv[:, k * 128:(k + 1) * 128], func=AF.Identity,
                                                    scale=gs1[:, k, r_:r_ + 1], bias=C.modT[:, l, sh_blk + k, r_:r_ + 1]),
                      r=[psT, gs1, C.modT], w=[h])
            for j in range(9):
                c0 = j * 512
                nc_ = 512 if j < 8 else 32
                pj = ps[1 + (j % 6)]
                for k in range(8):
                    kb.op("pe", lambda e: e.matmul(pj[:, 0:nc_], lhsT=h[:, k, :], rhs=W[:, k, c0:c0 + nc_],
                                                   start=(k == 0), stop=(k == 7)), r=[h, W], w=[pj])
                if j < 6:
                    if j % 2 == 0:
                        kb.op("act", lambda e: e.copy(out=z[:, c0:c0 + 512], in_=pj[:, :]), r=[pj], w=[z])
                    else:
                        kb.op("dve", lambda e: e.tensor_copy(out=z[:, c0:c0 + 512], in_=pj[:, :]), r=[pj], w=[z])
                elif j < 8:
                    kb.op("act", lambda e: e.activation(out=z[:, c0:c0 + 512], in_=pj[:, :], func=AF.Silu), r=[pj], w=[z])
                else:
                    kb.op("dve", lambda e: e.tensor_tensor(out=t16[:, :], in0=pj[:, 0:16], in1=prm[:, 16:32], op=ALU.add), r=[pj, prm], w=[t16])
                    kb.op("act", lambda e: e.activation(out=t16[:, :], in_=t16[:, :], func=AF.Exp), r=[t16], w=[t16])
                    kb.op("act", lambda e: e.activation(out=t16[:, :], in_=t16[:, :], func=AF.Ln, bias=1.0, scale=1.0), r=[t16], w=[t16])
                    kb.op("dve", lambda e: e.tensor_tensor(out=g[:, 0:16], in0=t16[:, :], in1=prm[:, 0:16], op=ALU.mult), r=[t16, prm], w=[g])
                    kb.op("act", lambda e: e.activation(out=g[:, 16:32], in_=pj[:, 16:32], func=AF.Exp, scale=-1.0), r=[pj], w=[g])
                    kb.op("dve", lambda e: e.tensor_scalar(out=g[:, 16:32], in0=g[:, 16:32], scalar1=1.0, scalar2=None, op0=ALU.add), r=[g], w=[g])
                    kb.op("dve", lambda e: e.reciprocal(out=g[:, 16:32], in_=g[:, 16:32]), r=[g], w=[g])
            zb = zbase(t)
            kb.dma("sp", C.ZP[zb:zb + 128, :], z[:, 0:3072], r=[z], w=[kb.tag("ZP", t)])
            kb.dma("sp", C.PA[t * 128:(t + 1) * 128, 3072:4096], z[:, 3072:4096], r=[z], w=[kb.tag("PAz", t)])
            kb.dma("sp", C.GB[t * 128:(t + 1) * 128, :], g[:, :], r=[g], w=[kb.tag("GB", t)])
    with kb.phase():
        cw = kb.sb("convw", [128, 3, 3072], F32)
        for j in range(3):
            kb.dma("sp", cw[:, j, :], C.od_conv[i, j].partition_broadcast(128), w=[cw])
        zmb = [kb.sb("zm", [128, 3, 3072], BF16) for _ in range(2)]
        acc = kb.sb("acc", [128, 3072], F32)
        acc2 = kb.sb("acc2", [128, 3072], F32)
        sq = kb.sb("sqd", [128, 2048], F32)
        ssh = kb.sb("sshd", [128, 16], F32)
        rs = kb.sb("rsd", [128, 16], F32)
        t16b = kb.sb("t16b", [128, 16], F32)
        outb = [kb.sb("qkvo", [128, 3072], BF16) for _ in range(2)]
        for n_, t in enumerate(tiles):
            zm = zmb[n_ % 2]
            ob = outb[n_ % 2]
            zb = zbase(t)
            deps = [kb.tag("ZP", tt) for tt in (t - 1, t, t + 1) if 0 <= tt < NT and tt in tiles] + [kb.tag("ZPz", q) for q in range(4)]
            for j in range(3):
                kb.dma("sp", zm[:, j, :], C.ZP[zb - 1 + j:zb + 127 + j, :], r=deps, w=[zm])
            kb.op("dve", lambda e: e.tensor_tensor(out=acc[:, :], in0=zm[:, 0, :], in1=cw[:, 0, :], op=ALU.mult), r=[zm, cw], w=[acc])
            kb.op("pool", lambda e: e.tensor_tensor(out=acc2[:, :], in0=zm[:, 1, :], in1=cw[:, 1, :], op=ALU.mult), r=[zm, cw], w=[acc2])
            kb.op("dve", lambda e: e.tensor_tensor(out=acc[:, :], in0=acc[:, :], in1=acc2[:, :], op=ALU.add), r=[acc, acc2], w=[acc])
            kb.op("pool", lambda e: e.tensor_tensor(out=acc2[:, :], in0=zm[:, 2, :], in1=cw[:, 2, :], op=ALU.mult), r=[zm, cw], w=[acc2])
            kb.op("dve", lambda e: e.tensor_tensor(out=acc[:, :], in0=acc[:, :], in1=acc2[:, :], op=ALU.add), r=[acc, acc2], w=[acc])
            kb.op("act", lambda e: e.activation(out=acc[:, :], in_=acc[:, :], func=AF.Silu), r=[acc], w=[acc])
            kb.op("act", lambda e: e.activation(out=sq[:, :], in_=acc[:, 0:2048], func=AF.Square), r=[acc], w=[sq])
            kb.op("dve", lambda e: e.reduce_sum(out=ssh[:, :], in_=sq[:, :].rearrange("p (h d) -> p h d", h=16), axis=AX.X), r=[sq], w=[ssh])
            rsqrt_mean(C, rs, ssh, 16, 1.0, t16b)
            kb.op("dve", lambda e: e.tensor_scalar(out=rs[:, 0:8], in0=rs[:, 0:8], scalar1=float(128 ** -0.5), scalar2=None, op0=ALU.mult),
                  r=[rs], w=[rs])
            kb.op("dve", lambda e: e.tensor_tensor(out=ob[:, 0:2048].rearrange("p (h d) -> p h d", h=16),
                                                   in0=acc[:, 0:2048].rearrange("p (h d) -> p h d", h=16),
                                                   in1=rs[:, :].unsqueeze(2).to_broadcast([128, 16, 128]), op=ALU.mult), r=[acc, rs], w=[ob])
            kb.op("pool", lambda e: e.tensor_copy(out=ob[:, 2048:3072], in_=acc[:, 2048:3072]), r=[acc], w=[ob])
            kb.dma("sp", C.PA[t * 128:(t + 1) * 128, 0:3072], ob[:, :], r=[ob], w=[kb.tag("PA", t)])


def phaseB_odd(C, l, n_ctx_tiles=2, lat_tiles=None, dirs=(0, 1)):
    kb = C.kb
    ps = C.ps
    if lat_tiles is None:
        lat_tiles = list(range(2, NT))
    with kb.phase():
        dc = kb.sb("dc", [128, 512], F32)
        kb.dma("sp", dc[:, :], C.dconst, w=[dc])
        MGE, MLE, SLT, SGT = [dc[:, q * 128:(q + 1) * 128] for q in range(4)]
        idf = kb.sb("idfb", [128, 128], F32)
        kb.op("dve", lambda e: e.tensor_copy(out=idf[:, :], in_=C.identb_s[:, :]), r=[C.identb_s], w=[idf])
        qkvb = [kb.sb("qkvd", [128, 3072], BF16) for _ in range(2)]
        gbb = [kb.sb("gbd", [128, 32], F32) for _ in range(2)]
        gcs = kb.sb("gcs", [128, 8], F32)
        egc = kb.sb("egc", [128, 8], F32)
        kbt = kb.sb("kbt", [128, 8, 128], BF16)
        vbt = kb.sb("vbt", [128, 8, 128], BF16)
        kbg = kb.sb("kbg", [128, 8, 128], BF16)
        qgt = kb.sb("qgt", [128, 8, 128], BF16)
        kdt = kb.sb("kdt", [128, 8, 128], BF16)
        kds = kb.sb("kds", [128, 8], F32)
        qT = kb.sb("qTd", [128, 8, 128], BF16)
        kT = kb.sb("kTd", [128, 8, 128], BF16)
        qgT = kb.sb("qgT", [128, 8, 128], BF16)
        wT = kb.sb("wTd", [128, 8, 128], BF16)
        attT = kb.sb("attTd", [128, 8, 128], BF16)
        uu = kb.sb("uu", [128, 8, 128], F32)
        glc = kb.sb("glc", [128, 8, 2], F32)
        grep_ = kb.sb("grep", [128, 128], F32)
        d1 = kb.sb("d1", [128, 128], F32)
        d2 = kb.sb("d2", [128, 128], F32)
        e1m = kb.sb("e1m", [128, 128], F32)
        e2m = kb.sb("e2m", [128, 128], F32)
        Lb = [kb.sb("Lb", [128, 128], BF16) for _ in range(2)]
        Ub = [kb.sb("Ub", [128, 128], BF16) for _ in range(2)]
        ILb = kb.sb("ILb", [128, 128], BF16)
        Mb = [kb.sb("Mb", [128, 128], BF16) for _ in range(2)]
        vn = kb.sb("vn", [128, 8, 128], BF16)
        S = kb.sb("Sd", [128, 8, 128], F32)
        Sb = kb.sb("Sbd", [128, 8, 128], BF16)
        osb = [kb.sb("osb", [128, D], F32) for _ in range(2)]
        n_ = 0
        for d_ in dirs:
            order = list(range(n_ctx_tiles)) + list(lat_tiles)
            if d_ == 1:
                order = list(range(n_ctx_tiles))[::-1] + list(lat_tiles)[::-1]
            CUM = MGE if d_ == 0 else MLE
            M_att = MGE if d_ == 0 else MLE
            M_lo = SLT if d_ == 0 else SGT
            chunks = (0, 1) if d_ == 0 else (1, 0)
            flast = (lambda c: c * 64 + 63) if d_ == 0 else (lambda c: c * 64)
            ODST = C.OF if d_ == 0 else C.OB
            otag = "OF" if d_ == 0 else "OB"
            kb.op("dve", lambda e: e.memset(S[:, :, :], 0.0), w=[S])
            kb.op("dve", lambda e: e.memset(Sb[:, :, :], 0.0), w=[Sb])
            for t in order:
                qkv = qkvb[n_ % 2]
                gbt = gbb[n_ % 2]
                ot = osb[n_ % 2]
                n_ += 1
                kb.dma("sp", qkv[:, :], C.PA[t * 128:(t + 1) * 128, 0:3072], r=[kb.tag("PA", t)], w=[qkv])
                kb.dma("sp", gbt[:, :], C.GB[t * 128:(t + 1) * 128, :], r=[kb.tag("GB", t)], w=[gbt])
                gcol = gbt[:, d_ * 8:(d_ + 1) * 8]
                bcol = gbt[:, 16 + d_ * 8:16 + (d_ + 1) * 8]
                pg = ps[0]
                kb.op("pe", lambda e: e.matmul(pg[:, 0:8], lhsT=CUM, rhs=gcol, start=True, stop=True), r=[dc, gbt], w=[pg])
                kb.op("dve", lambda e: e.tensor_copy(out=gcs[:, :], in_=pg[:, 0:8]), r=[pg], w=[gcs])
                kb.op("act", lambda e: e.activation(out=egc[:, :], in_=gcs[:, :], func=AF.Exp), r=[gcs], w=[egc])
                qv = qkv[:, 0:1024].rearrange("p (h d) -> p h d", h=8)
                kv = qkv[:, 1024:2048].rearrange("p (h d) -> p h d", h=8)
                vv = qkv[:, 2048:3072].rearrange("p (h d) -> p h d", h=8)
                bb = bcol.unsqueeze(2).to_broadcast([128, 8, 128])
                eb = egc[:, :].unsqueeze(2).to_broadcast([128, 8, 128])
                kb.op("dve", lambda e: e.tensor_tensor(out=kbt[:, :, :], in0=kv, in1=bb, op=ALU.mult), r=[qkv, gbt], w=[kbt])
                kb.op("pool", lambda e: e.tensor_tensor(out=vbt[:, :, :], in0=vv, in1=bb, op=ALU.mult), r=[qkv, gbt], w=[vbt])
                kb.op("dve", lambda e: e.tensor_tensor(out=kbg[:, :, :], in0=kbt[:, :, :], in1=eb, op=ALU.mult), r=[kbt, egc], w=[kbg])
                kb.op("pool", lambda e: e.tensor_tensor(out=qgt[:, :, :], in0=qv, in1=eb, op=ALU.mult), r=[qkv, egc], w=[qgt])
                for (src_ap, src_tl, dst) in ((qkv[:, 0:1024], qkv, qT), (qkv[:, 1024:2048], qkv, kT), (qgt[:, :, :].rearrange("p h d -> p (h d)"), qgt, qgT)):
                    pt = ps[1]
                    ptv = pt[:, :].bitcast(BF16)
                    for h in range(8):
                        kb.op("pe", lambda e: e.transpose(ptv[:, h * 128:(h + 1) * 128], src_ap[:, h * 128:(h + 1) * 128], C.identb_s[:, :]),
                              r=[src_tl, C.identb_s], w=[pt])
                    kb.op("act", lambda e: e.copy(out=dst[:, :, :].rearrange("p h t -> p (h t)"), in_=ptv[:, :]), r=[pt], w=[dst])
                for h in range(8):
                    pa = ps[2 + (h % 2)]
                    pn = ps[4 + (h % 2)]
                    GR, KK, QK, UT = pa[:, 0:128], pa[:, 128:256], pa[:, 256:384], pa[:, 384:512]
                    kb.op("dve", lambda e: e.tensor_copy(out=grep_[:, :], in_=gcol[:, h:h + 1].to_broadcast([128, 128])), r=[gbt], w=[grep_])
                    kb.op("pe", lambda e: e.matmul(GR, lhsT=grep_[:, :], rhs=CUM, start=True, stop=True), r=[grep_, dc], w=[pa])
                    kb.op("pe", lambda e: e.matmul(KK, lhsT=kT[:, h, :], rhs=kT[:, h, :], start=True, stop=True), r=[kT], w=[pa])
                    kb.op("pe", lambda e: e.matmul(QK, lhsT=kT[:, h, :], rhs=qT[:, h, :], start=True, stop=True), r=[kT, qT], w=[pa])
                    gch = gcs[:, h:h + 1]
                    kb.op("dve", lambda e: e.tensor_scalar(out=d1[:, :], in0=GR, scalar1=gch, scalar2=0.0, op0=ALU.subtract, op1=ALU.min),
                          r=[pa, gcs], w=[d1])
                    kb.op("dve", lambda e: e.tensor_scalar(out=d2[:, :], in0=d1[:, :], scalar1=-1.0, scalar2=None, op0=ALU.mult), r=[d1], w=[d2])
                    kb.op("dve", lambda e: e.tensor_scalar(out=d2[:, :], in0=GR, scalar1=gch, scalar2=-1.0, op0=ALU.subtract, op1=ALU.mult),
                          r=[pa, gcs], w=[d2])
                    kb.op("dve", lambda e: e.tensor_scalar(out=d2[:, :], in0=d2[:, :], scalar1=0.0, scalar2=None, op0=ALU.min), r=[d2], w=[d2])
                    kb.op("act", lambda e: e.activation(out=d1[:, :], in_=d1[:, :], func=AF.Exp), r=[d1], w=[d1])
                    kb.op("act", lambda e: e.activation(out=d2[:, :], in_=d2[:, :], func=AF.Exp), r=[d2], w=[d2])
                    kb.op("dve", lambda e: e.tensor_tensor(out=e1m[:, :], in0=d1[:, :], in1=M_att, op=ALU.mult), r=[d1, dc], w=[e1m])
                    kb.op("pool", lambda e: e.tensor_tensor(out=e2m[:, :], in0=d2[:, :], in1=M_lo, op=ALU.mult), r=[d2, dc], w=[e2m])
                    for ci, c in enumerate((0, 1)):
                        f = flast(c)
                        kb.op("act", lambda e: e.activation(out=glc[:, h, c:c + 1], in_=GR[:, f:f + 1], func=AF.Exp), r=[pa], w=[glc])
                    f0, f1 = flast(0), flast(1)
                    kb.op("dve", lambda e: e.tensor_tensor(out=kds[:, h:h + 1], in0=e1m[:, f0:f0 + 1], in1=e1m[:, f1:f1 + 1], op=ALU.add),
                          r=[e1m], w=[kds])
                    kb.op("dve", lambda e: e.tensor_tensor(out=attT[:, h, :], in0=QK, in1=e1m[:, :], op=ALU.mult), r=[pa, e1m], w=[attT])
                    L0, U0 = Lb[0], Ub[0]
                    kb.op("dve", lambda e: e.scalar_tensor_tensor(out=L0[:, :], in0=KK, scalar=bcol[:, h:h + 1], in1=e2m[:, :],
                                                                  op0=ALU.mult, op1=ALU.mult), r=[pa, gbt, e2m], w=[L0])
                    utv = pa[:, 384:512].bitcast(BF16)
                    kb.op("pe", lambda e: e.transpose(utv[:, 0:128], L0[:, :], C.identb_s[:, :]), r=[L0, C.identb_s], w=[pa])
                    kb.op("act", lambda e: e.copy(out=U0[:, :], in_=utv[:, 0:128]), r=[pa], w=[U0])
                    Mc, Mn = Mb[0], Mb[1]
                    kb.op("dve", lambda e: e.tensor_tensor(out=Mc[:, :], in0=idf[:, :], in1=U0[:, :], op=ALU.subtract), r=[idf, U0], w=[Mc])
                    Lc, Ln, Uc, Un = Lb[0], Lb[1], Ub[0], Ub[1]
                    for lev in range(5):
                        U2, L2, M2 = pn[:, 0:128], pn[:, 128:256], pn[:, 256:384]
                        kb.op("pe", lambda e: e.matmul(L2, lhsT=Uc[:, :], rhs=Lc[:, :], start=True, stop=True), r=[Uc, Lc], w=[pn])
                        if lev < 4:
                            kb.op("pe", lambda e: e.matmul(U2, lhsT=Lc[:, :], rhs=Uc[:, :], start=True, stop=True), r=[Uc, Lc], w=[pn])
                        kb.op("dve", lambda e: e.tensor_tensor(out=ILb[:, :], in0=L2, in1=idf[:, :], op=ALU.add), r=[pn, idf], w=[ILb])
                        if lev < 4:
                            kb.op("act", lambda e: e.copy(out=Ln[:, :], in_=L2), r=[pn], w=[Ln])
                            kb.op("act", lambda e: e.copy(out=Un[:, :], in_=U2), r=[pn], w=[Un])
                        kb.op("pe", lambda e: e.matmul(M2, lhsT=ILb[:, :], rhs=Mc[:, :], start=True, stop=True), r=[ILb, Mc], w=[pn])
                        kb.op("dve", lambda e: e.tensor_copy(out=Mn[:, :], in_=M2), r=[pn], w=[Mn])
                        Mc, Mn = Mn, Mc
                        Lc, Ln, Uc, Un = Ln, Lc, Un, Uc
                    UU, WT = pn[:, 0:128], pn[:, 128:256]
                    kb.op("pe", lambda e: e.matmul(UU, lhsT=Mc[:, :], rhs=vbt[:, h, :], start=True, stop=True), r=[Mc, vbt], w=[pn])
                    kb.op("pe", lambda e: e.matmul(WT, lhsT=kbg[:, h, :], rhs=Mc[:, :], start=True, stop=True), r=[Mc, kbg], w=[pn])
                    kb.op("dve", lambda e: e.tensor_copy(out=uu[:, h, :], in_=UU), r=[pn], w=[uu])
                    kb.op("act", lambda e: e.copy(out=wT[:, h, :], in_=WT), r=[pn], w=[wT])
                kb.op("dve", lambda e: e.tensor_tensor(out=kdt[:, :, :], in0=kv, in1=kds[:, :].unsqueeze(2).to_broadcast([128, 8, 128]), op=ALU.mult),
                      r=[qkv, kds], w=[kdt])
                for c in chunks:
                    r0 = c * 64
                    pW = [ps[6], ps[7]]
                    for h in range(8):
                        kb.op("pe", lambda e: e.matmul(pW[h // 4][:, (h % 4) * 128:(h % 4 + 1) * 128], lhsT=wT[:, h, :], rhs=Sb[:, h, :],
                                                       start=True, stop=True), r=[wT, Sb], w=[pW[h // 4]])
                    for hb in range(2):
                        kb.op("dve", lambda e: e.tensor_tensor(out=vn[r0:r0 + 64, hb * 4:(hb + 1) * 4, :].rearrange("p h d -> p (h d)"),
                                                               in0=uu[r0:r0 + 64, hb * 4:(hb + 1) * 4, :].rearrange("p h d -> p (h d)"),
                                                               in1=pW[hb][r0:r0 + 64, :], op=ALU.subtract), r=[uu, pW[hb]], w=[vn])
                    pO = [ps[4], ps[5]]
                    pK = [ps[2], ps[3]]
                    for h in range(8):
                        oc = (h % 4) * 128
                        kb.op("pe", lambda e: e.matmul(pO[h // 4][:, oc:oc + 128], lhsT=qgT[:, h, :], rhs=Sb[:, h, :], start=True, stop=False),
                              r=[qgT, Sb], w=[pO[h // 4]])
                        kb.op("pe", lambda e: e.matmul(pO[h // 4][:, oc:oc + 128], lhsT=attT[r0:r0 + 64, h, :], rhs=vn[r0:r0 + 64, h, :],
                                                       start=False, stop=True), r=[attT, vn], w=[pO[h // 4]])
                        kb.op("pe", lambda e: e.matmul(pK[h // 4][:, oc:oc + 128], lhsT=kdt[r0:r0 + 64, h, :], rhs=vn[r0:r0 + 64, h, :],
                                                       start=True, stop=True), r=[kdt, vn], w=[pK[h // 4]])
                    for hb in range(2):
                        kb.op("act", lambda e: e.copy(out=ot[r0:r0 + 64, hb * 512:(hb + 1) * 512], in_=pO[hb][r0:r0 + 64, :]), r=[pO[hb]], w=[ot])
                    for h in range(8):
                        oc = (h % 4) * 128
                        kb.op("dve", lambda e: e.scalar_tensor_tensor(out=S[:, h, :], in0=S[:, h, :], scalar=glc[:, h, c:c + 1],
                                                                      in1=pK[h // 4][:, oc:oc + 128], op0=ALU.mult, op1=ALU.add),
                              r=[S, glc, pK[h // 4]], w=[S])
                    kb.op("act", lambda e: e.copy(out=Sb[:, :, :].rearrange("p h d -> p (h d)"), in_=S[:, :, :].rearrange("p h d -> p (h d)")),
                          r=[S], w=[Sb])
                kb.dma("sp", ODST[t * 128:(t + 1) * 128, :], ot[:, :], r=[ot], w=[kb.tag(otag, t)])


def odd_mix_src(C, l):
    kb = C.kb
    i = l // 2
    gn = kb.sb("ogain", [128, 128], F32)
    kb.dma("sp", gn[:, :], C.od_gain[i].partition_broadcast(128), w=[gn])
    ofb = [kb.sb("ofo", [128, D], F32) for _ in range(2)]
    obb = [kb.sb("obo", [128, D], F32) for _ in range(2)]
    zgb = [kb.sb("zgo", [128, D], BF16) for _ in range(2)]
    junk = kb.sb("junko", [128, D], F32)
    ssh = kb.sb("ssho", [128, 8], F32)
    rs8 = kb.sb("rs8o", [128, 8], F32)
    tmp8 = kb.sb("tmp8o", [128, 8], F32)
    mr = kb.sb("mro", [128, D], BF16)

    def src(t, mT, n_):
        of, ob, zg = ofb[n_ % 2], obb[n_ % 2], zgb[n_ % 2]
        kb.dma("sp", of[:, :], C.OF[t * 128:(t + 1) * 128, :], r=[kb.tag("OF", t)], w=[of])
        kb.dma("sp", ob[:, :], C.OB[t * 128:(t + 1) * 128, :], r=[kb.tag("OB", t)], w=[ob])
        kb.dma("sp", zg[:, :], C.PA[t * 128:(t + 1) * 128, 3072:4096], r=[kb.tag("PAz", t)], w=[zg])
        kb.op("pool", lambda e: e.tensor_tensor(out=of[:, :], in0=of[:, :], in1=ob[:, :], op=ALU.add), r=[of, ob], w=[of])
        kb.op("act", lambda e: e.activation(out=junk[:, :], in_=of[:, :], func=AF.Square), r=[of], w=[junk])
        kb.op("dve", lambda e: e.reduce_sum(out=ssh[:, :], in_=junk[:, :].rearrange("p (h d) -> p h d", h=8), axis=AX.X), r=[junk], w=[ssh])
        rsqrt_mean(C, rs8, ssh, 8, 1.0 / 128, tmp8)
        kb.op("dve", lambda e: e.tensor_tensor(out=of[:, :].rearrange("p (h d) -> p h d", h=8), in0=of[:, :].rearrange("p (h d) -> p h d", h=8),
                                               in1=rs8[:, :].unsqueeze(2).to_broadcast([128, 8, 128]), op=ALU.mult), r=[of, rs8], w=[of])
        kb.op("dve", lambda e: e.tensor_tensor(out=of[:, :].rearrange("p (h d) -> p h d", h=8), in0=of[:, :].rearrange("p (h d) -> p h d", h=8),
                                               in1=gn[:, :].unsqueeze(1).to_broadcast([128, 8, 128]), op=ALU.mult), r=[of, gn], w=[of])
        kb.op("pool", lambda e: e.tensor_tensor(out=mr[:, :], in0=of[:, :], in1=zg[:, :], op=ALU.mult), r=[of, zg], w=[mr])
        pT = C.ps[0]
        pv = pT[:, :].bitcast(BF16)
        for k in range(8):
            kb.op("pe", lambda e: e.transpose(pv[:, k * 128:(k + 1) * 128], mr[:, k * 128:(k + 1) * 128], C.identb_s[:, :]),
                  r=[mr, C.identb_s], w=[pT])
        kb.op("act", lambda e: e.copy(out=mT[:, 0:8, :].rearrange("p k t -> p (k t)"), in_=pv[:, :]), r=[pT], w=[mT])
    return src


def final_phase(C, tiles):
    kb = C.kb
    with kb.phase():
        src = combine_src(C, DEPTH - 1, store=False)
        fn = kb.sb("fnrep", [128, D], F32)
        kb.dma("sp", fn[:, :], C.norms[2 * DEPTH].partition_broadcast(128), w=[fn])
        xb = [kb.sb("xf", [128, D], F32) for _ in range(2)]
        ob = [kb.sb("of_", [128, D], F32) for _ in range(2)]
        junk = kb.sb("junkf", [128, D], BF16)
        ss = kb.sb("ssf", [128, 1], F32)
        rstd = kb.sb("rstdf", [128, 1], F32)
        tmp1 = kb.sb("tmp1f", [128, 8], F32)
        for n_, t in enumerate(tiles):
            xt, o = xb[n_ % 2], ob[n_ % 2]
            src(t, xt)
            norm_tile(C, xt, ss, rstd, tmp1, junk, o)
            kb.op("dve", lambda e: e.tensor_tensor(out=o[:, :], in0=o[:, :], in1=fn[:, :], op=ALU.mult), r=[o, fn], w=[o])
            kb.dma("sp", C.out[(t - 2) * 128:(t - 1) * 128, :], o[:, :], r=[o], w=[kb.tag("out", t)])


def build_program():
    nc = bass.Bass("TRN2", target_bir_lowering=False)
    C = Ctx()
    C.nc = nc
    C.kb = KB(nc)
    C.dbg_out = set()
    C.moe_skip = True
    kb = C.kb
    declare_io(nc, C)
    C.out = nc.dram_tensor("out", [L, D], F32, kind="ExternalOutput").ap()
    setup_globals(C)
    setup_consts(C)
    phase0(C)
    all_tiles = list(range(NT))
    for l in range(DEPTH):
        last = l == DEPTH - 1
        if l == 0:
            def x_src(t, xt):
                kb.dma("sp", xt[:, :], C.xin[t * 128:(t + 1) * 128, :], w=[xt])
                kb.dma("sp", C.X[t * 128:(t + 1) * 128, :], xt[:, :], r=[xt], w=[kb.tag("X", t)])
            holder = None
        else:
            holder = kb.phase()
            holder.__enter__()
            x_src = combine_src(C, l - 1)
        if l % 2 == 0:
            phaseA_even(C, l, all_tiles, x_src)
        else:
            phaseA_odd(C, l, all_tiles, x_src)
        if holder is not None:
            holder.__exit__(None, None, None)
        if l % 2 == 0:
            phaseB_ret(C, l)
            phaseC_att(C, l)
            phaseD(C, l, all_tiles, C.ev_w_out[l // 2], 12, lambda: even_mix_src(C), set(all_tiles))
        else:
            phaseB_odd(C, l)
            tiles_d = all_tiles if not last else all_tiles[2:]
            phaseD(C, l, tiles_d, C.od_w_out[l // 2], 8, lambda: odd_mix_src(C, l), set(tiles_d))
        phaseE(C, l)
    final_phase(C, all_tiles[2:])
    kb.barrier()
    return nc


_CACHE = {}


def kernel(**inputs):
    inp = {k: np.asarray(v) for k, v in inputs.items()}
    maps = host_prep(inp)[:NCORES]
    if "nc" not in _CACHE:
        _CACHE["nc"] = build_program()
    res = run_bass_kernel_spmd(_CACHE["nc"], maps, core_ids=list(range(NCORES)))
    out = np.stack([np.asarray(res.results[b]["out"], np.float32) for b in range(4)], axis=0)
    return out


NCORES = 4
```

```python
import numpy as np
from contextlib import ExitStack
import concourse.bass as bass
import concourse.mybir as mybir
from concourse.bass_utils import run_bass_kernel_spmd

F32 = mybir.dt.float32
BF16 = mybir.dt.bfloat16
I32 = mybir.dt.int32
AF = mybir.ActivationFunctionType
ALU = mybir.AluOpType
AX = mybir.AxisListType

D = 1024
LC = 256
L = 8192
T = LC + L
NT = T // 128
DEPTH = 4
EPS = 1e-6
EVEN_IN = 3840
ODD_IN = 4128


class Buf:
    __slots__ = ("w", "r")

    def __init__(self):
        self.w = None
        self.r = {}


class Tl:
    __slots__ = ("t", "b", "psum")

    def __init__(self, t, psum=False):
        self.t = t
        self.b = Buf()
        self.psum = psum

    def __getitem__(self, k):
        return self.t[k]


class KB:
    NDMA = {"sp": 12, "pool": 12, "act": 4}

    def __init__(self, nc):
        self.nc = nc
        self.es = ExitStack()
        self.eng = {"pe": nc.tensor, "act": nc.scalar, "dve": nc.vector, "pool": nc.gpsimd, "sp": nc.sync}
        self.sem = {}
        self.cnt = {}
        self.seen = {e: {} for e in self.eng}
        for e in self.eng:
            self.sem[e] = self.es.enter_context(nc.semaphore("s_" + e))
            self.cnt[e] = 0
        self.dsem = {}
        self.dval = {}
        self.dnext = {}
        for q, n in self.NDMA.items():
            self.dsem[q] = [self.es.enter_context(nc.semaphore(f"d_{q}{i}")) for i in range(n)]
            self.dval[q] = [0] * n
            self.dnext[q] = 0
        self.tags = {}
        self.ninst = 0
        self.cur = self.es

    def semobj(self, key):
        if isinstance(key, tuple):
            return self.dsem[key[0]][key[1]]
        return self.sem[key]

    def tag(self, *key):
        b = self.tags.get(key)
        if b is None:
            b = Tl(None)
            self.tags[key] = b
        return b

    def _deps(self, eng, r, w):
        deps = {}

        def add(ev, raw):
            if ev is None:
                return
            k, v = ev
            if k == eng and (not raw or eng in ("pe", "sp")):
                return
            if deps.get(k, 0) < v:
                deps[k] = v

        for x in r:
            add(x.b.w, True)
        for x in w:
            add(x.b.w, False)
            for k, v in x.b.r.items():
                add((k, v), False)
        return deps

    def _wait(self, eng, deps):
        seen = self.seen[eng]
        e = self.eng[eng]
        for k, v in deps.items():
            if seen.get(k, 0) >= v:
                continue
            e.wait_ge(self.semobj(k), v)
            seen[k] = v
            self.ninst += 1

    def _mark(self, ev, r, w):
        k, v = ev
        for x in r:
            if x.b.r.get(k, 0) < v:
                x.b.r[k] = v
        for x in w:
            x.b.w = ev
            x.b.r = {}

    def op(self, eng, fn, r=(), w=()):
        if any(x.psum for x in r):
            w = list(w) + [x for x in r if x.psum]
            r = [x for x in r if not x.psum]
        self._wait(eng, self._deps(eng, r, w))
        ins = fn(self.eng[eng])
        self.cnt[eng] += 1
        ins.then_inc(self.sem[eng], 1)
        self._mark((eng, self.cnt[eng]), r, w)
        self.ninst += 1
        return ins

    def dma(self, q, out, in_, r=(), w=(), indirect=None, **kw):
        deps = self._deps("dma", r, w)
        i = self.dnext[q]
        self.dnext[q] = (i + 1) % len(self.dsem[q])
        key = (q, i)
        if self.dval[q][i] > 0:
            deps[key] = max(deps.get(key, 0), self.dval[q][i])
        self._wait(q, deps)
        e = self.eng[q]
        if indirect is not None:
            ins = e.indirect_dma_start(out=out, in_=in_, **indirect, **kw)
        else:
            ins = e.dma_start(out=out, in_=in_, **kw)
        self.dval[q][i] += 16
        ins.then_inc(self.dsem[q][i], 16)
        self._mark((key, self.dval[q][i]), r, w)
        self.ninst += 1
        return ins

    def barrier(self, engines=("pe", "act", "dve", "pool", "sp")):
        deps = {e: self.cnt[e] for e in self.eng if self.cnt[e] > 0}
        for q in self.dsem:
            for i, v in enumerate(self.dval[q]):
                if v > 0:
                    deps[(q, i)] = v
        for e in engines:
            d = {k: v for k, v in deps.items() if k != e}
            self._wait(e, d)

    def sb(self, name, shape, dtype):
        self.nalloc = getattr(self, "nalloc", 0) + 1
        return Tl(self.cur.enter_context(self.nc.sbuf_tensor(f"{name}_{self.nalloc}", shape, dtype)))

    def phase(self):
        kb = self

        class _P:
            def __enter__(self_):
                self_.prev = getattr(kb, "cur", kb.es)
                self_.st = ExitStack()
                kb.cur = self_.st
                return self_

            def __exit__(self_, *a):
                if a[0] is None:
                    kb.barrier()
                self_.st.close()
                kb.cur = self_.prev
                return False

        return _P()


class Ctx:
    pass


def declare_io(nc, C):
    def din(name, shape, dt=F32):
        return nc.dram_tensor(name, shape, dt, kind="ExternalInput").ap()
    C.xin = din("xin", [T, D])
    C.cT = din("cT", [128, 16])
    C.normT = din("normT", [128, 72])
    C.badaT = din("badaT", [128, 192])
    C.b_ada = din("b_ada", [DEPTH, 6 * D])
    C.rope = din("rope", [L, 128])
    C.identb = din("identb", [128, 128], BF16)
    C.identf = din("identf", [128, 128])
    C.w_ada = din("w_ada", [DEPTH, D, 6 * D])
    C.ev_w_in = din("ev_w_in", [2, D, EVEN_IN])
    C.ev_qk_gain = din("ev_qk_gain", [2, 128])
    C.ev_decay = din("ev_decay", [2, 16])
    C.ev_w_out = din("ev_w_out", [2, 1536, D])
    C.rconst = din("rconst", [128, 770])
    C.norms = din("norms", [9, D])
    C.od_w_in = din("od_w_in", [2, D, ODD_IN])
    C.od_prm = din("od_prm", [2, 32])
    C.od_conv = din("od_conv", [2, 3, 3072])
    C.od_gain = din("od_gain", [2, 128])
    C.od_w_out = din("od_w_out", [2, D, D])
    C.dconst = din("dconst", [128, 512])
    C.wr = din("wr", [DEPTH, D, 36])
    C.br = din("br", [DEPTH, 36])
    C.mconst = din("mconst", [128, 33 + NSLOT])
    C.triones = din("triones", [128, 256], BF16)
    C.srcidx = din("srcidx", [128, NT], I32)
    C.listinit = din("listinit", [128, (NSLOT + 1) * 4], I32)
    C.wgu = din("wgu", [DEPTH, 32 * 128, 8 * 1024])
    C.wd = din("wd", [DEPTH, 32 * 128, 4 * 1024])


def setup_globals(C):
    kb, nc = C.kb, C.nc
    C.ps = [Tl(kb.es.enter_context(nc.psum_tensor(f"ps{i}", [128, 512], F32)), psum=True) for i in range(8)]
    C.modT = kb.sb("modT", [128, DEPTH, 48, 2], F32)
    C.normTs = kb.sb("normTs", [128, 72], F32)
    C.identb_s = kb.sb("identb_s", [128, 128], BF16)
    C.identf_s = kb.sb("identf_s", [128, 128], F32)
    kb.dma("sp", C.normTs[:, :], C.normT, w=[C.normTs])
    kb.dma("sp", C.identb_s[:, :], C.identb, w=[C.identb_s])
    kb.dma("sp", C.identf_s[:, :], C.identf, w=[C.identf_s])
    def dscr(name, shape, dt=F32):
        kind = "ExternalOutput" if name in C.dbg_out else "Internal"
        return nc.dram_tensor(name, shape, dt, kind=kind).ap()
    C.G = dscr("G", [DEPTH, 128, 8192])
    C.X = dscr("X", [T, D])
    C.PA = dscr("PA", [T, ODD_IN], BF16)
    C.OF = dscr("OF", [T, D])
    C.MIX = dscr("MIX", [T, D], BF16)
    C.AT = dscr("AT", [512, T], BF16)
    C.F = dscr("F", [T, D], BF16)
    C.ZP = dscr("ZP", [TZ, 3072], BF16)
    C.GB = dscr("GB", [T, 32])
    C.OB = dscr("OB", [T, D])
    C.LIST = dscr("LIST", [(NSLOT + 1) * 128, 4], I32)
    C.YB = dscr("YB", [2 * TPAD, D])
    C.widx = kb.sb("widx", [128, NSLOT], I32)


def phase0(C):
    kb = C.kb
    ps0, ps1 = C.ps[0], C.ps[1]
    with kb.phase():
        cT = kb.sb("cT", [128, 16], F32)
        sc = kb.sb("sc", [128, 16], F32)
        screp = kb.sb("screp", [128, 16, 128], F32)
        badaT = kb.sb("badaT", [128, 192], F32)
        brep = kb.sb("brep", [128, 4096], F32)
        gout = kb.sb("gout", [128, 8192], F32)
        wblk = [kb.sb("wada", [128, 8, 512], F32) for _ in range(2)]
        kb.dma("sp", cT[:, :], C.cT, w=[cT])
        kb.dma("sp", badaT[:, :], C.badaT, w=[badaT])
        kb.op("act", lambda e: e.activation(out=sc[:, :], in_=cT[:, :], func=AF.Silu), r=[cT], w=[sc])
        kb.op("dve", lambda e: e.tensor_copy(out=screp[:, :, :], in_=sc[:, :].unsqueeze(2).to_broadcast([128, 16, 128])),
              r=[sc], w=[screp])
        for l in range(DEPTH):
            for gi, c0 in enumerate((2048, 5120, 3072, 4096)):
                kb.dma("sp", brep[:, gi * 1024:(gi + 1) * 1024], C.b_ada[l, c0:c0 + 1024].partition_broadcast(128), w=[brep])
            wv = C.w_ada[l].rearrange("(k p) c -> p k c", p=128)
            for j in range(12):
                wb = wblk[j % 2]
                kb.dma("sp", wb[:, :, :], wv[:, :, j * 512:(j + 1) * 512], w=[wb])
                for c4 in range(4):
                    for k in range(8):
                        kb.op("pe", lambda e: e.matmul(ps0[:, c4 * 2:(c4 + 1) * 2], lhsT=wb[:, k, c4 * 128:(c4 + 1) * 128],
                                                       rhs=sc[:, 2 * k:2 * k + 2], start=(k == 0), stop=(k == 7)),
                              r=[wb, sc], w=[ps0])
                kb.op("dve", lambda e: e.tensor_tensor(
                    out=C.modT[:, l, j * 4:(j + 1) * 4, :], in0=ps0[:, 0:8].rearrange("p (c r) -> p c r", r=2),
                    in1=badaT[:, l * 48 + j * 4:l * 48 + j * 4 + 4].unsqueeze(2).to_broadcast([128, 4, 2]), op=ALU.add),
                    r=[ps0, badaT], w=[C.modT])
                if j in (4, 5, 10, 11, 6, 7, 8, 9):
                    gi = {4: 0, 5: 0, 10: 1, 11: 1, 6: 2, 7: 2, 8: 3, 9: 3}[j]
                    half = j % 2
                    for r_ in range(2):
                        for k in range(8):
                            kb.op("pe", lambda e: e.matmul(ps1[:, :], lhsT=screp[:, 2 * k + r_, :], rhs=wb[:, k, :],
                                                           start=(k == 0), stop=(k == 7)), r=[screp, wb], w=[ps1])
                        o0 = ((r_ * 2 + gi) if gi < 2 else (4 + r_ * 2 + gi - 2)) * 1024 + half * 512
                        b0 = gi * 1024 + half * 512
                        kb.op("dve", lambda e: e.tensor_tensor(out=gout[:, o0:o0 + 512], in0=ps1[:, :], in1=brep[:, b0:b0 + 512],
                                                               op=ALU.add), r=[ps1, brep], w=[gout])
            kb.dma("sp", C.G[l], gout[:, :], r=[gout], w=[kb.tag("G", l)])


def setup_consts(C):
    kb = C.kb
    C.cneg = kb.sb("cneg", [128, 64], F32)
    kb.op("pool", lambda e: e.memset(C.cneg[:, :], -0.5), w=[C.cneg])


def rsqrt_mean(C, dst, src, n, inv_n, tmp):
    kb = C.kb
    kb.op("dve", lambda e: e.tensor_scalar(out=tmp[:, 0:n], in0=src[:, 0:n], scalar1=inv_n, scalar2=EPS,
                                           op0=ALU.mult, op1=ALU.add), r=[src], w=[tmp])
    kb.op("pool", lambda e: e.tensor_tensor(out=dst[:, 0:n], in0=tmp[:, 0:n], in1=C.cneg[:, 0:n], op=ALU.pow),
          r=[tmp, C.cneg], w=[dst])


def layer_mod_tables(C, l, which):
    kb = C.kb
    gs = kb.sb("gs", [128, 8, 2], F32)
    sc_blk = 8 if which == 0 else 32
    sh_blk = 0 if which == 0 else 24
    nrm = (l if which == 0 else DEPTH + l) * 8
    kb.op("dve", lambda e: e.tensor_scalar(out=gs[:, :, :], in0=C.modT[:, l, sc_blk:sc_blk + 8, :], scalar1=1.0, scalar2=None,
                                           op0=ALU.add), r=[C.modT], w=[gs])
    kb.op("dve", lambda e: e.tensor_tensor(out=gs[:, :, :], in0=gs[:, :, :],
                                           in1=C.normTs[:, nrm:nrm + 8].unsqueeze(2).to_broadcast([128, 8, 2]), op=ALU.mult),
          r=[gs, C.normTs], w=[gs])
    return gs, sh_blk


def rope_apply(C, dst_ap, dst_tl, src_ap, src_tl, nh, rp, t1, t2):
    kb = C.kb
    n = nh * 64
    kb.op("dve", lambda e: e.tensor_tensor(out=t1[:, 0:n].rearrange("p (h d) -> p h d", h=nh),
                                           in0=src_ap.rearrange("p (h d) -> p h d", h=nh),
                                           in1=rp[:, 0:64].unsqueeze(1).to_broadcast([128, nh, 64]), op=ALU.mult),
          r=[src_tl, rp], w=[t1])
    sv = src_ap.rearrange("p (h a b f) -> p h a b f", h=nh, a=2, b=2, f=16)
    tv = t2[:, 0:n].rearrange("p (h a b f) -> p h a b f", h=nh, a=2, b=2, f=16)
    sn = rp[:, 64:128].rearrange("p (a b f) -> p a b f", a=2, b=2, f=16)
    for b_ in range(2):
        kb.op("dve", lambda e: e.tensor_tensor(out=tv[:, :, :, b_, :], in0=sv[:, :, :, 1 - b_, :],
                                               in1=sn[:, :, b_, :].unsqueeze(1).to_broadcast([128, nh, 2, 16]), op=ALU.mult),
              r=[src_tl, rp], w=[t2])
    kb.op("pool", lambda e: e.tensor_tensor(out=dst_ap, in0=t1[:, 0:n], in1=t2[:, 0:n], op=ALU.add),
          r=[t1, t2], w=[dst_tl])


def norm_tile(C, xt, ss, rstd, tmp1, junk, xn):
    kb = C.kb
    kb.op("act", lambda e: e.activation(out=junk[:, :], in_=xt[:, :], func=AF.Square, accum_out=ss[:, 0:1]),
          r=[xt], w=[junk, ss])
    rsqrt_mean(C, rstd, ss, 1, 1.0 / D, tmp1)
    kb.op("act", lambda e: e.activation(out=xn[:, :], in_=xt[:, :], func=AF.Copy, scale=rstd[:, 0:1]),
          r=[xt, rstd], w=[xn])


def phaseA_even(C, l, tiles, x_src):
    kb = C.kb
    i = l // 2
    ps = C.ps
    with kb.phase():
        W = kb.sb("Win", [128, 8, EVEN_IN], BF16)
        wv = C.ev_w_in[i].rearrange("(k p) c -> p k c", p=128)
        for k in range(8):
            kb.dma("pool", W[:, k, :], wv[:, k, :], w=[W])
        gs1, sh_blk = layer_mod_tables(C, l, 0)
        gain = kb.sb("qkgain", [128, 128], F32)
        kb.dma("sp", gain[:, :], C.ev_qk_gain[i].partition_broadcast(128), w=[gain])
        kb.op("dve", lambda e: e.tensor_scalar(out=gain[:, 0:64], in0=gain[:, 0:64], scalar1=0.125, scalar2=None, op0=ALU.mult),
              r=[gain], w=[gain])
        xb = [kb.sb("xt", [128, D], F32) for _ in range(2)]
        rpb = [kb.sb("rp", [128, 128], F32) for _ in range(2)]
        junk = kb.sb("junk", [128, D], BF16)
        xn = kb.sb("xn", [128, D], BF16)
        hT = [kb.sb("hT", [128, 8, 128], BF16) for _ in range(2)]
        pa = [kb.sb("pa", [128, EVEN_IN], BF16) for _ in range(2)]
        ss = kb.sb("ss", [128, 1], F32)
        rstd = kb.sb("rstd", [128, 1], F32)
        tmp1 = kb.sb("tmp1", [128, 8], F32)
        ssh = kb.sb("ssh", [128, 8], F32)
        rs8 = kb.sb("rs8", [128, 8], F32)
        tA = kb.sb("tA", [128, 512], F32)
        tB = kb.sb("tB", [128, 512], F32)
        t1 = kb.sb("t1", [128, 512], F32)
        t2 = kb.sb("t2", [128, 512], F32)
        for n_, t in enumerate(tiles):
            lat = t >= 2
            r_ = 0 if lat else 1
            xt = xb[n_ % 2]
            rp = rpb[n_ % 2]
            h = hT[n_ % 2]
            po = pa[n_ % 2]
            x_src(t, xt)
            if lat:
                kb.dma("sp", rp[:, :], C.rope[(t - 2) * 128:(t - 1) * 128, :], w=[rp])
            norm_tile(C, xt, ss, rstd, tmp1, junk, xn)
            psT = ps[0]
            pv = psT[:, :].bitcast(BF16)
            for k in range(8):
                kb.op("pe", lambda e: e.transpose(pv[:, k * 128:(k + 1) * 128], xn[:, k * 128:(k + 1) * 128], C.identb_s[:, :]),
                      r=[xn, C.identb_s], w=[psT])
            for k in range(8):
                kb.op("act", lambda e: e.activation(out=h[:, k, :], in_=pv[:, k * 128:(k + 1) * 128], func=AF.Identity,
                                                    scale=gs1[:, k, r_:r_ + 1], bias=C.modT[:, l, sh_blk + k, r_:r_ + 1]),
                      r=[psT, gs1, C.modT], w=[h])
            for j in range(8):
                c0 = j * 512
                nc_ = 512 if j < 7 else 256
                pj = ps[1 + (j % 6)]
                for k in range(8):
                    kb.op("pe", lambda e: e.matmul(pj[:, 0:nc_], lhsT=h[:, k, :], rhs=W[:, k, c0:c0 + nc_],
                                                   start=(k == 0), stop=(k == 7)), r=[h, W], w=[pj])
                if j == 0:
                    if lat:
                        rope_apply(C, po[:, c0:c0 + 512], po, pj[:, :], pj, 8, rp, t1, t2)
                    else:
                        kb.op("act", lambda e: e.copy(out=po[:, c0:c0 + 512], in_=pj[:, :]), r=[pj], w=[po])
                elif j == 1:
                    if lat:
                        kb.op("act", lambda e: e.mul(out=tA[:, :], in_=pj[:, :], mul=0.125), r=[pj], w=[tA])
                        rope_apply(C, po[:, c0:c0 + 512], po, tA[:, :], tA, 8, rp, t1, t2)
                    else:
                        kb.op("act", lambda e: e.mul(out=po[:, c0:c0 + 512], in_=pj[:, :], mul=0.125), r=[pj], w=[po])
                elif j in (2, 3):
                    kb.op("act", lambda e: e.copy(out=po[:, c0:c0 + 512], in_=pj[:, :]), r=[pj], w=[po])
                elif j in (4, 5):
                    kb.op("act", lambda e: e.activation(out=po[:, c0:c0 + 512], in_=pj[:, :], func=AF.Silu), r=[pj], w=[po])
                else:
                    nh = 8 if j == 6 else 2
                    n = nh * 64
                    g0 = 0 if j == 6 else 64
                    kb.op("act", lambda e: e.activation(out=tA[:, 0:n], in_=pj[:, 0:n], func=AF.Square), r=[pj], w=[tA])
                    kb.op("dve", lambda e: e.reduce_sum(out=ssh[:, 0:nh], in_=tA[:, 0:n].rearrange("p (h d) -> p h d", h=nh),
                                                        axis=AX.X), r=[tA], w=[ssh])
                    rsqrt_mean(C, rs8, ssh, nh, 1.0 / 64, tmp1)
                    kb.op("dve", lambda e: e.tensor_tensor(out=tB[:, 0:n].rearrange("p (h d) -> p h d", h=nh),
                                                           in0=pj[:, 0:n].rearrange("p (h d) -> p h d", h=nh),
                                                           in1=rs8[:, 0:nh].unsqueeze(2).to_broadcast([128, nh, 64]), op=ALU.mult),
                          r=[pj, rs8], w=[tB])
                    if lat:
                        kb.op("dve", lambda e: e.tensor_tensor(out=tB[:, 0:n].rearrange("p (h d) -> p h d", h=nh),
                                                               in0=tB[:, 0:n].rearrange("p (h d) -> p h d", h=nh),
                                                               in1=gain[:, g0:g0 + 64].unsqueeze(1).to_broadcast([128, nh, 64]),
                                                               op=ALU.mult), r=[tB, gain], w=[tB])
                        rope_apply(C, po[:, c0:c0 + n], po, tB[:, 0:n], tB, nh, rp, t1, t2)
                    else:
                        kb.op("dve", lambda e: e.tensor_tensor(out=po[:, c0:c0 + n].rearrange("p (h d) -> p h d", h=nh),
                                                               in0=tB[:, 0:n].rearrange("p (h d) -> p h d", h=nh),
                                                               in1=gain[:, g0:g0 + 64].unsqueeze(1).to_broadcast([128, nh, 64]),
                                                               op=ALU.mult), r=[tB, gain], w=[po])
                    if j == 7:
                        kb.op("act", lambda e: e.copy(out=po[:, c0 + 128:c0 + 256], in_=pj[:, 128:256]), r=[pj], w=[po])
            kb.dma("sp", C.PA[t * 128:(t + 1) * 128, 0:EVEN_IN], po[:, :], r=[po], w=[kb.tag("PA", t)])


def rope_table():
    t = np.arange(L)
    rows = (t // 64).astype(np.float32)
    cols = (t % 64).astype(np.float32)
    inv = (10000.0 ** (-np.arange(16, dtype=np.float32) / 16)).astype(np.float32)
    ang = np.stack([rows[:, None] * inv, cols[:, None] * inv], axis=1).astype(np.float32)
    c, s = np.cos(ang), np.sin(ang)
    C64 = np.stack([c, c], axis=2)
    S64 = np.stack([-s, s], axis=2)
    return np.concatenate([C64.reshape(L, 64), S64.reshape(L, 64)], axis=1).astype(np.float32)


def fm(v):
    v = np.asarray(v, np.float32).reshape(-1)
    return np.ascontiguousarray(v.reshape(-1, 128).T)


def host_prep(inp):
    import ml_dtypes
    shared = {}
    shared["normT"] = np.concatenate([fm(inp["norm_mix"][l]) for l in range(DEPTH)] + [fm(inp["norm_ffn"][l]) for l in range(DEPTH)]
                                     + [fm(inp["final_norm"])], axis=1)
    shared["badaT"] = np.concatenate([fm(inp["b_ada"][l]) for l in range(DEPTH)], axis=1)
    shared["b_ada"] = np.ascontiguousarray(inp["b_ada"], np.float32)
    shared["rope"] = rope_table()
    shared["identb"] = np.eye(128, dtype=np.float32).astype(ml_dtypes.bfloat16)
    shared["identf"] = np.eye(128, dtype=np.float32)
    shared["rconst"] = ret_consts()
    shared["od_w_in"] = np.ascontiguousarray(inp["od_w_in"], np.float32)
    shared["od_prm"] = np.ascontiguousarray(np.concatenate([inp["od_a_log_f"], inp["od_a_log_b"], inp["od_dt_bias_f"], inp["od_dt_bias_b"]], axis=1), np.float32)
    shared["od_conv"] = np.ascontiguousarray(inp["od_conv"], np.float32)
    shared["od_gain"] = np.ascontiguousarray(inp["od_out_gain"], np.float32)
    shared["od_w_out"] = np.ascontiguousarray(inp["od_w_out"], np.float32)
    shared["dconst"] = dn_consts()
    shared["norms"] = np.ascontiguousarray(np.concatenate([inp["norm_mix"], inp["norm_ffn"], np.asarray(inp["final_norm"])[None, :]], axis=0), np.float32)
    shared["wr"] = np.ascontiguousarray(np.concatenate([inp["moe_w_group"], inp["moe_w_expert"]], axis=2), np.float32)
    shared["br"] = np.ascontiguousarray(np.concatenate([inp["moe_b_group"], inp["moe_b_expert"]], axis=1), np.float32)
    shared["triones"], shared["mconst"], shared["srcidx"], shared["listinit"] = moe_consts()
    shared["wgu"] = np.ascontiguousarray(np.asarray(inp["moe_w_gate_up"], np.float32).reshape(DEPTH, 32, 8, 128, 1024).transpose(0, 1, 3, 2, 4)).reshape(DEPTH, 32 * 128, 8 * 1024)
    shared["wd"] = np.ascontiguousarray(np.asarray(inp["moe_w_down"], np.float32).reshape(DEPTH, 32, 4, 128, 1024).transpose(0, 1, 3, 2, 4)).reshape(DEPTH, 32 * 128, 4 * 1024)
    shared["w_ada"] = np.ascontiguousarray(inp["w_ada"], np.float32)
    shared["ev_w_in"] = np.ascontiguousarray(inp["ev_w_in"], np.float32)
    shared["ev_qk_gain"] = np.ascontiguousarray(np.concatenate([inp["ev_q_gain"], inp["ev_k_gain"]], axis=1), np.float32)
    shared["ev_decay"] = np.ascontiguousarray(np.concatenate([inp["ev_decay_f"], inp["ev_decay_b"]], axis=1), np.float32)
    shared["ev_w_out"] = np.ascontiguousarray(inp["ev_w_out"], np.float32)
    maps = []
    for core in range(8):
        b = core % 4
        m = dict(shared)
        m["xin"] = np.ascontiguousarray(np.concatenate([inp["ctx"][b], inp["x"][b]], axis=0), np.float32)
        cv = np.stack([np.asarray(inp["c"][b], np.float32), np.asarray(inp["c_ctx"], np.float32)], axis=0)
        m["cT"] = np.ascontiguousarray(cv.reshape(2, 8, 128).transpose(2, 1, 0).reshape(128, 16))
        maps.append(m)
    return maps


def ret_consts():
    p = np.arange(128, dtype=np.float32)
    diff = p[None, :] - p[:, None]
    dpos = np.maximum(diff, 0)
    dneg = np.maximum(-diff, 0)
    mge = (diff >= 0).astype(np.float32)
    mle = (diff <= 0).astype(np.float32)
    pos1 = np.tile(p[None, :] + 1, (128, 1))
    rpos1 = np.tile(128 - p[None, :], (128, 1))
    pcol = np.stack([127 - p, p], axis=1)
    return np.ascontiguousarray(np.concatenate([dpos, dneg, mge, mle, pos1, rpos1, pcol], axis=1), np.float32)


def phaseB_ret(C, l, n_ctx_tiles=2, lat_tiles=None):
    kb = C.kb
    i = l // 2
    ps = C.ps
    if lat_tiles is None:
        lat_tiles = list(range(2, NT))
    with kb.phase():
        rc = kb.sb("rc", [128, 770], F32)
        kb.dma("sp", rc[:, :], C.rconst, w=[rc])
        ld = kb.sb("ld", [128, 16], F32)
        kb.dma("sp", ld[:, :], C.ev_decay[i].partition_broadcast(128), w=[ld])
        kb.op("act", lambda e: e.activation(out=ld[:, :], in_=ld[:, :], func=AF.Exp), r=[ld], w=[ld])
        kb.op("dve", lambda e: e.tensor_scalar(out=ld[:, :], in0=ld[:, :], scalar1=-1.0, scalar2=None, op0=ALU.mult), r=[ld], w=[ld])
        maskT = kb.sb("maskT", [128, 16, 128], BF16)
        DQ = kb.sb("DQ", [64, 16, 128], F32)
        DK = kb.sb("DK", [128, 16], F32)
        GC = kb.sb("GC", [64, 16], F32)
        tmpm = kb.sb("tmpm", [128, 128], F32)
        for d_ in range(2):
            for h in range(8):
                c = d_ * 8 + h
                kb.op("act", lambda e: e.activation(out=tmpm[:, :], in_=rc[:, d_ * 128:(d_ + 1) * 128], func=AF.Exp,
                                                    scale=ld[:, c:c + 1]), r=[rc, ld], w=[tmpm])
                kb.op("dve", lambda e: e.tensor_tensor(out=maskT[:, c, :], in0=tmpm[:, :], in1=rc[:, (2 + d_) * 128:(3 + d_) * 128],
                                                       op=ALU.mult), r=[tmpm, rc], w=[maskT])
                kb.op("act", lambda e: e.activation(out=DQ[:, c, :], in_=rc[0:64, (4 + d_) * 128:(5 + d_) * 128], func=AF.Exp,
                                                    scale=ld[0:64, c:c + 1]), r=[rc, ld], w=[DQ])
            kb.op("dve", lambda e: e.tensor_scalar(out=DK[:, d_ * 8:(d_ + 1) * 8], in0=ld[:, d_ * 8:(d_ + 1) * 8],
                                                   scalar1=rc[:, 768 + d_:769 + d_], scalar2=None, op0=ALU.mult), r=[ld, rc], w=[DK])
        kb.op("act", lambda e: e.activation(out=DK[:, :], in_=DK[:, :], func=AF.Exp), r=[DK], w=[DK])
        kb.op("act", lambda e: e.activation(out=GC[:, :], in_=ld[0:64, :], func=AF.Exp, scale=128.0), r=[ld], w=[GC])
        qkvb = [kb.sb("qkv", [128, 2048], BF16) for _ in range(2)]
        gateb = [kb.sb("gate", [128, 1024], BF16) for _ in range(2)]
        ofb = [kb.sb("of", [128, 1024], F32) for _ in range(2)]
        qT = kb.sb("qT", [64, 8, 128], BF16)
        qTd = kb.sb("qTd", [64, 8, 128], BF16)
        kT = kb.sb("kT", [64, 8, 128], BF16)
        kdec = kb.sb("kdec", [128, 8, 64], BF16)
        smb = [kb.sb("sm", [128, 128], BF16) for _ in range(8)]
        S = kb.sb("S", [64, 8, 128], F32)
        Sb = kb.sb("Sb", [64, 8, 128], BF16)
        osum = kb.sb("osum", [128, 1024], F32)
        junk = kb.sb("junkr", [128, 1024], F32)
        ssh = kb.sb("sshr", [128, 8], F32)
        rs8 = kb.sb("rs8r", [128, 8], F32)
        tmp8 = kb.sb("tmp8r", [128, 8], F32)
        mixb = [kb.sb("mixr", [128, 1024], BF16) for _ in range(2)]
        psQ, psK = ps[0], ps[1]
        psS = [ps[2], ps[3]]
        psO = [ps[4], ps[5]]
        psKV = [ps[6], ps[7]]
        pq = psQ[:, :].bitcast(BF16)
        pk = psK[:, :].bitcast(BF16)
        n_ = 0
        for d_ in range(2):
            order = list(range(n_ctx_tiles)) + list(lat_tiles)
            if d_ == 1:
                order = list(range(n_ctx_tiles))[::-1] + list(lat_tiles)[::-1]
            kb.op("dve", lambda e: e.memset(S[:, :, :], 0.0), w=[S])
            kb.op("dve", lambda e: e.memset(Sb[:, :, :], 0.0), w=[Sb])
            for t in order:
                if getattr(C, "dbgB", 9) < 1:
                    break
                qkv = qkvb[n_ % 2]
                gate = gateb[n_ % 2]
                of = ofb[n_ % 2]
                mix = mixb[n_ % 2]
                n_ += 1
                kb.dma("sp", qkv[:, :], C.PA[t * 128:(t + 1) * 128, 0:2048], r=[kb.tag("PA", t)], w=[qkv])
                if d_ == 1:
                    kb.dma("sp", gate[:, :], C.PA[t * 128:(t + 1) * 128, 2048:3072], r=[kb.tag("PA", t)], w=[gate])
                    kb.dma("sp", of[:, :], C.OF[t * 128:(t + 1) * 128, :], r=[kb.tag("OF", t)], w=[of])
                for h in range(8):
                    kb.op("pe", lambda e: e.transpose(pq[0:64, h * 128:(h + 1) * 128], qkv[:, h * 64:(h + 1) * 64], C.identb_s[:, :]),
                          r=[qkv, C.identb_s], w=[psQ])
                for h in range(8):
                    kb.op("pe", lambda e: e.transpose(pk[0:64, h * 128:(h + 1) * 128], qkv[:, 512 + h * 64:512 + (h + 1) * 64],
                                                      C.identb_s[:, :]), r=[qkv, C.identb_s], w=[psK])
                kb.op("act", lambda e: e.copy(out=qT[:, :, :].rearrange("p h t -> p (h t)"), in_=pq[0:64, :]), r=[psQ], w=[qT])
                kb.op("dve", lambda e: e.tensor_tensor(out=qTd[:, :, :], in0=pq[0:64, :].rearrange("p (h t) -> p h t", h=8),
                                                       in1=DQ[:, d_ * 8:(d_ + 1) * 8, :], op=ALU.mult), r=[psQ, DQ], w=[qTd])
                kb.op("act", lambda e: e.copy(out=kT[:, :, :].rearrange("p h t -> p (h t)"), in_=pk[0:64, :]), r=[psK], w=[kT])
                kb.op("dve", lambda e: e.tensor_tensor(out=kdec[:, :, :], in0=qkv[:, 512:1024].rearrange("p (h d) -> p h d", h=8),
                                                        in1=DK[:, d_ * 8:(d_ + 1) * 8].unsqueeze(2).to_broadcast([128, 8, 64]),
                                                        op=ALU.mult), r=[qkv, DK], w=[kdec])
                if getattr(C, "dbgB", 9) < 2:
                    continue
                for h in range(8):
                    pS = psS[h // 4]
                    sc_ = (h % 4) * 128
                    kb.op("pe", lambda e: e.matmul(pS[:, sc_:sc_ + 128], lhsT=kT[:, h, :], rhs=qT[:, h, :], start=True, stop=True),
                          r=[kT, qT], w=[pS])
                for h in range(8):
                    pS = psS[h // 4]
                    sc_ = (h % 4) * 128
                    kb.op("dve", lambda e: e.tensor_tensor(out=smb[h][:, :], in0=pS[:, sc_:sc_ + 128], in1=maskT[:, d_ * 8 + h, :], op=ALU.mult),
                          r=[pS, maskT], w=[smb[h]])
                for h in range(8):
                    sm = smb[h]
                    pO = psO[h // 4]
                    pKV = psKV[h // 4]
                    oc = (h % 4) * 128
                    vh = qkv[:, 1024 + h * 128:1024 + (h + 1) * 128]
                    kb.op("pe", lambda e: e.matmul(pO[:, oc:oc + 128], lhsT=sm[:, :], rhs=vh, start=True, stop=False),
                          r=[sm, qkv], w=[pO])
                    kb.op("pe", lambda e: e.matmul(pO[:, oc:oc + 128], lhsT=qTd[:, h, :], rhs=Sb[:, h, :], start=False, stop=True),
                          r=[qTd, Sb], w=[pO])
                    kb.op("pe", lambda e: e.matmul(pKV[0:64, oc:oc + 128], lhsT=kdec[:, h, :], rhs=vh, start=True, stop=True),
                          r=[kdec, qkv], w=[pKV])
                if getattr(C, "dbgB", 9) < 3:
                    continue
                kb.op("dve", lambda e: e.tensor_tensor(out=S[:, :, :], in0=S[:, :, :],
                                                       in1=GC[:, d_ * 8:(d_ + 1) * 8].unsqueeze(2).to_broadcast([64, 8, 128]), op=ALU.mult),
                      r=[S, GC], w=[S])
                for hb in range(2):
                    kb.op("dve", lambda e: e.tensor_tensor(out=S[:, hb * 4:(hb + 1) * 4, :], in0=S[:, hb * 4:(hb + 1) * 4, :],
                                                           in1=psKV[hb][0:64, :].rearrange("p (h t) -> p h t", h=4), op=ALU.add),
                          r=[S, psKV[hb]], w=[S])
                kb.op("act", lambda e: e.copy(out=Sb[:, :, :].rearrange("p h t -> p (h t)"), in_=S[:, :, :].rearrange("p h t -> p (h t)")), r=[S], w=[Sb])
                if d_ == 0:
                    for hb in range(2):
                        kb.op("act", lambda e: e.copy(out=of[:, hb * 512:(hb + 1) * 512], in_=psO[hb][:, :]), r=[psO[hb]], w=[of])
                    kb.dma("sp", C.OF[t * 128:(t + 1) * 128, :], of[:, :], r=[of], w=[kb.tag("OF", t)])
                else:
                    for hb in range(2):
                        kb.op("dve", lambda e: e.tensor_tensor(out=osum[:, hb * 512:(hb + 1) * 512], in0=psO[hb][:, :],
                                                               in1=of[:, hb * 512:(hb + 1) * 512], op=ALU.add),
                              r=[psO[hb], of], w=[osum])
                    kb.op("act", lambda e: e.activation(out=junk[:, :], in_=osum[:, :], func=AF.Square), r=[osum], w=[junk])
                    kb.op("dve", lambda e: e.reduce_sum(out=ssh[:, :], in_=junk[:, :].rearrange("p (h d) -> p h d", h=8), axis=AX.X),
                          r=[junk], w=[ssh])
                    rsqrt_mean(C, rs8, ssh, 8, 1.0 / 128, tmp8)
                    kb.op("dve", lambda e: e.tensor_tensor(out=osum[:, :].rearrange("p (h d) -> p h d", h=8),
                                                           in0=osum[:, :].rearrange("p (h d) -> p h d", h=8),
                                                           in1=rs8[:, :].unsqueeze(2).to_broadcast([128, 8, 128]), op=ALU.mult),
                          r=[osum, rs8], w=[osum])
                    kb.op("pool", lambda e: e.tensor_tensor(out=mix[:, :], in0=osum[:, :], in1=gate[:, :], op=ALU.mult),
                          r=[osum, gate], w=[mix])
                    kb.dma("sp", C.MIX[t * 128:(t + 1) * 128, 0:1024], mix[:, :], r=[mix], w=[kb.tag("MIXr", t)])


def phaseC_att(C, l, qblocks=None, key_tiles=None):
    kb = C.kb
    ps = C.ps
    if key_tiles is None:
        key_tiles = list(range(NT))
    if qblocks is None:
        qblocks = [([0, 1], [0, 1])] + [([2 + 4 * b + j for j in range(4)], key_tiles) for b in range(16)]
    with kb.phase():
        kTa = kb.sb("kTa", [64, 2, T], BF16)
        Va = kb.sb("Va", [128, NT, 2, 128], BF16)
        kb.op("pool", lambda e: e.memset(Va[:, :, :, :], 1.0), w=[Va])
        kvb = [kb.sb("kvld", [128, 256], BF16) for _ in range(2)]
        pT0 = ps[0]
        pt0 = pT0[:, :].bitcast(BF16)
        for n_, t in enumerate(key_tiles):
            kv = kvb[n_ % 2]
            kb.dma("sp", kv[:, :], C.PA[t * 128:(t + 1) * 128, 3584:3840], r=[kb.tag("PA", t)], w=[kv])
            for g in range(2):
                kb.op("pe", lambda e: e.transpose(pt0[0:64, g * 128:(g + 1) * 128], kv[:, g * 64:(g + 1) * 64], C.identb_s[:, :]),
                      r=[kv, C.identb_s], w=[pT0])
            for g in range(2):
                kb.op("act", lambda e: e.copy(out=kTa[:, g, t * 128:(t + 1) * 128], in_=pt0[0:64, g * 128:(g + 1) * 128]),
                      r=[pT0], w=[kTa])
            kb.op("pool", lambda e: e.tensor_copy(out=Va[:, t, :, 0:64], in_=kv[:, 128:256].rearrange("p (g d) -> p g d", g=2)),
                  r=[kv], w=[Va])
        aqb = [kb.sb("aq", [128, 4, 512], BF16) for _ in range(2)]
        qTb = kb.sb("qTb", [64, 8, 512], BF16)
        pTb = [kb.sb("pT", [128, 512], BF16) for _ in range(3)]
        rsb = kb.sb("rsb", [128, 512], F32)
        atb = [kb.sb("attT", [64, 512], BF16) for _ in range(2)]
        psS = [ps[2], ps[3], ps[4]]
        psO = [ps[5], ps[6]]
        psQ = [ps[0], ps[1]]
        for bi, (qt, keys) in enumerate(qblocks):
            nq = len(qt) * 128
            aq = aqb[bi % 2]
            for j, t in enumerate(qt):
                kb.dma("sp", aq[:, j, :], C.PA[t * 128:(t + 1) * 128, 3072:3584], r=[kb.tag("PA", t)], w=[aq])
            for hp in range(4):
                pQ = psQ[hp % 2]
                pqv = pQ[:, :].bitcast(BF16)
                for hh in range(2):
                    h = hp * 2 + hh
                    for j in range(len(qt)):
                        kb.op("pe", lambda e: e.transpose(pqv[0:64, hh * 512 + j * 128:hh * 512 + (j + 1) * 128],
                                                          aq[:, j, h * 64:(h + 1) * 64], C.identb_s[:, :]),
                              r=[aq, C.identb_s], w=[pQ])
                kb.op("dve", lambda e: e.tensor_copy(out=qTb[:, hp * 2:hp * 2 + 2, 0:nq],
                                                     in_=pqv[0:64, :].rearrange("p (h q) -> p h q", h=2)[:, :, 0:nq]),
                      r=[pQ], w=[qTb])
            tok0 = qt[0] * 128
            for h in range(8):
                g = h // 4
                pO = psO[h % 2]
                at = atb[h % 2]
                nk = len(keys)

                def s_mm(ki):
                    kt = keys[ki]
                    pS = psS[ki % 3]
                    kb.op("pe", lambda e: e.matmul(pS[:, 0:nq], lhsT=kTa[:, g, kt * 128:(kt + 1) * 128], rhs=qTb[:, h, 0:nq],
                                                   start=True, stop=True), r=[kTa, qTb], w=[pS])
                s_mm(0)
                if nk > 1:
                    s_mm(1)
                for ki in range(nk):
                    kt = keys[ki]
                    pS = psS[ki % 3]
                    pT = pTb[ki % 3]
                    kb.op("act", lambda e: e.activation(out=pT[:, 0:nq], in_=pS[:, 0:nq], func=AF.Exp), r=[pS], w=[pT])
                    if ki + 2 < nk:
                        s_mm(ki + 2)
                    kb.op("pe", lambda e: e.matmul(pO[:, 0:nq], lhsT=Va[:, kt, g, :], rhs=pT[:, 0:nq], start=(ki == 0), stop=(ki == nk - 1)),
                          r=[Va, pT], w=[pO])
                kb.op("dve", lambda e: e.reciprocal(out=rsb[64:128, 0:nq], in_=pO[64:128, 0:nq]), r=[pO], w=[rsb])
                kb.op("dve", lambda e: e.tensor_tensor(out=at[:, 0:nq], in0=pO[0:64, 0:nq], in1=rsb[64:128, 0:nq], op=ALU.mult),
                      r=[pO, rsb], w=[at])
                kb.dma("sp", C.AT[h * 64:(h + 1) * 64, tok0:tok0 + nq], at[:, 0:nq], r=[at], w=[kb.tag("AT", bi)])


def phaseD_out(C, l, tiles, w_out_ap, nk, mix_src, post):
    kb = C.kb
    ps = C.ps
    with kb.phase():
        Wo = kb.sb("Wo", [128, nk, D], BF16)
        wv = w_out_ap.rearrange("(k p) c -> p k c", p=128)
        for k in range(nk):
            kb.dma("pool", Wo[:, k, :], wv[:, k, :], w=[Wo])
        Gt = kb.sb("Gt", [128, 4096], F32)
        kb.dma("sp", Gt[:, :], C.G[l], r=[kb.tag("G", l)], w=[Gt])
        mTb = [kb.sb("mT", [128, nk, 128], BF16) for _ in range(2)]
        xb = [kb.sb("xd", [128, D], F32) for _ in range(2)]
        yb = kb.sb("yb", [128, D], F32)
        st = post(None, None, None, setup=True)
        for n_, t in enumerate(tiles):
            lat = t >= 2
            mT = mTb[n_ % 2]
            xt = xb[n_ % 2]
            kb.dma("sp", xt[:, :], C.X[t * 128:(t + 1) * 128, :], r=[kb.tag("X", t)], w=[xt])
            mix_src(t, mT, n_)
            psY = [ps[6], ps[7]]
            for nb in range(2):
                for k in range(nk):
                    kb.op("pe", lambda e: e.matmul(psY[nb][:, :], lhsT=mT[:, k, :], rhs=Wo[:, k, nb * 512:(nb + 1) * 512],
                                                   start=(k == 0), stop=(k == nk - 1)), r=[mT, Wo], w=[psY[nb]])
            g0 = 0 if lat else 2048
            for nb in range(2):
                kb.op("dve", lambda e: e.tensor_tensor(out=yb[:, nb * 512:(nb + 1) * 512], in0=psY[nb][:, :],
                                                       in1=Gt[:, g0 + nb * 512:g0 + (nb + 1) * 512], op=ALU.mult),
                      r=[psY[nb], Gt], w=[yb])
            kb.op("pool", lambda e: e.tensor_tensor(out=xt[:, :], in0=xt[:, :], in1=yb[:, :], op=ALU.add), r=[xt, yb], w=[xt])
            kb.dma("sp", C.X[t * 128:(t + 1) * 128, :], xt[:, :], r=[xt], w=[kb.tag("X", t)])
            post(t, xt, Gt, st=st)
        post(None, None, None, st=st, finish=True)


def even_mix_src(C):
    kb = C.kb
    mrb = [kb.sb("mr", [128, D], BF16) for _ in range(2)]
    ATv = C.AT.rearrange("(c p) t -> p c t", p=128)

    def src(t, mT, n_):
        mr = mrb[n_ % 2]
        bi = 0 if t < 2 else 1 + (t - 2) // 4
        kb.dma("sp", mr[:, :], C.MIX[t * 128:(t + 1) * 128, 0:1024], r=[kb.tag("MIXr", t)], w=[mr])
        kb.dma("sp", mT[:, 8:12, :], ATv[:, :, t * 128:(t + 1) * 128], r=[kb.tag("AT", bi)], w=[mT])
        pT = C.ps[0]
        pv = pT[:, :].bitcast(BF16)
        for k in range(8):
            kb.op("pe", lambda e: e.transpose(pv[:, k * 128:(k + 1) * 128], mr[:, k * 128:(k + 1) * 128], C.identb_s[:, :]),
                  r=[mr, C.identb_s], w=[pT])
        kb.op("act", lambda e: e.copy(out=mT[:, 0:8, :].rearrange("p k t -> p (k t)"), in_=pv[:, :]), r=[pT], w=[mT])
    return src


NSLOT = 164
TPAD = T + 128
BIGI = 1.0e6
NEG = -1.0e30


def moe_consts():
    import ml_dtypes
    p = np.arange(128)
    tri = (p[:, None] < p[None, :]).astype(np.float32).astype(ml_dtypes.bfloat16)
    ones = np.ones((128, 128), np.float32).astype(ml_dtypes.bfloat16)
    eidx = np.tile(np.arange(32, dtype=np.float32)[None, :], (128, 1))
    pidx = p.astype(np.float32)[:, None]
    sidx = np.tile(np.arange(NSLOT, dtype=np.float32)[None, :], (128, 1))
    mconst = np.ascontiguousarray(np.concatenate([eidx, pidx, sidx], axis=1), np.float32)
    src = (np.arange(NT)[None, :] * 128 + p[:, None]).astype(np.int32)
    li = np.zeros((NSLOT + 1, 128, 4), np.int32)
    li[:, :, 1] = T + p[None, :]
    li = np.ascontiguousarray(li.transpose(1, 0, 2).reshape(128, (NSLOT + 1) * 4))
    return np.ascontiguousarray(np.concatenate([tri, ones], axis=1)), mconst, src, li


def phaseD(C, l, tiles, w_out_ap, nk, mix_src_factory, route_tiles):
    kb = C.kb
    ps = C.ps
    with kb.phase():
        Wo = kb.sb("Wo", [128, nk, D], BF16)
        wv = w_out_ap.rearrange("(k p) c -> p k c", p=128)
        for k in range(nk):
            kb.dma("pool", Wo[:, k, :], wv[:, k, :], w=[Wo])
        Gt = kb.sb("Gt", [128, 8192], F32)
        kb.dma("sp", Gt[:, :], C.G[l], r=[kb.tag("G", l)], w=[Gt])
        nrep = kb.sb("nrep", [128, D], F32)
        kb.dma("sp", nrep[:, :], C.norms[DEPTH + l].partition_broadcast(128), w=[nrep])
        for v_ in range(2):
            o0 = (5 + 2 * v_) * 1024
            kb.op("dve", lambda e: e.scalar_tensor_tensor(out=Gt[:, o0:o0 + 1024], in0=Gt[:, o0:o0 + 1024], scalar=1.0, in1=nrep[:, :],
                                                          op0=ALU.add, op1=ALU.mult), r=[Gt, nrep], w=[Gt])
        Wr = kb.sb("Wr", [128, 8, 36], F32)
        kb.dma("sp", Wr[:, :, :], C.wr[l].rearrange("(k p) c -> p k c", p=128), w=[Wr])
        brr = kb.sb("brr", [128, 36], F32)
        kb.dma("sp", brr[:, :], C.br[l].partition_broadcast(128), w=[brr])
        mc = kb.sb("mc", [128, 33 + NSLOT], F32)
        kb.dma("sp", mc[:, :], C.mconst, w=[mc])
        tro = kb.sb("tro", [128, 256], BF16)
        kb.dma("sp", tro[:, :], C.triones, w=[tro])
        srci = kb.sb("srci", [128, NT], I32)
        kb.dma("sp", srci[:, :], C.srcidx, w=[srci])
        linit = kb.sb("linit", [128, (NSLOT + 1) * 4], I32)
        kb.dma("sp", linit[:, :], C.listinit, w=[linit])
        kb.dma("sp", C.LIST.rearrange("(s p) c -> p s c", p=128), linit[:, :].rearrange("p (s c) -> p s c", c=4), r=[linit],
               w=[kb.tag("LISTinit")])
        base = kb.sb("base", [128, 32], F32)
        kb.op("dve", lambda e: e.memset(base[:, :], 0.0), w=[base])
        RT = kb.sb("RT", [128, NT, 8], F32)
        kb.op("dve", lambda e: e.memset(RT[:, :, :], 0.0), w=[RT])
        mTb = [kb.sb("mT", [128, nk, 128], BF16) for _ in range(2)]
        xb = [kb.sb("xd", [128, D], F32) for _ in range(2)]
        yb = kb.sb("yb", [128, D], F32)
        junk = kb.sb("junkd", [128, D], BF16)
        xn2 = kb.sb("xn2", [128, D], F32)
        fb = [kb.sb("fb", [128, D], BF16) for _ in range(2)]
        fT = kb.sb("fT", [128, 8, 128], F32)
        ss = kb.sb("ssd", [128, 1], F32)
        rstd = kb.sb("rstdd", [128, 1], F32)
        tmp1 = kb.sb("tmp1d", [128, 8], F32)
        lg = kb.sb("lg", [128, 36], F32)
        sm = kb.sb("smalls", [128, 16], F32)
        geq = kb.sb("geq", [128, 4], F32)
        gex = kb.sb("gex", [128, 4], F32)
        ml = kb.sb("ml", [128, 32], F32)
        ml2 = kb.sb("ml2", [128, 32], F32)
        oh1 = kb.sb("oh1", [128, 32], F32)
        oh2 = kb.sb("oh2", [128, 32], F32)
        ind = kb.sb("ind", [128, 32], BF16)
        pos = kb.sb("pos", [128, 32], F32)
        t32 = kb.sb("t32", [128, 32], F32)
        mix_src = mix_src_factory()
        EIDX = mc[:, 0:32]
        for n_, t in enumerate(tiles):
            lat = t >= 2
            mT = mTb[n_ % 2]
            xt = xb[n_ % 2]
            kb.dma("sp", xt[:, :], C.X[t * 128:(t + 1) * 128, :], r=[kb.tag("X", t)], w=[xt])
            mix_src(t, mT, n_)
            psY = [ps[6], ps[7]]
            for nb in range(2):
                for k in range(nk):
                    kb.op("pe", lambda e: e.matmul(psY[nb][:, :], lhsT=mT[:, k, :], rhs=Wo[:, k, nb * 512:(nb + 1) * 512],
                                                   start=(k == 0), stop=(k == nk - 1)), r=[mT, Wo], w=[psY[nb]])
            g0 = 0 if lat else 2048
            for nb in range(2):
                kb.op("dve", lambda e: e.tensor_tensor(out=yb[:, nb * 512:(nb + 1) * 512], in0=psY[nb][:, :],
                                                       in1=Gt[:, g0 + nb * 512:g0 + (nb + 1) * 512], op=ALU.mult),
                      r=[psY[nb], Gt], w=[yb])
            kb.op("pool", lambda e: e.tensor_tensor(out=xt[:, :], in0=xt[:, :], in1=yb[:, :], op=ALU.add), r=[xt, yb], w=[xt])
            kb.dma("sp", C.X[t * 128:(t + 1) * 128, :], xt[:, :], r=[xt], w=[kb.tag("X", t)])
            if t not in route_tiles:
                continue
            f = fb[n_ % 2]
            norm_tile(C, xt, ss, rstd, tmp1, junk, xn2)
            v0 = 4096 if lat else 6144
            kb.op("dve", lambda e: e.tensor_tensor(out=xn2[:, :], in0=xn2[:, :], in1=Gt[:, v0 + 1024:v0 + 2048], op=ALU.mult),
                  r=[xn2, Gt], w=[xn2])
            kb.op("pool", lambda e: e.tensor_tensor(out=xn2[:, :], in0=xn2[:, :], in1=Gt[:, v0:v0 + 1024], op=ALU.add),
                  r=[xn2, Gt], w=[xn2])
            kb.op("act", lambda e: e.copy(out=f[:, :], in_=xn2[:, :]), r=[xn2], w=[f])
            kb.dma("sp", C.F[t * 128:(t + 1) * 128, :], f[:, :], r=[f], w=[kb.tag("F", t)])
            psT = [ps[0], ps[1]]
            for k in range(8):
                kb.op("pe", lambda e: e.transpose(psT[k // 4][:, (k % 4) * 128:(k % 4 + 1) * 128], xn2[:, k * 128:(k + 1) * 128],
                                                  C.identf_s[:, :]), r=[xn2, C.identf_s], w=[psT[k // 4]])
            for hb in range(2):
                kb.op("act", lambda e: e.copy(out=fT[:, hb * 4:(hb + 1) * 4, :].rearrange("p k t -> p (k t)"), in_=psT[hb][:, :]),
                      r=[psT[hb]], w=[fT])
            pL = ps[2]
            for k in range(8):
                kb.op("pe", lambda e: e.matmul(pL[:, 0:36], lhsT=fT[:, k, :], rhs=Wr[:, k, :], start=(k == 0), stop=(k == 7)),
                      r=[fT, Wr], w=[pL])
            kb.op("dve", lambda e: e.tensor_tensor(out=lg[:, :], in0=pL[:, 0:36], in1=brr[:, :], op=ALU.add), r=[pL, brr], w=[lg])
            gmax, ngmax, gsum, pg, m1, m2, dd, ed, w1, w2 = [sm[:, i:i + 1] for i in range(10)]
            kb.op("dve", lambda e: e.reduce_max(out=gmax, in_=lg[:, 0:4], axis=AX.X), r=[lg], w=[sm])
            kb.op("dve", lambda e: e.tensor_scalar(out=ngmax, in0=gmax, scalar1=-1.0, scalar2=None, op0=ALU.mult), r=[sm], w=[sm])
            kb.op("dve", lambda e: e.tensor_scalar(out=geq[:, :], in0=lg[:, 0:4], scalar1=gmax, scalar2=None, op0=ALU.is_equal),
                  r=[lg, sm], w=[geq])
            kb.op("act", lambda e: e.activation(out=gex[:, :], in_=lg[:, 0:4], func=AF.Exp, bias=ngmax, scale=1.0, accum_out=gsum),
                  r=[lg, sm], w=[gex, sm])
            kb.op("dve", lambda e: e.reciprocal(out=pg, in_=gsum), r=[sm], w=[sm])
            kb.op("dve", lambda e: e.tensor_scalar(out=geq[:, :], in0=geq[:, :], scalar1=-NEG, scalar2=NEG, op0=ALU.mult, op1=ALU.add),
                  r=[geq], w=[geq])
            kb.op("dve", lambda e: e.tensor_tensor(out=ml[:, :].rearrange("p (g e) -> p g e", g=4),
                                                   in0=lg[:, 4:36].rearrange("p (g e) -> p g e", g=4),
                                                   in1=geq[:, :].unsqueeze(2).to_broadcast([128, 4, 8]), op=ALU.add), r=[lg, geq], w=[ml])
            kb.op("dve", lambda e: e.reduce_max(out=m1, in_=ml[:, :], axis=AX.X), r=[ml], w=[sm])
            kb.op("dve", lambda e: e.tensor_scalar(out=oh1[:, :], in0=ml[:, :], scalar1=m1, scalar2=None, op0=ALU.is_equal),
                  r=[ml, sm], w=[oh1])
            kb.op("dve", lambda e: e.scalar_tensor_tensor(out=ml2[:, :], in0=oh1[:, :], scalar=NEG, in1=ml[:, :], op0=ALU.mult, op1=ALU.add),
                  r=[oh1, ml], w=[ml2])
            kb.op("dve", lambda e: e.reduce_max(out=m2, in_=ml2[:, :], axis=AX.X), r=[ml2], w=[sm])
            kb.op("dve", lambda e: e.tensor_scalar(out=oh2[:, :], in0=ml2[:, :], scalar1=m2, scalar2=None, op0=ALU.is_equal),
                  r=[ml2, sm], w=[oh2])
            kb.op("dve", lambda e: e.tensor_tensor(out=dd, in0=m2, in1=m1, op=ALU.subtract), r=[sm], w=[sm])
            kb.op("act", lambda e: e.activation(out=ed, in_=dd, func=AF.Exp), r=[sm], w=[sm])
            kb.op("dve", lambda e: e.tensor_scalar(out=w1, in0=ed, scalar1=1.0, scalar2=None, op0=ALU.add), r=[sm], w=[sm])
            kb.op("dve", lambda e: e.reciprocal(out=w1, in_=w1), r=[sm], w=[sm])
            kb.op("dve", lambda e: e.tensor_tensor(out=w2, in0=ed, in1=w1, op=ALU.mult), r=[sm], w=[sm])
            kb.op("dve", lambda e: e.tensor_tensor(out=RT[:, t, 2:3], in0=w1, in1=pg, op=ALU.mult), r=[sm], w=[RT])
            kb.op("dve", lambda e: e.tensor_tensor(out=RT[:, t, 5:6], in0=w2, in1=pg, op=ALU.mult), r=[sm], w=[RT])
            kb.op("dve", lambda e: e.tensor_tensor(out=ind[:, :], in0=oh1[:, :], in1=oh2[:, :], op=ALU.add), r=[oh1, oh2], w=[ind])
            pR = ps[3]
            kb.op("pe", lambda e: e.matmul(pR[:, 0:32], lhsT=tro[:, 0:128], rhs=ind[:, :], start=True, stop=True), r=[tro, ind], w=[pR])
            kb.op("pe", lambda e: e.matmul(pR[:, 32:64], lhsT=tro[:, 128:256], rhs=ind[:, :], start=True, stop=True), r=[tro, ind], w=[pR])
            kb.op("dve", lambda e: e.tensor_tensor(out=pos[:, :], in0=pR[:, 0:32], in1=base[:, :], op=ALU.add), r=[pR, base], w=[pos])
            kb.op("dve", lambda e: e.tensor_tensor(out=base[:, :], in0=pR[:, 32:64], in1=base[:, :], op=ALU.add), r=[pR, base], w=[base])
            for k_, oh in enumerate((oh1, oh2)):
                kb.op("dve", lambda e: e.tensor_tensor(out=t32[:, :], in0=oh[:, :], in1=EIDX, op=ALU.mult), r=[oh, mc], w=[t32])
                kb.op("dve", lambda e: e.reduce_sum(out=RT[:, t, 3 * k_:3 * k_ + 1], in_=t32[:, :], axis=AX.X), r=[t32], w=[RT])
                kb.op("dve", lambda e: e.tensor_tensor(out=t32[:, :], in0=oh[:, :], in1=pos[:, :], op=ALU.mult), r=[oh, pos], w=[t32])
                kb.op("dve", lambda e: e.reduce_sum(out=RT[:, t, 3 * k_ + 1:3 * k_ + 2], in_=t32[:, :], axis=AX.X), r=[t32], w=[RT])
        ni = kb.sb("ni", [128, 32], I32)
        ca = kb.sb("ca", [128, 32], F32)
        cb = kb.sb("cb", [128, 32], F32)
        tl_ = kb.sb("tl", [128, 32], F32)
        kb.op("dve", lambda e: e.tensor_scalar(out=t32[:, :], in0=base[:, :], scalar1=127.0, scalar2=None, op0=ALU.add), r=[base], w=[t32])
        kb.op("dve", lambda e: e.tensor_copy(out=ni[:, :], in_=t32[:, :]), r=[t32], w=[ni])
        kb.op("dve", lambda e: e.tensor_scalar(out=ni[:, :], in0=ni[:, :], scalar1=7, scalar2=None, op0=ALU.arith_shift_right), r=[ni], w=[ni])
        kb.op("dve", lambda e: e.tensor_copy(out=tl_[:, :], in_=ni[:, :]), r=[ni], w=[tl_])
        kb.op("dve", lambda e: e.tensor_copy(out=ca[:, :], in_=tl_[:, :]), r=[tl_], w=[ca])
        a_, b_ = ca, cb
        for d_ in (1, 2, 4, 8, 16):
            kb.op("dve", lambda e: e.tensor_copy(out=b_[:, 0:d_], in_=a_[:, 0:d_]), r=[a_], w=[b_])
            kb.op("dve", lambda e: e.tensor_tensor(out=b_[:, d_:32], in0=a_[:, d_:32], in1=a_[:, 0:32 - d_], op=ALU.add), r=[a_], w=[b_])
            a_, b_ = b_, a_
        cum = a_
        ss128 = b_
        kb.op("dve", lambda e: e.tensor_tensor(out=ss128[:, :], in0=cum[:, :], in1=tl_[:, :], op=ALU.subtract), r=[cum, tl_], w=[ss128])
        kb.op("dve", lambda e: e.tensor_scalar(out=ss128[:, :], in0=ss128[:, :], scalar1=128.0, scalar2=None, op0=ALU.mult),
              r=[ss128], w=[ss128])
        big = kb.sb("big", [128, NT, 32], F32)
        offf = kb.sb("offf", [128, 2, NT], F32)
        offi = kb.sb("offi", [128, 2, NT], I32)
        ent = kb.sb("ent", [128, 2, NT, 4], I32)
        kb.op("dve", lambda e: e.memset(ent[:, :, :, :], 0), w=[ent])
        for k_ in range(2):
            kb.op("dve", lambda e: e.tensor_tensor(out=big[:, :, :], in0=mc[:, 0:32].unsqueeze(1).to_broadcast([128, NT, 32]),
                                                   in1=RT[:, :, 3 * k_:3 * k_ + 1].to_broadcast([128, NT, 32]), op=ALU.is_equal),
                  r=[mc, RT], w=[big])
            kb.op("dve", lambda e: e.tensor_tensor(out=big[:, :, :], in0=big[:, :, :],
                                                   in1=ss128[:, :].unsqueeze(1).to_broadcast([128, NT, 32]), op=ALU.mult),
                  r=[big, ss128], w=[big])
            kb.op("dve", lambda e: e.reduce_sum(out=offf[:, k_, :], in_=big[:, :, :], axis=AX.X), r=[big], w=[offf])
            kb.op("dve", lambda e: e.tensor_tensor(out=offf[:, k_, :], in0=offf[:, k_, :], in1=RT[:, :, 3 * k_ + 1], op=ALU.add),
                  r=[offf, RT], w=[offf])
            kb.op("dve", lambda e: e.tensor_copy(out=offi[:, k_, :], in_=offf[:, k_, :]), r=[offf], w=[offi])
            kb.op("dve", lambda e: e.tensor_copy(out=ent[:, k_, :, 0], in_=srci[:, :]), r=[srci], w=[ent])
            kb.op("dve", lambda e: e.tensor_scalar(out=ent[:, k_, :, 1], in0=srci[:, :], scalar1=k_ * TPAD, scalar2=None, op0=ALU.add),
                  r=[srci], w=[ent])
            kb.op("dve", lambda e: e.tensor_copy(out=ent[:, k_, :, 2], in_=RT[:, :, 3 * k_ + 2].bitcast(I32)), r=[RT], w=[ent])
        if hasattr(C, "dbgD"):
            kb.dma("sp", C.dbgD["offi"], offi[:, :, :].rearrange("p k t -> p (k t)"), r=[offi], w=[kb.tag("dbg1")])
            kb.dma("sp", C.dbgD["RT"], RT[:, :, :].rearrange("p t c -> p (t c)"), r=[RT], w=[kb.tag("dbg2")])
            kb.dma("sp", C.dbgD["base"], base[:, :], r=[base], w=[kb.tag("dbg4")])
            kb.dma("sp", C.dbgD["ent"], ent[:, :, :, :].rearrange("p k t c -> p (k t c)"), r=[ent], w=[kb.tag("dbg5")])
        for t in (route_tiles if not getattr(C, "skip_scatter", False) else []):
            for k_ in range(2):
                kb.dma("pool", C.LIST, ent[:, k_, t, :], r=[ent, offi, kb.tag("LISTinit")], w=[kb.tag("LISTs", t, k_)],
                       indirect=dict(out_offset=bass.IndirectOffsetOnAxis(ap=offi[:, k_, t:t + 1], axis=0), in_offset=None))
        SIDX = mc[:, 33:33 + NSLOT]
        cmp_ = kb.sb("cmp", [128, NSLOT, 32], F32)
        eid = kb.sb("eid", [128, NSLOT + 2], F32)
        kb.op("dve", lambda e: e.memset(eid[:, 0:2], -1.0), w=[eid])
        kb.op("dve", lambda e: e.tensor_tensor(out=cmp_[:, :, :], in0=cum[:, :].unsqueeze(1).to_broadcast([128, NSLOT, 32]),
                                               in1=SIDX.unsqueeze(2).to_broadcast([128, NSLOT, 32]), op=ALU.is_le), r=[cum, mc], w=[cmp_])
        kb.op("dve", lambda e: e.reduce_sum(out=eid[:, 2:2 + NSLOT], in_=cmp_[:, :, :], axis=AX.X), r=[cmp_], w=[eid])
        ldf = kb.sb("ldf", [128, NSLOT], F32)
        vld = kb.sb("vld", [128, NSLOT], F32)
        wix = kb.sb("wix", [128, NSLOT], F32)
        kb.op("dve", lambda e: e.tensor_tensor(out=ldf[:, :], in0=eid[:, 2:2 + NSLOT], in1=eid[:, 0:NSLOT], op=ALU.not_equal), r=[eid], w=[ldf])
        kb.op("dve", lambda e: e.tensor_scalar(out=vld[:, :], in0=eid[:, 2:2 + NSLOT], scalar1=31.5, scalar2=None, op0=ALU.is_lt), r=[eid], w=[vld])
        kb.op("dve", lambda e: e.tensor_tensor(out=ldf[:, :], in0=ldf[:, :], in1=vld[:, :], op=ALU.mult), r=[ldf, vld], w=[ldf])
        kb.op("dve", lambda e: e.tensor_scalar(out=wix[:, :], in0=eid[:, 2:2 + NSLOT], scalar1=31.0, scalar2=128.0,
                                               op0=ALU.min, op1=ALU.mult), r=[eid], w=[wix])
        kb.op("dve", lambda e: e.tensor_scalar(out=wix[:, :], in0=wix[:, :], scalar1=mc[:, 32:33], scalar2=float(l * 4096), op0=ALU.add,
                                               op1=ALU.add), r=[wix, mc], w=[wix])
        if getattr(C, "moe_skip", False):
            kb.op("dve", lambda e: e.tensor_scalar(out=wix[:, :], in0=wix[:, :], scalar1=-BIGI, scalar2=None, op0=ALU.add), r=[wix], w=[wix])
            kb.op("dve", lambda e: e.tensor_tensor(out=wix[:, :], in0=wix[:, :], in1=ldf[:, :], op=ALU.mult), r=[wix, ldf], w=[wix])
            kb.op("dve", lambda e: e.tensor_scalar(out=wix[:, :], in0=wix[:, :], scalar1=BIGI, scalar2=None, op0=ALU.add), r=[wix], w=[wix])
        kb.op("dve", lambda e: e.tensor_copy(out=C.widx[:, :], in_=wix[:, :]), r=[wix], w=[C.widx])
        if hasattr(C, "dbgD"):
            kb.dma("sp", C.dbgD["widx"], C.widx[:, :], r=[C.widx], w=[kb.tag("dbg6")])
            kb.dma("sp", C.dbgD["eid"], eid[:, :], r=[eid], w=[kb.tag("dbg3")])


def phaseE(C, l, nslot=None, lvl=9):
    nslot = NSLOT if nslot is None else nslot
    kb = C.kb
    ps = C.ps
    with kb.phase():
        Wgu = [kb.sb("Wgu", [128, 8 * 1024], BF16) for _ in range(2)]
        Wd = [kb.sb("Wd", [128, 4 * 1024], BF16) for _ in range(2)]
        for a in range(2):
            kb.op("pool", lambda e: e.memset(Wgu[a][:, :], 0.0), w=[Wgu[a]])
            kb.op("pool", lambda e: e.memset(Wd[a][:, :], 0.0), w=[Wd[a]])
        entb = [kb.sb("ente", [128, 4], I32) for _ in range(2)]
        xsb = [kb.sb("xs", [128, D], BF16) for _ in range(2)]
        xsT = kb.sb("xsT", [128, 8, 128], BF16)
        actf = kb.sb("actf", [128, 512], F32)
        actb = kb.sb("actb", [128, 512], BF16)
        actT = kb.sb("actT", [128, 4, 128], BF16)
        ywb = [kb.sb("yw", [128, D], F32) for _ in range(2)]
        if not hasattr(C, "bc_reg"):
            C.bc_reg = C.nc.gpsimd.to_reg(DEPTH * 4096 - 1)
        wgu_v = C.wgu[l].rearrange("r (a c) -> r a c", c=2048)
        wd_v = C.wd[l].rearrange("r (a c) -> r a c", c=2048)

        def loads(s):
            A = s % 2
            ent = entb[A]
            msk = getattr(C, "ldmask", 15)
            kb.dma("sp", ent[:, :], C.LIST[s * 128:(s + 1) * 128, :], w=[ent])
            extra = dict(bounds_check=C.bc_reg, oob_is_err=False) if (msk & 8) else {}
            if msk & 1:
                kb.dma("pool", Wgu[A][:, :], C.wgu.rearrange("l r c -> (l r) c"), r=[C.widx], w=[Wgu[A]],
                       indirect=dict(out_offset=None, in_offset=bass.IndirectOffsetOnAxis(ap=C.widx[:, s:s + 1], axis=0), **extra))
            if msk & 2:
                kb.dma("pool", Wd[A][:, :], C.wd.rearrange("l r c -> (l r) c"), r=[C.widx], w=[Wd[A]],
                       indirect=dict(out_offset=None, in_offset=bass.IndirectOffsetOnAxis(ap=C.widx[:, s:s + 1], axis=0), **extra))
            if msk & 4:
                kb.dma("pool", xsb[A][:, :], C.F, r=[ent], w=[xsb[A]],
                       indirect=dict(out_offset=None, in_offset=bass.IndirectOffsetOnAxis(ap=ent[:, 0:1], axis=0)))

        loads(0)
        for s in range(nslot):
            A = s % 2
            ent, xs, yw = entb[A], xsb[A], ywb[A]
            if s + 1 < nslot:
                loads(s + 1)
            if lvl < 1:
                continue
            pT = ps[0]
            pv = pT[:, :].bitcast(BF16)
            for k in range(8):
                kb.op("pe", lambda e: e.transpose(pv[:, k * 128:(k + 1) * 128], xs[:, k * 128:(k + 1) * 128], C.identb_s[:, :]),
                      r=[xs, C.identb_s], w=[pT])
            kb.op("dve", lambda e: e.tensor_copy(out=xsT[:, :, :].rearrange("p k t -> p (k t)"), in_=pv[:, :]), r=[pT], w=[xsT])
            pG, pU = ps[1], ps[2]
            for nb, pp in enumerate((pG, pU)):
                for k in range(8):
                    kb.op("pe", lambda e: e.matmul(pp[:, :], lhsT=xsT[:, k, :], rhs=Wgu[A][:, k * 1024 + nb * 512:k * 1024 + (nb + 1) * 512],
                                                   start=(k == 0), stop=(k == 7)), r=[xsT, Wgu[A]], w=[pp])
            kb.op("act", lambda e: e.activation(out=actf[:, :], in_=pG[:, :], func=AF.Silu), r=[pG], w=[actf])
            kb.op("dve", lambda e: e.tensor_tensor(out=actb[:, :], in0=pU[:, :], in1=actf[:, :], op=ALU.mult), r=[pU, actf], w=[actb])
            pA = ps[3]
            pav = pA[:, :].bitcast(BF16)
            for c in range(4):
                kb.op("pe", lambda e: e.transpose(pav[:, c * 128:(c + 1) * 128], actb[:, c * 128:(c + 1) * 128], C.identb_s[:, :]),
                      r=[actb, C.identb_s], w=[pA])
            kb.op("act", lambda e: e.copy(out=actT[:, :, :].rearrange("p k t -> p (k t)"), in_=pav[:, 0:512]), r=[pA], w=[actT])
            pY = [ps[4 + 2 * (s % 2)], ps[5 + 2 * (s % 2)]]
            for nb in range(2):
                for c in range(4):
                    kb.op("pe", lambda e: e.matmul(pY[nb][:, :], lhsT=actT[:, c, :], rhs=Wd[A][:, c * 1024 + nb * 512:c * 1024 + (nb + 1) * 512],
                                                   start=(c == 0), stop=(c == 3)), r=[actT, Wd[A]], w=[pY[nb]])
            if lvl < 2:
                continue
            wcol = ent[:, 2:3].bitcast(F32)
            kb.op("dve", lambda e: e.tensor_scalar(out=yw[:, 0:512], in0=pY[0][:, :], scalar1=wcol, scalar2=None, op0=ALU.mult),
                  r=[pY[0], ent], w=[yw])
            kb.op("act", lambda e: e.activation(out=yw[:, 512:1024], in_=pY[1][:, :], func=AF.Copy, scale=wcol), r=[pY[1], ent], w=[yw])
            if lvl < 3:
                continue
            kb.dma("pool", C.YB, yw[:, :], r=[yw, ent], w=[kb.tag("YBs", s)],
                   indirect=dict(out_offset=bass.IndirectOffsetOnAxis(ap=ent[:, 1:2], axis=0), in_offset=None))


def combine_src(C, l_prev, store=True):
    kb = C.kb
    Gt = kb.sb("Gc", [128, 2048], F32)
    kb.dma("sp", Gt[:, 0:1024], C.G[l_prev][:, 1024:2048], r=[kb.tag("G", l_prev)], w=[Gt])
    kb.dma("sp", Gt[:, 1024:2048], C.G[l_prev][:, 3072:4096], r=[kb.tag("G", l_prev)], w=[Gt])
    y1b = [kb.sb("y1", [128, D], F32) for _ in range(2)]
    y2b = [kb.sb("y2", [128, D], F32) for _ in range(2)]
    cnt = [0]

    def src(t, xt):
        n_ = cnt[0]
        cnt[0] += 1
        y1, y2 = y1b[n_ % 2], y2b[n_ % 2]
        kb.dma("sp", xt[:, :], C.X[t * 128:(t + 1) * 128, :], r=[kb.tag("X", t)], w=[xt])
        kb.dma("sp", y1[:, :], C.YB[t * 128:(t + 1) * 128, :], w=[y1])
        kb.dma("sp", y2[:, :], C.YB[TPAD + t * 128:TPAD + (t + 1) * 128, :], w=[y2])
        g0 = 0 if t >= 2 else 1024
        kb.op("pool", lambda e: e.tensor_tensor(out=y1[:, :], in0=y1[:, :], in1=y2[:, :], op=ALU.add), r=[y1, y2], w=[y1])
        kb.op("dve", lambda e: e.tensor_tensor(out=y1[:, :], in0=y1[:, :], in1=Gt[:, g0:g0 + 1024], op=ALU.mult), r=[y1, Gt], w=[y1])
        kb.op("pool", lambda e: e.tensor_tensor(out=xt[:, :], in0=xt[:, :], in1=y1[:, :], op=ALU.add), r=[xt, y1], w=[xt])
        if store:
            kb.dma("sp", C.X[t * 128:(t + 1) * 128, :], xt[:, :], r=[xt], w=[kb.tag("X", t)])
    return src


TZ = T + 4


def zbase(t):
    return 1 + t * 128 if t < 2 else 259 + (t - 2) * 128


def dn_consts():
    p = np.arange(128)
    same = (p[:, None] // 64) == (p[None, :] // 64)
    mge = (same & (p[None, :] >= p[:, None])).astype(np.float32)
    mle = (same & (p[None, :] <= p[:, None])).astype(np.float32)
    slt = (same & (p[None, :] < p[:, None])).astype(np.float32)
    sgt = (same & (p[None, :] > p[:, None])).astype(np.float32)
    return np.ascontiguousarray(np.concatenate([mge, mle, slt, sgt], axis=1), np.float32)


def phaseA_odd(C, l, tiles, x_src):
    kb = C.kb
    i = l // 2
    ps = C.ps
    with kb.phase():
        W = kb.sb("Wino", [128, 8, ODD_IN], BF16)
        wv = C.od_w_in[i].rearrange("(k p) c -> p k c", p=128)
        for k in range(8):
            kb.dma("pool", W[:, k, :], wv[:, k, :], w=[W])
        gs1, sh_blk = layer_mod_tables(C, l, 0)
        prm = kb.sb("dnprm", [128, 32], F32)
        kb.dma("sp", prm[:, :], C.od_prm[i].partition_broadcast(128), w=[prm])
        kb.op("act", lambda e: e.activation(out=prm[:, 0:16], in_=prm[:, 0:16], func=AF.Exp), r=[prm], w=[prm])
        kb.op("dve", lambda e: e.tensor_scalar(out=prm[:, 0:16], in0=prm[:, 0:16], scalar1=-1.0, scalar2=None, op0=ALU.mult), r=[prm], w=[prm])
        zrow = kb.sb("zrow", [4, 3072], BF16)
        kb.op("dve", lambda e: e.memset(zrow[:, :], 0.0), w=[zrow])
        for n_, r0 in enumerate((0, 257, 258, TZ - 1)):
            kb.dma("sp", C.ZP[r0:r0 + 1, :], zrow[0:1, :], r=[zrow], w=[kb.tag("ZPz", n_)])
        xb = [kb.sb("xt", [128, D], F32) for _ in range(2)]
        junk = kb.sb("junk", [128, D], BF16)
        xn = kb.sb("xn", [128, D], BF16)
        hT = [kb.sb("hT", [128, 8, 128], BF16) for _ in range(2)]
        zt = [kb.sb("zt", [128, 4096], BF16) for _ in range(2)]
        gb = [kb.sb("gb", [128, 32], F32) for _ in range(2)]
        ss = kb.sb("ss", [128, 1], F32)
        rstd = kb.sb("rstd", [128, 1], F32)
        tmp1 = kb.sb("tmp1", [128, 8], F32)
        t16 = kb.sb("t16", [128, 16], F32)
        for n_, t in enumerate(tiles):
            r_ = 0 if t >= 2 else 1
            xt = xb[n_ % 2]
            h = hT[n_ % 2]
            z = zt[n_ % 2]
            g = gb[n_ % 2]
            x_src(t, xt)
            norm_tile(C, xt, ss, rstd, tmp1, junk, xn)
            psT = ps[0]
            pv = psT[:, :].bitcast(BF16)
            for k in range(8):
                kb.op("pe", lambda e: e.transpose(pv[:, k * 128:(k + 1) * 128], xn[:, k * 128:(k + 1) * 128], C.identb_s[:, :]),
                      r=[xn, C.identb_s], w=[psT])
            for k in range(8):
                kb.op("act", lambda e: e.activation(out=h[:, k, :], in_=pv[:, k * 128:(k + 1) * 128], func=AF.Identity,
                                                    scale=gs1[:, k, r_:r_ + 1], bias=C.modT[:, l, sh_blk + k, r_:r_ + 1]),
                      r=[psT, gs1, C.modT], w=[h])
            for j in range(9):
                c0 = j * 512
                nc_ = 512 if j < 8 else 32
                pj = ps[1 + (j % 6)]
                for k in range(8):
                    kb.op("pe", lambda e: e.matmul(pj[:, 0:nc_], lhsT=h[:, k, :], rhs=W[:, k, c0:c0 + nc_],
                                                   start=(k == 0), stop=(k == 7)), r=[h, W], w=[pj])
                if j < 6:
                    if j % 2 == 0:
                        kb.op("act", lambda e: e.copy(out=z[:, c0:c0 + 512], in_=pj[:, :]), r=[pj], w=[z])
                    else:
                        kb.op("dve", lambda e: e.tensor_copy(out=z[:, c0:c0 + 512], in_=pj[:, :]), r=[pj], w=[z])
                elif j < 8:
                    kb.op("act", lambda e: e.activation(out=z[:, c0:c0 + 512], in_=pj[:, :], func=AF.Silu), r=[pj], w=[z])
                else:
                    kb.op("dve", lambda e: e.tensor_tensor(out=t16[:, :], in0=pj[:, 0:16], in1=prm[:, 16:32], op=ALU.add), r=[pj, prm], w=[t16])
                    kb.op("act", lambda e: e.activation(out=t16[:, :], in_=t16[:, :], func=AF.Exp), r=[t16], w=[t16])
                    kb.op("act", lambda e: e.activation(out=t16[:, :], in_=t16[:, :], func=AF.Ln, bias=1.0, scale=1.0), r=[t16], w=[t16])
                    kb.op("dve", lambda e: e.tensor_tensor(out=g[:, 0:16], in0=t16[:, :], in1=prm[:, 0:16], op=ALU.mult), r=[t16, prm], w=[g])
                    kb.op("act", lambda e: e.activation(out=g[:, 16:32], in_=pj[:, 16:32], func=AF.Exp, scale=-1.0), r=[pj], w=[g])
                    kb.op("dve", lambda e: e.tensor_scalar(out=g[:, 16:32], in0=g[:, 16:32], scalar1=1.0, scalar2=None, op0=ALU.add), r=[g], w=[g])
                    kb.op("dve", lambda e: e.reciprocal(out=g[:, 16:32], in_=g[:, 16:32]), r=[g], w=[g])
            zb = zbase(t)
            kb.dma("sp", C.ZP[zb:zb + 128, :], z[:, 0:3072], r=[z], w=[kb.tag("ZP", t)])
            kb.dma("sp", C.PA[t * 128:(t + 1) * 128, 3072:4096], z[:, 3072:4096], r=[z], w=[kb.tag("PAz", t)])
            kb.dma("sp", C.GB[t * 128:(t + 1) * 128, :], g[:, :], r=[g], w=[kb.tag("GB", t)])
    with kb.phase():
        cw = kb.sb("convw", [128, 3, 3072], F32)
        for j in range(3):
            kb.dma("sp", cw[:, j, :], C.od_conv[i, j].partition_broadcast(128), w=[cw])
        zmb = [kb.sb("zm", [128, 3, 3072], BF16) for _ in range(2)]
        acc = kb.sb("acc", [128, 3072], F32)
        acc2 = kb.sb("acc2", [128, 3072], F32)
        sq = kb.sb("sqd", [128, 2048], F32)
        ssh = kb.sb("sshd", [128, 16], F32)
        rs = kb.sb("rsd", [128, 16], F32)
        t16b = kb.sb("t16b", [128, 16], F32)
        outb = [kb.sb("qkvo", [128, 3072], BF16) for _ in range(2)]
        for n_, t in enumerate(tiles):
            zm = zmb[n_ % 2]
            ob = outb[n_ % 2]
            zb = zbase(t)
            deps = [kb.tag("ZP", tt) for tt in (t - 1, t, t + 1) if 0 <= tt < NT and tt in tiles] + [kb.tag("ZPz", q) for q in range(4)]
            for j in range(3):
                kb.dma("sp", zm[:, j, :], C.ZP[zb - 1 + j:zb + 127 + j, :], r=deps, w=[zm])
            kb.op("dve", lambda e: e.tensor_tensor(out=acc[:, :], in0=zm[:, 0, :], in1=cw[:, 0, :], op=ALU.mult), r=[zm, cw], w=[acc])
            kb.op("pool", lambda e: e.tensor_tensor(out=acc2[:, :], in0=zm[:, 1, :], in1=cw[:, 1, :], op=ALU.mult), r=[zm, cw], w=[acc2])
            kb.op("dve", lambda e: e.tensor_tensor(out=acc[:, :], in0=acc[:, :], in1=acc2[:, :], op=ALU.add), r=[acc, acc2], w=[acc])
            kb.op("pool", lambda e: e.tensor_tensor(out=acc2[:, :], in0=zm[:, 2, :], in1=cw[:, 2, :], op=ALU.mult), r=[zm, cw], w=[acc2])
            kb.op("dve", lambda e: e.tensor_tensor(out=acc[:, :], in0=acc[:, :], in1=acc2[:, :], op=ALU.add), r=[acc, acc2], w=[acc])
            kb.op("act", lambda e: e.activation(out=acc[:, :], in_=acc[:, :], func=AF.Silu), r=[acc], w=[acc])
            kb.op("act", lambda e: e.activation(out=sq[:, :], in_=acc[:, 0:2048], func=AF.Square), r=[acc], w=[sq])
            kb.op("dve", lambda e: e.reduce_sum(out=ssh[:, :], in_=sq[:, :].rearrange("p (h d) -> p h d", h=16), axis=AX.X), r=[sq], w=[ssh])
            rsqrt_mean(C, rs, ssh, 16, 1.0, t16b)
            kb.op("dve", lambda e: e.tensor_scalar(out=rs[:, 0:8], in0=rs[:, 0:8], scalar1=float(128 ** -0.5), scalar2=None, op0=ALU.mult),
                  r=[rs], w=[rs])
            kb.op("dve", lambda e: e.tensor_tensor(out=ob[:, 0:2048].rearrange("p (h d) -> p h d", h=16),
                                                   in0=acc[:, 0:2048].rearrange("p (h d) -> p h d", h=16),
                                                   in1=rs[:, :].unsqueeze(2).to_broadcast([128, 16, 128]), op=ALU.mult), r=[acc, rs], w=[ob])
            kb.op("pool", lambda e: e.tensor_copy(out=ob[:, 2048:3072], in_=acc[:, 2048:3072]), r=[acc], w=[ob])
            kb.dma("sp", C.PA[t * 128:(t + 1) * 128, 0:3072], ob[:, :], r=[ob], w=[kb.tag("PA", t)])


def phaseB_odd(C, l, n_ctx_tiles=2, lat_tiles=None, dirs=(0, 1)):
    kb = C.kb
    ps = C.ps
    if lat_tiles is None:
        lat_tiles = list(range(2, NT))
    with kb.phase():
        dc = kb.sb("dc", [128, 512], F32)
        kb.dma("sp", dc[:, :], C.dconst, w=[dc])
        MGE, MLE, SLT, SGT = [dc[:, q * 128:(q + 1) * 128] for q in range(4)]
        idf = kb.sb("idfb", [128, 128], F32)
        kb.op("dve", lambda e: e.tensor_copy(out=idf[:, :], in_=C.identb_s[:, :]), r=[C.identb_s], w=[idf])
        qkvb = [kb.sb("qkvd", [128, 3072], BF16) for _ in range(2)]
        gbb = [kb.sb("gbd", [128, 32], F32) for _ in range(2)]
        gcs = kb.sb("gcs", [128, 8], F32)
        egc = kb.sb("egc", [128, 8], F32)
        kbt = kb.sb("kbt", [128, 8, 128], BF16)
        vbt = kb.sb("vbt", [128, 8, 128], BF16)
        kbg = kb.sb("kbg", [128, 8, 128], BF16)
        qgt = kb.sb("qgt", [128, 8, 128], BF16)
        kdt = kb.sb("kdt", [128, 8, 128], BF16)
        kds = kb.sb("kds", [128, 8], F32)
        qT = kb.sb("qTd", [128, 8, 128], BF16)
        kT = kb.sb("kTd", [128, 8, 128], BF16)
        qgT = kb.sb("qgT", [128, 8, 128], BF16)
        wT = kb.sb("wTd", [128, 8, 128], BF16)
        attT = kb.sb("attTd", [128, 8, 128], BF16)
        uu = kb.sb("uu", [128, 8, 128], F32)
        glc = kb.sb("glc", [128, 8, 2], F32)
        grep_ = [kb.sb("grep", [128, 128], F32) for _ in range(8)]
        d1 = [kb.sb("d1", [128, 128], F32) for _ in range(8)]
        d2 = [kb.sb("d2", [128, 128], F32) for _ in range(8)]
        e1m = [kb.sb("e1m", [128, 128], F32) for _ in range(8)]
        e2m = [kb.sb("e2m", [128, 128], F32) for _ in range(8)]
        Lb = [[kb.sb("Lb", [128, 128], BF16) for _ in range(2)] for _ in range(8)]
        Ub = [[kb.sb("Ub", [128, 128], BF16) for _ in range(2)] for _ in range(8)]
        ILb = [kb.sb("ILb", [128, 128], BF16) for _ in range(8)]
        Mb = [[kb.sb("Mb", [128, 128], BF16) for _ in range(2)] for _ in range(8)]
        vn = kb.sb("vn", [128, 8, 128], BF16)
        S = kb.sb("Sd", [128, 8, 128], F32)
        Sb = kb.sb("Sbd", [128, 8, 128], BF16)
        osb = [kb.sb("osb", [128, D], F32) for _ in range(2)]
        n_ = 0
        for d_ in dirs:
            order = list(range(n_ctx_tiles)) + list(lat_tiles)
            if d_ == 1:
                order = list(range(n_ctx_tiles))[::-1] + list(lat_tiles)[::-1]
            CUM = MGE if d_ == 0 else MLE
            M_att = MGE if d_ == 0 else MLE
            M_lo = SLT if d_ == 0 else SGT
            chunks = (0, 1) if d_ == 0 else (1, 0)
            flast = (lambda c: c * 64 + 63) if d_ == 0 else (lambda c: c * 64)
            ODST = C.OF if d_ == 0 else C.OB
            otag = "OF" if d_ == 0 else "OB"
            kb.op("dve", lambda e: e.memset(S[:, :, :], 0.0), w=[S])
            kb.op("dve", lambda e: e.memset(Sb[:, :, :], 0.0), w=[Sb])
            for t in order:
                qkv = qkvb[n_ % 2]
                gbt = gbb[n_ % 2]
                ot = osb[n_ % 2]
                n_ += 1
                kb.dma("sp", qkv[:, :], C.PA[t * 128:(t + 1) * 128, 0:3072], r=[kb.tag("PA", t)], w=[qkv])
                kb.dma("sp", gbt[:, :], C.GB[t * 128:(t + 1) * 128, :], r=[kb.tag("GB", t)], w=[gbt])
                gcol = gbt[:, d_ * 8:(d_ + 1) * 8]
                bcol = gbt[:, 16 + d_ * 8:16 + (d_ + 1) * 8]
                pg = ps[6]
                kb.op("pe", lambda e: e.matmul(pg[:, 0:8], lhsT=CUM, rhs=gcol, start=True, stop=True), r=[dc, gbt], w=[pg])
                kb.op("dve", lambda e: e.tensor_copy(out=gcs[:, :], in_=pg[:, 0:8]), r=[pg], w=[gcs])
                kb.op("act", lambda e: e.activation(out=egc[:, :], in_=gcs[:, :], func=AF.Exp), r=[gcs], w=[egc])
                qv = qkv[:, 0:1024].rearrange("p (h d) -> p h d", h=8)
                kv = qkv[:, 1024:2048].rearrange("p (h d) -> p h d", h=8)
                vv = qkv[:, 2048:3072].rearrange("p (h d) -> p h d", h=8)
                bb = bcol.unsqueeze(2).to_broadcast([128, 8, 128])
                eb = egc[:, :].unsqueeze(2).to_broadcast([128, 8, 128])
                kb.op("dve", lambda e: e.tensor_tensor(out=kbt[:, :, :], in0=kv, in1=bb, op=ALU.mult), r=[qkv, gbt], w=[kbt])
                kb.op("pool", lambda e: e.tensor_tensor(out=vbt[:, :, :], in0=vv, in1=bb, op=ALU.mult), r=[qkv, gbt], w=[vbt])
                kb.op("dve", lambda e: e.tensor_tensor(out=kbg[:, :, :], in0=kbt[:, :, :], in1=eb, op=ALU.mult), r=[kbt, egc], w=[kbg])
                kb.op("pool", lambda e: e.tensor_tensor(out=qgt[:, :, :], in0=qv, in1=eb, op=ALU.mult), r=[qkv, egc], w=[qgt])
                for (src_ap, src_tl, dst) in ((qkv[:, 0:1024], qkv, qT), (qkv[:, 1024:2048], qkv, kT), (qgt[:, :, :].rearrange("p h d -> p (h d)"), qgt, qgT)):
                    pt = ps[7]
                    ptv = pt[:, :].bitcast(BF16)
                    for h in range(8):
                        kb.op("pe", lambda e: e.transpose(ptv[:, h * 128:(h + 1) * 128], src_ap[:, h * 128:(h + 1) * 128], C.identb_s[:, :]),
                              r=[src_tl, C.identb_s], w=[pt])
                    kb.op("act", lambda e: e.copy(out=dst[:, :, :].rearrange("p h t -> p (h t)"), in_=ptv[:, :]), r=[pt], w=[dst])
                def reg(h, j):
                    c = h * 3 + j
                    return ps[c // 4], slice((c % 4) * 128, (c % 4 + 1) * 128)
                for h in range(8):
                    kb.op("dve", lambda e: e.tensor_copy(out=grep_[h][:, :], in_=gcol[:, h:h + 1].to_broadcast([128, 128])), r=[gbt], w=[grep_[h]])
                for h in range(8):
                    (b0, c0), (b1, c1), (b2, c2) = reg(h, 0), reg(h, 1), reg(h, 2)
                    kb.op("pe", lambda e: e.matmul(b0[:, c0], lhsT=grep_[h][:, :], rhs=CUM, start=True, stop=True), r=[grep_[h], dc], w=[b0])
                    kb.op("pe", lambda e: e.matmul(b1[:, c1], lhsT=kT[:, h, :], rhs=kT[:, h, :], start=True, stop=True), r=[kT], w=[b1])
                    kb.op("pe", lambda e: e.matmul(b2[:, c2], lhsT=kT[:, h, :], rhs=qT[:, h, :], start=True, stop=True), r=[kT, qT], w=[b2])
                for h in range(8):
                    b0, c0 = reg(h, 0)
                    gch = gcs[:, h:h + 1]
                    kb.op("dve", lambda e: e.tensor_scalar(out=d1[h][:, :], in0=b0[:, c0], scalar1=gch, scalar2=0.0, op0=ALU.subtract, op1=ALU.min),
                          r=[b0, gcs], w=[d1[h]])
                    kb.op("dve", lambda e: e.tensor_scalar(out=d2[h][:, :], in0=b0[:, c0], scalar1=gch, scalar2=-1.0, op0=ALU.subtract, op1=ALU.mult),
                          r=[b0, gcs], w=[d2[h]])
                    kb.op("dve", lambda e: e.tensor_scalar(out=d2[h][:, :], in0=d2[h][:, :], scalar1=0.0, scalar2=None, op0=ALU.min), r=[d2[h]], w=[d2[h]])
                for h in range(8):
                    b0, c0 = reg(h, 0)
                    for c in (0, 1):
                        f = flast(c)
                        kb.op("act", lambda e: e.activation(out=glc[:, h, c:c + 1], in_=b0[:, c0.start + f:c0.start + f + 1], func=AF.Exp), r=[b0], w=[glc])
                    kb.op("act", lambda e: e.activation(out=d1[h][:, :], in_=d1[h][:, :], func=AF.Exp), r=[d1[h]], w=[d1[h]])
                    kb.op("act", lambda e: e.activation(out=d2[h][:, :], in_=d2[h][:, :], func=AF.Exp), r=[d2[h]], w=[d2[h]])
                f0, f1 = flast(0), flast(1)
                for h in range(8):
                    kb.op("dve", lambda e: e.tensor_tensor(out=e1m[h][:, :], in0=d1[h][:, :], in1=M_att, op=ALU.mult), r=[d1[h], dc], w=[e1m[h]])
                    kb.op("pool", lambda e: e.tensor_tensor(out=e2m[h][:, :], in0=d2[h][:, :], in1=M_lo, op=ALU.mult), r=[d2[h], dc], w=[e2m[h]])
                for h in range(8):
                    (b1, c1), (b2, c2) = reg(h, 1), reg(h, 2)
                    kb.op("dve", lambda e: e.tensor_tensor(out=kds[:, h:h + 1], in0=e1m[h][:, f0:f0 + 1], in1=e1m[h][:, f1:f1 + 1], op=ALU.add),
                          r=[e1m[h]], w=[kds])
                    kb.op("dve", lambda e: e.tensor_tensor(out=attT[:, h, :], in0=b2[:, c2], in1=e1m[h][:, :], op=ALU.mult), r=[b2, e1m[h]], w=[attT])
                    kb.op("dve", lambda e: e.scalar_tensor_tensor(out=Lb[h][0][:, :], in0=b1[:, c1], scalar=bcol[:, h:h + 1], in1=e2m[h][:, :],
                                                                  op0=ALU.mult, op1=ALU.mult), r=[b1, gbt, e2m[h]], w=[Lb[h][0]])
                for h in range(8):
                    b0, c0 = reg(h, 0)
                    utv = b0[:, c0].bitcast(BF16)
                    kb.op("pe", lambda e: e.transpose(utv[:, 0:128], Lb[h][0][:, :], C.identb_s[:, :]), r=[Lb[h][0], C.identb_s], w=[b0])
                for h in range(8):
                    b0, c0 = reg(h, 0)
                    utv = b0[:, c0].bitcast(BF16)
                    kb.op("act", lambda e: e.copy(out=Ub[h][0][:, :], in_=utv[:, 0:128]), r=[b0], w=[Ub[h][0]])
                    kb.op("dve", lambda e: e.tensor_tensor(out=Mb[h][0][:, :], in0=idf[:, :], in1=Ub[h][0][:, :], op=ALU.subtract),
                          r=[idf, Ub[h][0]], w=[Mb[h][0]])
                cur = 0
                for lev in range(5):
                    nxt = 1 - cur
                    for h in range(8):
                        (b0, c0), (b1, c1) = reg(h, 0), reg(h, 1)
                        kb.op("pe", lambda e: e.matmul(b0[:, c0], lhsT=Ub[h][cur][:, :], rhs=Lb[h][cur][:, :], start=True, stop=True),
                              r=[Ub[h][cur], Lb[h][cur]], w=[b0])
                        if lev < 4:
                            kb.op("pe", lambda e: e.matmul(b1[:, c1], lhsT=Lb[h][cur][:, :], rhs=Ub[h][cur][:, :], start=True, stop=True),
                                  r=[Ub[h][cur], Lb[h][cur]], w=[b1])
                    for h in range(8):
                        (b0, c0), (b1, c1) = reg(h, 0), reg(h, 1)
                        kb.op("dve", lambda e: e.tensor_tensor(out=ILb[h][:, :], in0=b0[:, c0], in1=idf[:, :], op=ALU.add), r=[b0, idf], w=[ILb[h]])
                        if lev < 4:
                            kb.op("act", lambda e: e.copy(out=Lb[h][nxt][:, :], in_=b0[:, c0]), r=[b0], w=[Lb[h][nxt]])
                            kb.op("act", lambda e: e.copy(out=Ub[h][nxt][:, :], in_=b1[:, c1]), r=[b1], w=[Ub[h][nxt]])
                    for h in range(8):
                        b2, c2 = reg(h, 2)
                        kb.op("pe", lambda e: e.matmul(b2[:, c2], lhsT=ILb[h][:, :], rhs=Mb[h][cur][:, :], start=True, stop=True),
                              r=[ILb[h], Mb[h][cur]], w=[b2])
                    for h in range(8):
                        b2, c2 = reg(h, 2)
                        kb.op("dve", lambda e: e.tensor_copy(out=Mb[h][nxt][:, :], in_=b2[:, c2]), r=[b2], w=[Mb[h][nxt]])
                    cur = nxt
                for h in range(8):
                    (b0, c0), (b1, c1) = reg(h, 0), reg(h, 1)
                    kb.op("pe", lambda e: e.matmul(b0[:, c0], lhsT=Mb[h][cur][:, :], rhs=vbt[:, h, :], start=True, stop=True), r=[Mb[h][cur], vbt], w=[b0])
                    kb.op("pe", lambda e: e.matmul(b1[:, c1], lhsT=kbg[:, h, :], rhs=Mb[h][cur][:, :], start=True, stop=True), r=[Mb[h][cur], kbg], w=[b1])
                for h in range(8):
                    (b0, c0), (b1, c1) = reg(h, 0), reg(h, 1)
                    kb.op("dve", lambda e: e.tensor_copy(out=uu[:, h, :], in_=b0[:, c0]), r=[b0], w=[uu])
                    kb.op("act", lambda e: e.copy(out=wT[:, h, :], in_=b1[:, c1]), r=[b1], w=[wT])
                kb.op("dve", lambda e: e.tensor_tensor(out=kdt[:, :, :], in0=kv, in1=kds[:, :].unsqueeze(2).to_broadcast([128, 8, 128]), op=ALU.mult),
                      r=[qkv, kds], w=[kdt])
                for c in chunks:
                    r0 = c * 64
                    pW = [ps[6], ps[7]]
                    for h in range(8):
                        kb.op("pe", lambda e: e.matmul(pW[h // 4][:, (h % 4) * 128:(h % 4 + 1) * 128], lhsT=wT[:, h, :], rhs=Sb[:, h, :],
                                                       start=True, stop=True), r=[wT, Sb], w=[pW[h // 4]])
                    for hb in range(2):
                        kb.op("dve", lambda e: e.tensor_tensor(out=vn[r0:r0 + 64, hb * 4:(hb + 1) * 4, :].rearrange("p h d -> p (h d)"),
                                                               in0=uu[r0:r0 + 64, hb * 4:(hb + 1) * 4, :].rearrange("p h d -> p (h d)"),
                                                               in1=pW[hb][r0:r0 + 64, :], op=ALU.subtract), r=[uu, pW[hb]], w=[vn])
                    pO = [ps[4], ps[5]]
                    pK = [ps[2], ps[3]]
                    for h in range(8):
                        oc = (h % 4) * 128
                        kb.op("pe", lambda e: e.matmul(pO[h // 4][:, oc:oc + 128], lhsT=qgT[:, h, :], rhs=Sb[:, h, :], start=True, stop=False),
                              r=[qgT, Sb], w=[pO[h // 4]])
                        kb.op("pe", lambda e: e.matmul(pO[h // 4][:, oc:oc + 128], lhsT=attT[r0:r0 + 64, h, :], rhs=vn[r0:r0 + 64, h, :],
                                                       start=False, stop=True), r=[attT, vn], w=[pO[h // 4]])
                        kb.op("pe", lambda e: e.matmul(pK[h // 4][:, oc:oc + 128], lhsT=kdt[r0:r0 + 64, h, :], rhs=vn[r0:r0 + 64, h, :],
                                                       start=True, stop=True), r=[kdt, vn], w=[pK[h // 4]])
                    for hb in range(2):
                        kb.op("act", lambda e: e.copy(out=ot[r0:r0 + 64, hb * 512:(hb + 1) * 512], in_=pO[hb][r0:r0 + 64, :]), r=[pO[hb]], w=[ot])
                    for h in range(8):
                        oc = (h % 4) * 128
                        kb.op("dve", lambda e: e.scalar_tensor_tensor(out=S[:, h, :], in0=S[:, h, :], scalar=glc[:, h, c:c + 1],
                                                                      in1=pK[h // 4][:, oc:oc + 128], op0=ALU.mult, op1=ALU.add),
                              r=[S, glc, pK[h // 4]], w=[S])
                    kb.op("act", lambda e: e.copy(out=Sb[:, :, :].rearrange("p h d -> p (h d)"), in_=S[:, :, :].rearrange("p h d -> p (h d)")),
                          r=[S], w=[Sb])
                kb.dma("sp", ODST[t * 128:(t + 1) * 128, :], ot[:, :], r=[ot], w=[kb.tag(otag, t)])


def odd_mix_src(C, l):
    kb = C.kb
    i = l // 2
    gn = kb.sb("ogain", [128, 128], F32)
    kb.dma("sp", gn[:, :], C.od_gain[i].partition_broadcast(128), w=[gn])
    ofb = [kb.sb("ofo", [128, D], F32) for _ in range(2)]
    obb = [kb.sb("obo", [128, D], F32) for _ in range(2)]
    zgb = [kb.sb("zgo", [128, D], BF16) for _ in range(2)]
    junk = kb.sb("junko", [128, D], F32)
    ssh = kb.sb("ssho", [128, 8], F32)
    rs8 = kb.sb("rs8o", [128, 8], F32)
    tmp8 = kb.sb("tmp8o", [128, 8], F32)
    mr = kb.sb("mro", [128, D], BF16)

    def src(t, mT, n_):
        of, ob, zg = ofb[n_ % 2], obb[n_ % 2], zgb[n_ % 2]
        kb.dma("sp", of[:, :], C.OF[t * 128:(t + 1) * 128, :], r=[kb.tag("OF", t)], w=[of])
        kb.dma("sp", ob[:, :], C.OB[t * 128:(t + 1) * 128, :], r=[kb.tag("OB", t)], w=[ob])
        kb.dma("sp", zg[:, :], C.PA[t * 128:(t + 1) * 128, 3072:4096], r=[kb.tag("PAz", t)], w=[zg])
        kb.op("pool", lambda e: e.tensor_tensor(out=of[:, :], in0=of[:, :], in1=ob[:, :], op=ALU.add), r=[of, ob], w=[of])
        kb.op("act", lambda e: e.activation(out=junk[:, :], in_=of[:, :], func=AF.Square), r=[of], w=[junk])
        kb.op("dve", lambda e: e.reduce_sum(out=ssh[:, :], in_=junk[:, :].rearrange("p (h d) -> p h d", h=8), axis=AX.X), r=[junk], w=[ssh])
        rsqrt_mean(C, rs8, ssh, 8, 1.0 / 128, tmp8)
        kb.op("dve", lambda e: e.tensor_tensor(out=of[:, :].rearrange("p (h d) -> p h d", h=8), in0=of[:, :].rearrange("p (h d) -> p h d", h=8),
                                               in1=rs8[:, :].unsqueeze(2).to_broadcast([128, 8, 128]), op=ALU.mult), r=[of, rs8], w=[of])
        kb.op("dve", lambda e: e.tensor_tensor(out=of[:, :].rearrange("p (h d) -> p h d", h=8), in0=of[:, :].rearrange("p (h d) -> p h d", h=8),
                                               in1=gn[:, :].unsqueeze(1).to_broadcast([128, 8, 128]), op=ALU.mult), r=[of, gn], w=[of])
        kb.op("pool", lambda e: e.tensor_tensor(out=mr[:, :], in0=of[:, :], in1=zg[:, :], op=ALU.mult), r=[of, zg], w=[mr])
        pT = C.ps[0]
        pv = pT[:, :].bitcast(BF16)
        for k in range(8):
            kb.op("pe", lambda e: e.transpose(pv[:, k * 128:(k + 1) * 128], mr[:, k * 128:(k + 1) * 128], C.identb_s[:, :]),
                  r=[mr, C.identb_s], w=[pT])
        kb.op("act", lambda e: e.copy(out=mT[:, 0:8, :].rearrange("p k t -> p (k t)"), in_=pv[:, :]), r=[pT], w=[mT])
    return src


def final_phase(C, tiles):
    kb = C.kb
    with kb.phase():
        src = combine_src(C, DEPTH - 1, store=False)
        fn = kb.sb("fnrep", [128, D], F32)
        kb.dma("sp", fn[:, :], C.norms[2 * DEPTH].partition_broadcast(128), w=[fn])
        xb = [kb.sb("xf", [128, D], F32) for _ in range(2)]
        ob = [kb.sb("of_", [128, D], F32) for _ in range(2)]
        junk = kb.sb("junkf", [128, D], BF16)
        ss = kb.sb("ssf", [128, 1], F32)
        rstd = kb.sb("rstdf", [128, 1], F32)
        tmp1 = kb.sb("tmp1f", [128, 8], F32)
        for n_, t in enumerate(tiles):
            xt, o = xb[n_ % 2], ob[n_ % 2]
            src(t, xt)
            norm_tile(C, xt, ss, rstd, tmp1, junk, o)
            kb.op("dve", lambda e: e.tensor_tensor(out=o[:, :], in0=o[:, :], in1=fn[:, :], op=ALU.mult), r=[o, fn], w=[o])
            kb.dma("sp", C.out[(t - 2) * 128:(t - 1) * 128, :], o[:, :], r=[o], w=[kb.tag("out", t)])


def build_program():
    nc = bass.Bass("TRN2", target_bir_lowering=False)
    C = Ctx()
    C.nc = nc
    C.kb = KB(nc)
    C.dbg_out = set()
    C.moe_skip = True
    kb = C.kb
    declare_io(nc, C)
    C.out = nc.dram_tensor("out", [L, D], F32, kind="ExternalOutput").ap()
    setup_globals(C)
    setup_consts(C)
    phase0(C)
    all_tiles = list(range(NT))
    for l in range(DEPTH):
        last = l == DEPTH - 1
        if l == 0:
            def x_src(t, xt):
                kb.dma("sp", xt[:, :], C.xin[t * 128:(t + 1) * 128, :], w=[xt])
                kb.dma("sp", C.X[t * 128:(t + 1) * 128, :], xt[:, :], r=[xt], w=[kb.tag("X", t)])
            holder = None
        else:
            holder = kb.phase()
            holder.__enter__()
            x_src = combine_src(C, l - 1)
        if l % 2 == 0:
            phaseA_even(C, l, all_tiles, x_src)
        else:
            phaseA_odd(C, l, all_tiles, x_src)
        if holder is not None:
            holder.__exit__(None, None, None)
        if l % 2 == 0:
            phaseB_ret(C, l)
            phaseC_att(C, l)
            phaseD(C, l, all_tiles, C.ev_w_out[l // 2], 12, lambda: even_mix_src(C), set(all_tiles))
        else:
            phaseB_odd(C, l)
            tiles_d = all_tiles if not last else all_tiles[2:]
            phaseD(C, l, tiles_d, C.od_w_out[l // 2], 8, lambda: odd_mix_src(C, l), set(tiles_d))
        phaseE(C, l)
    final_phase(C, all_tiles[2:])
    kb.barrier()
    return nc


_CACHE = {}


def kernel(**inputs):
    inp = {k: np.asarray(v) for k, v in inputs.items()}
    maps = host_prep(inp)[:NCORES]
    if "nc" not in _CACHE:
        _CACHE["nc"] = build_program()
    res = run_bass_kernel_spmd(_CACHE["nc"], maps, core_ids=list(range(NCORES)))
    out = np.stack([np.asarray(res.results[b]["out"], np.float32) for b in range(4)], axis=0)
    return out


NCORES = 4
```

```python
import numpy as np
from contextlib import ExitStack
import concourse.bass as bass
import concourse.mybir as mybir
from concourse.bass_utils import run_bass_kernel_spmd

F32 = mybir.dt.float32
BF16 = mybir.dt.bfloat16
I32 = mybir.dt.int32
AF = mybir.ActivationFunctionType
ALU = mybir.AluOpType
AX = mybir.AxisListType

D = 1024
LC = 256
L = 8192
T = LC + L
NT = T // 128
DEPTH = 4
EPS = 1e-6
EVEN_IN = 3840
ODD_IN = 4128


class Buf:
    __slots__ = ("w", "r")

    def __init__(self):
        self.w = None
        self.r = {}


class Tl:
    __slots__ = ("t", "b", "psum")

    def __init__(self, t, psum=False):
        self.t = t
        self.b = Buf()
        self.psum = psum

    def __getitem__(self, k):
        return self.t[k]


class KB:
    NDMA = {"sp": 12, "pool": 12, "act": 4}

    def __init__(self, nc):
        self.nc = nc
        self.es = ExitStack()
        self.eng = {"pe": nc.tensor, "act": nc.scalar, "dve": nc.vector, "pool": nc.gpsimd, "sp": nc.sync}
        self.sem = {}
        self.cnt = {}
        self.seen = {e: {} for e in self.eng}
        for e in self.eng:
            self.sem[e] = self.es.enter_context(nc.semaphore("s_" + e))
            self.cnt[e] = 0
        self.dsem = {}
        self.dval = {}
        self.dnext = {}
        for q, n in self.NDMA.items():
            self.dsem[q] = [self.es.enter_context(nc.semaphore(f"d_{q}{i}")) for i in range(n)]
            self.dval[q] = [0] * n
            self.dnext[q] = 0
        self.tags = {}
        self.ninst = 0
        self.cur = self.es

    def semobj(self, key):
        if isinstance(key, tuple):
            return self.dsem[key[0]][key[1]]
        return self.sem[key]

    def tag(self, *key):
        b = self.tags.get(key)
        if b is None:
            b = Tl(None)
            self.tags[key] = b
        return b

    def _deps(self, eng, r, w):
        deps = {}

        def add(ev, raw):
            if ev is None:
                return
            k, v = ev
            if k == eng and (not raw or eng in ("pe", "sp")):
                return
            if deps.get(k, 0) < v:
                deps[k] = v

        for x in r:
            add(x.b.w, True)
        for x in w:
            add(x.b.w, False)
            for k, v in x.b.r.items():
                add((k, v), False)
        return deps

    def _wait(self, eng, deps):
        seen = self.seen[eng]
        e = self.eng[eng]
        for k, v in deps.items():
            if seen.get(k, 0) >= v:
                continue
            e.wait_ge(self.semobj(k), v)
            seen[k] = v
            self.ninst += 1

    def _mark(self, ev, r, w):
        k, v = ev
        for x in r:
            if x.b.r.get(k, 0) < v:
                x.b.r[k] = v
        for x in w:
            x.b.w = ev
            x.b.r = {}

    def op(self, eng, fn, r=(), w=()):
        if any(x.psum for x in r):
            w = list(w) + [x for x in r if x.psum]
            r = [x for x in r if not x.psum]
        self._wait(eng, self._deps(eng, r, w))
        ins = fn(self.eng[eng])
        self.cnt[eng] += 1
        ins.then_inc(self.sem[eng], 1)
        self._mark((eng, self.cnt[eng]), r, w)
        self.ninst += 1
        return ins

    def dma(self, q, out, in_, r=(), w=(), indirect=None, **kw):
        deps = self._deps("dma", r, w)
        i = self.dnext[q]
        self.dnext[q] = (i + 1) % len(self.dsem[q])
        key = (q, i)
        if self.dval[q][i] > 0:
            deps[key] = max(deps.get(key, 0), self.dval[q][i])
        self._wait(q, deps)
        e = self.eng[q]
        if indirect is not None:
            ins = e.indirect_dma_start(out=out, in_=in_, **indirect, **kw)
        else:
            ins = e.dma_start(out=out, in_=in_, **kw)
        self.dval[q][i] += 16
        ins.then_inc(self.dsem[q][i], 16)
        self._mark((key, self.dval[q][i]), r, w)
        self.ninst += 1
        return ins

    def barrier(self, engines=("pe", "act", "dve", "pool", "sp")):
        deps = {e: self.cnt[e] for e in self.eng if self.cnt[e] > 0}
        for q in self.dsem:
            for i, v in enumerate(self.dval[q]):
                if v > 0:
                    deps[(q, i)] = v
        for e in engines:
            d = {k: v for k, v in deps.items() if k != e}
            self._wait(e, d)

    def sb(self, name, shape, dtype):
        self.nalloc = getattr(self, "nalloc", 0) + 1
        return Tl(self.cur.enter_context(self.nc.sbuf_tensor(f"{name}_{self.nalloc}", shape, dtype)))

    def phase(self):
        kb = self

        class _P:
            def __enter__(self_):
                self_.prev = getattr(kb, "cur", kb.es)
                self_.st = ExitStack()
                kb.cur = self_.st
                return self_

            def __exit__(self_, *a):
                if a[0] is None:
                    kb.barrier()
                self_.st.close()
                kb.cur = self_.prev
                return False

        return _P()


class Ctx:
    pass


def declare_io(nc, C):
    def din(name, shape, dt=F32):
        return nc.dram_tensor(name, shape, dt, kind="ExternalInput").ap()
    C.xin = din("xin", [T, D])
    C.cT = din("cT", [128, 16])
    C.normT = din("normT", [128, 72])
    C.badaT = din("badaT", [128, 192])
    C.b_ada = din("b_ada", [DEPTH, 6 * D])
    C.rope = din("rope", [L, 128])
    C.identb = din("identb", [128, 128], BF16)
    C.identf = din("identf", [128, 128])
    C.w_ada = din("w_ada", [DEPTH, D, 6 * D])
    C.ev_w_in = din("ev_w_in", [2, D, EVEN_IN])
    C.ev_qk_gain = din("ev_qk_gain", [2, 128])
    C.ev_decay = din("ev_decay", [2, 16])
    C.ev_w_out = din("ev_w_out", [2, 1536, D])
    C.rconst = din("rconst", [128, 770])
    C.norms = din("norms", [9, D])
    C.od_w_in = din("od_w_in", [2, D, ODD_IN])
    C.od_prm = din("od_prm", [2, 32])
    C.od_conv = din("od_conv", [2, 3, 3072])
    C.od_gain = din("od_gain", [2, 128])
    C.od_w_out = din("od_w_out", [2, D, D])
    C.dconst = din("dconst", [128, 512])
    C.wr = din("wr", [DEPTH, D, 36])
    C.br = din("br", [DEPTH, 36])
    C.mconst = din("mconst", [128, 33 + NSLOT])
    C.triones = din("triones", [128, 256], BF16)
    C.srcidx = din("srcidx", [128, NT], I32)
    C.listinit = din("listinit", [128, (NSLOT + 1) * 4], I32)
    C.wgu = din("wgu", [DEPTH, 32 * 128, 8 * 1024])
    C.wd = din("wd", [DEPTH, 32 * 128, 4 * 1024])


def setup_globals(C):
    kb, nc = C.kb, C.nc
    C.ps = [Tl(kb.es.enter_context(nc.psum_tensor(f"ps{i}", [128, 512], F32)), psum=True) for i in range(8)]
    C.modT = kb.sb("modT", [128, DEPTH, 48, 2], F32)
    C.normTs = kb.sb("normTs", [128, 72], F32)
    C.identb_s = kb.sb("identb_s", [128, 128], BF16)
    C.identf_s = kb.sb("identf_s", [128, 128], F32)
    kb.dma("sp", C.normTs[:, :], C.normT, w=[C.normTs])
    kb.dma("sp", C.identb_s[:, :], C.identb, w=[C.identb_s])
    kb.dma("sp", C.identf_s[:, :], C.identf, w=[C.identf_s])
    def dscr(name, shape, dt=F32):
        kind = "ExternalOutput" if name in C.dbg_out else "Internal"
        return nc.dram_tensor(name, shape, dt, kind=kind).ap()
    C.G = dscr("G", [DEPTH, 128, 8192])
    C.X = dscr("X", [T, D])
    C.PA = dscr("PA", [T, ODD_IN], BF16)
    C.OF = dscr("OF", [T, D])
    C.MIX = dscr("MIX", [T, D], BF16)
    C.AT = dscr("AT", [512, T], BF16)
    C.F = dscr("F", [T, D], BF16)
    C.ZP = dscr("ZP", [TZ, 3072], BF16)
    C.GB = dscr("GB", [T, 32])
    C.OB = dscr("OB", [T, D])
    C.LIST = dscr("LIST", [(NSLOT + 1) * 128, 4], I32)
    C.YB = dscr("YB", [2 * TPAD, D])
    C.widx = kb.sb("widx", [128, NSLOT], I32)


def phase0(C):
    kb = C.kb
    ps0, ps1 = C.ps[0], C.ps[1]
    with kb.phase():
        cT = kb.sb("cT", [128, 16], F32)
        sc = kb.sb("sc", [128, 16], F32)
        screp = kb.sb("screp", [128, 16, 128], F32)
        badaT = kb.sb("badaT", [128, 192], F32)
        brep = kb.sb("brep", [128, 4096], F32)
        gout = kb.sb("gout", [128, 8192], F32)
        wblk = [kb.sb("wada", [128, 8, 512], F32) for _ in range(2)]
        kb.dma("sp", cT[:, :], C.cT, w=[cT])
        kb.dma("sp", badaT[:, :], C.badaT, w=[badaT])
        kb.op("act", lambda e: e.activation(out=sc[:, :], in_=cT[:, :], func=AF.Silu), r=[cT], w=[sc])
        kb.op("dve", lambda e: e.tensor_copy(out=screp[:, :, :], in_=sc[:, :].unsqueeze(2).to_broadcast([128, 16, 128])),
              r=[sc], w=[screp])
        for l in range(DEPTH):
            for gi, c0 in enumerate((2048, 5120, 3072, 4096)):
                kb.dma("sp", brep[:, gi * 1024:(gi + 1) * 1024], C.b_ada[l, c0:c0 + 1024].partition_broadcast(128), w=[brep])
            wv = C.w_ada[l].rearrange("(k p) c -> p k c", p=128)
            for j in range(12):
                wb = wblk[j % 2]
                kb.dma("sp", wb[:, :, :], wv[:, :, j * 512:(j + 1) * 512], w=[wb])
                for c4 in range(4):
                    for k in range(8):
                        kb.op("pe", lambda e: e.matmul(ps0[:, c4 * 2:(c4 + 1) * 2], lhsT=wb[:, k, c4 * 128:(c4 + 1) * 128],
                                                       rhs=sc[:, 2 * k:2 * k + 2], start=(k == 0), stop=(k == 7)),
                              r=[wb, sc], w=[ps0])
                kb.op("dve", lambda e: e.tensor_tensor(
                    out=C.modT[:, l, j * 4:(j + 1) * 4, :], in0=ps0[:, 0:8].rearrange("p (c r) -> p c r", r=2),
                    in1=badaT[:, l * 48 + j * 4:l * 48 + j * 4 + 4].unsqueeze(2).to_broadcast([128, 4, 2]), op=ALU.add),
                    r=[ps0, badaT], w=[C.modT])
                if j in (4, 5, 10, 11, 6, 7, 8, 9):
                    gi = {4: 0, 5: 0, 10: 1, 11: 1, 6: 2, 7: 2, 8: 3, 9: 3}[j]
                    half = j % 2
                    for r_ in range(2):
                        for k in range(8):
                            kb.op("pe", lambda e: e.matmul(ps1[:, :], lhsT=screp[:, 2 * k + r_, :], rhs=wb[:, k, :],
                                                           start=(k == 0), stop=(k == 7)), r=[screp, wb], w=[ps1])
                        o0 = ((r_ * 2 + gi) if gi < 2 else (4 + r_ * 2 + gi - 2)) * 1024 + half * 512
                        b0 = gi * 1024 + half * 512
                        kb.op("dve", lambda e: e.tensor_tensor(out=gout[:, o0:o0 + 512], in0=ps1[:, :], in1=brep[:, b0:b0 + 512],
                                                               op=ALU.add), r=[ps1, brep], w=[gout])
            kb.dma("sp", C.G[l], gout[:, :], r=[gout], w=[kb.tag("G", l)])


def setup_consts(C):
    kb = C.kb
    C.cneg = kb.sb("cneg", [128, 64], F32)
    kb.op("pool", lambda e: e.memset(C.cneg[:, :], -0.5), w=[C.cneg])


def rsqrt_mean(C, dst, src, n, inv_n, tmp):
    kb = C.kb
    kb.op("dve", lambda e: e.tensor_scalar(out=tmp[:, 0:n], in0=src[:, 0:n], scalar1=inv_n, scalar2=EPS,
                                           op0=ALU.mult, op1=ALU.add), r=[src], w=[tmp])
    kb.op("pool", lambda e: e.tensor_tensor(out=dst[:, 0:n], in0=tmp[:, 0:n], in1=C.cneg[:, 0:n], op=ALU.pow),
          r=[tmp, C.cneg], w=[dst])


def layer_mod_tables(C, l, which):
    kb = C.kb
    gs = kb.sb("gs", [128, 8, 2], F32)
    sc_blk = 8 if which == 0 else 32
    sh_blk = 0 if which == 0 else 24
    nrm = (l if which == 0 else DEPTH + l) * 8
    kb.op("dve", lambda e: e.tensor_scalar(out=gs[:, :, :], in0=C.modT[:, l, sc_blk:sc_blk + 8, :], scalar1=1.0, scalar2=None,
                                           op0=ALU.add), r=[C.modT], w=[gs])
    kb.op("dve", lambda e: e.tensor_tensor(out=gs[:, :, :], in0=gs[:, :, :],
                                           in1=C.normTs[:, nrm:nrm + 8].unsqueeze(2).to_broadcast([128, 8, 2]), op=ALU.mult),
          r=[gs, C.normTs], w=[gs])
    return gs, sh_blk


def rope_apply(C, dst_ap, dst_tl, src_ap, src_tl, nh, rp, t1, t2):
    kb = C.kb
    n = nh * 64
    kb.op("dve", lambda e: e.tensor_tensor(out=t1[:, 0:n].rearrange("p (h d) -> p h d", h=nh),
                                           in0=src_ap.rearrange("p (h d) -> p h d", h=nh),
                                           in1=rp[:, 0:64].unsqueeze(1).to_broadcast([128, nh, 64]), op=ALU.mult),
          r=[src_tl, rp], w=[t1])
    sv = src_ap.rearrange("p (h a b f) -> p h a b f", h=nh, a=2, b=2, f=16)
    tv = t2[:, 0:n].rearrange("p (h a b f) -> p h a b f", h=nh, a=2, b=2, f=16)
    sn = rp[:, 64:128].rearrange("p (a b f) -> p a b f", a=2, b=2, f=16)
    for b_ in range(2):
        kb.op("dve", lambda e: e.tensor_tensor(out=tv[:, :, :, b_, :], in0=sv[:, :, :, 1 - b_, :],
                                               in1=sn[:, :, b_, :].unsqueeze(1).to_broadcast([128, nh, 2, 16]), op=ALU.mult),
              r=[src_tl, rp], w=[t2])
    kb.op("pool", lambda e: e.tensor_tensor(out=dst_ap, in0=t1[:, 0:n], in1=t2[:, 0:n], op=ALU.add),
          r=[t1, t2], w=[dst_tl])


def norm_tile(C, xt, ss, rstd, tmp1, junk, xn):
    kb = C.kb
    kb.op("act", lambda e: e.activation(out=junk[:, :], in_=xt[:, :], func=AF.Square, accum_out=ss[:, 0:1]),
          r=[xt], w=[junk, ss])
    rsqrt_mean(C, rstd, ss, 1, 1.0 / D, tmp1)
    kb.op("act", lambda e: e.activation(out=xn[:, :], in_=xt[:, :], func=AF.Copy, scale=rstd[:, 0:1]),
          r=[xt, rstd], w=[xn])


def phaseA_even(C, l, tiles, x_src):
    kb = C.kb
    i = l // 2
    ps = C.ps
    with kb.phase():
        W = kb.sb("Win", [128, 8, EVEN_IN], BF16)
        wv = C.ev_w_in[i].rearrange("(k p) c -> p k c", p=128)
        for k in range(8):
            kb.dma("pool", W[:, k, :], wv[:, k, :], w=[W])
        gs1, sh_blk = layer_mod_tables(C, l, 0)
        gain = kb.sb("qkgain", [128, 128], F32)
        kb.dma("sp", gain[:, :], C.ev_qk_gain[i].partition_broadcast(128), w=[gain])
        kb.op("dve", lambda e: e.tensor_scalar(out=gain[:, 0:64], in0=gain[:, 0:64], scalar1=0.125, scalar2=None, op0=ALU.mult),
              r=[gain], w=[gain])
        xb = [kb.sb("xt", [128, D], F32) for _ in range(2)]
        rpb = [kb.sb("rp", [128, 128], F32) for _ in range(2)]
        junk = kb.sb("junk", [128, D], BF16)
        xn = kb.sb("xn", [128, D], BF16)
        hT = [kb.sb("hT", [128, 8, 128], BF16) for _ in range(2)]
        pa = [kb.sb("pa", [128, EVEN_IN], BF16) for _ in range(2)]
        ss = kb.sb("ss", [128, 1], F32)
        rstd = kb.sb("rstd", [128, 1], F32)
        tmp1 = kb.sb("tmp1", [128, 8], F32)
        ssh = kb.sb("ssh", [128, 8], F32)
        rs8 = kb.sb("rs8", [128, 8], F32)
        tA = kb.sb("tA", [128, 512], F32)
        tB = kb.sb("tB", [128, 512], F32)
        t1 = kb.sb("t1", [128, 512], F32)
        t2 = kb.sb("t2", [128, 512], F32)
        for n_, t in enumerate(tiles):
            lat = t >= 2
            r_ = 0 if lat else 1
            xt = xb[n_ % 2]
            rp = rpb[n_ % 2]
            h = hT[n_ % 2]
            po = pa[n_ % 2]
            x_src(t, xt)
            if lat:
                kb.dma("sp", rp[:, :], C.rope[(t - 2) * 128:(t - 1) * 128, :], w=[rp])
            norm_tile(C, xt, ss, rstd, tmp1, junk, xn)
            psT = ps[0]
            pv = psT[:, :].bitcast(BF16)
            for k in range(8):
                kb.op("pe", lambda e: e.transpose(pv[:, k * 128:(k + 1) * 128], xn[:, k * 128:(k + 1) * 128], C.identb_s[:, :]),
                      r=[xn, C.identb_s], w=[psT])
            for k in range(8):
                kb.op("act", lambda e: e.activation(out=h[:, k, :], in_=pv[:, k * 128:(k + 1) * 128], func=AF.Identity,
                                                    scale=gs1[:, k, r_:r_ + 1], bias=C.modT[:, l, sh_blk + k, r_:r_ + 1]),
                      r=[psT, gs1, C.modT], w=[h])
            for j in range(8):
                c0 = j * 512
                nc_ = 512 if j < 7 else 256
                pj = ps[1 + (j % 6)]
                for k in range(8):
                    kb.op("pe", lambda e: e.matmul(pj[:, 0:nc_], lhsT=h[:, k, :], rhs=W[:, k, c0:c0 + nc_],
                                                   start=(k == 0), stop=(k == 7)), r=[h, W], w=[pj])
                if j == 0:
                    if lat:
                        rope_apply(C, po[:, c0:c0 + 512], po, pj[:, :], pj, 8, rp, t1, t2)
                    else:
                        kb.op("act", lambda e: e.copy(out=po[:, c0:c0 + 512], in_=pj[:, :]), r=[pj], w=[po])
                elif j == 1:
                    if lat:
                        kb.op("act", lambda e: e.mul(out=tA[:, :], in_=pj[:, :], mul=0.125), r=[pj], w=[tA])
                        rope_apply(C, po[:, c0:c0 + 512], po, tA[:, :], tA, 8, rp, t1, t2)
                    else:
                        kb.op("act", lambda e: e.mul(out=po[:, c0:c0 + 512], in_=pj[:, :], mul=0.125), r=[pj], w=[po])
                elif j in (2, 3):
                    kb.op("act", lambda e: e.copy(out=po[:, c0:c0 + 512], in_=pj[:, :]), r=[pj], w=[po])
                elif j in (4, 5):
                    kb.op("act", lambda e: e.activation(out=po[:, c0:c0 + 512], in_=pj[:, :], func=AF.Silu), r=[pj], w=[po])
                else:
                    nh = 8 if j == 6 else 2
                    n = nh * 64
                    g0 = 0 if j == 6 else 64
                    kb.op("act", lambda e: e.activation(out=tA[:, 0:n], in_=pj[:, 0:n], func=AF.Square), r=[pj], w=[tA])
                    kb.op("dve", lambda e: e.reduce_sum(out=ssh[:, 0:nh], in_=tA[:, 0:n].rearrange("p (h d) -> p h d", h=nh),
                                                        axis=AX.X), r=[tA], w=[ssh])
                    rsqrt_mean(C, rs8, ssh, nh, 1.0 / 64, tmp1)
                    kb.op("dve", lambda e: e.tensor_tensor(out=tB[:, 0:n].rearrange("p (h d) -> p h d", h=nh),
                                                           in0=pj[:, 0:n].rearrange("p (h d) -> p h d", h=nh),
                                                           in1=rs8[:, 0:nh].unsqueeze(2).to_broadcast([128, nh, 64]), op=ALU.mult),
                          r=[pj, rs8], w=[tB])
                    if lat:
                        kb.op("dve", lambda e: e.tensor_tensor(out=tB[:, 0:n].rearrange("p (h d) -> p h d", h=nh),
                                                               in0=tB[:, 0:n].rearrange("p (h d) -> p h d", h=nh),
                                                               in1=gain[:, g0:g0 + 64].unsqueeze(1).to_broadcast([128, nh, 64]),
                                                               op=ALU.mult), r=[tB, gain], w=[tB])
                        rope_apply(C, po[:, c0:c0 + n], po, tB[:, 0:n], tB, nh, rp, t1, t2)
                    else:
                        kb.op("dve", lambda e: e.tensor_tensor(out=po[:, c0:c0 + n].rearrange("p (h d) -> p h d", h=nh),
                                                               in0=tB[:, 0:n].rearrange("p (h d) -> p h d", h=nh),
                                                               in1=gain[:, g0:g0 + 64].unsqueeze(1).to_broadcast([128, nh, 64]),
                                                               op=ALU.mult), r=[tB, gain], w=[po])
                    if j == 7:
                        kb.op("act", lambda e: e.copy(out=po[:, c0 + 128:c0 + 256], in_=pj[:, 128:256]), r=[pj], w=[po])
            kb.dma("sp", C.PA[t * 128:(t + 1) * 128, 0:EVEN_IN], po[:, :], r=[po], w=[kb.tag("PA", t)])


def rope_table():
    t = np.arange(L)
    rows = (t // 64).astype(np.float32)
    cols = (t % 64).astype(np.float32)
    inv = (10000.0 ** (-np.arange(16, dtype=np.float32) / 16)).astype(np.float32)
    ang = np.stack([rows[:, None] * inv, cols[:, None] * inv], axis=1).astype(np.float32)
    c, s = np.cos(ang), np.sin(ang)
    C64 = np.stack([c, c], axis=2)
    S64 = np.stack([-s, s], axis=2)
    return np.concatenate([C64.reshape(L, 64), S64.reshape(L, 64)], axis=1).astype(np.float32)


def fm(v):
    v = np.asarray(v, np.float32).reshape(-1)
    return np.ascontiguousarray(v.reshape(-1, 128).T)


def host_prep(inp):
    import ml_dtypes
    shared = {}
    shared["normT"] = np.concatenate([fm(inp["norm_mix"][l]) for l in range(DEPTH)] + [fm(inp["norm_ffn"][l]) for l in range(DEPTH)]
                                     + [fm(inp["final_norm"])], axis=1)
    shared["badaT"] = np.concatenate([fm(inp["b_ada"][l]) for l in range(DEPTH)], axis=1)
    shared["b_ada"] = np.ascontiguousarray(inp["b_ada"], np.float32)
    shared["rope"] = rope_table()
    shared["identb"] = np.eye(128, dtype=np.float32).astype(ml_dtypes.bfloat16)
    shared["identf"] = np.eye(128, dtype=np.float32)
    shared["rconst"] = ret_consts()
    shared["od_w_in"] = np.ascontiguousarray(inp["od_w_in"], np.float32)
    shared["od_prm"] = np.ascontiguousarray(np.concatenate([inp["od_a_log_f"], inp["od_a_log_b"], inp["od_dt_bias_f"], inp["od_dt_bias_b"]], axis=1), np.float32)
    shared["od_conv"] = np.ascontiguousarray(inp["od_conv"], np.float32)
    shared["od_gain"] = np.ascontiguousarray(inp["od_out_gain"], np.float32)
    shared["od_w_out"] = np.ascontiguousarray(inp["od_w_out"], np.float32)
    shared["dconst"] = dn_consts()
    shared["norms"] = np.ascontiguousarray(np.concatenate([inp["norm_mix"], inp["norm_ffn"], np.asarray(inp["final_norm"])[None, :]], axis=0), np.float32)
    shared["wr"] = np.ascontiguousarray(np.concatenate([inp["moe_w_group"], inp["moe_w_expert"]], axis=2), np.float32)
    shared["br"] = np.ascontiguousarray(np.concatenate([inp["moe_b_group"], inp["moe_b_expert"]], axis=1), np.float32)
    shared["triones"], shared["mconst"], shared["srcidx"], shared["listinit"] = moe_consts()
    shared["wgu"] = np.ascontiguousarray(np.asarray(inp["moe_w_gate_up"], np.float32).reshape(DEPTH, 32, 8, 128, 1024).transpose(0, 1, 3, 2, 4)).reshape(DEPTH, 32 * 128, 8 * 1024)
    shared["wd"] = np.ascontiguousarray(np.asarray(inp["moe_w_down"], np.float32).reshape(DEPTH, 32, 4, 128, 1024).transpose(0, 1, 3, 2, 4)).reshape(DEPTH, 32 * 128, 4 * 1024)
    shared["w_ada"] = np.ascontiguousarray(inp["w_ada"], np.float32)
    shared["ev_w_in"] = np.ascontiguousarray(inp["ev_w_in"], np.float32)
    shared["ev_qk_gain"] = np.ascontiguousarray(np.concatenate([inp["ev_q_gain"], inp["ev_k_gain"]], axis=1), np.float32)
    shared["ev_decay"] = np.ascontiguousarray(np.concatenate([inp["ev_decay_f"], inp["ev_decay_b"]], axis=1), np.float32)
    shared["ev_w_out"] = np.ascontiguousarray(inp["ev_w_out"], np.float32)
    maps = []
    for core in range(8):
        b = core % 4
        m = dict(shared)
        m["xin"] = np.ascontiguousarray(np.concatenate([inp["ctx"][b], inp["x"][b]], axis=0), np.float32)
        cv = np.stack([np.asarray(inp["c"][b], np.float32), np.asarray(inp["c_ctx"], np.float32)], axis=0)
        m["cT"] = np.ascontiguousarray(cv.reshape(2, 8, 128).transpose(2, 1, 0).reshape(128, 16))
        maps.append(m)
    return maps


def ret_consts():
    p = np.arange(128, dtype=np.float32)
    diff = p[None, :] - p[:, None]
    dpos = np.maximum(diff, 0)
    dneg = np.maximum(-diff, 0)
    mge = (diff >= 0).astype(np.float32)
    mle = (diff <= 0).astype(np.float32)
    pos1 = np.tile(p[None, :] + 1, (128, 1))
    rpos1 = np.tile(128 - p[None, :], (128, 1))
    pcol = np.stack([127 - p, p], axis=1)
    return np.ascontiguousarray(np.concatenate([dpos, dneg, mge, mle, pos1, rpos1, pcol], axis=1), np.float32)


def phaseB_ret(C, l, n_ctx_tiles=2, lat_tiles=None):
    kb = C.kb
    i = l // 2
    ps = C.ps
    if lat_tiles is None:
        lat_tiles = list(range(2, NT))
    with kb.phase():
        rc = kb.sb("rc", [128, 770], F32)
        kb.dma("sp", rc[:, :], C.rconst, w=[rc])
        ld = kb.sb("ld", [128, 16], F32)
        kb.dma("sp", ld[:, :], C.ev_decay[i].partition_broadcast(128), w=[ld])
        kb.op("act", lambda e: e.activation(out=ld[:, :], in_=ld[:, :], func=AF.Exp), r=[ld], w=[ld])
        kb.op("dve", lambda e: e.tensor_scalar(out=ld[:, :], in0=ld[:, :], scalar1=-1.0, scalar2=None, op0=ALU.mult), r=[ld], w=[ld])
        maskT = kb.sb("maskT", [128, 16, 128], BF16)
        DQ = kb.sb("DQ", [64, 16, 128], F32)
        DK = kb.sb("DK", [128, 16], F32)
        GC = kb.sb("GC", [64, 16], F32)
        tmpm = kb.sb("tmpm", [128, 128], F32)
        for d_ in range(2):
            for h in range(8):
                c = d_ * 8 + h
                kb.op("act", lambda e: e.activation(out=tmpm[:, :], in_=rc[:, d_ * 128:(d_ + 1) * 128], func=AF.Exp,
                                                    scale=ld[:, c:c + 1]), r=[rc, ld], w=[tmpm])
                kb.op("dve", lambda e: e.tensor_tensor(out=maskT[:, c, :], in0=tmpm[:, :], in1=rc[:, (2 + d_) * 128:(3 + d_) * 128],
                                                       op=ALU.mult), r=[tmpm, rc], w=[maskT])
                kb.op("act", lambda e: e.activation(out=DQ[:, c, :], in_=rc[0:64, (4 + d_) * 128:(5 + d_) * 128], func=AF.Exp,
                                                    scale=ld[0:64, c:c + 1]), r=[rc, ld], w=[DQ])
            kb.op("dve", lambda e: e.tensor_scalar(out=DK[:, d_ * 8:(d_ + 1) * 8], in0=ld[:, d_ * 8:(d_ + 1) * 8],
                                                   scalar1=rc[:, 768 + d_:769 + d_], scalar2=None, op0=ALU.mult), r=[ld, rc], w=[DK])
        kb.op("act", lambda e: e.activation(out=DK[:, :], in_=DK[:, :], func=AF.Exp), r=[DK], w=[DK])
        kb.op("act", lambda e: e.activation(out=GC[:, :], in_=ld[0:64, :], func=AF.Exp, scale=128.0), r=[ld], w=[GC])
        qkvb = [kb.sb("qkv", [128, 2048], BF16) for _ in range(2)]
        gateb = [kb.sb("gate", [128, 1024], BF16) for _ in range(2)]
        ofb = [kb.sb("of", [128, 1024], F32) for _ in range(2)]
        qT = kb.sb("qT", [64, 8, 128], BF16)
        qTd = kb.sb("qTd", [64, 8, 128], BF16)
        kT = kb.sb("kT", [64, 8, 128], BF16)
        kdec = kb.sb("kdec", [128, 8, 64], BF16)
        smb = [kb.sb("sm", [128, 128], BF16) for _ in range(8)]
        S = kb.sb("S", [64, 8, 128], F32)
        Sb = kb.sb("Sb", [64, 8, 128], BF16)
        osum = kb.sb("osum", [128, 1024], F32)
        junk = kb.sb("junkr", [128, 1024], F32)
        ssh = kb.sb("sshr", [128, 8], F32)
        rs8 = kb.sb("rs8r", [128, 8], F32)
        tmp8 = kb.sb("tmp8r", [128, 8], F32)
        mixb = [kb.sb("mixr", [128, 1024], BF16) for _ in range(2)]
        psQ, psK = ps[0], ps[1]
        psS = [ps[2], ps[3]]
        psO = [ps[4], ps[5]]
        psKV = [ps[6], ps[7]]
        pq = psQ[:, :].bitcast(BF16)
        pk = psK[:, :].bitcast(BF16)
        n_ = 0
        for d_ in range(2):
            order = list(range(n_ctx_tiles)) + list(lat_tiles)
            if d_ == 1:
                order = list(range(n_ctx_tiles))[::-1] + list(lat_tiles)[::-1]
            kb.op("dve", lambda e: e.memset(S[:, :, :], 0.0), w=[S])
            kb.op("dve", lambda e: e.memset(Sb[:, :, :], 0.0), w=[Sb])
            for t in order:
                if getattr(C, "dbgB", 9) < 1:
                    break
                qkv = qkvb[n_ % 2]
                gate = gateb[n_ % 2]
                of = ofb[n_ % 2]
                mix = mixb[n_ % 2]
                n_ += 1
                kb.dma("sp", qkv[:, :], C.PA[t * 128:(t + 1) * 128, 0:2048], r=[kb.tag("PA", t)], w=[qkv])
                if d_ == 1:
                    kb.dma("sp", gate[:, :], C.PA[t * 128:(t + 1) * 128, 2048:3072], r=[kb.tag("PA", t)], w=[gate])
                    kb.dma("sp", of[:, :], C.OF[t * 128:(t + 1) * 128, :], r=[kb.tag("OF", t)], w=[of])
                for h in range(8):
                    kb.op("pe", lambda e: e.transpose(pq[0:64, h * 128:(h + 1) * 128], qkv[:, h * 64:(h + 1) * 64], C.identb_s[:, :]),
                          r=[qkv, C.identb_s], w=[psQ])
                for h in range(8):
                    kb.op("pe", lambda e: e.transpose(pk[0:64, h * 128:(h + 1) * 128], qkv[:, 512 + h * 64:512 + (h + 1) * 64],
                                                      C.identb_s[:, :]), r=[qkv, C.identb_s], w=[psK])
                kb.op("act", lambda e: e.copy(out=qT[:, :, :].rearrange("p h t -> p (h t)"), in_=pq[0:64, :]), r=[psQ], w=[qT])
                kb.op("dve", lambda e: e.tensor_tensor(out=qTd[:, :, :], in0=pq[0:64, :].rearrange("p (h t) -> p h t", h=8),
                                                       in1=DQ[:, d_ * 8:(d_ + 1) * 8, :], op=ALU.mult), r=[psQ, DQ], w=[qTd])
                kb.op("act", lambda e: e.copy(out=kT[:, :, :].rearrange("p h t -> p (h t)"), in_=pk[0:64, :]), r=[psK], w=[kT])
                kb.op("dve", lambda e: e.tensor_tensor(out=kdec[:, :, :], in0=qkv[:, 512:1024].rearrange("p (h d) -> p h d", h=8),
                                                        in1=DK[:, d_ * 8:(d_ + 1) * 8].unsqueeze(2).to_broadcast([128, 8, 64]),
                                                        op=ALU.mult), r=[qkv, DK], w=[kdec])
                if getattr(C, "dbgB", 9) < 2:
                    continue
                for h in range(8):
                    pS = psS[h // 4]
                    sc_ = (h % 4) * 128
                    kb.op("pe", lambda e: e.matmul(pS[:, sc_:sc_ + 128], lhsT=kT[:, h, :], rhs=qT[:, h, :], start=True, stop=True),
                          r=[kT, qT], w=[pS])
                for h in range(8):
                    pS = psS[h // 4]
                    sc_ = (h % 4) * 128
                    kb.op("dve", lambda e: e.tensor_tensor(out=smb[h][:, :], in0=pS[:, sc_:sc_ + 128], in1=maskT[:, d_ * 8 + h, :], op=ALU.mult),
                          r=[pS, maskT], w=[smb[h]])
                for h in range(8):
                    sm = smb[h]
                    pO = psO[h // 4]
                    pKV = psKV[h // 4]
                    oc = (h % 4) * 128
                    vh = qkv[:, 1024 + h * 128:1024 + (h + 1) * 128]
                    kb.op("pe", lambda e: e.matmul(pO[:, oc:oc + 128], lhsT=sm[:, :], rhs=vh, start=True, stop=False),
                          r=[sm, qkv], w=[pO])
                    kb.op("pe", lambda e: e.matmul(pO[:, oc:oc + 128], lhsT=qTd[:, h, :], rhs=Sb[:, h, :], start=False, stop=True),
                          r=[qTd, Sb], w=[pO])
                    kb.op("pe", lambda e: e.matmul(pKV[0:64, oc:oc + 128], lhsT=kdec[:, h, :], rhs=vh, start=True, stop=True),
                          r=[kdec, qkv], w=[pKV])
                if getattr(C, "dbgB", 9) < 3:
                    continue
                kb.op("dve", lambda e: e.tensor_tensor(out=S[:, :, :], in0=S[:, :, :],
                                                       in1=GC[:, d_ * 8:(d_ + 1) * 8].unsqueeze(2).to_broadcast([64, 8, 128]), op=ALU.mult),
                      r=[S, GC], w=[S])
                for hb in range(2):
                    kb.op("dve", lambda e: e.tensor_tensor(out=S[:, hb * 4:(hb + 1) * 4, :], in0=S[:, hb * 4:(hb + 1) * 4, :],
                                                           in1=psKV[hb][0:64, :].rearrange("p (h t) -> p h t", h=4), op=ALU.add),
                          r=[S, psKV[hb]], w=[S])
                kb.op("act", lambda e: e.copy(out=Sb[:, :, :].rearrange("p h t -> p (h t)"), in_=S[:, :, :].rearrange("p h t -> p (h t)")), r=[S], w=[Sb])
                if d_ == 0:
                    for hb in range(2):
                        kb.op("act", lambda e: e.copy(out=of[:, hb * 512:(hb + 1) * 512], in_=psO[hb][:, :]), r=[psO[hb]], w=[of])
                    kb.dma("sp", C.OF[t * 128:(t + 1) * 128, :], of[:, :], r=[of], w=[kb.tag("OF", t)])
                else:
                    for hb in range(2):
                        kb.op("dve", lambda e: e.tensor_tensor(out=osum[:, hb * 512:(hb + 1) * 512], in0=psO[hb][:, :],
                                                               in1=of[:, hb * 512:(hb + 1) * 512], op=ALU.add),
                              r=[psO[hb], of], w=[osum])
                    kb.op("act", lambda e: e.activation(out=junk[:, :], in_=osum[:, :], func=AF.Square), r=[osum], w=[junk])
                    kb.op("dve", lambda e: e.reduce_sum(out=ssh[:, :], in_=junk[:, :].rearrange("p (h d) -> p h d", h=8), axis=AX.X),
                          r=[junk], w=[ssh])
                    rsqrt_mean(C, rs8, ssh, 8, 1.0 / 128, tmp8)
                    kb.op("dve", lambda e: e.tensor_tensor(out=osum[:, :].rearrange("p (h d) -> p h d", h=8),
                                                           in0=osum[:, :].rearrange("p (h d) -> p h d", h=8),
                                                           in1=rs8[:, :].unsqueeze(2).to_broadcast([128, 8, 128]), op=ALU.mult),
                          r=[osum, rs8], w=[osum])
                    kb.op("pool", lambda e: e.tensor_tensor(out=mix[:, :], in0=osum[:, :], in1=gate[:, :], op=ALU.mult),
                          r=[osum, gate], w=[mix])
                    kb.dma("sp", C.MIX[t * 128:(t + 1) * 128, 0:1024], mix[:, :], r=[mix], w=[kb.tag("MIXr", t)])


def phaseC_att(C, l, qblocks=None, key_tiles=None):
    kb = C.kb
    ps = C.ps
    if key_tiles is None:
        key_tiles = list(range(NT))
    if qblocks is None:
        qblocks = [([0, 1], [0, 1])] + [([2 + 4 * b + j for j in range(4)], key_tiles) for b in range(16)]
    with kb.phase():
        kTa = kb.sb("kTa", [64, 2, T], BF16)
        Va = kb.sb("Va", [128, NT, 2, 128], BF16)
        kb.op("pool", lambda e: e.memset(Va[:, :, :, :], 1.0), w=[Va])
        kvb = [kb.sb("kvld", [128, 256], BF16) for _ in range(2)]
        pT0 = ps[0]
        pt0 = pT0[:, :].bitcast(BF16)
        for n_, t in enumerate(key_tiles):
            kv = kvb[n_ % 2]
            kb.dma("sp", kv[:, :], C.PA[t * 128:(t + 1) * 128, 3584:3840], r=[kb.tag("PA", t)], w=[kv])
            for g in range(2):
                kb.op("pe", lambda e: e.transpose(pt0[0:64, g * 128:(g + 1) * 128], kv[:, g * 64:(g + 1) * 64], C.identb_s[:, :]),
                      r=[kv, C.identb_s], w=[pT0])
            for g in range(2):
                kb.op("act", lambda e: e.copy(out=kTa[:, g, t * 128:(t + 1) * 128], in_=pt0[0:64, g * 128:(g + 1) * 128]),
                      r=[pT0], w=[kTa])
            kb.op("pool", lambda e: e.tensor_copy(out=Va[:, t, :, 0:64], in_=kv[:, 128:256].rearrange("p (g d) -> p g d", g=2)),
                  r=[kv], w=[Va])
        aqb = [kb.sb("aq", [128, 4, 512], BF16) for _ in range(2)]
        qTb = kb.sb("qTb", [64, 8, 512], BF16)
        pTb = [kb.sb("pT", [128, 512], BF16) for _ in range(3)]
        rsb = kb.sb("rsb", [128, 512], F32)
        atb = [kb.sb("attT", [64, 512], BF16) for _ in range(2)]
        psS = [ps[2], ps[3], ps[4]]
        psO = [ps[5], ps[6]]
        psQ = [ps[0], ps[1]]
        for bi, (qt, keys) in enumerate(qblocks):
            nq = len(qt) * 128
            aq = aqb[bi % 2]
            for j, t in enumerate(qt):
                kb.dma("sp", aq[:, j, :], C.PA[t * 128:(t + 1) * 128, 3072:3584], r=[kb.tag("PA", t)], w=[aq])
            for hp in range(4):
                pQ = psQ[hp % 2]
                pqv = pQ[:, :].bitcast(BF16)
                for hh in range(2):
                    h = hp * 2 + hh
                    for j in range(len(qt)):
                        kb.op("pe", lambda e: e.transpose(pqv[0:64, hh * 512 + j * 128:hh * 512 + (j + 1) * 128],
                                                          aq[:, j, h * 64:(h + 1) * 64], C.identb_s[:, :]),
                              r=[aq, C.identb_s], w=[pQ])
                kb.op("dve", lambda e: e.tensor_copy(out=qTb[:, hp * 2:hp * 2 + 2, 0:nq],
                                                     in_=pqv[0:64, :].rearrange("p (h q) -> p h q", h=2)[:, :, 0:nq]),
                      r=[pQ], w=[qTb])
            tok0 = qt[0] * 128
            for h in range(8):
                g = h // 4
                pO = psO[h % 2]
                at = atb[h % 2]
                nk = len(keys)

                def s_mm(ki):
                    kt = keys[ki]
                    pS = psS[ki % 3]
                    kb.op("pe", lambda e: e.matmul(pS[:, 0:nq], lhsT=kTa[:, g, kt * 128:(kt + 1) * 128], rhs=qTb[:, h, 0:nq],
                                                   start=True, stop=True), r=[kTa, qTb], w=[pS])
                s_mm(0)
                if nk > 1:
                    s_mm(1)
                for ki in range(nk):
                    kt = keys[ki]
                    pS = psS[ki % 3]
                    pT = pTb[ki % 3]
                    kb.op("act", lambda e: e.activation(out=pT[:, 0:nq], in_=pS[:, 0:nq], func=AF.Exp), r=[pS], w=[pT])
                    if ki + 2 < nk:
                        s_mm(ki + 2)
                    kb.op("pe", lambda e: e.matmul(pO[:, 0:nq], lhsT=Va[:, kt, g, :], rhs=pT[:, 0:nq], start=(ki == 0), stop=(ki == nk - 1)),
                          r=[Va, pT], w=[pO])
                kb.op("dve", lambda e: e.reciprocal(out=rsb[64:128, 0:nq], in_=pO[64:128, 0:nq]), r=[pO], w=[rsb])
                kb.op("dve", lambda e: e.tensor_tensor(out=at[:, 0:nq], in0=pO[0:64, 0:nq], in1=rsb[64:128, 0:nq], op=ALU.mult),
                      r=[pO, rsb], w=[at])
                kb.dma("sp", C.AT[h * 64:(h + 1) * 64, tok0:tok0 + nq], at[:, 0:nq], r=[at], w=[kb.tag("AT", bi)])


def phaseD_out(C, l, tiles, w_out_ap, nk, mix_src, post):
    kb = C.kb
    ps = C.ps
    with kb.phase():
        Wo = kb.sb("Wo", [128, nk, D], BF16)
        wv = w_out_ap.rearrange("(k p) c -> p k c", p=128)
        for k in range(nk):
            kb.dma("pool", Wo[:, k, :], wv[:, k, :], w=[Wo])
        Gt = kb.sb("Gt", [128, 4096], F32)
        kb.dma("sp", Gt[:, :], C.G[l], r=[kb.tag("G", l)], w=[Gt])
        mTb = [kb.sb("mT", [128, nk, 128], BF16) for _ in range(2)]
        xb = [kb.sb("xd", [128, D], F32) for _ in range(2)]
        yb = kb.sb("yb", [128, D], F32)
        st = post(None, None, None, setup=True)
        for n_, t in enumerate(tiles):
            lat = t >= 2
            mT = mTb[n_ % 2]
            xt = xb[n_ % 2]
            kb.dma("sp", xt[:, :], C.X[t * 128:(t + 1) * 128, :], r=[kb.tag("X", t)], w=[xt])
            mix_src(t, mT, n_)
            psY = [ps[6], ps[7]]
            for nb in range(2):
                for k in range(nk):
                    kb.op("pe", lambda e: e.matmul(psY[nb][:, :], lhsT=mT[:, k, :], rhs=Wo[:, k, nb * 512:(nb + 1) * 512],
                                                   start=(k == 0), stop=(k == nk - 1)), r=[mT, Wo], w=[psY[nb]])
            g0 = 0 if lat else 2048
            for nb in range(2):
                kb.op("dve", lambda e: e.tensor_tensor(out=yb[:, nb * 512:(nb + 1) * 512], in0=psY[nb][:, :],
                                                       in1=Gt[:, g0 + nb * 512:g0 + (nb + 1) * 512], op=ALU.mult),
                      r=[psY[nb], Gt], w=[yb])
            kb.op("pool", lambda e: e.tensor_tensor(out=xt[:, :], in0=xt[:, :], in1=yb[:, :], op=ALU.add), r=[xt, yb], w=[xt])
            kb.dma("sp", C.X[t * 128:(t + 1) * 128, :], xt[:, :], r=[xt], w=[kb.tag("X", t)])
            post(t, xt, Gt, st=st)
        post(None, None, None, st=st, finish=True)


def even_mix_src(C):
    kb = C.kb
    mrb = [kb.sb("mr", [128, D], BF16) for _ in range(2)]
    ATv = C.AT.rearrange("(c p) t -> p c t", p=128)

    def src(t, mT, n_):
        mr = mrb[n_ % 2]
        bi = 0 if t < 2 else 1 + (t - 2) // 4
        kb.dma("sp", mr[:, :], C.MIX[t * 128:(t + 1) * 128, 0:1024], r=[kb.tag("MIXr", t)], w=[mr])
        kb.dma("sp", mT[:, 8:12, :], ATv[:, :, t * 128:(t + 1) * 128], r=[kb.tag("AT", bi)], w=[mT])
        pT = C.ps[0]
        pv = pT[:, :].bitcast(BF16)
        for k in range(8):
            kb.op("pe", lambda e: e.transpose(pv[:, k * 128:(k + 1) * 128], mr[:, k * 128:(k + 1) * 128], C.identb_s[:, :]),
                  r=[mr, C.identb_s], w=[pT])
        kb.op("act", lambda e: e.copy(out=mT[:, 0:8, :].rearrange("p k t -> p (k t)"), in_=pv[:, :]), r=[pT], w=[mT])
    return src


NSLOT = 164
TPAD = T + 128
BIGI = 1.0e6
NEG = -1.0e30


def moe_consts():
    import ml_dtypes
    p = np.arange(128)
    tri = (p[:, None] < p[None, :]).astype(np.float32).astype(ml_dtypes.bfloat16)
    ones = np.ones((128, 128), np.float32).astype(ml_dtypes.bfloat16)
    eidx = np.tile(np.arange(32, dtype=np.float32)[None, :], (128, 1))
    pidx = p.astype(np.float32)[:, None]
    sidx = np.tile(np.arange(NSLOT, dtype=np.float32)[None, :], (128, 1))
    mconst = np.ascontiguousarray(np.concatenate([eidx, pidx, sidx], axis=1), np.float32)
    src = (np.arange(NT)[None, :] * 128 + p[:, None]).astype(np.int32)
    li = np.zeros((NSLOT + 1, 128, 4), np.int32)
    li[:, :, 1] = T + p[None, :]
    li = np.ascontiguousarray(li.transpose(1, 0, 2).reshape(128, (NSLOT + 1) * 4))
    return np.ascontiguousarray(np.concatenate([tri, ones], axis=1)), mconst, src, li


def phaseD(C, l, tiles, w_out_ap, nk, mix_src_factory, route_tiles):
    kb = C.kb
    ps = C.ps
    with kb.phase():
        Wo = kb.sb("Wo", [128, nk, D], BF16)
        wv = w_out_ap.rearrange("(k p) c -> p k c", p=128)
        for k in range(nk):
            kb.dma("pool", Wo[:, k, :], wv[:, k, :], w=[Wo])
        Gt = kb.sb("Gt", [128, 8192], F32)
        kb.dma("sp", Gt[:, :], C.G[l], r=[kb.tag("G", l)], w=[Gt])
        nrep = kb.sb("nrep", [128, D], F32)
        kb.dma("sp", nrep[:, :], C.norms[DEPTH + l].partition_broadcast(128), w=[nrep])
        for v_ in range(2):
            o0 = (5 + 2 * v_) * 1024
            kb.op("dve", lambda e: e.scalar_tensor_tensor(out=Gt[:, o0:o0 + 1024], in0=Gt[:, o0:o0 + 1024], scalar=1.0, in1=nrep[:, :],
                                                          op0=ALU.add, op1=ALU.mult), r=[Gt, nrep], w=[Gt])
        Wr = kb.sb("Wr", [128, 8, 36], F32)
        kb.dma("sp", Wr[:, :, :], C.wr[l].rearrange("(k p) c -> p k c", p=128), w=[Wr])
        brr = kb.sb("brr", [128, 36], F32)
        kb.dma("sp", brr[:, :], C.br[l].partition_broadcast(128), w=[brr])
        mc = kb.sb("mc", [128, 33 + NSLOT], F32)
        kb.dma("sp", mc[:, :], C.mconst, w=[mc])
        tro = kb.sb("tro", [128, 256], BF16)
        kb.dma("sp", tro[:, :], C.triones, w=[tro])
        srci = kb.sb("srci", [128, NT], I32)
        kb.dma("sp", srci[:, :], C.srcidx, w=[srci])
        linit = kb.sb("linit", [128, (NSLOT + 1) * 4], I32)
        kb.dma("sp", linit[:, :], C.listinit, w=[linit])
        kb.dma("sp", C.LIST.rearrange("(s p) c -> p s c", p=128), linit[:, :].rearrange("p (s c) -> p s c", c=4), r=[linit],
               w=[kb.tag("LISTinit")])
        base = kb.sb("base", [128, 32], F32)
        kb.op("dve", lambda e: e.memset(base[:, :], 0.0), w=[base])
        RT = kb.sb("RT", [128, NT, 8], F32)
        kb.op("dve", lambda e: e.memset(RT[:, :, :], 0.0), w=[RT])
        mTb = [kb.sb("mT", [128, nk, 128], BF16) for _ in range(2)]
        xb = [kb.sb("xd", [128, D], F32) for _ in range(2)]
        yb = kb.sb("yb", [128, D], F32)
        junk = kb.sb("junkd", [128, D], BF16)
        xn2 = kb.sb("xn2", [128, D], F32)
        fb = [kb.sb("fb", [128, D], BF16) for _ in range(2)]
        fT = kb.sb("fT", [128, 8, 128], F32)
        ss = kb.sb("ssd", [128, 1], F32)
        rstd = kb.sb("rstdd", [128, 1], F32)
        tmp1 = kb.sb("tmp1d", [128, 8], F32)
        lg = kb.sb("lg", [128, 36], F32)
        sm = kb.sb("smalls", [128, 16], F32)
        geq = kb.sb("geq", [128, 4], F32)
        gex = kb.sb("gex", [128, 4], F32)
        ml = kb.sb("ml", [128, 32], F32)
        ml2 = kb.sb("ml2", [128, 32], F32)
        oh1 = kb.sb("oh1", [128, 32], F32)
        oh2 = kb.sb("oh2", [128, 32], F32)
        ind = kb.sb("ind", [128, 32], BF16)
        pos = kb.sb("pos", [128, 32], F32)
        t32 = kb.sb("t32", [128, 32], F32)
        mix_src = mix_src_factory()
        EIDX = mc[:, 0:32]
        for n_, t in enumerate(tiles):
            lat = t >= 2
            mT = mTb[n_ % 2]
            xt = xb[n_ % 2]
            kb.dma("sp", xt[:, :], C.X[t * 128:(t + 1) * 128, :], r=[kb.tag("X", t)], w=[xt])
            mix_src(t, mT, n_)
            psY = [ps[6], ps[7]]
            for nb in range(2):
                for k in range(nk):
                    kb.op("pe", lambda e: e.matmul(psY[nb][:, :], lhsT=mT[:, k, :], rhs=Wo[:, k, nb * 512:(nb + 1) * 512],
                                                   start=(k == 0), stop=(k == nk - 1)), r=[mT, Wo], w=[psY[nb]])
            g0 = 0 if lat else 2048
            for nb in range(2):
                kb.op("dve", lambda e: e.tensor_tensor(out=yb[:, nb * 512:(nb + 1) * 512], in0=psY[nb][:, :],
                                                       in1=Gt[:, g0 + nb * 512:g0 + (nb + 1) * 512], op=ALU.mult),
                      r=[psY[nb], Gt], w=[yb])
            kb.op("pool", lambda e: e.tensor_tensor(out=xt[:, :], in0=xt[:, :], in1=yb[:, :], op=ALU.add), r=[xt, yb], w=[xt])
            kb.dma("sp", C.X[t * 128:(t + 1) * 128, :], xt[:, :], r=[xt], w=[kb.tag("X", t)])
            if t not in route_tiles:
                continue
            f = fb[n_ % 2]
            norm_tile(C, xt, ss, rstd, tmp1, junk, xn2)
            v0 = 4096 if lat else 6144
            kb.op("dve", lambda e: e.tensor_tensor(out=xn2[:, :], in0=xn2[:, :], in1=Gt[:, v0 + 1024:v0 + 2048], op=ALU.mult),
                  r=[xn2, Gt], w=[xn2])
            kb.op("pool", lambda e: e.tensor_tensor(out=xn2[:, :], in0=xn2[:, :], in1=Gt[:, v0:v0 + 1024], op=ALU.add),
                  r=[xn2, Gt], w=[xn2])
            kb.op("act", lambda e: e.copy(out=f[:, :], in_=xn2[:, :]), r=[xn2], w=[f])
            kb.dma("sp", C.F[t * 128:(t + 1) * 128, :], f[:, :], r=[f], w=[kb.tag("F", t)])
            psT = [ps[0], ps[1]]
            for k in range(8):
                kb.op("pe", lambda e: e.transpose(psT[k // 4][:, (k % 4) * 128:(k % 4 + 1) * 128], xn2[:, k * 128:(k + 1) * 128],
                                                  C.identf_s[:, :]), r=[xn2, C.identf_s], w=[psT[k // 4]])
            for hb in range(2):
                kb.op("act", lambda e: e.copy(out=fT[:, hb * 4:(hb + 1) * 4, :].rearrange("p k t -> p (k t)"), in_=psT[hb][:, :]),
                      r=[psT[hb]], w=[fT])
            pL = ps[2]
            for k in range(8):
                kb.op("pe", lambda e: e.matmul(pL[:, 0:36], lhsT=fT[:, k, :], rhs=Wr[:, k, :], start=(k == 0), stop=(k == 7)),
                      r=[fT, Wr], w=[pL])
            kb.op("dve", lambda e: e.tensor_tensor(out=lg[:, :], in0=pL[:, 0:36], in1=brr[:, :], op=ALU.add), r=[pL, brr], w=[lg])
            gmax, ngmax, gsum, pg, m1, m2, dd, ed, w1, w2 = [sm[:, i:i + 1] for i in range(10)]
            kb.op("dve", lambda e: e.reduce_max(out=gmax, in_=lg[:, 0:4], axis=AX.X), r=[lg], w=[sm])
            kb.op("dve", lambda e: e.tensor_scalar(out=ngmax, in0=gmax, scalar1=-1.0, scalar2=None, op0=ALU.mult), r=[sm], w=[sm])
            kb.op("dve", lambda e: e.tensor_scalar(out=geq[:, :], in0=lg[:, 0:4], scalar1=gmax, scalar2=None, op0=ALU.is_equal),
                  r=[lg, sm], w=[geq])
            kb.op("act", lambda e: e.activation(out=gex[:, :], in_=lg[:, 0:4], func=AF.Exp, bias=ngmax, scale=1.0, accum_out=gsum),
                  r=[lg, sm], w=[gex, sm])
            kb.op("dve", lambda e: e.reciprocal(out=pg, in_=gsum), r=[sm], w=[sm])
            kb.op("dve", lambda e: e.tensor_scalar(out=geq[:, :], in0=geq[:, :], scalar1=-NEG, scalar2=NEG, op0=ALU.mult, op1=ALU.add),
                  r=[geq], w=[geq])
            kb.op("dve", lambda e: e.tensor_tensor(out=ml[:, :].rearrange("p (g e) -> p g e", g=4),
                                                   in0=lg[:, 4:36].rearrange("p (g e) -> p g e", g=4),
                                                   in1=geq[:, :].unsqueeze(2).to_broadcast([128, 4, 8]), op=ALU.add), r=[lg, geq], w=[ml])
            kb.op("dve", lambda e: e.reduce_max(out=m1, in_=ml[:, :], axis=AX.X), r=[ml], w=[sm])
            kb.op("dve", lambda e: e.tensor_scalar(out=oh1[:, :], in0=ml[:, :], scalar1=m1, scalar2=None, op0=ALU.is_equal),
                  r=[ml, sm], w=[oh1])
            kb.op("dve", lambda e: e.scalar_tensor_tensor(out=ml2[:, :], in0=oh1[:, :], scalar=NEG, in1=ml[:, :], op0=ALU.mult, op1=ALU.add),
                  r=[oh1, ml], w=[ml2])
            kb.op("dve", lambda e: e.reduce_max(out=m2, in_=ml2[:, :], axis=AX.X), r=[ml2], w=[sm])
            kb.op("dve", lambda e: e.tensor_scalar(out=oh2[:, :], in0=ml2[:, :], scalar1=m2, scalar2=None, op0=ALU.is_equal),
                  r=[ml2, sm], w=[oh2])
            kb.op("dve", lambda e: e.tensor_tensor(out=dd, in0=m2, in1=m1, op=ALU.subtract), r=[sm], w=[sm])
            kb.op("act", lambda e: e.activation(out=ed, in_=dd, func=AF.Exp), r=[sm], w=[sm])
            kb.op("dve", lambda e: e.tensor_scalar(out=w1, in0=ed, scalar1=1.0, scalar2=None, op0=ALU.add), r=[sm], w=[sm])
            kb.op("dve", lambda e: e.reciprocal(out=w1, in_=w1), r=[sm], w=[sm])
            kb.op("dve", lambda e: e.tensor_tensor(out=w2, in0=ed, in1=w1, op=ALU.mult), r=[sm], w=[sm])
            kb.op("dve", lambda e: e.tensor_tensor(out=RT[:, t, 2:3], in0=w1, in1=pg, op=ALU.mult), r=[sm], w=[RT])
            kb.op("dve", lambda e: e.tensor_tensor(out=RT[:, t, 5:6], in0=w2, in1=pg, op=ALU.mult), r=[sm], w=[RT])
            kb.op("dve", lambda e: e.tensor_tensor(out=ind[:, :], in0=oh1[:, :], in1=oh2[:, :], op=ALU.add), r=[oh1, oh2], w=[ind])
            pR = ps[3]
            kb.op("pe", lambda e: e.matmul(pR[:, 0:32], lhsT=tro[:, 0:128], rhs=ind[:, :], start=True, stop=True), r=[tro, ind], w=[pR])
            kb.op("pe", lambda e: e.matmul(pR[:, 32:64], lhsT=tro[:, 128:256], rhs=ind[:, :], start=True, stop=True), r=[tro, ind], w=[pR])
            kb.op("dve", lambda e: e.tensor_tensor(out=pos[:, :], in0=pR[:, 0:32], in1=base[:, :], op=ALU.add), r=[pR, base], w=[pos])
            kb.op("dve", lambda e: e.tensor_tensor(out=base[:, :], in0=pR[:, 32:64], in1=base[:, :], op=ALU.add), r=[pR, base], w=[base])
            for k_, oh in enumerate((oh1, oh2)):
                kb.op("dve", lambda e: e.tensor_tensor(out=t32[:, :], in0=oh[:, :], in1=EIDX, op=ALU.mult), r=[oh, mc], w=[t32])
                kb.op("dve", lambda e: e.reduce_sum(out=RT[:, t, 3 * k_:3 * k_ + 1], in_=t32[:, :], axis=AX.X), r=[t32], w=[RT])
                kb.op("dve", lambda e: e.tensor_tensor(out=t32[:, :], in0=oh[:, :], in1=pos[:, :], op=ALU.mult), r=[oh, pos], w=[t32])
                kb.op("dve", lambda e: e.reduce_sum(out=RT[:, t, 3 * k_ + 1:3 * k_ + 2], in_=t32[:, :], axis=AX.X), r=[t32], w=[RT])
        ni = kb.sb("ni", [128, 32], I32)
        ca = kb.sb("ca", [128, 32], F32)
        cb = kb.sb("cb", [128, 32], F32)
        tl_ = kb.sb("tl", [128, 32], F32)
        kb.op("dve", lambda e: e.tensor_scalar(out=t32[:, :], in0=base[:, :], scalar1=127.0, scalar2=None, op0=ALU.add), r=[base], w=[t32])
        kb.op("dve", lambda e: e.tensor_copy(out=ni[:, :], in_=t32[:, :]), r=[t32], w=[ni])
        kb.op("dve", lambda e: e.tensor_scalar(out=ni[:, :], in0=ni[:, :], scalar1=7, scalar2=None, op0=ALU.arith_shift_right), r=[ni], w=[ni])
        kb.op("dve", lambda e: e.tensor_copy(out=tl_[:, :], in_=ni[:, :]), r=[ni], w=[tl_])
        kb.op("dve", lambda e: e.tensor_copy(out=ca[:, :], in_=tl_[:, :]), r=[tl_], w=[ca])
        a_, b_ = ca, cb
        for d_ in (1, 2, 4, 8, 16):
            kb.op("dve", lambda e: e.tensor_copy(out=b_[:, 0:d_], in_=a_[:, 0:d_]), r=[a_], w=[b_])
            kb.op("dve", lambda e: e.tensor_tensor(out=b_[:, d_:32], in0=a_[:, d_:32], in1=a_[:, 0:32 - d_], op=ALU.add), r=[a_], w=[b_])
            a_, b_ = b_, a_
        cum = a_
        ss128 = b_
        kb.op("dve", lambda e: e.tensor_tensor(out=ss128[:, :], in0=cum[:, :], in1=tl_[:, :], op=ALU.subtract), r=[cum, tl_], w=[ss128])
        kb.op("dve", lambda e: e.tensor_scalar(out=ss128[:, :], in0=ss128[:, :], scalar1=128.0, scalar2=None, op0=ALU.mult),
              r=[ss128], w=[ss128])
        big = kb.sb("big", [128, NT, 32], F32)
        offf = kb.sb("offf", [128, 2, NT], F32)
        offi = kb.sb("offi", [128, 2, NT], I32)
        ent = kb.sb("ent", [128, 2, NT, 4], I32)
        kb.op("dve", lambda e: e.memset(ent[:, :, :, :], 0), w=[ent])
        for k_ in range(2):
            kb.op("dve", lambda e: e.tensor_tensor(out=big[:, :, :], in0=mc[:, 0:32].unsqueeze(1).to_broadcast([128, NT, 32]),
                                                   in1=RT[:, :, 3 * k_:3 * k_ + 1].to_broadcast([128, NT, 32]), op=ALU.is_equal),
                  r=[mc, RT], w=[big])
            kb.op("dve", lambda e: e.tensor_tensor(out=big[:, :, :], in0=big[:, :, :],
                                                   in1=ss128[:, :].unsqueeze(1).to_broadcast([128, NT, 32]), op=ALU.mult),
                  r=[big, ss128], w=[big])
            kb.op("dve", lambda e: e.reduce_sum(out=offf[:, k_, :], in_=big[:, :, :], axis=AX.X), r=[big], w=[offf])
            kb.op("dve", lambda e: e.tensor_tensor(out=offf[:, k_, :], in0=offf[:, k_, :], in1=RT[:, :, 3 * k_ + 1], op=ALU.add),
                  r=[offf, RT], w=[offf])
            kb.op("dve", lambda e: e.tensor_copy(out=offi[:, k_, :], in_=offf[:, k_, :]), r=[offf], w=[offi])
            kb.op("dve", lambda e: e.tensor_copy(out=ent[:, k_, :, 0], in_=srci[:, :]), r=[srci], w=[ent])
            kb.op("dve", lambda e: e.tensor_scalar(out=ent[:, k_, :, 1], in0=srci[:, :], scalar1=k_ * TPAD, scalar2=None, op0=ALU.add),
                  r=[srci], w=[ent])
            kb.op("dve", lambda e: e.tensor_copy(out=ent[:, k_, :, 2], in_=RT[:, :, 3 * k_ + 2].bitcast(I32)), r=[RT], w=[ent])
        if hasattr(C, "dbgD"):
            kb.dma("sp", C.dbgD["offi"], offi[:, :, :].rearrange("p k t -> p (k t)"), r=[offi], w=[kb.tag("dbg1")])
            kb.dma("sp", C.dbgD["RT"], RT[:, :, :].rearrange("p t c -> p (t c)"), r=[RT], w=[kb.tag("dbg2")])
            kb.dma("sp", C.dbgD["base"], base[:, :], r=[base], w=[kb.tag("dbg4")])
            kb.dma("sp", C.dbgD["ent"], ent[:, :, :, :].rearrange("p k t c -> p (k t c)"), r=[ent], w=[kb.tag("dbg5")])
        for t in (route_tiles if not getattr(C, "skip_scatter", False) else []):
            for k_ in range(2):
                kb.dma("pool", C.LIST, ent[:, k_, t, :], r=[ent, offi, kb.tag("LISTinit")], w=[kb.tag("LISTs", t, k_)],
                       indirect=dict(out_offset=bass.IndirectOffsetOnAxis(ap=offi[:, k_, t:t + 1], axis=0), in_offset=None))
        SIDX = mc[:, 33:33 + NSLOT]
        cmp_ = kb.sb("cmp", [128, NSLOT, 32], F32)
        eid = kb.sb("eid", [128, NSLOT + 2], F32)
        kb.op("dve", lambda e: e.memset(eid[:, 0:2], -1.0), w=[eid])
        kb.op("dve", lambda e: e.tensor_tensor(out=cmp_[:, :, :], in0=cum[:, :].unsqueeze(1).to_broadcast([128, NSLOT, 32]),
                                               in1=SIDX.unsqueeze(2).to_broadcast([128, NSLOT, 32]), op=ALU.is_le), r=[cum, mc], w=[cmp_])
        kb.op("dve", lambda e: e.reduce_sum(out=eid[:, 2:2 + NSLOT], in_=cmp_[:, :, :], axis=AX.X), r=[cmp_], w=[eid])
        ldf = kb.sb("ldf", [128, NSLOT], F32)
        vld = kb.sb("vld", [128, NSLOT], F32)
        wix = kb.sb("wix", [128, NSLOT], F32)
        kb.op("dve", lambda e: e.tensor_tensor(out=ldf[:, :], in0=eid[:, 2:2 + NSLOT], in1=eid[:, 0:NSLOT], op=ALU.not_equal), r=[eid], w=[ldf])
        kb.op("dve", lambda e: e.tensor_scalar(out=vld[:, :], in0=eid[:, 2:2 + NSLOT], scalar1=31.5, scalar2=None, op0=ALU.is_lt), r=[eid], w=[vld])
        kb.op("dve", lambda e: e.tensor_tensor(out=ldf[:, :], in0=ldf[:, :], in1=vld[:, :], op=ALU.mult), r=[ldf, vld], w=[ldf])
        kb.op("dve", lambda e: e.tensor_scalar(out=wix[:, :], in0=eid[:, 2:2 + NSLOT], scalar1=31.0, scalar2=128.0,
                                               op0=ALU.min, op1=ALU.mult), r=[eid], w=[wix])
        kb.op("dve", lambda e: e.tensor_scalar(out=wix[:, :], in0=wix[:, :], scalar1=mc[:, 32:33], scalar2=float(l * 4096), op0=ALU.add,
                                               op1=ALU.add), r=[wix, mc], w=[wix])
        if getattr(C, "moe_skip", False):
            kb.op("dve", lambda e: e.tensor_scalar(out=wix[:, :], in0=wix[:, :], scalar1=-BIGI, scalar2=None, op0=ALU.add), r=[wix], w=[wix])
            kb.op("dve", lambda e: e.tensor_tensor(out=wix[:, :], in0=wix[:, :], in1=ldf[:, :], op=ALU.mult), r=[wix, ldf], w=[wix])
            kb.op("dve", lambda e: e.tensor_scalar(out=wix[:, :], in0=wix[:, :], scalar1=BIGI, scalar2=None, op0=ALU.add), r=[wix], w=[wix])
        kb.op("dve", lambda e: e.tensor_copy(out=C.widx[:, :], in_=wix[:, :]), r=[wix], w=[C.widx])
        if hasattr(C, "dbgD"):
            kb.dma("sp", C.dbgD["widx"], C.widx[:, :], r=[C.widx], w=[kb.tag("dbg6")])
            kb.dma("sp", C.dbgD["eid"], eid[:, :], r=[eid], w=[kb.tag("dbg3")])


def phaseE(C, l, nslot=None, lvl=9):
    nslot = NSLOT if nslot is None else nslot
    kb = C.kb
    ps = C.ps
    with kb.phase():
        Wgu = [kb.sb("Wgu", [128, 8 * 1024], BF16) for _ in range(2)]
        Wd = [kb.sb("Wd", [128, 4 * 1024], BF16) for _ in range(2)]
        for a in range(2):
            kb.op("pool", lambda e: e.memset(Wgu[a][:, :], 0.0), w=[Wgu[a]])
            kb.op("pool", lambda e: e.memset(Wd[a][:, :], 0.0), w=[Wd[a]])
        entb = [kb.sb("ente", [128, 4], I32) for _ in range(2)]
        xsb = [kb.sb("xs", [128, D], BF16) for _ in range(2)]
        xsT = kb.sb("xsT", [128, 8, 128], BF16)
        actf = kb.sb("actf", [128, 512], F32)
        actb = kb.sb("actb", [128, 512], BF16)
        actT = kb.sb("actT", [128, 4, 128], BF16)
        ywb = [kb.sb("yw", [128, D], F32) for _ in range(2)]
        if not hasattr(C, "bc_reg"):
            C.bc_reg = C.nc.gpsimd.to_reg(DEPTH * 4096 - 1)
        wgu_v = C.wgu[l].rearrange("r (a c) -> r a c", c=2048)
        wd_v = C.wd[l].rearrange("r (a c) -> r a c", c=2048)

        def loads(s):
            A = s % 2
            ent = entb[A]
            msk = getattr(C, "ldmask", 15)
            kb.dma("sp", ent[:, :], C.LIST[s * 128:(s + 1) * 128, :], w=[ent])
            extra = dict(bounds_check=C.bc_reg, oob_is_err=False) if (msk & 8) else {}
            if msk & 1:
                kb.dma("pool", Wgu[A][:, :], C.wgu.rearrange("l r c -> (l r) c"), r=[C.widx], w=[Wgu[A]],
                       indirect=dict(out_offset=None, in_offset=bass.IndirectOffsetOnAxis(ap=C.widx[:, s:s + 1], axis=0), **extra))
            if msk & 2:
                kb.dma("pool", Wd[A][:, :], C.wd.rearrange("l r c -> (l r) c"), r=[C.widx], w=[Wd[A]],
                       indirect=dict(out_offset=None, in_offset=bass.IndirectOffsetOnAxis(ap=C.widx[:, s:s + 1], axis=0), **extra))
            if msk & 4:
                kb.dma("pool", xsb[A][:, :], C.F, r=[ent], w=[xsb[A]],
                       indirect=dict(out_offset=None, in_offset=bass.IndirectOffsetOnAxis(ap=ent[:, 0:1], axis=0)))

        loads(0)
        for s in range(nslot):
            A = s % 2
            ent, xs, yw = entb[A], xsb[A], ywb[A]
            if s + 1 < nslot:
                loads(s + 1)
            if lvl < 1:
                continue
            pT = ps[0]
            pv = pT[:, :].bitcast(BF16)
            for k in range(8):
                kb.op("pe", lambda e: e.transpose(pv[:, k * 128:(k + 1) * 128], xs[:, k * 128:(k + 1) * 128], C.identb_s[:, :]),
                      r=[xs, C.identb_s], w=[pT])
            kb.op("dve", lambda e: e.tensor_copy(out=xsT[:, :, :].rearrange("p k t -> p (k t)"), in_=pv[:, :]), r=[pT], w=[xsT])
            pG, pU = ps[1], ps[2]
            for nb, pp in enumerate((pG, pU)):
                for k in range(8):
                    kb.op("pe", lambda e: e.matmul(pp[:, :], lhsT=xsT[:, k, :], rhs=Wgu[A][:, k * 1024 + nb * 512:k * 1024 + (nb + 1) * 512],
                                                   start=(k == 0), stop=(k == 7)), r=[xsT, Wgu[A]], w=[pp])
            kb.op("act", lambda e: e.activation(out=actf[:, :], in_=pG[:, :], func=AF.Silu), r=[pG], w=[actf])
            kb.op("dve", lambda e: e.tensor_tensor(out=actb[:, :], in0=pU[:, :], in1=actf[:, :], op=ALU.mult), r=[pU, actf], w=[actb])
            pA = ps[3]
            pav = pA[:, :].bitcast(BF16)
            for c in range(4):
                kb.op("pe", lambda e: e.transpose(pav[:, c * 128:(c + 1) * 128], actb[:, c * 128:(c + 1) * 128], C.identb_s[:, :]),
                      r=[actb, C.identb_s], w=[pA])
            kb.op("act", lambda e: e.copy(out=actT[:, :, :].rearrange("p k t -> p (k t)"), in_=pav[:, 0:512]), r=[pA], w=[actT])
            pY = [ps[4 + 2 * (s % 2)], ps[5 + 2 * (s % 2)]]
            for nb in range(2):
                for c in range(4):
                    kb.op("pe", lambda e: e.matmul(pY[nb][:, :], lhsT=actT[:, c, :], rhs=Wd[A][:, c * 1024 + nb * 512:c * 1024 + (nb + 1) * 512],
                                                   start=(c == 0), stop=(c == 3)), r=[actT, Wd[A]], w=[pY[nb]])
            if lvl < 2:
                continue
            wcol = ent[:, 2:3].bitcast(F32)
            kb.op("dve", lambda e: e.tensor_scalar(out=yw[:, 0:512], in0=pY[0][:, :], scalar1=wcol, scalar2=None, op0=ALU.mult),
                  r=[pY[0], ent], w=[yw])
            kb.op("act", lambda e: e.activation(out=yw[:, 512:1024], in_=pY[1][:, :], func=AF.Copy, scale=wcol), r=[pY[1], ent], w=[yw])
            if lvl < 3:
                continue
            kb.dma("pool", C.YB, yw[:, :], r=[yw, ent], w=[kb.tag("YBs", s)],
                   indirect=dict(out_offset=bass.IndirectOffsetOnAxis(ap=ent[:, 1:2], axis=0), in_offset=None))


def combine_src(C, l_prev, store=True):
    kb = C.kb
    Gt = kb.sb("Gc", [128, 2048], F32)
    kb.dma("sp", Gt[:, 0:1024], C.G[l_prev][:, 1024:2048], r=[kb.tag("G", l_prev)], w=[Gt])
    kb.dma("sp", Gt[:, 1024:2048], C.G[l_prev][:, 3072:4096], r=[kb.tag("G", l_prev)], w=[Gt])
    y1b = [kb.sb("y1", [128, D], F32) for _ in range(2)]
    y2b = [kb.sb("y2", [128, D], F32) for _ in range(2)]
    cnt = [0]

    def src(t, xt):
        n_ = cnt[0]
        cnt[0] += 1
        y1, y2 = y1b[n_ % 2], y2b[n_ % 2]
        kb.dma("sp", xt[:, :], C.X[t * 128:(t + 1) * 128, :], r=[kb.tag("X", t)], w=[xt])
        kb.dma("sp", y1[:, :], C.YB[t * 128:(t + 1) * 128, :], w=[y1])
        kb.dma("sp", y2[:, :], C.YB[TPAD + t * 128:TPAD + (t + 1) * 128, :], w=[y2])
        g0 = 0 if t >= 2 else 1024
        kb.op("pool", lambda e: e.tensor_tensor(out=y1[:, :], in0=y1[:, :], in1=y2[:, :], op=ALU.add), r=[y1, y2], w=[y1])
        kb.op("dve", lambda e: e.tensor_tensor(out=y1[:, :], in0=y1[:, :], in1=Gt[:, g0:g0 + 1024], op=ALU.mult), r=[y1, Gt], w=[y1])
        kb.op("pool", lambda e: e.tensor_tensor(out=xt[:, :], in0=xt[:, :], in1=y1[:, :], op=ALU.add), r=[xt, y1], w=[xt])
        if store:
            kb.dma("sp", C.X[t * 128:(t + 1) * 128, :], xt[:, :], r=[xt], w=[kb.tag("X", t)])
    return src


TZ = T + 4


def zbase(t):
    return 1 + t * 128 if t < 2 else 259 + (t - 2) * 128


def dn_consts():
    p = np.arange(128)
    same = (p[:, None] // 64) == (p[None, :] // 64)
    mge = (same & (p[None, :] >= p[:, None])).astype(np.float32)
    mle = (same & (p[None, :] <= p[:, None])).astype(np.float32)
    slt = (same & (p[None, :] < p[:, None])).astype(np.float32)
    sgt = (same & (p[None, :] > p[:, None])).astype(np.float32)
    return np.ascontiguousarray(np.concatenate([mge, mle, slt, sgt], axis=1), np.float32)


def phaseA_odd(C, l, tiles, x_src):
    kb = C.kb
    i = l // 2
    ps = C.ps
    with kb.phase():
        W = kb.sb("Wino", [128, 8, ODD_IN], BF16)
        wv = C.od_w_in[i].rearrange("(k p) c -> p k c", p=128)
        for k in range(8):
            kb.dma("pool", W[:, k, :], wv[:, k, :], w=[W])
        gs1, sh_blk = layer_mod_tables(C, l, 0)
        prm = kb.sb("dnprm", [128, 32], F32)
        kb.dma("sp", prm[:, :], C.od_prm[i].partition_broadcast(128), w=[prm])
        kb.op("act", lambda e: e.activation(out=prm[:, 0:16], in_=prm[:, 0:16], func=AF.Exp), r=[prm], w=[prm])
        kb.op("dve", lambda e: e.tensor_scalar(out=prm[:, 0:16], in0=prm[:, 0:16], scalar1=-1.0, scalar2=None, op0=ALU.mult), r=[prm], w=[prm])
        zrow = kb.sb("zrow", [4, 3072], BF16)
        kb.op("dve", lambda e: e.memset(zrow[:, :], 0.0), w=[zrow])
        for n_, r0 in enumerate((0, 257, 258, TZ - 1)):
            kb.dma("sp", C.ZP[r0:r0 + 1, :], zrow[0:1, :], r=[zrow], w=[kb.tag("ZPz", n_)])
        xb = [kb.sb("xt", [128, D], F32) for _ in range(2)]
        junk = kb.sb("junk", [128, D], BF16)
        xn = kb.sb("xn", [128, D], BF16)
        hT = [kb.sb("hT", [128, 8, 128], BF16) for _ in range(2)]
        zt = [kb.sb("zt", [128, 4096], BF16) for _ in range(2)]
        gb = [kb.sb("gb", [128, 32], F32) for _ in range(2)]
        ss = kb.sb("ss", [128, 1], F32)
        rstd = kb.sb("rstd", [128, 1], F32)
        tmp1 = kb.sb("tmp1", [128, 8], F32)
        t16 = kb.sb("t16", [128, 16], F32)
        for n_, t in enumerate(tiles):
            r_ = 0 if t >= 2 else 1
            xt = xb[n_ % 2]
            h = hT[n_ % 2]
            z = zt[n_ % 2]
            g = gb[n_ % 2]
            x_src(t, xt)
            norm_tile(C, xt, ss, rstd, tmp1, junk, xn)
            psT = ps[0]
            pv = psT[:, :].bitcast(BF16)
            for k in range(8):
                kb.op("pe", lambda e: e.transpose(pv[:, k * 128:(k + 1) * 128], xn[:, k * 128:(k + 1) * 128], C.identb_s[:, :]),
                      r=[xn, C.identb_s], w=[psT])
            for k in range(8):
                kb.op("act", lambda e: e.activation(out=h[:, k, :], in_=pv[:, k * 128:(k + 1) * 128], func=AF.Identity,
                                                    scale=gs1[:, k, r_:r_ + 1], bias=C.modT[:, l, sh_blk + k, r_:r_ + 1]),
                      r=[psT, gs1, C.modT], w=[h])
            for j in range(9):
                c0 = j * 512
                nc_ = 512 if j < 8 else 32
                pj = ps[1 + (j % 6)]
                for k in range(8):
                    kb.op("pe", lambda e: e.matmul(pj[:, 0:nc_], lhsT=h[:, k, :], rhs=W[:, k, c0:c0 + nc_],
                                                   start=(k == 0), stop=(k == 7)), r=[h, W], w=[pj])
                if j < 6:
                    if j % 2 == 0:
                        kb.op("act", lambda e: e.copy(out=z[:, c0:c0 + 512], in_=pj[:, :]), r=[pj], w=[z])
                    else:
                        kb.op("dve", lambda e: e.tensor_copy(out=z[:, c0:c0 + 512], in_=pj[:, :]), r=[pj], w=[z])
                elif j < 8:
                    kb.op("act", lambda e: e.activation(out=z[:, c0:c0 + 512], in_=pj[:, :], func=AF.Silu), r=[pj], w=[z])
                else:
                    kb.op("dve", lambda e: e.tensor_tensor(out=t16[:, :], in0=pj[:, 0:16], in1=prm[:, 16:32], op=ALU.add), r=[pj, prm], w=[t16])
                    kb.op("act", lambda e: e.activation(out=t16[:, :], in_=t16[:, :], func=AF.Exp), r=[t16], w=[t16])
                    kb.op("act", lambda e: e.activation(out=t16[:, :], in_=t16[:, :], func=AF.Ln, bias=1.0, scale=1.0), r=[t16], w=[t16])
                    kb.op("dve", lambda e: e.tensor_tensor(out=g[:, 0:16], in0=t16[:, :], in1=prm[:, 0:16], op=ALU.mult), r=[t16, prm], w=[g])
                    kb.op("act", lambda e: e.activation(out=g[:, 16:32], in_=pj[:, 16:32], func=AF.Exp, scale=-1.0), r=[pj], w=[g])
                    kb.op("dve", lambda e: e.tensor_scalar(out=g[:, 16:32], in0=g[:, 16:32], scalar1=1.0, scalar2=None, op0=ALU.add), r=[g], w=[g])
                    kb.op("dve", lambda e: e.reciprocal(out=g[:, 16:32], in_=g[:, 16:32]), r=[g], w=[g])
            zb = zbase(t)
            kb.dma("sp", C.ZP[zb:zb + 128, :], z[:, 0:3072], r=[z], w=[kb.tag("ZP", t)])
            kb.dma("sp", C.PA[t * 128:(t + 1) * 128, 3072:4096], z[:, 3072:4096], r=[z], w=[kb.tag("PAz", t)])
            kb.dma("sp", C.GB[t * 128:(t + 1) * 128, :], g[:, :], r=[g], w=[kb.tag("GB", t)])
    with kb.phase():
        cw = kb.sb("convw", [128, 3, 3072], F32)
        for j in range(3):
            kb.dma("sp", cw[:, j, :], C.od_conv[i, j].partition_broadcast(128), w=[cw])
        zmb = [kb.sb("zm", [128, 3, 3072], BF16) for _ in range(2)]
        accb = [kb.sb("acc", [128, 3072], F32) for _ in range(2)]
        acc2b = [kb.sb("acc2", [128, 3072], F32) for _ in range(2)]
        sqb = [kb.sb("sqd", [128, 2048], F32) for _ in range(2)]
        ssh = kb.sb("sshd", [128, 16], F32)
        rs = kb.sb("rsd", [128, 16], F32)
        t16b = kb.sb("t16b", [128, 16], F32)
        outb = [kb.sb("qkvo", [128, 3072], BF16) for _ in range(2)]
        for n_, t in enumerate(tiles):
            zm = zmb[n_ % 2]
            ob = outb[n_ % 2]
            acc, acc2, sq = accb[n_ % 2], acc2b[n_ % 2], sqb[n_ % 2]
            zb = zbase(t)
            deps = [kb.tag("ZP", tt) for tt in (t - 1, t, t + 1) if 0 <= tt < NT and tt in tiles] + [kb.tag("ZPz", q) for q in range(4)]
            for j in range(3):
                kb.dma("sp", zm[:, j, :], C.ZP[zb - 1 + j:zb + 127 + j, :], r=deps, w=[zm])
            kb.op("dve", lambda e: e.tensor_tensor(out=acc[:, :], in0=zm[:, 0, :], in1=cw[:, 0, :], op=ALU.mult), r=[zm, cw], w=[acc])
            kb.op("pool", lambda e: e.tensor_tensor(out=acc2[:, :], in0=zm[:, 1, :], in1=cw[:, 1, :], op=ALU.mult), r=[zm, cw], w=[acc2])
            kb.op("dve", lambda e: e.tensor_tensor(out=acc[:, :], in0=acc[:, :], in1=acc2[:, :], op=ALU.add), r=[acc, acc2], w=[acc])
            kb.op("pool", lambda e: e.tensor_tensor(out=acc2[:, :], in0=zm[:, 2, :], in1=cw[:, 2, :], op=ALU.mult), r=[zm, cw], w=[acc2])
            kb.op("dve", lambda e: e.tensor_tensor(out=acc[:, :], in0=acc[:, :], in1=acc2[:, :], op=ALU.add), r=[acc, acc2], w=[acc])
            kb.op("act", lambda e: e.activation(out=acc[:, :], in_=acc[:, :], func=AF.Silu), r=[acc], w=[acc])
            kb.op("act", lambda e: e.activation(out=sq[:, :], in_=acc[:, 0:2048], func=AF.Square), r=[acc], w=[sq])
            kb.op("dve", lambda e: e.reduce_sum(out=ssh[:, :], in_=sq[:, :].rearrange("p (h d) -> p h d", h=16), axis=AX.X), r=[sq], w=[ssh])
            rsqrt_mean(C, rs, ssh, 16, 1.0, t16b)
            kb.op("dve", lambda e: e.tensor_scalar(out=rs[:, 0:8], in0=rs[:, 0:8], scalar1=float(128 ** -0.5), scalar2=None, op0=ALU.mult),
                  r=[rs], w=[rs])
            kb.op("dve", lambda e: e.tensor_tensor(out=ob[:, 0:2048].rearrange("p (h d) -> p h d", h=16),
                                                   in0=acc[:, 0:2048].rearrange("p (h d) -> p h d", h=16),
                                                   in1=rs[:, :].unsqueeze(2).to_broadcast([128, 16, 128]), op=ALU.mult), r=[acc, rs], w=[ob])
            kb.op("pool", lambda e: e.tensor_copy(out=ob[:, 2048:3072], in_=acc[:, 2048:3072]), r=[acc], w=[ob])
            kb.dma("sp", C.PA[t * 128:(t + 1) * 128, 0:3072], ob[:, :], r=[ob], w=[kb.tag("PA", t)])


def phaseB_odd(C, l, n_ctx_tiles=2, lat_tiles=None, dirs=(0, 1)):
    kb = C.kb
    ps = C.ps
    if lat_tiles is None:
        lat_tiles = list(range(2, NT))
    with kb.phase():
        dc = kb.sb("dc", [128, 512], F32)
        kb.dma("sp", dc[:, :], C.dconst, w=[dc])
        MGE, MLE, SLT, SGT = [dc[:, q * 128:(q + 1) * 128] for q in range(4)]
        idf = kb.sb("idfb", [128, 128], F32)
        kb.op("dve", lambda e: e.tensor_copy(out=idf[:, :], in_=C.identb_s[:, :]), r=[C.identb_s], w=[idf])
        qkvb = [kb.sb("qkvd", [128, 3072], BF16) for _ in range(2)]
        gbb = [kb.sb("gbd", [128, 32], F32) for _ in range(2)]
        gcs = kb.sb("gcs", [128, 8], F32)
        egc = kb.sb("egc", [128, 8], F32)
        kbt = kb.sb("kbt", [128, 8, 128], BF16)
        vbt = kb.sb("vbt", [128, 8, 128], BF16)
        kbg = kb.sb("kbg", [128, 8, 128], BF16)
        qgt = kb.sb("qgt", [128, 8, 128], BF16)
        kdt = kb.sb("kdt", [128, 8, 128], BF16)
        kds = kb.sb("kds", [128, 8], F32)
        qT = kb.sb("qTd", [128, 8, 128], BF16)
        kT = kb.sb("kTd", [128, 8, 128], BF16)
        qgT = kb.sb("qgT", [128, 8, 128], BF16)
        wT = kb.sb("wTd", [128, 8, 128], BF16)
        attT = kb.sb("attTd", [128, 8, 128], BF16)
        uu = kb.sb("uu", [128, 8, 128], F32)
        glc = kb.sb("glc", [128, 8, 2], F32)
        grep_ = [kb.sb("grep", [128, 128], F32) for _ in range(8)]
        d1 = [kb.sb("d1", [128, 128], F32) for _ in range(8)]
        d2 = [kb.sb("d2", [128, 128], F32) for _ in range(8)]
        e1m = [kb.sb("e1m", [128, 128], F32) for _ in range(8)]
        e2m = [kb.sb("e2m", [128, 128], F32) for _ in range(8)]
        Lb = [[kb.sb("Lb", [128, 128], BF16) for _ in range(2)] for _ in range(8)]
        Ub = [[kb.sb("Ub", [128, 128], BF16) for _ in range(2)] for _ in range(8)]
        ILb = [kb.sb("ILb", [128, 128], BF16) for _ in range(8)]
        Mb = [[kb.sb("Mb", [128, 128], BF16) for _ in range(2)] for _ in range(8)]
        vn = kb.sb("vn", [128, 8, 128], BF16)
        S = kb.sb("Sd", [128, 8, 128], F32)
        Sb = kb.sb("Sbd", [128, 8, 128], BF16)
        osb = [kb.sb("osb", [128, D], F32) for _ in range(2)]
        n_ = 0
        for d_ in dirs:
            order = list(range(n_ctx_tiles)) + list(lat_tiles)
            if d_ == 1:
                order = list(range(n_ctx_tiles))[::-1] + list(lat_tiles)[::-1]
            CUM = MGE if d_ == 0 else MLE
            M_att = MGE if d_ == 0 else MLE
            M_lo = SLT if d_ == 0 else SGT
            chunks = (0, 1) if d_ == 0 else (1, 0)
            flast = (lambda c: c * 64 + 63) if d_ == 0 else (lambda c: c * 64)
            ODST = C.OF if d_ == 0 else C.OB
            otag = "OF" if d_ == 0 else "OB"
            kb.op("dve", lambda e: e.memset(S[:, :, :], 0.0), w=[S])
            kb.op("dve", lambda e: e.memset(Sb[:, :, :], 0.0), w=[Sb])
            for t in order:
                qkv = qkvb[n_ % 2]
                gbt = gbb[n_ % 2]
                ot = osb[n_ % 2]
                n_ += 1
                kb.dma("sp", qkv[:, :], C.PA[t * 128:(t + 1) * 128, 0:3072], r=[kb.tag("PA", t)], w=[qkv])
                kb.dma("sp", gbt[:, :], C.GB[t * 128:(t + 1) * 128, :], r=[kb.tag("GB", t)], w=[gbt])
                gcol = gbt[:, d_ * 8:(d_ + 1) * 8]
                bcol = gbt[:, 16 + d_ * 8:16 + (d_ + 1) * 8]
                pg = ps[6]
                kb.op("pe", lambda e: e.matmul(pg[:, 0:8], lhsT=CUM, rhs=gcol, start=True, stop=True), r=[dc, gbt], w=[pg])
                kb.op("dve", lambda e: e.tensor_copy(out=gcs[:, :], in_=pg[:, 0:8]), r=[pg], w=[gcs])
                kb.op("act", lambda e: e.activation(out=egc[:, :], in_=gcs[:, :], func=AF.Exp), r=[gcs], w=[egc])
                qv = qkv[:, 0:1024].rearrange("p (h d) -> p h d", h=8)
                kv = qkv[:, 1024:2048].rearrange("p (h d) -> p h d", h=8)
                vv = qkv[:, 2048:3072].rearrange("p (h d) -> p h d", h=8)
                bb = bcol.unsqueeze(2).to_broadcast([128, 8, 128])
                eb = egc[:, :].unsqueeze(2).to_broadcast([128, 8, 128])
                kb.op("dve", lambda e: e.tensor_tensor(out=kbt[:, :, :], in0=kv, in1=bb, op=ALU.mult), r=[qkv, gbt], w=[kbt])
                kb.op("pool", lambda e: e.tensor_tensor(out=vbt[:, :, :], in0=vv, in1=bb, op=ALU.mult), r=[qkv, gbt], w=[vbt])
                kb.op("dve", lambda e: e.tensor_tensor(out=kbg[:, :, :], in0=kbt[:, :, :], in1=eb, op=ALU.mult), r=[kbt, egc], w=[kbg])
                kb.op("pool", lambda e: e.tensor_tensor(out=qgt[:, :, :], in0=qv, in1=eb, op=ALU.mult), r=[qkv, egc], w=[qgt])
                for (src_ap, src_tl, dst) in ((qkv[:, 0:1024], qkv, qT), (qkv[:, 1024:2048], qkv, kT), (qgt[:, :, :].rearrange("p h d -> p (h d)"), qgt, qgT)):
                    pt = ps[7]
                    ptv = pt[:, :].bitcast(BF16)
                    for h in range(8):
                        kb.op("pe", lambda e: e.transpose(ptv[:, h * 128:(h + 1) * 128], src_ap[:, h * 128:(h + 1) * 128], C.identb_s[:, :]),
                              r=[src_tl, C.identb_s], w=[pt])
                    kb.op("act", lambda e: e.copy(out=dst[:, :, :].rearrange("p h t -> p (h t)"), in_=ptv[:, :]), r=[pt], w=[dst])
                def reg(h, j):
                    c = h * 3 + j
                    return ps[c // 4], slice((c % 4) * 128, (c % 4 + 1) * 128)
                for h in range(8):
                    kb.op("dve", lambda e: e.tensor_copy(out=grep_[h][:, :], in_=gcol[:, h:h + 1].to_broadcast([128, 128])), r=[gbt], w=[grep_[h]])
                for h in range(8):
                    (b0, c0), (b1, c1), (b2, c2) = reg(h, 0), reg(h, 1), reg(h, 2)
                    kb.op("pe", lambda e: e.matmul(b0[:, c0], lhsT=grep_[h][:, :], rhs=CUM, start=True, stop=True), r=[grep_[h], dc], w=[b0])
                    kb.op("pe", lambda e: e.matmul(b1[:, c1], lhsT=kT[:, h, :], rhs=kT[:, h, :], start=True, stop=True), r=[kT], w=[b1])
                    kb.op("pe", lambda e: e.matmul(b2[:, c2], lhsT=kT[:, h, :], rhs=qT[:, h, :], start=True, stop=True), r=[kT, qT], w=[b2])
                for h in range(8):
                    b0, c0 = reg(h, 0)
                    gch = gcs[:, h:h + 1]
                    kb.op("dve", lambda e: e.tensor_scalar(out=d1[h][:, :], in0=b0[:, c0], scalar1=gch, scalar2=0.0, op0=ALU.subtract, op1=ALU.min),
                          r=[b0, gcs], w=[d1[h]])
                    kb.op("dve", lambda e: e.tensor_scalar(out=d2[h][:, :], in0=b0[:, c0], scalar1=gch, scalar2=0.0, op0=ALU.subtract, op1=ALU.max),
                          r=[b0, gcs], w=[d2[h]])
                for h in range(8):
                    b0, c0 = reg(h, 0)
                    for c in (0, 1):
                        f = flast(c)
                        kb.op("act", lambda e: e.activation(out=glc[:, h, c:c + 1], in_=b0[:, c0.start + f:c0.start + f + 1], func=AF.Exp), r=[b0], w=[glc])
                    kb.op("act", lambda e: e.activation(out=d1[h][:, :], in_=d1[h][:, :], func=AF.Exp), r=[d1[h]], w=[d1[h]])
                    kb.op("act", lambda e: e.activation(out=d2[h][:, :], in_=d2[h][:, :], func=AF.Exp, scale=-1.0), r=[d2[h]], w=[d2[h]])
                f0, f1 = flast(0), flast(1)
                for h in range(8):
                    kb.op("pool", lambda e: e.tensor_tensor(out=e1m[h][:, :], in0=d1[h][:, :], in1=M_att, op=ALU.mult), r=[d1[h], dc], w=[e1m[h]])
                    kb.op("pool", lambda e: e.tensor_tensor(out=e2m[h][:, :], in0=d2[h][:, :], in1=M_lo, op=ALU.mult), r=[d2[h], dc], w=[e2m[h]])
                for h in range(8):
                    (b1, c1), (b2, c2) = reg(h, 1), reg(h, 2)
                    kb.op("dve", lambda e: e.tensor_tensor(out=kds[:, h:h + 1], in0=e1m[h][:, f0:f0 + 1], in1=e1m[h][:, f1:f1 + 1], op=ALU.add),
                          r=[e1m[h]], w=[kds])
                    kb.op("dve", lambda e: e.tensor_tensor(out=attT[:, h, :], in0=b2[:, c2], in1=e1m[h][:, :], op=ALU.mult), r=[b2, e1m[h]], w=[attT])
                    kb.op("dve", lambda e: e.scalar_tensor_tensor(out=Lb[h][0][:, :], in0=b1[:, c1], scalar=bcol[:, h:h + 1], in1=e2m[h][:, :],
                                                                  op0=ALU.mult, op1=ALU.mult), r=[b1, gbt, e2m[h]], w=[Lb[h][0]])
                for h in range(8):
                    b0, c0 = reg(h, 0)
                    utv = b0[:, c0].bitcast(BF16)
                    kb.op("pe", lambda e: e.transpose(utv[:, 0:128], Lb[h][0][:, :], C.identb_s[:, :]), r=[Lb[h][0], C.identb_s], w=[b0])
                for h in range(8):
                    b0, c0 = reg(h, 0)
                    utv = b0[:, c0].bitcast(BF16)
                    kb.op("act", lambda e: e.copy(out=Ub[h][0][:, :], in_=utv[:, 0:128]), r=[b0], w=[Ub[h][0]])
                    kb.op("pool", lambda e: e.tensor_tensor(out=Mb[h][0][:, :], in0=idf[:, :], in1=Ub[h][0][:, :], op=ALU.subtract),
                          r=[idf, Ub[h][0]], w=[Mb[h][0]])
                cur = 0
                for lev in range(5):
                    nxt = 1 - cur
                    for h in range(8):
                        (b0, c0), (b1, c1) = reg(h, 0), reg(h, 1)
                        kb.op("pe", lambda e: e.matmul(b0[:, c0], lhsT=Ub[h][cur][:, :], rhs=Lb[h][cur][:, :], start=True, stop=True),
                              r=[Ub[h][cur], Lb[h][cur]], w=[b0])
                        if lev < 4:
                            kb.op("pe", lambda e: e.matmul(b1[:, c1], lhsT=Lb[h][cur][:, :], rhs=Ub[h][cur][:, :], start=True, stop=True),
                                  r=[Ub[h][cur], Lb[h][cur]], w=[b1])
                    for h in range(8):
                        (b0, c0), (b1, c1) = reg(h, 0), reg(h, 1)
                        kb.op("dve", lambda e: e.tensor_tensor(out=ILb[h][:, :], in0=b0[:, c0], in1=idf[:, :], op=ALU.add), r=[b0, idf], w=[ILb[h]])
                        if lev < 4:
                            kb.op("act", lambda e: e.copy(out=Lb[h][nxt][:, :], in_=b0[:, c0]), r=[b0], w=[Lb[h][nxt]])
                            kb.op("act", lambda e: e.copy(out=Ub[h][nxt][:, :], in_=b1[:, c1]), r=[b1], w=[Ub[h][nxt]])
                    for h in range(8):
                        b2, c2 = reg(h, 2)
                        kb.op("pe", lambda e: e.matmul(b2[:, c2], lhsT=ILb[h][:, :], rhs=Mb[h][cur][:, :], start=True, stop=True),
                              r=[ILb[h], Mb[h][cur]], w=[b2])
                    for h in range(8):
                        b2, c2 = reg(h, 2)
                        kb.op("dve", lambda e: e.tensor_copy(out=Mb[h][nxt][:, :], in_=b2[:, c2]), r=[b2], w=[Mb[h][nxt]])
                    cur = nxt
                for h in range(8):
                    (b0, c0), (b1, c1) = reg(h, 0), reg(h, 1)
                    kb.op("pe", lambda e: e.matmul(b0[:, c0], lhsT=Mb[h][cur][:, :], rhs=vbt[:, h, :], start=True, stop=True), r=[Mb[h][cur], vbt], w=[b0])
                    kb.op("pe", lambda e: e.matmul(b1[:, c1], lhsT=kbg[:, h, :], rhs=Mb[h][cur][:, :], start=True, stop=True), r=[Mb[h][cur], kbg], w=[b1])
                for h in range(8):
                    (b0, c0), (b1, c1) = reg(h, 0), reg(h, 1)
                    kb.op("dve", lambda e: e.tensor_copy(out=uu[:, h, :], in_=b0[:, c0]), r=[b0], w=[uu])
                    kb.op("act", lambda e: e.copy(out=wT[:, h, :], in_=b1[:, c1]), r=[b1], w=[wT])
                kb.op("dve", lambda e: e.tensor_tensor(out=kdt[:, :, :], in0=kv, in1=kds[:, :].unsqueeze(2).to_broadcast([128, 8, 128]), op=ALU.mult),
                      r=[qkv, kds], w=[kdt])
                for c in chunks:
                    r0 = c * 64
                    pW = [ps[6], ps[7]]
                    for h in range(8):
                        kb.op("pe", lambda e: e.matmul(pW[h // 4][:, (h % 4) * 128:(h % 4 + 1) * 128], lhsT=wT[:, h, :], rhs=Sb[:, h, :],
                                                       start=True, stop=True), r=[wT, Sb], w=[pW[h // 4]])
                    for hb in range(2):
                        kb.op("dve", lambda e: e.tensor_tensor(out=vn[r0:r0 + 64, hb * 4:(hb + 1) * 4, :].rearrange("p h d -> p (h d)"),
                                                               in0=uu[r0:r0 + 64, hb * 4:(hb + 1) * 4, :].rearrange("p h d -> p (h d)"),
                                                               in1=pW[hb][r0:r0 + 64, :], op=ALU.subtract), r=[uu, pW[hb]], w=[vn])
                    pO = [ps[4], ps[5]]
                    pK = [ps[2], ps[3]]
                    for h in range(8):
                        oc = (h % 4) * 128
                        kb.op("pe", lambda e: e.matmul(pO[h // 4][:, oc:oc + 128], lhsT=qgT[:, h, :], rhs=Sb[:, h, :], start=True, stop=False),
                              r=[qgT, Sb], w=[pO[h // 4]])
                        kb.op("pe", lambda e: e.matmul(pO[h // 4][:, oc:oc + 128], lhsT=attT[r0:r0 + 64, h, :], rhs=vn[r0:r0 + 64, h, :],
                                                       start=False, stop=True), r=[attT, vn], w=[pO[h // 4]])
                        kb.op("pe", lambda e: e.matmul(pK[h // 4][:, oc:oc + 128], lhsT=kdt[r0:r0 + 64, h, :], rhs=vn[r0:r0 + 64, h, :],
                                                       start=True, stop=True), r=[kdt, vn], w=[pK[h // 4]])
                    for hb in range(2):
                        kb.op("act", lambda e: e.copy(out=ot[r0:r0 + 64, hb * 512:(hb + 1) * 512], in_=pO[hb][r0:r0 + 64, :]), r=[pO[hb]], w=[ot])
                    for h in range(8):
                        oc = (h % 4) * 128
                        kb.op("dve", lambda e: e.scalar_tensor_tensor(out=S[:, h, :], in0=S[:, h, :], scalar=glc[:, h, c:c + 1],
                                                                      in1=pK[h // 4][:, oc:oc + 128], op0=ALU.mult, op1=ALU.add),
                              r=[S, glc, pK[h // 4]], w=[S])
                    kb.op("act", lambda e: e.copy(out=Sb[:, :, :].rearrange("p h d -> p (h d)"), in_=S[:, :, :].rearrange("p h d -> p (h d)")),
                          r=[S], w=[Sb])
                kb.dma("sp", ODST[t * 128:(t + 1) * 128, :], ot[:, :], r=[ot], w=[kb.tag(otag, t)])


def odd_mix_src(C, l):
    kb = C.kb
    i = l // 2
    gn = kb.sb("ogain", [128, 128], F32)
    kb.dma("sp", gn[:, :], C.od_gain[i].partition_broadcast(128), w=[gn])
    ofb = [kb.sb("ofo", [128, D], F32) for _ in range(2)]
    obb = [kb.sb("obo", [128, D], F32) for _ in range(2)]
    zgb = [kb.sb("zgo", [128, D], BF16) for _ in range(2)]
    junk = kb.sb("junko", [128, D], F32)
    ssh = kb.sb("ssho", [128, 8], F32)
    rs8 = kb.sb("rs8o", [128, 8], F32)
    tmp8 = kb.sb("tmp8o", [128, 8], F32)
    mr = kb.sb("mro", [128, D], BF16)

    def src(t, mT, n_):
        of, ob, zg = ofb[n_ % 2], obb[n_ % 2], zgb[n_ % 2]
        kb.dma("sp", of[:, :], C.OF[t * 128:(t + 1) * 128, :], r=[kb.tag("OF", t)], w=[of])
        kb.dma("sp", ob[:, :], C.OB[t * 128:(t + 1) * 128, :], r=[kb.tag("OB", t)], w=[ob])
        kb.dma("sp", zg[:, :], C.PA[t * 128:(t + 1) * 128, 3072:4096], r=[kb.tag("PAz", t)], w=[zg])
        kb.op("pool", lambda e: e.tensor_tensor(out=of[:, :], in0=of[:, :], in1=ob[:, :], op=ALU.add), r=[of, ob], w=[of])
        kb.op("act", lambda e: e.activation(out=junk[:, :], in_=of[:, :], func=AF.Square), r=[of], w=[junk])
        kb.op("dve", lambda e: e.reduce_sum(out=ssh[:, :], in_=junk[:, :].rearrange("p (h d) -> p h d", h=8), axis=AX.X), r=[junk], w=[ssh])
        rsqrt_mean(C, rs8, ssh, 8, 1.0 / 128, tmp8)
        kb.op("dve", lambda e: e.tensor_tensor(out=of[:, :].rearrange("p (h d) -> p h d", h=8), in0=of[:, :].rearrange("p (h d) -> p h d", h=8),
                                               in1=rs8[:, :].unsqueeze(2).to_broadcast([128, 8, 128]), op=ALU.mult), r=[of, rs8], w=[of])
        kb.op("dve", lambda e: e.tensor_tensor(out=of[:, :].rearrange("p (h d) -> p h d", h=8), in0=of[:, :].rearrange("p (h d) -> p h d", h=8),
                                               in1=gn[:, :].unsqueeze(1).to_broadcast([128, 8, 128]), op=ALU.mult), r=[of, gn], w=[of])
        kb.op("pool", lambda e: e.tensor_tensor(out=mr[:, :], in0=of[:, :], in1=zg[:, :], op=ALU.mult), r=[of, zg], w=[mr])
        pT = C.ps[0]
        pv = pT[:, :].bitcast(BF16)
        for k in range(8):
            kb.op("pe", lambda e: e.transpose(pv[:, k * 128:(k + 1) * 128], mr[:, k * 128:(k + 1) * 128], C.identb_s[:, :]),
                  r=[mr, C.identb_s], w=[pT])
        kb.op("act", lambda e: e.copy(out=mT[:, 0:8, :].rearrange("p k t -> p (k t)"), in_=pv[:, :]), r=[pT], w=[mT])
    return src


def final_phase(C, tiles):
    kb = C.kb
    with kb.phase():
        src = combine_src(C, DEPTH - 1, store=False)
        fn = kb.sb("fnrep", [128, D], F32)
        kb.dma("sp", fn[:, :], C.norms[2 * DEPTH].partition_broadcast(128), w=[fn])
        xb = [kb.sb("xf", [128, D], F32) for _ in range(2)]
        ob = [kb.sb("of_", [128, D], F32) for _ in range(2)]
        junk = kb.sb("junkf", [128, D], BF16)
        ss = kb.sb("ssf", [128, 1], F32)
        rstd = kb.sb("rstdf", [128, 1], F32)
        tmp1 = kb.sb("tmp1f", [128, 8], F32)
        for n_, t in enumerate(tiles):
            xt, o = xb[n_ % 2], ob[n_ % 2]
            src(t, xt)
            norm_tile(C, xt, ss, rstd, tmp1, junk, o)
            kb.op("dve", lambda e: e.tensor_tensor(out=o[:, :], in0=o[:, :], in1=fn[:, :], op=ALU.mult), r=[o, fn], w=[o])
            kb.dma("sp", C.out[(t - 2) * 128:(t - 1) * 128, :], o[:, :], r=[o], w=[kb.tag("out", t)])


def build_program():
    nc = bass.Bass("TRN2", target_bir_lowering=False)
    C = Ctx()
    C.nc = nc
    C.kb = KB(nc)
    C.dbg_out = set()
    C.moe_skip = True
    kb = C.kb
    declare_io(nc, C)
    C.out = nc.dram_tensor("out", [L, D], F32, kind="ExternalOutput").ap()
    setup_globals(C)
    setup_consts(C)
    phase0(C)
    all_tiles = list(range(NT))
    for l in range(DEPTH):
        last = l == DEPTH - 1
        if l == 0:
            def x_src(t, xt):
                kb.dma("sp", xt[:, :], C.xin[t * 128:(t + 1) * 128, :], w=[xt])
                kb.dma("sp", C.X[t * 128:(t + 1) * 128, :], xt[:, :], r=[xt], w=[kb.tag("X", t)])
            holder = None
        else:
            holder = kb.phase()
            holder.__enter__()
            x_src = combine_src(C, l - 1)
        if l % 2 == 0:
            phaseA_even(C, l, all_tiles, x_src)
        else:
            phaseA_odd(C, l, all_tiles, x_src)
        if holder is not None:
            holder.__exit__(None, None, None)
        if l % 2 == 0:
            phaseB_ret(C, l)
            phaseC_att(C, l)
            phaseD(C, l, all_tiles, C.ev_w_out[l // 2], 12, lambda: even_mix_src(C), set(all_tiles))
        else:
            phaseB_odd(C, l)
            tiles_d = all_tiles if not last else all_tiles[2:]
            phaseD(C, l, tiles_d, C.od_w_out[l // 2], 8, lambda: odd_mix_src(C, l), set(tiles_d))
        phaseE(C, l)
    final_phase(C, all_tiles[2:])
    kb.barrier()
    return nc


_CACHE = {}


def kernel(**inputs):
    inp = {k: np.asarray(v) for k, v in inputs.items()}
    maps = host_prep(inp)[:NCORES]
    if "nc" not in _CACHE:
        _CACHE["nc"] = build_program()
    res = run_bass_kernel_spmd(_CACHE["nc"], maps, core_ids=list(range(NCORES)))
    out = np.stack([np.asarray(res.results[b]["out"], np.float32) for b in range(4)], axis=0)
    return out


NCORES = 4
```

```python
import numpy as np
from contextlib import ExitStack
import concourse.bass as bass
import concourse.mybir as mybir
from concourse.bass_utils import run_bass_kernel_spmd

F32 = mybir.dt.float32
BF16 = mybir.dt.bfloat16
I32 = mybir.dt.int32
AF = mybir.ActivationFunctionType
ALU = mybir.AluOpType
AX = mybir.AxisListType

D = 1024
LC = 256
L = 8192
T = LC + L
NT = T // 128
DEPTH = 4
EPS = 1e-6
EVEN_IN = 3840
ODD_IN = 4128


class Buf:
    __slots__ = ("w", "r")

    def __init__(self):
        self.w = None
        self.r = {}


class Tl:
    __slots__ = ("t", "b", "psum")

    def __init__(self, t, psum=False):
        self.t = t
        self.b = Buf()
        self.psum = psum

    def __getitem__(self, k):
        return self.t[k]


class KB:
    NDMA = {"sp": 12, "pool": 12, "act": 4}

    def __init__(self, nc):
        self.nc = nc
        self.es = ExitStack()
        self.eng = {"pe": nc.tensor, "act": nc.scalar, "dve": nc.vector, "pool": nc.gpsimd, "sp": nc.sync}
        self.sem = {}
        self.cnt = {}
        self.seen = {e: {} for e in self.eng}
        for e in self.eng:
            self.sem[e] = self.es.enter_context(nc.semaphore("s_" + e))
            self.cnt[e] = 0
        self.dsem = {}
        self.dval = {}
        self.dnext = {}
        for q, n in self.NDMA.items():
            self.dsem[q] = [self.es.enter_context(nc.semaphore(f"d_{q}{i}")) for i in range(n)]
            self.dval[q] = [0] * n
            self.dnext[q] = 0
        self.tags = {}
        self.ninst = 0
        self.cur = self.es
        self.deferred = []
        self.loads_since_defer = 0

    def semobj(self, key):
        if isinstance(key, tuple):
            return self.dsem[key[0]][key[1]]
        return self.sem[key]

    def tag(self, *key):
        b = self.tags.get(key)
        if b is None:
            b = Tl(None)
            self.tags[key] = b
        return b

    def _deps(self, eng, r, w):
        deps = {}

        def add(ev, raw):
            if ev is None:
                return
            k, v = ev
            if k == eng and (not raw or eng in ("pe", "sp")):
                return
            if deps.get(k, 0) < v:
                deps[k] = v

        for x in r:
            add(x.b.w, True)
        for x in w:
            add(x.b.w, False)
            for k, v in x.b.r.items():
                add((k, v), False)
        return deps

    def _wait(self, eng, deps):
        seen = self.seen[eng]
        e = self.eng[eng]
        for k, v in deps.items():
            if seen.get(k, 0) >= v:
                continue
            e.wait_ge(self.semobj(k), v)
            seen[k] = v
            self.ninst += 1

    def _mark(self, ev, r, w):
        k, v = ev
        for x in r:
            if x.b.r.get(k, 0) < v:
                x.b.r[k] = v
        for x in w:
            x.b.w = ev
            x.b.r = {}

    def op(self, eng, fn, r=(), w=()):
        if self.deferred:
            if self.loads_since_defer > 0:
                self.flush()
            else:
                pr = set(id(x) for d in self.deferred for x in d[3])
                if any(id(x) in pr for x in w):
                    self.flush()
        if any(x.psum for x in r):
            w = list(w) + [x for x in r if x.psum]
            r = [x for x in r if not x.psum]
        self._wait(eng, self._deps(eng, r, w))
        ins = fn(self.eng[eng])
        self.cnt[eng] += 1
        ins.then_inc(self.sem[eng], 1)
        self._mark((eng, self.cnt[eng]), r, w)
        self.ninst += 1
        return ins

    def _is_dram(self, ap):
        return "DRam" in type(ap.tensor).__name__

    def flush(self):
        d, self.deferred = self.deferred, []
        for (q, out, in_, r, w, kw) in d:
            self._dma(q, out, in_, r, w, None, **kw)

    def dma(self, q, out, in_, r=(), w=(), indirect=None, **kw):
        if q == "sp" and indirect is None and self._is_dram(out):
            self.deferred.append((q, out, in_, list(r), list(w), kw))
            self.loads_since_defer = 0
            return None
        if self.deferred:
            if q == "sp":
                self.loads_since_defer += 1
            pw = set(id(x) for d in self.deferred for x in d[4])
            pr = set(id(x) for d in self.deferred for x in d[3])
            if any(id(x) in pw for x in r) or any(id(x) in pw or id(x) in pr for x in w):
                self.flush()
        return self._dma(q, out, in_, r, w, indirect, **kw)

    def _dma(self, q, out, in_, r=(), w=(), indirect=None, **kw):
        deps = self._deps("dma", r, w)
        i = self.dnext[q]
        self.dnext[q] = (i + 1) % len(self.dsem[q])
        key = (q, i)
        if self.dval[q][i] > 0:
            deps[key] = max(deps.get(key, 0), self.dval[q][i])
        self._wait(q, deps)
        e = self.eng[q]
        if indirect is not None:
            ins = e.indirect_dma_start(out=out, in_=in_, **indirect, **kw)
        else:
            ins = e.dma_start(out=out, in_=in_, **kw)
        self.dval[q][i] += 16
        ins.then_inc(self.dsem[q][i], 16)
        self._mark((key, self.dval[q][i]), r, w)
        self.ninst += 1
        return ins

    def barrier(self, engines=("pe", "act", "dve", "pool", "sp")):
        self.flush()
        deps = {e: self.cnt[e] for e in self.eng if self.cnt[e] > 0}
        for q in self.dsem:
            for i, v in enumerate(self.dval[q]):
                if v > 0:
                    deps[(q, i)] = v
        for e in engines:
            d = {k: v for k, v in deps.items() if k != e}
            self._wait(e, d)

    def sb(self, name, shape, dtype):
        self.nalloc = getattr(self, "nalloc", 0) + 1
        return Tl(self.cur.enter_context(self.nc.sbuf_tensor(f"{name}_{self.nalloc}", shape, dtype)))

    def phase(self):
        kb = self

        class _P:
            def __enter__(self_):
                self_.prev = getattr(kb, "cur", kb.es)
                self_.st = ExitStack()
                kb.cur = self_.st
                return self_

            def __exit__(self_, *a):
                if a[0] is None:
                    kb.barrier()
                self_.st.close()
                kb.cur = self_.prev
                return False

        return _P()


class Ctx:
    pass


def declare_io(nc, C):
    def din(name, shape, dt=F32):
        return nc.dram_tensor(name, shape, dt, kind="ExternalInput").ap()
    C.xin = din("xin", [T, D])
    C.cT = din("cT", [128, 16])
    C.normT = din("normT", [128, 72])
    C.badaT = din("badaT", [128, 192])
    C.b_ada = din("b_ada", [DEPTH, 6 * D])
    C.rope = din("rope", [L, 128])
    C.identb = din("identb", [128, 128], BF16)
    C.identf = din("identf", [128, 128])
    C.w_ada = din("w_ada", [DEPTH, D, 6 * D])
    C.ev_w_in = din("ev_w_in", [2, D, EVEN_IN])
    C.ev_qk_gain = din("ev_qk_gain", [2, 128])
    C.ev_decay = din("ev_decay", [2, 16])
    C.ev_w_out = din("ev_w_out", [2, 1536, D])
    C.rconst = din("rconst", [128, 770])
    C.norms = din("norms", [9, D])
    C.od_w_in = din("od_w_in", [2, D, ODD_IN])
    C.od_prm = din("od_prm", [2, 32])
    C.od_conv = din("od_conv", [2, 3, 3072])
    C.od_gain = din("od_gain", [2, 128])
    C.od_w_out = din("od_w_out", [2, D, D])
    C.dconst = din("dconst", [128, 512])
    C.wr = din("wr", [DEPTH, D, 36])
    C.br = din("br", [DEPTH, 36])
    C.mconst = din("mconst", [128, 33 + NSLOT])
    C.triones = din("triones", [128, 256], BF16)
    C.srcidx = din("srcidx", [128, NT], I32)
    C.listinit = din("listinit", [128, (NSLOT + 1) * 4], I32)
    C.wgu = din("wgu", [DEPTH, 32 * 128, 8 * 1024])
    C.wd = din("wd", [DEPTH, 32 * 128, 4 * 1024])


def setup_globals(C):
    kb, nc = C.kb, C.nc
    C.ps = [Tl(kb.es.enter_context(nc.psum_tensor(f"ps{i}", [128, 512], F32)), psum=True) for i in range(8)]
    C.modT = kb.sb("modT", [128, DEPTH, 48, 2], F32)
    C.normTs = kb.sb("normTs", [128, 72], F32)
    C.identb_s = kb.sb("identb_s", [128, 128], BF16)
    C.identf_s = kb.sb("identf_s", [128, 128], F32)
    kb.dma("sp", C.normTs[:, :], C.normT, w=[C.normTs])
    kb.dma("sp", C.identb_s[:, :], C.identb, w=[C.identb_s])
    kb.dma("sp", C.identf_s[:, :], C.identf, w=[C.identf_s])
    def dscr(name, shape, dt=F32):
        kind = "ExternalOutput" if name in C.dbg_out else "Internal"
        return nc.dram_tensor(name, shape, dt, kind=kind).ap()
    C.G = dscr("G", [DEPTH, 128, 8192])
    C.X = dscr("X", [T, D])
    C.PA = dscr("PA", [T, ODD_IN], BF16)
    C.OF = dscr("OF", [T, D])
    C.MIX = dscr("MIX", [T, D], BF16)
    C.AT = dscr("AT", [512, T], BF16)
    C.F = dscr("F", [T, D], BF16)
    C.ZP = dscr("ZP", [TZ, 3072], BF16)
    C.GB = dscr("GB", [T, 32])
    C.OB = dscr("OB", [T, D])
    C.LIST = dscr("LIST", [(NSLOT + 1) * 128, 4], I32)
    C.YB = dscr("YB", [2 * TPAD, D])
    C.widx = kb.sb("widx", [128, NSLOT], I32)


def phase0(C):
    kb = C.kb
    ps0, ps1 = C.ps[0], C.ps[1]
    with kb.phase():
        cT = kb.sb("cT", [128, 16], F32)
        sc = kb.sb("sc", [128, 16], F32)
        screp = kb.sb("screp", [128, 16, 128], F32)
        badaT = kb.sb("badaT", [128, 192], F32)
        brep = kb.sb("brep", [128, 4096], F32)
        gout = kb.sb("gout", [128, 8192], F32)
        wblk = [kb.sb("wada", [128, 8, 512], F32) for _ in range(2)]
        kb.dma("sp", cT[:, :], C.cT, w=[cT])
        kb.dma("sp", badaT[:, :], C.badaT, w=[badaT])
        kb.op("act", lambda e: e.activation(out=sc[:, :], in_=cT[:, :], func=AF.Silu), r=[cT], w=[sc])
        kb.op("dve", lambda e: e.tensor_copy(out=screp[:, :, :], in_=sc[:, :].unsqueeze(2).to_broadcast([128, 16, 128])),
              r=[sc], w=[screp])
        for l in range(DEPTH):
            for gi, c0 in enumerate((2048, 5120, 3072, 4096)):
                kb.dma("sp", brep[:, gi * 1024:(gi + 1) * 1024], C.b_ada[l, c0:c0 + 1024].partition_broadcast(128), w=[brep])
            wv = C.w_ada[l].rearrange("(k p) c -> p k c", p=128)
            for j in range(12):
                wb = wblk[j % 2]
                kb.dma("sp", wb[:, :, :], wv[:, :, j * 512:(j + 1) * 512], w=[wb])
                for c4 in range(4):
                    for k in range(8):
                        kb.op("pe", lambda e: e.matmul(ps0[:, c4 * 2:(c4 + 1) * 2], lhsT=wb[:, k, c4 * 128:(c4 + 1) * 128],
                                                       rhs=sc[:, 2 * k:2 * k + 2], start=(k == 0), stop=(k == 7)),
                              r=[wb, sc], w=[ps0])
                kb.op("dve", lambda e: e.tensor_tensor(
                    out=C.modT[:, l, j * 4:(j + 1) * 4, :], in0=ps0[:, 0:8].rearrange("p (c r) -> p c r", r=2),
                    in1=badaT[:, l * 48 + j * 4:l * 48 + j * 4 + 4].unsqueeze(2).to_broadcast([128, 4, 2]), op=ALU.add),
                    r=[ps0, badaT], w=[C.modT])
                if j in (4, 5, 10, 11, 6, 7, 8, 9):
                    gi = {4: 0, 5: 0, 10: 1, 11: 1, 6: 2, 7: 2, 8: 3, 9: 3}[j]
                    half = j % 2
                    for r_ in range(2):
                        for k in range(8):
                            kb.op("pe", lambda e: e.matmul(ps1[:, :], lhsT=screp[:, 2 * k + r_, :], rhs=wb[:, k, :],
                                                           start=(k == 0), stop=(k == 7)), r=[screp, wb], w=[ps1])
                        o0 = ((r_ * 2 + gi) if gi < 2 else (4 + r_ * 2 + gi - 2)) * 1024 + half * 512
                        b0 = gi * 1024 + half * 512
                        kb.op("dve", lambda e: e.tensor_tensor(out=gout[:, o0:o0 + 512], in0=ps1[:, :], in1=brep[:, b0:b0 + 512],
                                                               op=ALU.add), r=[ps1, brep], w=[gout])
            kb.dma("sp", C.G[l], gout[:, :], r=[gout], w=[kb.tag("G", l)])


def setup_consts(C):
    kb = C.kb
    C.cneg = kb.sb("cneg", [128, 64], F32)
    kb.op("pool", lambda e: e.memset(C.cneg[:, :], -0.5), w=[C.cneg])


def rsqrt_mean(C, dst, src, n, inv_n, tmp):
    kb = C.kb
    kb.op("dve", lambda e: e.tensor_scalar(out=tmp[:, 0:n], in0=src[:, 0:n], scalar1=inv_n, scalar2=EPS,
                                           op0=ALU.mult, op1=ALU.add), r=[src], w=[tmp])
    kb.op("pool", lambda e: e.tensor_tensor(out=dst[:, 0:n], in0=tmp[:, 0:n], in1=C.cneg[:, 0:n], op=ALU.pow),
          r=[tmp, C.cneg], w=[dst])


def layer_mod_tables(C, l, which):
    kb = C.kb
    gs = kb.sb("gs", [128, 8, 2], F32)
    sc_blk = 8 if which == 0 else 32
    sh_blk = 0 if which == 0 else 24
    nrm = (l if which == 0 else DEPTH + l) * 8
    kb.op("dve", lambda e: e.tensor_scalar(out=gs[:, :, :], in0=C.modT[:, l, sc_blk:sc_blk + 8, :], scalar1=1.0, scalar2=None,
                                           op0=ALU.add), r=[C.modT], w=[gs])
    kb.op("dve", lambda e: e.tensor_tensor(out=gs[:, :, :], in0=gs[:, :, :],
                                           in1=C.normTs[:, nrm:nrm + 8].unsqueeze(2).to_broadcast([128, 8, 2]), op=ALU.mult),
          r=[gs, C.normTs], w=[gs])
    return gs, sh_blk


def rope_apply(C, dst_ap, dst_tl, src_ap, src_tl, nh, rp, t1, t2):
    kb = C.kb
    n = nh * 64
    kb.op("dve", lambda e: e.tensor_tensor(out=t1[:, 0:n].rearrange("p (h d) -> p h d", h=nh),
                                           in0=src_ap.rearrange("p (h d) -> p h d", h=nh),
                                           in1=rp[:, 0:64].unsqueeze(1).to_broadcast([128, nh, 64]), op=ALU.mult),
          r=[src_tl, rp], w=[t1])
    sv = src_ap.rearrange("p (h a b f) -> p h a b f", h=nh, a=2, b=2, f=16)
    tv = t2[:, 0:n].rearrange("p (h a b f) -> p h a b f", h=nh, a=2, b=2, f=16)
    sn = rp[:, 64:128].rearrange("p (a b f) -> p a b f", a=2, b=2, f=16)
    for b_ in range(2):
        kb.op("dve", lambda e: e.tensor_tensor(out=tv[:, :, :, b_, :], in0=sv[:, :, :, 1 - b_, :],
                                               in1=sn[:, :, b_, :].unsqueeze(1).to_broadcast([128, nh, 2, 16]), op=ALU.mult),
              r=[src_tl, rp], w=[t2])
    kb.op("pool", lambda e: e.tensor_tensor(out=dst_ap, in0=t1[:, 0:n], in1=t2[:, 0:n], op=ALU.add),
          r=[t1, t2], w=[dst_tl])


def norm_tile(C, xt, ss, rstd, tmp1, junk, xn):
    kb = C.kb
    kb.op("act", lambda e: e.activation(out=junk[:, :], in_=xt[:, :], func=AF.Square, accum_out=ss[:, 0:1]),
          r=[xt], w=[junk, ss])
    rsqrt_mean(C, rstd, ss, 1, 1.0 / D, tmp1)
    kb.op("act", lambda e: e.activation(out=xn[:, :], in_=xt[:, :], func=AF.Copy, scale=rstd[:, 0:1]),
          r=[xt, rstd], w=[xn])


def phaseA_even(C, l, tiles, x_src):
    kb = C.kb
    i = l // 2
    ps = C.ps
    with kb.phase():
        W = kb.sb("Win", [128, 8, EVEN_IN], BF16)
        wv = C.ev_w_in[i].rearrange("(k p) c -> p k c", p=128)
        for k in range(8):
            kb.dma("pool", W[:, k, :], wv[:, k, :], w=[W])
        gs1, sh_blk = layer_mod_tables(C, l, 0)
        gain = kb.sb("qkgain", [128, 128], F32)
        kb.dma("sp", gain[:, :], C.ev_qk_gain[i].partition_broadcast(128), w=[gain])
        kb.op("dve", lambda e: e.tensor_scalar(out=gain[:, 0:64], in0=gain[:, 0:64], scalar1=0.125, scalar2=None, op0=ALU.mult),
              r=[gain], w=[gain])
        xb = [kb.sb("xt", [128, D], F32) for _ in range(2)]
        rpb = [kb.sb("rp", [128, 128], F32) for _ in range(2)]
        junk = kb.sb("junk", [128, D], BF16)
        xn = kb.sb("xn", [128, D], BF16)
        hT = [kb.sb("hT", [128, 8, 128], BF16) for _ in range(2)]
        pa = [kb.sb("pa", [128, EVEN_IN], BF16) for _ in range(2)]
        ss = kb.sb("ss", [128, 1], F32)
        rstd = kb.sb("rstd", [128, 1], F32)
        tmp1 = kb.sb("tmp1", [128, 8], F32)
        ssh = kb.sb("ssh", [128, 8], F32)
        rs8 = kb.sb("rs8", [128, 8], F32)
        tA = kb.sb("tA", [128, 512], F32)
        tB = kb.sb("tB", [128, 512], F32)
        t1 = kb.sb("t1", [128, 512], F32)
        t2 = kb.sb("t2", [128, 512], F32)
        for n_, t in enumerate(tiles):
            lat = t >= 2
            r_ = 0 if lat else 1
            xt = xb[n_ % 2]
            rp = rpb[n_ % 2]
            h = hT[n_ % 2]
            po = pa[n_ % 2]
            x_src(t, xt)
            if lat:
                kb.dma("sp", rp[:, :], C.rope[(t - 2) * 128:(t - 1) * 128, :], w=[rp])
            norm_tile(C, xt, ss, rstd, tmp1, junk, xn)
            psT = ps[0]
            pv = psT[:, :].bitcast(BF16)
            for k in range(8):
                kb.op("pe", lambda e: e.transpose(pv[:, k * 128:(k + 1) * 128], xn[:, k * 128:(k + 1) * 128], C.identb_s[:, :]),
                      r=[xn, C.identb_s], w=[psT])
            for k in range(8):
                kb.op("act", lambda e: e.activation(out=h[:, k, :], in_=pv[:, k * 128:(k + 1) * 128], func=AF.Identity,
                                                    scale=gs1[:, k, r_:r_ + 1], bias=C.modT[:, l, sh_blk + k, r_:r_ + 1]),
                      r=[psT, gs1, C.modT], w=[h])
            for j in range(8):
                c0 = j * 512
                nc_ = 512 if j < 7 else 256
                pj = ps[1 + (j % 6)]
                for k in range(8):
                    kb.op("pe", lambda e: e.matmul(pj[:, 0:nc_], lhsT=h[:, k, :], rhs=W[:, k, c0:c0 + nc_],
                                                   start=(k == 0), stop=(k == 7)), r=[h, W], w=[pj])
                if j == 0:
                    if lat:
                        rope_apply(C, po[:, c0:c0 + 512], po, pj[:, :], pj, 8, rp, t1, t2)
                    else:
                        kb.op("act", lambda e: e.copy(out=po[:, c0:c0 + 512], in_=pj[:, :]), r=[pj], w=[po])
                elif j == 1:
                    if lat:
                        kb.op("act", lambda e: e.mul(out=tA[:, :], in_=pj[:, :], mul=0.125), r=[pj], w=[tA])
                        rope_apply(C, po[:, c0:c0 + 512], po, tA[:, :], tA, 8, rp, t1, t2)
                    else:
                        kb.op("act", lambda e: e.mul(out=po[:, c0:c0 + 512], in_=pj[:, :], mul=0.125), r=[pj], w=[po])
                elif j in (2, 3):
                    kb.op("act", lambda e: e.copy(out=po[:, c0:c0 + 512], in_=pj[:, :]), r=[pj], w=[po])
                elif j in (4, 5):
                    kb.op("act", lambda e: e.activation(out=po[:, c0:c0 + 512], in_=pj[:, :], func=AF.Silu), r=[pj], w=[po])
                else:
                    nh = 8 if j == 6 else 2
                    n = nh * 64
                    g0 = 0 if j == 6 else 64
                    kb.op("act", lambda e: e.activation(out=tA[:, 0:n], in_=pj[:, 0:n], func=AF.Square), r=[pj], w=[tA])
                    kb.op("dve", lambda e: e.reduce_sum(out=ssh[:, 0:nh], in_=tA[:, 0:n].rearrange("p (h d) -> p h d", h=nh),
                                                        axis=AX.X), r=[tA], w=[ssh])
                    rsqrt_mean(C, rs8, ssh, nh, 1.0 / 64, tmp1)
                    kb.op("dve", lambda e: e.tensor_tensor(out=tB[:, 0:n].rearrange("p (h d) -> p h d", h=nh),
                                                           in0=pj[:, 0:n].rearrange("p (h d) -> p h d", h=nh),
                                                           in1=rs8[:, 0:nh].unsqueeze(2).to_broadcast([128, nh, 64]), op=ALU.mult),
                          r=[pj, rs8], w=[tB])
                    if lat:
                        kb.op("dve", lambda e: e.tensor_tensor(out=tB[:, 0:n].rearrange("p (h d) -> p h d", h=nh),
                                                               in0=tB[:, 0:n].rearrange("p (h d) -> p h d", h=nh),
                                                               in1=gain[:, g0:g0 + 64].unsqueeze(1).to_broadcast([128, nh, 64]),
                                                               op=ALU.mult), r=[tB, gain], w=[tB])
                        rope_apply(C, po[:, c0:c0 + n], po, tB[:, 0:n], tB, nh, rp, t1, t2)
                    else:
                        kb.op("dve", lambda e: e.tensor_tensor(out=po[:, c0:c0 + n].rearrange("p (h d) -> p h d", h=nh),
                                                               in0=tB[:, 0:n].rearrange("p (h d) -> p h d", h=nh),
                                                               in1=gain[:, g0:g0 + 64].unsqueeze(1).to_broadcast([128, nh, 64]),
                                                               op=ALU.mult), r=[tB, gain], w=[po])
                    if j == 7:
                        kb.op("act", lambda e: e.copy(out=po[:, c0 + 128:c0 + 256], in_=pj[:, 128:256]), r=[pj], w=[po])
            kb.dma("sp", C.PA[t * 128:(t + 1) * 128, 0:EVEN_IN], po[:, :], r=[po], w=[kb.tag("PA", t)])


def rope_table():
    t = np.arange(L)
    rows = (t // 64).astype(np.float32)
    cols = (t % 64).astype(np.float32)
    inv = (10000.0 ** (-np.arange(16, dtype=np.float32) / 16)).astype(np.float32)
    ang = np.stack([rows[:, None] * inv, cols[:, None] * inv], axis=1).astype(np.float32)
    c, s = np.cos(ang), np.sin(ang)
    C64 = np.stack([c, c], axis=2)
    S64 = np.stack([-s, s], axis=2)
    return np.concatenate([C64.reshape(L, 64), S64.reshape(L, 64)], axis=1).astype(np.float32)


def fm(v):
    v = np.asarray(v, np.float32).reshape(-1)
    return np.ascontiguousarray(v.reshape(-1, 128).T)


def host_prep(inp):
    import ml_dtypes
    shared = {}
    shared["normT"] = np.concatenate([fm(inp["norm_mix"][l]) for l in range(DEPTH)] + [fm(inp["norm_ffn"][l]) for l in range(DEPTH)]
                                     + [fm(inp["final_norm"])], axis=1)
    shared["badaT"] = np.concatenate([fm(inp["b_ada"][l]) for l in range(DEPTH)], axis=1)
    shared["b_ada"] = np.ascontiguousarray(inp["b_ada"], np.float32)
    shared["rope"] = rope_table()
    shared["identb"] = np.eye(128, dtype=np.float32).astype(ml_dtypes.bfloat16)
    shared["identf"] = np.eye(128, dtype=np.float32)
    shared["rconst"] = ret_consts()
    shared["od_w_in"] = np.ascontiguousarray(inp["od_w_in"], np.float32)
    shared["od_prm"] = np.ascontiguousarray(np.concatenate([inp["od_a_log_f"], inp["od_a_log_b"], inp["od_dt_bias_f"], inp["od_dt_bias_b"]], axis=1), np.float32)
    shared["od_conv"] = np.ascontiguousarray(inp["od_conv"], np.float32)
    shared["od_gain"] = np.ascontiguousarray(inp["od_out_gain"], np.float32)
    shared["od_w_out"] = np.ascontiguousarray(inp["od_w_out"], np.float32)
    shared["dconst"] = dn_consts()
    shared["norms"] = np.ascontiguousarray(np.concatenate([inp["norm_mix"], inp["norm_ffn"], np.asarray(inp["final_norm"])[None, :]], axis=0), np.float32)
    shared["wr"] = np.ascontiguousarray(np.concatenate([inp["moe_w_group"], inp["moe_w_expert"]], axis=2), np.float32)
    shared["br"] = np.ascontiguousarray(np.concatenate([inp["moe_b_group"], inp["moe_b_expert"]], axis=1), np.float32)
    shared["triones"], shared["mconst"], shared["srcidx"], shared["listinit"] = moe_consts()
    shared["wgu"] = np.ascontiguousarray(np.asarray(inp["moe_w_gate_up"], np.float32).reshape(DEPTH, 32, 8, 128, 1024).transpose(0, 1, 3, 2, 4)).reshape(DEPTH, 32 * 128, 8 * 1024)
    shared["wd"] = np.ascontiguousarray(np.asarray(inp["moe_w_down"], np.float32).reshape(DEPTH, 32, 4, 128, 1024).transpose(0, 1, 3, 2, 4)).reshape(DEPTH, 32 * 128, 4 * 1024)
    shared["w_ada"] = np.ascontiguousarray(inp["w_ada"], np.float32)
    shared["ev_w_in"] = np.ascontiguousarray(inp["ev_w_in"], np.float32)
    shared["ev_qk_gain"] = np.ascontiguousarray(np.concatenate([inp["ev_q_gain"], inp["ev_k_gain"]], axis=1), np.float32)
    shared["ev_decay"] = np.ascontiguousarray(np.concatenate([inp["ev_decay_f"], inp["ev_decay_b"]], axis=1), np.float32)
    shared["ev_w_out"] = np.ascontiguousarray(inp["ev_w_out"], np.float32)
    maps = []
    for core in range(8):
        b = core % 4
        m = dict(shared)
        m["xin"] = np.ascontiguousarray(np.concatenate([inp["ctx"][b], inp["x"][b]], axis=0), np.float32)
        cv = np.stack([np.asarray(inp["c"][b], np.float32), np.asarray(inp["c_ctx"], np.float32)], axis=0)
        m["cT"] = np.ascontiguousarray(cv.reshape(2, 8, 128).transpose(2, 1, 0).reshape(128, 16))
        maps.append(m)
    return maps


def ret_consts():
    p = np.arange(128, dtype=np.float32)
    diff = p[None, :] - p[:, None]
    dpos = np.maximum(diff, 0)
    dneg = np.maximum(-diff, 0)
    mge = (diff >= 0).astype(np.float32)
    mle = (diff <= 0).astype(np.float32)
    pos1 = np.tile(p[None, :] + 1, (128, 1))
    rpos1 = np.tile(128 - p[None, :], (128, 1))
    pcol = np.stack([127 - p, p], axis=1)
    return np.ascontiguousarray(np.concatenate([dpos, dneg, mge, mle, pos1, rpos1, pcol], axis=1), np.float32)


def phaseB_ret(C, l, n_ctx_tiles=2, lat_tiles=None):
    kb = C.kb
    i = l // 2
    ps = C.ps
    if lat_tiles is None:
        lat_tiles = list(range(2, NT))
    with kb.phase():
        rc = kb.sb("rc", [128, 770], F32)
        kb.dma("sp", rc[:, :], C.rconst, w=[rc])
        ld = kb.sb("ld", [128, 16], F32)
        kb.dma("sp", ld[:, :], C.ev_decay[i].partition_broadcast(128), w=[ld])
        kb.op("act", lambda e: e.activation(out=ld[:, :], in_=ld[:, :], func=AF.Exp), r=[ld], w=[ld])
        kb.op("dve", lambda e: e.tensor_scalar(out=ld[:, :], in0=ld[:, :], scalar1=-1.0, scalar2=None, op0=ALU.mult), r=[ld], w=[ld])
        maskT = kb.sb("maskT", [128, 16, 128], BF16)
        DQ = kb.sb("DQ", [64, 16, 128], F32)
        DK = kb.sb("DK", [128, 16], F32)
        GC = kb.sb("GC", [64, 16], F32)
        tmpm = kb.sb("tmpm", [128, 128], F32)
        for d_ in range(2):
            for h in range(8):
                c = d_ * 8 + h
                kb.op("act", lambda e: e.activation(out=tmpm[:, :], in_=rc[:, d_ * 128:(d_ + 1) * 128], func=AF.Exp,
                                                    scale=ld[:, c:c + 1]), r=[rc, ld], w=[tmpm])
                kb.op("dve", lambda e: e.tensor_tensor(out=maskT[:, c, :], in0=tmpm[:, :], in1=rc[:, (2 + d_) * 128:(3 + d_) * 128],
                                                       op=ALU.mult), r=[tmpm, rc], w=[maskT])
                kb.op("act", lambda e: e.activation(out=DQ[:, c, :], in_=rc[0:64, (4 + d_) * 128:(5 + d_) * 128], func=AF.Exp,
                                                    scale=ld[0:64, c:c + 1]), r=[rc, ld], w=[DQ])
            kb.op("dve", lambda e: e.tensor_scalar(out=DK[:, d_ * 8:(d_ + 1) * 8], in0=ld[:, d_ * 8:(d_ + 1) * 8],
                                                   scalar1=rc[:, 768 + d_:769 + d_], scalar2=None, op0=ALU.mult), r=[ld, rc], w=[DK])
        kb.op("act", lambda e: e.activation(out=DK[:, :], in_=DK[:, :], func=AF.Exp), r=[DK], w=[DK])
        kb.op("act", lambda e: e.activation(out=GC[:, :], in_=ld[0:64, :], func=AF.Exp, scale=128.0), r=[ld], w=[GC])
        qkvb = [kb.sb("qkv", [128, 2048], BF16) for _ in range(2)]
        gateb = [kb.sb("gate", [128, 1024], BF16) for _ in range(2)]
        ofb = [kb.sb("of", [128, 1024], F32) for _ in range(2)]
        qT = kb.sb("qT", [64, 8, 128], BF16)
        qTd = kb.sb("qTd", [64, 8, 128], BF16)
        kT = kb.sb("kT", [64, 8, 128], BF16)
        kdec = kb.sb("kdec", [128, 8, 64], BF16)
        smb = [kb.sb("sm", [128, 128], BF16) for _ in range(8)]
        S = kb.sb("S", [64, 8, 128], F32)
        Sb = kb.sb("Sb", [64, 8, 128], BF16)
        osum = kb.sb("osum", [128, 1024], F32)
        junk = kb.sb("junkr", [128, 1024], F32)
        ssh = kb.sb("sshr", [128, 8], F32)
        rs8 = kb.sb("rs8r", [128, 8], F32)
        tmp8 = kb.sb("tmp8r", [128, 8], F32)
        mixb = [kb.sb("mixr", [128, 1024], BF16) for _ in range(2)]
        psQ, psK = ps[0], ps[1]
        psS = [ps[2], ps[3]]
        psO = [ps[4], ps[5]]
        psKV = [ps[6], ps[7]]
        pq = psQ[:, :].bitcast(BF16)
        pk = psK[:, :].bitcast(BF16)
        n_ = 0
        for d_ in range(2):
            order = list(range(n_ctx_tiles)) + list(lat_tiles)
            if d_ == 1:
                order = list(range(n_ctx_tiles))[::-1] + list(lat_tiles)[::-1]
            kb.op("dve", lambda e: e.memset(S[:, :, :], 0.0), w=[S])
            kb.op("dve", lambda e: e.memset(Sb[:, :, :], 0.0), w=[Sb])
            for t in order:
                if getattr(C, "dbgB", 9) < 1:
                    break
                qkv = qkvb[n_ % 2]
                gate = gateb[n_ % 2]
                of = ofb[n_ % 2]
                mix = mixb[n_ % 2]
                n_ += 1
                kb.dma("sp", qkv[:, :], C.PA[t * 128:(t + 1) * 128, 0:2048], r=[kb.tag("PA", t)], w=[qkv])
                if d_ == 1:
                    kb.dma("sp", gate[:, :], C.PA[t * 128:(t + 1) * 128, 2048:3072], r=[kb.tag("PA", t)], w=[gate])
                    kb.dma("sp", of[:, :], C.OF[t * 128:(t + 1) * 128, :], r=[kb.tag("OF", t)], w=[of])
                for h in range(8):
                    kb.op("pe", lambda e: e.transpose(pq[0:64, h * 128:(h + 1) * 128], qkv[:, h * 64:(h + 1) * 64], C.identb_s[:, :]),
                          r=[qkv, C.identb_s], w=[psQ])
                for h in range(8):
                    kb.op("pe", lambda e: e.transpose(pk[0:64, h * 128:(h + 1) * 128], qkv[:, 512 + h * 64:512 + (h + 1) * 64],
                                                      C.identb_s[:, :]), r=[qkv, C.identb_s], w=[psK])
                kb.op("act", lambda e: e.copy(out=qT[:, :, :].rearrange("p h t -> p (h t)"), in_=pq[0:64, :]), r=[psQ], w=[qT])
                kb.op("dve", lambda e: e.tensor_tensor(out=qTd[:, :, :], in0=pq[0:64, :].rearrange("p (h t) -> p h t", h=8),
                                                       in1=DQ[:, d_ * 8:(d_ + 1) * 8, :], op=ALU.mult), r=[psQ, DQ], w=[qTd])
                kb.op("act", lambda e: e.copy(out=kT[:, :, :].rearrange("p h t -> p (h t)"), in_=pk[0:64, :]), r=[psK], w=[kT])
                kb.op("dve", lambda e: e.tensor_tensor(out=kdec[:, :, :], in0=qkv[:, 512:1024].rearrange("p (h d) -> p h d", h=8),
                                                        in1=DK[:, d_ * 8:(d_ + 1) * 8].unsqueeze(2).to_broadcast([128, 8, 64]),
                                                        op=ALU.mult), r=[qkv, DK], w=[kdec])
                if getattr(C, "dbgB", 9) < 2:
                    continue
                for h in range(8):
                    pS = psS[h // 4]
                    sc_ = (h % 4) * 128
                    kb.op("pe", lambda e: e.matmul(pS[:, sc_:sc_ + 128], lhsT=kT[:, h, :], rhs=qT[:, h, :], start=True, stop=True),
                          r=[kT, qT], w=[pS])
                for h in range(8):
                    pS = psS[h // 4]
                    sc_ = (h % 4) * 128
                    kb.op("dve", lambda e: e.tensor_tensor(out=smb[h][:, :], in0=pS[:, sc_:sc_ + 128], in1=maskT[:, d_ * 8 + h, :], op=ALU.mult),
                          r=[pS, maskT], w=[smb[h]])
                for h in range(8):
                    sm = smb[h]
                    pO = psO[h // 4]
                    pKV = psKV[h // 4]
                    oc = (h % 4) * 128
                    vh = qkv[:, 1024 + h * 128:1024 + (h + 1) * 128]
                    kb.op("pe", lambda e: e.matmul(pO[:, oc:oc + 128], lhsT=sm[:, :], rhs=vh, start=True, stop=False),
                          r=[sm, qkv], w=[pO])
                    kb.op("pe", lambda e: e.matmul(pO[:, oc:oc + 128], lhsT=qTd[:, h, :], rhs=Sb[:, h, :], start=False, stop=True),
                          r=[qTd, Sb], w=[pO])
                    kb.op("pe", lambda e: e.matmul(pKV[0:64, oc:oc + 128], lhsT=kdec[:, h, :], rhs=vh, start=True, stop=True),
                          r=[kdec, qkv], w=[pKV])
                if getattr(C, "dbgB", 9) < 3:
                    continue
                kb.op("dve", lambda e: e.tensor_tensor(out=S[:, :, :], in0=S[:, :, :],
                                                       in1=GC[:, d_ * 8:(d_ + 1) * 8].unsqueeze(2).to_broadcast([64, 8, 128]), op=ALU.mult),
                      r=[S, GC], w=[S])
                for hb in range(2):
                    kb.op("dve", lambda e: e.tensor_tensor(out=S[:, hb * 4:(hb + 1) * 4, :], in0=S[:, hb * 4:(hb + 1) * 4, :],
                                                           in1=psKV[hb][0:64, :].rearrange("p (h t) -> p h t", h=4), op=ALU.add),
                          r=[S, psKV[hb]], w=[S])
                kb.op("act", lambda e: e.copy(out=Sb[:, :, :].rearrange("p h t -> p (h t)"), in_=S[:, :, :].rearrange("p h t -> p (h t)")), r=[S], w=[Sb])
                if d_ == 0:
                    for hb in range(2):
                        kb.op("act", lambda e: e.copy(out=of[:, hb * 512:(hb + 1) * 512], in_=psO[hb][:, :]), r=[psO[hb]], w=[of])
                    kb.dma("sp", C.OF[t * 128:(t + 1) * 128, :], of[:, :], r=[of], w=[kb.tag("OF", t)])
                else:
                    for hb in range(2):
                        kb.op("dve", lambda e: e.tensor_tensor(out=osum[:, hb * 512:(hb + 1) * 512], in0=psO[hb][:, :],
                                                               in1=of[:, hb * 512:(hb + 1) * 512], op=ALU.add),
                              r=[psO[hb], of], w=[osum])
                    kb.op("act", lambda e: e.activation(out=junk[:, :], in_=osum[:, :], func=AF.Square), r=[osum], w=[junk])
                    kb.op("dve", lambda e: e.reduce_sum(out=ssh[:, :], in_=junk[:, :].rearrange("p (h d) -> p h d", h=8), axis=AX.X),
                          r=[junk], w=[ssh])
                    rsqrt_mean(C, rs8, ssh, 8, 1.0 / 128, tmp8)
                    kb.op("dve", lambda e: e.tensor_tensor(out=osum[:, :].rearrange("p (h d) -> p h d", h=8),
                                                           in0=osum[:, :].rearrange("p (h d) -> p h d", h=8),
                                                           in1=rs8[:, :].unsqueeze(2).to_broadcast([128, 8, 128]), op=ALU.mult),
                          r=[osum, rs8], w=[osum])
                    kb.op("pool", lambda e: e.tensor_tensor(out=mix[:, :], in0=osum[:, :], in1=gate[:, :], op=ALU.mult),
                          r=[osum, gate], w=[mix])
                    kb.dma("sp", C.MIX[t * 128:(t + 1) * 128, 0:1024], mix[:, :], r=[mix], w=[kb.tag("MIXr", t)])


def phaseC_att(C, l, qblocks=None, key_tiles=None):
    kb = C.kb
    ps = C.ps
    if key_tiles is None:
        key_tiles = list(range(NT))
    if qblocks is None:
        qblocks = [([0, 1], [0, 1])] + [([2 + 4 * b + j for j in range(4)], key_tiles) for b in range(16)]
    with kb.phase():
        kTa = kb.sb("kTa", [64, 2, T], BF16)
        Va = kb.sb("Va", [128, NT, 2, 128], BF16)
        kb.op("pool", lambda e: e.memset(Va[:, :, :, :], 1.0), w=[Va])
        kvb = [kb.sb("kvld", [128, 256], BF16) for _ in range(2)]
        pT0 = ps[0]
        pt0 = pT0[:, :].bitcast(BF16)
        for n_, t in enumerate(key_tiles):
            kv = kvb[n_ % 2]
            kb.dma("sp", kv[:, :], C.PA[t * 128:(t + 1) * 128, 3584:3840], r=[kb.tag("PA", t)], w=[kv])
            for g in range(2):
                kb.op("pe", lambda e: e.transpose(pt0[0:64, g * 128:(g + 1) * 128], kv[:, g * 64:(g + 1) * 64], C.identb_s[:, :]),
                      r=[kv, C.identb_s], w=[pT0])
            for g in range(2):
                kb.op("act", lambda e: e.copy(out=kTa[:, g, t * 128:(t + 1) * 128], in_=pt0[0:64, g * 128:(g + 1) * 128]),
                      r=[pT0], w=[kTa])
            kb.op("pool", lambda e: e.tensor_copy(out=Va[:, t, :, 0:64], in_=kv[:, 128:256].rearrange("p (g d) -> p g d", g=2)),
                  r=[kv], w=[Va])
        aqb = [kb.sb("aq", [128, 4, 512], BF16) for _ in range(2)]
        qTb = kb.sb("qTb", [64, 8, 512], BF16)
        pTb = [kb.sb("pT", [128, 512], BF16) for _ in range(3)]
        rsb = kb.sb("rsb", [128, 512], F32)
        atb = [kb.sb("attT", [64, 512], BF16) for _ in range(2)]
        psS = [ps[2], ps[3], ps[4]]
        psO = [ps[5], ps[6]]
        psQ = [ps[0], ps[1]]
        for bi, (qt, keys) in enumerate(qblocks):
            nq = len(qt) * 128
            aq = aqb[bi % 2]
            for j, t in enumerate(qt):
                kb.dma("sp", aq[:, j, :], C.PA[t * 128:(t + 1) * 128, 3072:3584], r=[kb.tag("PA", t)], w=[aq])
            for hp in range(4):
                pQ = psQ[hp % 2]
                pqv = pQ[:, :].bitcast(BF16)
                for hh in range(2):
                    h = hp * 2 + hh
                    for j in range(len(qt)):
                        kb.op("pe", lambda e: e.transpose(pqv[0:64, hh * 512 + j * 128:hh * 512 + (j + 1) * 128],
                                                          aq[:, j, h * 64:(h + 1) * 64], C.identb_s[:, :]),
                              r=[aq, C.identb_s], w=[pQ])
                kb.op("dve", lambda e: e.tensor_copy(out=qTb[:, hp * 2:hp * 2 + 2, 0:nq],
                                                     in_=pqv[0:64, :].rearrange("p (h q) -> p h q", h=2)[:, :, 0:nq]),
                      r=[pQ], w=[qTb])
            tok0 = qt[0] * 128
            for h in range(8):
                g = h // 4
                pO = psO[h % 2]
                at = atb[h % 2]
                nk = len(keys)

                def s_mm(ki):
                    kt = keys[ki]
                    pS = psS[ki % 3]
                    kb.op("pe", lambda e: e.matmul(pS[:, 0:nq], lhsT=kTa[:, g, kt * 128:(kt + 1) * 128], rhs=qTb[:, h, 0:nq],
                                                   start=True, stop=True), r=[kTa, qTb], w=[pS])
                s_mm(0)
                if nk > 1:
                    s_mm(1)
                for ki in range(nk):
                    kt = keys[ki]
                    pS = psS[ki % 3]
                    pT = pTb[ki % 3]
                    kb.op("act", lambda e: e.activation(out=pT[:, 0:nq], in_=pS[:, 0:nq], func=AF.Exp), r=[pS], w=[pT])
                    if ki + 2 < nk:
                        s_mm(ki + 2)
                    kb.op("pe", lambda e: e.matmul(pO[:, 0:nq], lhsT=Va[:, kt, g, :], rhs=pT[:, 0:nq], start=(ki == 0), stop=(ki == nk - 1)),
                          r=[Va, pT], w=[pO])
                kb.op("dve", lambda e: e.reciprocal(out=rsb[64:128, 0:nq], in_=pO[64:128, 0:nq]), r=[pO], w=[rsb])
                kb.op("dve", lambda e: e.tensor_tensor(out=at[:, 0:nq], in0=pO[0:64, 0:nq], in1=rsb[64:128, 0:nq], op=ALU.mult),
                      r=[pO, rsb], w=[at])
                kb.dma("sp", C.AT[h * 64:(h + 1) * 64, tok0:tok0 + nq], at[:, 0:nq], r=[at], w=[kb.tag("AT", bi)])


def phaseD_out(C, l, tiles, w_out_ap, nk, mix_src, post):
    kb = C.kb
    ps = C.ps
    with kb.phase():
        Wo = kb.sb("Wo", [128, nk, D], BF16)
        wv = w_out_ap.rearrange("(k p) c -> p k c", p=128)
        for k in range(nk):
            kb.dma("pool", Wo[:, k, :], wv[:, k, :], w=[Wo])
        Gt = kb.sb("Gt", [128, 4096], F32)
        kb.dma("sp", Gt[:, :], C.G[l], r=[kb.tag("G", l)], w=[Gt])
        mTb = [kb.sb("mT", [128, nk, 128], BF16) for _ in range(2)]
        xb = [kb.sb("xd", [128, D], F32) for _ in range(2)]
        yb = kb.sb("yb", [128, D], F32)
        st = post(None, None, None, setup=True)
        for n_, t in enumerate(tiles):
            lat = t >= 2
            mT = mTb[n_ % 2]
            xt = xb[n_ % 2]
            kb.dma("sp", xt[:, :], C.X[t * 128:(t + 1) * 128, :], r=[kb.tag("X", t)], w=[xt])
            mix_src(t, mT, n_)
            psY = [ps[6], ps[7]]
            for nb in range(2):
                for k in range(nk):
                    kb.op("pe", lambda e: e.matmul(psY[nb][:, :], lhsT=mT[:, k, :], rhs=Wo[:, k, nb * 512:(nb + 1) * 512],
                                                   start=(k == 0), stop=(k == nk - 1)), r=[mT, Wo], w=[psY[nb]])
            g0 = 0 if lat else 2048
            for nb in range(2):
                kb.op("dve", lambda e: e.tensor_tensor(out=yb[:, nb * 512:(nb + 1) * 512], in0=psY[nb][:, :],
                                                       in1=Gt[:, g0 + nb * 512:g0 + (nb + 1) * 512], op=ALU.mult),
                      r=[psY[nb], Gt], w=[yb])
            kb.op("pool", lambda e: e.tensor_tensor(out=xt[:, :], in0=xt[:, :], in1=yb[:, :], op=ALU.add), r=[xt, yb], w=[xt])
            kb.dma("sp", C.X[t * 128:(t + 1) * 128, :], xt[:, :], r=[xt], w=[kb.tag("X", t)])
            post(t, xt, Gt, st=st)
        post(None, None, None, st=st, finish=True)


def even_mix_src(C):
    kb = C.kb
    mrb = [kb.sb("mr", [128, D], BF16) for _ in range(2)]
    ATv = C.AT.rearrange("(c p) t -> p c t", p=128)

    def src(t, mT, n_):
        mr = mrb[n_ % 2]
        bi = 0 if t < 2 else 1 + (t - 2) // 4
        kb.dma("sp", mr[:, :], C.MIX[t * 128:(t + 1) * 128, 0:1024], r=[kb.tag("MIXr", t)], w=[mr])
        kb.dma("sp", mT[:, 8:12, :], ATv[:, :, t * 128:(t + 1) * 128], r=[kb.tag("AT", bi)], w=[mT])
        pT = C.ps[0]
        pv = pT[:, :].bitcast(BF16)
        for k in range(8):
            kb.op("pe", lambda e: e.transpose(pv[:, k * 128:(k + 1) * 128], mr[:, k * 128:(k + 1) * 128], C.identb_s[:, :]),
                  r=[mr, C.identb_s], w=[pT])
        kb.op("act", lambda e: e.copy(out=mT[:, 0:8, :].rearrange("p k t -> p (k t)"), in_=pv[:, :]), r=[pT], w=[mT])
    return src


NSLOT = 164
TPAD = T + 128
BIGI = 1.0e6
NEG = -1.0e30


def moe_consts():
    import ml_dtypes
    p = np.arange(128)
    tri = (p[:, None] < p[None, :]).astype(np.float32).astype(ml_dtypes.bfloat16)
    ones = np.ones((128, 128), np.float32).astype(ml_dtypes.bfloat16)
    eidx = np.tile(np.arange(32, dtype=np.float32)[None, :], (128, 1))
    pidx = p.astype(np.float32)[:, None]
    sidx = np.tile(np.arange(NSLOT, dtype=np.float32)[None, :], (128, 1))
    mconst = np.ascontiguousarray(np.concatenate([eidx, pidx, sidx], axis=1), np.float32)
    src = (np.arange(NT)[None, :] * 128 + p[:, None]).astype(np.int32)
    li = np.zeros((NSLOT + 1, 128, 4), np.int32)
    li[:, :, 1] = T + p[None, :]
    li = np.ascontiguousarray(li.transpose(1, 0, 2).reshape(128, (NSLOT + 1) * 4))
    return np.ascontiguousarray(np.concatenate([tri, ones], axis=1)), mconst, src, li


def phaseD(C, l, tiles, w_out_ap, nk, mix_src_factory, route_tiles):
    kb = C.kb
    ps = C.ps
    with kb.phase():
        Wo = kb.sb("Wo", [128, nk, D], BF16)
        wv = w_out_ap.rearrange("(k p) c -> p k c", p=128)
        for k in range(nk):
            kb.dma("pool", Wo[:, k, :], wv[:, k, :], w=[Wo])
        Gt = kb.sb("Gt", [128, 8192], F32)
        kb.dma("sp", Gt[:, :], C.G[l], r=[kb.tag("G", l)], w=[Gt])
        nrep = kb.sb("nrep", [128, D], F32)
        kb.dma("sp", nrep[:, :], C.norms[DEPTH + l].partition_broadcast(128), w=[nrep])
        for v_ in range(2):
            o0 = (5 + 2 * v_) * 1024
            kb.op("dve", lambda e: e.scalar_tensor_tensor(out=Gt[:, o0:o0 + 1024], in0=Gt[:, o0:o0 + 1024], scalar=1.0, in1=nrep[:, :],
                                                          op0=ALU.add, op1=ALU.mult), r=[Gt, nrep], w=[Gt])
        Wr = kb.sb("Wr", [128, 8, 36], F32)
        kb.dma("sp", Wr[:, :, :], C.wr[l].rearrange("(k p) c -> p k c", p=128), w=[Wr])
        brr = kb.sb("brr", [128, 36], F32)
        kb.dma("sp", brr[:, :], C.br[l].partition_broadcast(128), w=[brr])
        mc = kb.sb("mc", [128, 33 + NSLOT], F32)
        kb.dma("sp", mc[:, :], C.mconst, w=[mc])
        tro = kb.sb("tro", [128, 256], BF16)
        kb.dma("sp", tro[:, :], C.triones, w=[tro])
        srci = kb.sb("srci", [128, NT], I32)
        kb.dma("sp", srci[:, :], C.srcidx, w=[srci])
        linit = kb.sb("linit", [128, (NSLOT + 1) * 4], I32)
        kb.dma("sp", linit[:, :], C.listinit, w=[linit])
        kb.dma("sp", C.LIST.rearrange("(s p) c -> p s c", p=128), linit[:, :].rearrange("p (s c) -> p s c", c=4), r=[linit],
               w=[kb.tag("LISTinit")])
        base = kb.sb("base", [128, 32], F32)
        kb.op("dve", lambda e: e.memset(base[:, :], 0.0), w=[base])
        RT = kb.sb("RT", [128, NT, 8], F32)
        kb.op("dve", lambda e: e.memset(RT[:, :, :], 0.0), w=[RT])
        mTb = [kb.sb("mT", [128, nk, 128], BF16) for _ in range(2)]
        xb = [kb.sb("xd", [128, D], F32) for _ in range(2)]
        yb = kb.sb("yb", [128, D], F32)
        junk = kb.sb("junkd", [128, D], BF16)
        xn2 = kb.sb("xn2", [128, D], F32)
        fb = [kb.sb("fb", [128, D], BF16) for _ in range(2)]
        fT = kb.sb("fT", [128, 8, 128], F32)
        ss = kb.sb("ssd", [128, 1], F32)
        rstd = kb.sb("rstdd", [128, 1], F32)
        tmp1 = kb.sb("tmp1d", [128, 8], F32)
        lg = kb.sb("lg", [128, 36], F32)
        sm = kb.sb("smalls", [128, 16], F32)
        geq = kb.sb("geq", [128, 4], F32)
        gex = kb.sb("gex", [128, 4], F32)
        ml = kb.sb("ml", [128, 32], F32)
        ml2 = kb.sb("ml2", [128, 32], F32)
        oh1 = kb.sb("oh1", [128, 32], F32)
        oh2 = kb.sb("oh2", [128, 32], F32)
        ind = kb.sb("ind", [128, 32], BF16)
        pos = kb.sb("pos", [128, 32], F32)
        t32 = kb.sb("t32", [128, 32], F32)
        mix_src = mix_src_factory()
        EIDX = mc[:, 0:32]
        for n_, t in enumerate(tiles):
            lat = t >= 2
            mT = mTb[n_ % 2]
            xt = xb[n_ % 2]
            kb.dma("sp", xt[:, :], C.X[t * 128:(t + 1) * 128, :], r=[kb.tag("X", t)], w=[xt])
            mix_src(t, mT, n_)
            psY = [ps[6], ps[7]]
            for nb in range(2):
                for k in range(nk):
                    kb.op("pe", lambda e: e.matmul(psY[nb][:, :], lhsT=mT[:, k, :], rhs=Wo[:, k, nb * 512:(nb + 1) * 512],
                                                   start=(k == 0), stop=(k == nk - 1)), r=[mT, Wo], w=[psY[nb]])
            g0 = 0 if lat else 2048
            for nb in range(2):
                kb.op("dve", lambda e: e.tensor_tensor(out=yb[:, nb * 512:(nb + 1) * 512], in0=psY[nb][:, :],
                                                       in1=Gt[:, g0 + nb * 512:g0 + (nb + 1) * 512], op=ALU.mult),
                      r=[psY[nb], Gt], w=[yb])
            kb.op("pool", lambda e: e.tensor_tensor(out=xt[:, :], in0=xt[:, :], in1=yb[:, :], op=ALU.add), r=[xt, yb], w=[xt])
            kb.dma("sp", C.X[t * 128:(t + 1) * 128, :], xt[:, :], r=[xt], w=[kb.tag("X", t)])
            if t not in route_tiles:
                continue
            f = fb[n_ % 2]
            norm_tile(C, xt, ss, rstd, tmp1, junk, xn2)
            v0 = 4096 if lat else 6144
            kb.op("dve", lambda e: e.tensor_tensor(out=xn2[:, :], in0=xn2[:, :], in1=Gt[:, v0 + 1024:v0 + 2048], op=ALU.mult),
                  r=[xn2, Gt], w=[xn2])
            kb.op("pool", lambda e: e.tensor_tensor(out=xn2[:, :], in0=xn2[:, :], in1=Gt[:, v0:v0 + 1024], op=ALU.add),
                  r=[xn2, Gt], w=[xn2])
            kb.op("act", lambda e: e.copy(out=f[:, :], in_=xn2[:, :]), r=[xn2], w=[f])
            kb.dma("sp", C.F[t * 128:(t + 1) * 128, :], f[:, :], r=[f], w=[kb.tag("F", t)])
            psT = [ps[0], ps[1]]
            for k in range(8):
                kb.op("pe", lambda e: e.transpose(psT[k // 4][:, (k % 4) * 128:(k % 4 + 1) * 128], xn2[:, k * 128:(k + 1) * 128],
                                                  C.identf_s[:, :]), r=[xn2, C.identf_s], w=[psT[k // 4]])
            for hb in range(2):
                kb.op("act", lambda e: e.copy(out=fT[:, hb * 4:(hb + 1) * 4, :].rearrange("p k t -> p (k t)"), in_=psT[hb][:, :]),
                      r=[psT[hb]], w=[fT])
            pL = ps[2]
            for k in range(8):
                kb.op("pe", lambda e: e.matmul(pL[:, 0:36], lhsT=fT[:, k, :], rhs=Wr[:, k, :], start=(k == 0), stop=(k == 7)),
                      r=[fT, Wr], w=[pL])
            kb.op("dve", lambda e: e.tensor_tensor(out=lg[:, :], in0=pL[:, 0:36], in1=brr[:, :], op=ALU.add), r=[pL, brr], w=[lg])
            gmax, ngmax, gsum, pg, m1, m2, dd, ed, w1, w2 = [sm[:, i:i + 1] for i in range(10)]
            kb.op("dve", lambda e: e.reduce_max(out=gmax, in_=lg[:, 0:4], axis=AX.X), r=[lg], w=[sm])
            kb.op("dve", lambda e: e.tensor_scalar(out=ngmax, in0=gmax, scalar1=-1.0, scalar2=None, op0=ALU.mult), r=[sm], w=[sm])
            kb.op("dve", lambda e: e.tensor_scalar(out=geq[:, :], in0=lg[:, 0:4], scalar1=gmax, scalar2=None, op0=ALU.is_equal),
                  r=[lg, sm], w=[geq])
            kb.op("act", lambda e: e.activation(out=gex[:, :], in_=lg[:, 0:4], func=AF.Exp, bias=ngmax, scale=1.0, accum_out=gsum),
                  r=[lg, sm], w=[gex, sm])
            kb.op("dve", lambda e: e.reciprocal(out=pg, in_=gsum), r=[sm], w=[sm])
            kb.op("dve", lambda e: e.tensor_scalar(out=geq[:, :], in0=geq[:, :], scalar1=-NEG, scalar2=NEG, op0=ALU.mult, op1=ALU.add),
                  r=[geq], w=[geq])
            kb.op("dve", lambda e: e.tensor_tensor(out=ml[:, :].rearrange("p (g e) -> p g e", g=4),
                                                   in0=lg[:, 4:36].rearrange("p (g e) -> p g e", g=4),
                                                   in1=geq[:, :].unsqueeze(2).to_broadcast([128, 4, 8]), op=ALU.add), r=[lg, geq], w=[ml])
            kb.op("dve", lambda e: e.reduce_max(out=m1, in_=ml[:, :], axis=AX.X), r=[ml], w=[sm])
            kb.op("dve", lambda e: e.tensor_scalar(out=oh1[:, :], in0=ml[:, :], scalar1=m1, scalar2=None, op0=ALU.is_equal),
                  r=[ml, sm], w=[oh1])
            kb.op("dve", lambda e: e.scalar_tensor_tensor(out=ml2[:, :], in0=oh1[:, :], scalar=NEG, in1=ml[:, :], op0=ALU.mult, op1=ALU.add),
                  r=[oh1, ml], w=[ml2])
            kb.op("dve", lambda e: e.reduce_max(out=m2, in_=ml2[:, :], axis=AX.X), r=[ml2], w=[sm])
            kb.op("dve", lambda e: e.tensor_scalar(out=oh2[:, :], in0=ml2[:, :], scalar1=m2, scalar2=None, op0=ALU.is_equal),
                  r=[ml2, sm], w=[oh2])
            kb.op("dve", lambda e: e.tensor_tensor(out=dd, in0=m2, in1=m1, op=ALU.subtract), r=[sm], w=[sm])
            kb.op("act", lambda e: e.activation(out=ed, in_=dd, func=AF.Exp), r=[sm], w=[sm])
            kb.op("dve", lambda e: e.tensor_scalar(out=w1, in0=ed, scalar1=1.0, scalar2=None, op0=ALU.add), r=[sm], w=[sm])
            kb.op("dve", lambda e: e.reciprocal(out=w1, in_=w1), r=[sm], w=[sm])
            kb.op("dve", lambda e: e.tensor_tensor(out=w2, in0=ed, in1=w1, op=ALU.mult), r=[sm], w=[sm])
            kb.op("dve", lambda e: e.tensor_tensor(out=RT[:, t, 2:3], in0=w1, in1=pg, op=ALU.mult), r=[sm], w=[RT])
            kb.op("dve", lambda e: e.tensor_tensor(out=RT[:, t, 5:6], in0=w2, in1=pg, op=ALU.mult), r=[sm], w=[RT])
            kb.op("dve", lambda e: e.tensor_tensor(out=ind[:, :], in0=oh1[:, :], in1=oh2[:, :], op=ALU.add), r=[oh1, oh2], w=[ind])
            pR = ps[3]
            kb.op("pe", lambda e: e.matmul(pR[:, 0:32], lhsT=tro[:, 0:128], rhs=ind[:, :], start=True, stop=True), r=[tro, ind], w=[pR])
            kb.op("pe", lambda e: e.matmul(pR[:, 32:64], lhsT=tro[:, 128:256], rhs=ind[:, :], start=True, stop=True), r=[tro, ind], w=[pR])
            kb.op("dve", lambda e: e.tensor_tensor(out=pos[:, :], in0=pR[:, 0:32], in1=base[:, :], op=ALU.add), r=[pR, base], w=[pos])
            kb.op("dve", lambda e: e.tensor_tensor(out=base[:, :], in0=pR[:, 32:64], in1=base[:, :], op=ALU.add), r=[pR, base], w=[base])
            for k_, oh in enumerate((oh1, oh2)):
                kb.op("dve", lambda e: e.tensor_tensor(out=t32[:, :], in0=oh[:, :], in1=EIDX, op=ALU.mult), r=[oh, mc], w=[t32])
                kb.op("dve", lambda e: e.reduce_sum(out=RT[:, t, 3 * k_:3 * k_ + 1], in_=t32[:, :], axis=AX.X), r=[t32], w=[RT])
                kb.op("dve", lambda e: e.tensor_tensor(out=t32[:, :], in0=oh[:, :], in1=pos[:, :], op=ALU.mult), r=[oh, pos], w=[t32])
                kb.op("dve", lambda e: e.reduce_sum(out=RT[:, t, 3 * k_ + 1:3 * k_ + 2], in_=t32[:, :], axis=AX.X), r=[t32], w=[RT])
        ni = kb.sb("ni", [128, 32], I32)
        ca = kb.sb("ca", [128, 32], F32)
        cb = kb.sb("cb", [128, 32], F32)
        tl_ = kb.sb("tl", [128, 32], F32)
        kb.op("dve", lambda e: e.tensor_scalar(out=t32[:, :], in0=base[:, :], scalar1=127.0, scalar2=None, op0=ALU.add), r=[base], w=[t32])
        kb.op("dve", lambda e: e.tensor_copy(out=ni[:, :], in_=t32[:, :]), r=[t32], w=[ni])
        kb.op("dve", lambda e: e.tensor_scalar(out=ni[:, :], in0=ni[:, :], scalar1=7, scalar2=None, op0=ALU.arith_shift_right), r=[ni], w=[ni])
        kb.op("dve", lambda e: e.tensor_copy(out=tl_[:, :], in_=ni[:, :]), r=[ni], w=[tl_])
        kb.op("dve", lambda e: e.tensor_copy(out=ca[:, :], in_=tl_[:, :]), r=[tl_], w=[ca])
        a_, b_ = ca, cb
        for d_ in (1, 2, 4, 8, 16):
            kb.op("dve", lambda e: e.tensor_copy(out=b_[:, 0:d_], in_=a_[:, 0:d_]), r=[a_], w=[b_])
            kb.op("dve", lambda e: e.tensor_tensor(out=b_[:, d_:32], in0=a_[:, d_:32], in1=a_[:, 0:32 - d_], op=ALU.add), r=[a_], w=[b_])
            a_, b_ = b_, a_
        cum = a_
        ss128 = b_
        kb.op("dve", lambda e: e.tensor_tensor(out=ss128[:, :], in0=cum[:, :], in1=tl_[:, :], op=ALU.subtract), r=[cum, tl_], w=[ss128])
        kb.op("dve", lambda e: e.tensor_scalar(out=ss128[:, :], in0=ss128[:, :], scalar1=128.0, scalar2=None, op0=ALU.mult),
              r=[ss128], w=[ss128])
        big = kb.sb("big", [128, NT, 32], F32)
        offf = kb.sb("offf", [128, 2, NT], F32)
        offi = kb.sb("offi", [128, 2, NT], I32)
        ent = kb.sb("ent", [128, 2, NT, 4], I32)
        kb.op("dve", lambda e: e.memset(ent[:, :, :, :], 0), w=[ent])
        for k_ in range(2):
            kb.op("dve", lambda e: e.tensor_tensor(out=big[:, :, :], in0=mc[:, 0:32].unsqueeze(1).to_broadcast([128, NT, 32]),
                                                   in1=RT[:, :, 3 * k_:3 * k_ + 1].to_broadcast([128, NT, 32]), op=ALU.is_equal),
                  r=[mc, RT], w=[big])
            kb.op("dve", lambda e: e.tensor_tensor(out=big[:, :, :], in0=big[:, :, :],
                                                   in1=ss128[:, :].unsqueeze(1).to_broadcast([128, NT, 32]), op=ALU.mult),
                  r=[big, ss128], w=[big])
            kb.op("dve", lambda e: e.reduce_sum(out=offf[:, k_, :], in_=big[:, :, :], axis=AX.X), r=[big], w=[offf])
            kb.op("dve", lambda e: e.tensor_tensor(out=offf[:, k_, :], in0=offf[:, k_, :], in1=RT[:, :, 3 * k_ + 1], op=ALU.add),
                  r=[offf, RT], w=[offf])
            kb.op("dve", lambda e: e.tensor_copy(out=offi[:, k_, :], in_=offf[:, k_, :]), r=[offf], w=[offi])
            kb.op("dve", lambda e: e.tensor_copy(out=ent[:, k_, :, 0], in_=srci[:, :]), r=[srci], w=[ent])
            kb.op("dve", lambda e: e.tensor_scalar(out=ent[:, k_, :, 1], in0=srci[:, :], scalar1=k_ * TPAD, scalar2=None, op0=ALU.add),
                  r=[srci], w=[ent])
            kb.op("dve", lambda e: e.tensor_copy(out=ent[:, k_, :, 2], in_=RT[:, :, 3 * k_ + 2].bitcast(I32)), r=[RT], w=[ent])
        if hasattr(C, "dbgD"):
            kb.dma("sp", C.dbgD["offi"], offi[:, :, :].rearrange("p k t -> p (k t)"), r=[offi], w=[kb.tag("dbg1")])
            kb.dma("sp", C.dbgD["RT"], RT[:, :, :].rearrange("p t c -> p (t c)"), r=[RT], w=[kb.tag("dbg2")])
            kb.dma("sp", C.dbgD["base"], base[:, :], r=[base], w=[kb.tag("dbg4")])
            kb.dma("sp", C.dbgD["ent"], ent[:, :, :, :].rearrange("p k t c -> p (k t c)"), r=[ent], w=[kb.tag("dbg5")])
        for t in (route_tiles if not getattr(C, "skip_scatter", False) else []):
            for k_ in range(2):
                kb.dma("pool", C.LIST, ent[:, k_, t, :], r=[ent, offi, kb.tag("LISTinit")], w=[kb.tag("LISTs", t, k_)],
                       indirect=dict(out_offset=bass.IndirectOffsetOnAxis(ap=offi[:, k_, t:t + 1], axis=0), in_offset=None))
        SIDX = mc[:, 33:33 + NSLOT]
        cmp_ = kb.sb("cmp", [128, NSLOT, 32], F32)
        eid = kb.sb("eid", [128, NSLOT + 2], F32)
        kb.op("dve", lambda e: e.memset(eid[:, 0:2], -1.0), w=[eid])
        kb.op("dve", lambda e: e.tensor_tensor(out=cmp_[:, :, :], in0=cum[:, :].unsqueeze(1).to_broadcast([128, NSLOT, 32]),
                                               in1=SIDX.unsqueeze(2).to_broadcast([128, NSLOT, 32]), op=ALU.is_le), r=[cum, mc], w=[cmp_])
        kb.op("dve", lambda e: e.reduce_sum(out=eid[:, 2:2 + NSLOT], in_=cmp_[:, :, :], axis=AX.X), r=[cmp_], w=[eid])
        ldf = kb.sb("ldf", [128, NSLOT], F32)
        vld = kb.sb("vld", [128, NSLOT], F32)
        wix = kb.sb("wix", [128, NSLOT], F32)
        kb.op("dve", lambda e: e.tensor_tensor(out=ldf[:, :], in0=eid[:, 2:2 + NSLOT], in1=eid[:, 0:NSLOT], op=ALU.not_equal), r=[eid], w=[ldf])
        kb.op("dve", lambda e: e.tensor_scalar(out=vld[:, :], in0=eid[:, 2:2 + NSLOT], scalar1=31.5, scalar2=None, op0=ALU.is_lt), r=[eid], w=[vld])
        kb.op("dve", lambda e: e.tensor_tensor(out=ldf[:, :], in0=ldf[:, :], in1=vld[:, :], op=ALU.mult), r=[ldf, vld], w=[ldf])
        kb.op("dve", lambda e: e.tensor_scalar(out=wix[:, :], in0=eid[:, 2:2 + NSLOT], scalar1=31.0, scalar2=128.0,
                                               op0=ALU.min, op1=ALU.mult), r=[eid], w=[wix])
        kb.op("dve", lambda e: e.tensor_scalar(out=wix[:, :], in0=wix[:, :], scalar1=mc[:, 32:33], scalar2=float(l * 4096), op0=ALU.add,
                                               op1=ALU.add), r=[wix, mc], w=[wix])
        if getattr(C, "moe_skip", False):
            kb.op("dve", lambda e: e.tensor_scalar(out=wix[:, :], in0=wix[:, :], scalar1=-BIGI, scalar2=None, op0=ALU.add), r=[wix], w=[wix])
            kb.op("dve", lambda e: e.tensor_tensor(out=wix[:, :], in0=wix[:, :], in1=ldf[:, :], op=ALU.mult), r=[wix, ldf], w=[wix])
            kb.op("dve", lambda e: e.tensor_scalar(out=wix[:, :], in0=wix[:, :], scalar1=BIGI, scalar2=None, op0=ALU.add), r=[wix], w=[wix])
        kb.op("dve", lambda e: e.tensor_copy(out=C.widx[:, :], in_=wix[:, :]), r=[wix], w=[C.widx])
        if hasattr(C, "dbgD"):
            kb.dma("sp", C.dbgD["widx"], C.widx[:, :], r=[C.widx], w=[kb.tag("dbg6")])
            kb.dma("sp", C.dbgD["eid"], eid[:, :], r=[eid], w=[kb.tag("dbg3")])


def phaseE(C, l, nslot=None, lvl=9):
    nslot = NSLOT if nslot is None else nslot
    kb = C.kb
    ps = C.ps
    with kb.phase():
        Wgu = [kb.sb("Wgu", [128, 8 * 1024], BF16) for _ in range(2)]
        Wd = [kb.sb("Wd", [128, 4 * 1024], BF16) for _ in range(2)]
        for a in range(2):
            kb.op("pool", lambda e: e.memset(Wgu[a][:, :], 0.0), w=[Wgu[a]])
            kb.op("pool", lambda e: e.memset(Wd[a][:, :], 0.0), w=[Wd[a]])
        entb = [kb.sb("ente", [128, 4], I32) for _ in range(2)]
        xsb = [kb.sb("xs", [128, D], BF16) for _ in range(2)]
        xsT = kb.sb("xsT", [128, 8, 128], BF16)
        actf = kb.sb("actf", [128, 512], F32)
        actb = kb.sb("actb", [128, 512], BF16)
        actT = kb.sb("actT", [128, 4, 128], BF16)
        ywb = [kb.sb("yw", [128, D], F32) for _ in range(2)]
        if not hasattr(C, "bc_reg"):
            C.bc_reg = C.nc.gpsimd.to_reg(DEPTH * 4096 - 1)
        wgu_v = C.wgu[l].rearrange("r (a c) -> r a c", c=2048)
        wd_v = C.wd[l].rearrange("r (a c) -> r a c", c=2048)

        def loads(s):
            A = s % 2
            ent = entb[A]
            msk = getattr(C, "ldmask", 15)
            kb.dma("sp", ent[:, :], C.LIST[s * 128:(s + 1) * 128, :], w=[ent])
            extra = dict(bounds_check=C.bc_reg, oob_is_err=False) if (msk & 8) else {}
            if msk & 1:
                kb.dma("pool", Wgu[A][:, :], C.wgu.rearrange("l r c -> (l r) c"), r=[C.widx], w=[Wgu[A]],
                       indirect=dict(out_offset=None, in_offset=bass.IndirectOffsetOnAxis(ap=C.widx[:, s:s + 1], axis=0), **extra))
            if msk & 2:
                kb.dma("pool", Wd[A][:, :], C.wd.rearrange("l r c -> (l r) c"), r=[C.widx], w=[Wd[A]],
                       indirect=dict(out_offset=None, in_offset=bass.IndirectOffsetOnAxis(ap=C.widx[:, s:s + 1], axis=0), **extra))
            if msk & 4:
                kb.dma("pool", xsb[A][:, :], C.F, r=[ent], w=[xsb[A]],
                       indirect=dict(out_offset=None, in_offset=bass.IndirectOffsetOnAxis(ap=ent[:, 0:1], axis=0)))

        loads(0)
        for s in range(nslot):
            A = s % 2
            ent, xs, yw = entb[A], xsb[A], ywb[A]
            if s + 1 < nslot:
                loads(s + 1)
            if lvl < 1:
                continue
            pT = ps[0]
            pv = pT[:, :].bitcast(BF16)
            for k in range(8):
                kb.op("pe", lambda e: e.transpose(pv[:, k * 128:(k + 1) * 128], xs[:, k * 128:(k + 1) * 128], C.identb_s[:, :]),
                      r=[xs, C.identb_s], w=[pT])
            kb.op("dve", lambda e: e.tensor_copy(out=xsT[:, :, :].rearrange("p k t -> p (k t)"), in_=pv[:, :]), r=[pT], w=[xsT])
            pG, pU = ps[1], ps[2]
            for nb, pp in enumerate((pG, pU)):
                for k in range(8):
                    kb.op("pe", lambda e: e.matmul(pp[:, :], lhsT=xsT[:, k, :], rhs=Wgu[A][:, k * 1024 + nb * 512:k * 1024 + (nb + 1) * 512],
                                                   start=(k == 0), stop=(k == 7)), r=[xsT, Wgu[A]], w=[pp])
            kb.op("act", lambda e: e.activation(out=actf[:, :], in_=pG[:, :], func=AF.Silu), r=[pG], w=[actf])
            kb.op("dve", lambda e: e.tensor_tensor(out=actb[:, :], in0=pU[:, :], in1=actf[:, :], op=ALU.mult), r=[pU, actf], w=[actb])
            pA = ps[3]
            pav = pA[:, :].bitcast(BF16)
            for c in range(4):
                kb.op("pe", lambda e: e.transpose(pav[:, c * 128:(c + 1) * 128], actb[:, c * 128:(c + 1) * 128], C.identb_s[:, :]),
                      r=[actb, C.identb_s], w=[pA])
            kb.op("act", lambda e: e.copy(out=actT[:, :, :].rearrange("p k t -> p (k t)"), in_=pav[:, 0:512]), r=[pA], w=[actT])
            pY = [ps[4 + 2 * (s % 2)], ps[5 + 2 * (s % 2)]]
            for nb in range(2):
                for c in range(4):
                    kb.op("pe", lambda e: e.matmul(pY[nb][:, :], lhsT=actT[:, c, :], rhs=Wd[A][:, c * 1024 + nb * 512:c * 1024 + (nb + 1) * 512],
                                                   start=(c == 0), stop=(c == 3)), r=[actT, Wd[A]], w=[pY[nb]])
            if lvl < 2:
                continue
            wcol = ent[:, 2:3].bitcast(F32)
            kb.op("dve", lambda e: e.tensor_scalar(out=yw[:, 0:512], in0=pY[0][:, :], scalar1=wcol, scalar2=None, op0=ALU.mult),
                  r=[pY[0], ent], w=[yw])
            kb.op("act", lambda e: e.activation(out=yw[:, 512:1024], in_=pY[1][:, :], func=AF.Copy, scale=wcol), r=[pY[1], ent], w=[yw])
            if lvl < 3:
                continue
            kb.dma("pool", C.YB, yw[:, :], r=[yw, ent], w=[kb.tag("YBs", s)],
                   indirect=dict(out_offset=bass.IndirectOffsetOnAxis(ap=ent[:, 1:2], axis=0), in_offset=None))


def combine_src(C, l_prev, store=True):
    kb = C.kb
    Gt = kb.sb("Gc", [128, 2048], F32)
    kb.dma("sp", Gt[:, 0:1024], C.G[l_prev][:, 1024:2048], r=[kb.tag("G", l_prev)], w=[Gt])
    kb.dma("sp", Gt[:, 1024:2048], C.G[l_prev][:, 3072:4096], r=[kb.tag("G", l_prev)], w=[Gt])
    y1b = [kb.sb("y1", [128, D], F32) for _ in range(2)]
    y2b = [kb.sb("y2", [128, D], F32) for _ in range(2)]
    cnt = [0]

    def src(t, xt):
        n_ = cnt[0]
        cnt[0] += 1
        y1, y2 = y1b[n_ % 2], y2b[n_ % 2]
        kb.dma("sp", xt[:, :], C.X[t * 128:(t + 1) * 128, :], r=[kb.tag("X", t)], w=[xt])
        kb.dma("sp", y1[:, :], C.YB[t * 128:(t + 1) * 128, :], w=[y1])
        kb.dma("sp", y2[:, :], C.YB[TPAD + t * 128:TPAD + (t + 1) * 128, :], w=[y2])
        g0 = 0 if t >= 2 else 1024
        kb.op("pool", lambda e: e.tensor_tensor(out=y1[:, :], in0=y1[:, :], in1=y2[:, :], op=ALU.add), r=[y1, y2], w=[y1])
        kb.op("dve", lambda e: e.tensor_tensor(out=y1[:, :], in0=y1[:, :], in1=Gt[:, g0:g0 + 1024], op=ALU.mult), r=[y1, Gt], w=[y1])
        kb.op("pool", lambda e: e.tensor_tensor(out=xt[:, :], in0=xt[:, :], in1=y1[:, :], op=ALU.add), r=[xt, y1], w=[xt])
        if store:
            kb.dma("sp", C.X[t * 128:(t + 1) * 128, :], xt[:, :], r=[xt], w=[kb.tag("X", t)])
    return src


TZ = T + 4


def zbase(t):
    return 1 + t * 128 if t < 2 else 259 + (t - 2) * 128


def dn_consts():
    p = np.arange(128)
    same = (p[:, None] // 64) == (p[None, :] // 64)
    mge = (same & (p[None, :] >= p[:, None])).astype(np.float32)
    mle = (same & (p[None, :] <= p[:, None])).astype(np.float32)
    slt = (same & (p[None, :] < p[:, None])).astype(np.float32)
    sgt = (same & (p[None, :] > p[:, None])).astype(np.float32)
    return np.ascontiguousarray(np.concatenate([mge, mle, slt, sgt], axis=1), np.float32)


def phaseA_odd(C, l, tiles, x_src):
    kb = C.kb
    i = l // 2
    ps = C.ps
    with kb.phase():
        W = kb.sb("Wino", [128, 8, ODD_IN], BF16)
        wv = C.od_w_in[i].rearrange("(k p) c -> p k c", p=128)
        for k in range(8):
            kb.dma("pool", W[:, k, :], wv[:, k, :], w=[W])
        gs1, sh_blk = layer_mod_tables(C, l, 0)
        prm = kb.sb("dnprm", [128, 32], F32)
        kb.dma("sp", prm[:, :], C.od_prm[i].partition_broadcast(128), w=[prm])
        kb.op("act", lambda e: e.activation(out=prm[:, 0:16], in_=prm[:, 0:16], func=AF.Exp), r=[prm], w=[prm])
        kb.op("dve", lambda e: e.tensor_scalar(out=prm[:, 0:16], in0=prm[:, 0:16], scalar1=-1.0, scalar2=None, op0=ALU.mult), r=[prm], w=[prm])
        zrow = kb.sb("zrow", [4, 3072], BF16)
        kb.op("dve", lambda e: e.memset(zrow[:, :], 0.0), w=[zrow])
        for n_, r0 in enumerate((0, 257, 258, TZ - 1)):
            kb.dma("sp", C.ZP[r0:r0 + 1, :], zrow[0:1, :], r=[zrow], w=[kb.tag("ZPz", n_)])
        xb = [kb.sb("xt", [128, D], F32) for _ in range(2)]
        junk = kb.sb("junk", [128, D], BF16)
        xn = kb.sb("xn", [128, D], BF16)
        hT = [kb.sb("hT", [128, 8, 128], BF16) for _ in range(2)]
        zt = [kb.sb("zt", [128, 4096], BF16) for _ in range(2)]
        gb = [kb.sb("gb", [128, 32], F32) for _ in range(2)]
        ss = kb.sb("ss", [128, 1], F32)
        rstd = kb.sb("rstd", [128, 1], F32)
        tmp1 = kb.sb("tmp1", [128, 8], F32)
        t16 = kb.sb("t16", [128, 16], F32)
        for n_, t in enumerate(tiles):
            r_ = 0 if t >= 2 else 1
            xt = xb[n_ % 2]
            h = hT[n_ % 2]
            z = zt[n_ % 2]
            g = gb[n_ % 2]
            x_src(t, xt)
            norm_tile(C, xt, ss, rstd, tmp1, junk, xn)
            psT = ps[0]
            pv = psT[:, :].bitcast(BF16)
            for k in range(8):
                kb.op("pe", lambda e: e.transpose(pv[:, k * 128:(k + 1) * 128], xn[:, k * 128:(k + 1) * 128], C.identb_s[:, :]),
                      r=[xn, C.identb_s], w=[psT])
            for k in range(8):
                kb.op("act", lambda e: e.activation(out=h[:, k, :], in_=pv[:, k * 128:(k + 1) * 128], func=AF.Identity,
                                                    scale=gs1[:, k, r_:r_ + 1], bias=C.modT[:, l, sh_blk + k, r_:r_ + 1]),
                      r=[psT, gs1, C.modT], w=[h])
            for j in range(9):
                c0 = j * 512
                nc_ = 512 if j < 8 else 32
                pj = ps[1 + (j % 6)]
                for k in range(8):
                    kb.op("pe", lambda e: e.matmul(pj[:, 0:nc_], lhsT=h[:, k, :], rhs=W[:, k, c0:c0 + nc_],
                                                   start=(k == 0), stop=(k == 7)), r=[h, W], w=[pj])
                if j < 6:
                    if j % 2 == 0:
                        kb.op("act", lambda e: e.copy(out=z[:, c0:c0 + 512], in_=pj[:, :]), r=[pj], w=[z])
                    else:
                        kb.op("dve", lambda e: e.tensor_copy(out=z[:, c0:c0 + 512], in_=pj[:, :]), r=[pj], w=[z])
                elif j < 8:
                    kb.op("act", lambda e: e.activation(out=z[:, c0:c0 + 512], in_=pj[:, :], func=AF.Silu), r=[pj], w=[z])
                else:
                    kb.op("dve", lambda e: e.tensor_tensor(out=t16[:, :], in0=pj[:, 0:16], in1=prm[:, 16:32], op=ALU.add), r=[pj, prm], w=[t16])
                    kb.op("act", lambda e: e.activation(out=t16[:, :], in_=t16[:, :], func=AF.Exp), r=[t16], w=[t16])
                    kb.op("act", lambda e: e.activation(out=t16[:, :], in_=t16[:, :], func=AF.Ln, bias=1.0, scale=1.0), r=[t16], w=[t16])
                    kb.op("dve", lambda e: e.tensor_tensor(out=g[:, 0:16], in0=t16[:, :], in1=prm[:, 0:16], op=ALU.mult), r=[t16, prm], w=[g])
                    kb.op("act", lambda e: e.activation(out=g[:, 16:32], in_=pj[:, 16:32], func=AF.Exp, scale=-1.0), r=[pj], w=[g])
                    kb.op("dve", lambda e: e.tensor_scalar(out=g[:, 16:32], in0=g[:, 16:32], scalar1=1.0, scalar2=None, op0=ALU.add), r=[g], w=[g])
                    kb.op("dve", lambda e: e.reciprocal(out=g[:, 16:32], in_=g[:, 16:32]), r=[g], w=[g])
            zb = zbase(t)
            kb.dma("sp", C.ZP[zb:zb + 128, :], z[:, 0:3072], r=[z], w=[kb.tag("ZP", t)])
            kb.dma("sp", C.PA[t * 128:(t + 1) * 128, 3072:4096], z[:, 3072:4096], r=[z], w=[kb.tag("PAz", t)])
            kb.dma("sp", C.GB[t * 128:(t + 1) * 128, :], g[:, :], r=[g], w=[kb.tag("GB", t)])
    with kb.phase():
        cw = kb.sb("convw", [128, 3, 3072], F32)
        for j in range(3):
            kb.dma("sp", cw[:, j, :], C.od_conv[i, j].partition_broadcast(128), w=[cw])
        zmb = [kb.sb("zm", [128, 3, 3072], BF16) for _ in range(2)]
        accb = [kb.sb("acc", [128, 3072], F32) for _ in range(2)]
        acc2b = [kb.sb("acc2", [128, 3072], F32) for _ in range(2)]
        sqb = [kb.sb("sqd", [128, 2048], F32) for _ in range(2)]
        ssh = kb.sb("sshd", [128, 16], F32)
        rs = kb.sb("rsd", [128, 16], F32)
        t16b = kb.sb("t16b", [128, 16], F32)
        outb = [kb.sb("qkvo", [128, 3072], BF16) for _ in range(2)]
        for n_, t in enumerate(tiles):
            zm = zmb[n_ % 2]
            ob = outb[n_ % 2]
            acc, acc2, sq = accb[n_ % 2], acc2b[n_ % 2], sqb[n_ % 2]
            zb = zbase(t)
            deps = [kb.tag("ZP", tt) for tt in (t - 1, t, t + 1) if 0 <= tt < NT and tt in tiles] + [kb.tag("ZPz", q) for q in range(4)]
            for j in range(3):
                kb.dma("sp", zm[:, j, :], C.ZP[zb - 1 + j:zb + 127 + j, :], r=deps, w=[zm])
            kb.op("dve", lambda e: e.tensor_tensor(out=acc[:, :], in0=zm[:, 0, :], in1=cw[:, 0, :], op=ALU.mult), r=[zm, cw], w=[acc])
            kb.op("dve", lambda e: e.tensor_tensor(out=acc2[:, :], in0=zm[:, 1, :], in1=cw[:, 1, :], op=ALU.mult), r=[zm, cw], w=[acc2])
            kb.op("dve", lambda e: e.tensor_tensor(out=acc[:, :], in0=acc[:, :], in1=acc2[:, :], op=ALU.add), r=[acc, acc2], w=[acc])
            kb.op("dve", lambda e: e.tensor_tensor(out=acc2[:, :], in0=zm[:, 2, :], in1=cw[:, 2, :], op=ALU.mult), r=[zm, cw], w=[acc2])
            kb.op("dve", lambda e: e.tensor_tensor(out=acc[:, :], in0=acc[:, :], in1=acc2[:, :], op=ALU.add), r=[acc, acc2], w=[acc])
            kb.op("act", lambda e: e.activation(out=acc[:, :], in_=acc[:, :], func=AF.Silu), r=[acc], w=[acc])
            kb.op("act", lambda e: e.activation(out=sq[:, :], in_=acc[:, 0:2048], func=AF.Square), r=[acc], w=[sq])
            kb.op("dve", lambda e: e.reduce_sum(out=ssh[:, :], in_=sq[:, :].rearrange("p (h d) -> p h d", h=16), axis=AX.X), r=[sq], w=[ssh])
            rsqrt_mean(C, rs, ssh, 16, 1.0, t16b)
            kb.op("dve", lambda e: e.tensor_scalar(out=rs[:, 0:8], in0=rs[:, 0:8], scalar1=float(128 ** -0.5), scalar2=None, op0=ALU.mult),
                  r=[rs], w=[rs])
            kb.op("dve", lambda e: e.tensor_tensor(out=ob[:, 0:2048].rearrange("p (h d) -> p h d", h=16),
                                                   in0=acc[:, 0:2048].rearrange("p (h d) -> p h d", h=16),
                                                   in1=rs[:, :].unsqueeze(2).to_broadcast([128, 16, 128]), op=ALU.mult), r=[acc, rs], w=[ob])
            kb.op("pool", lambda e: e.tensor_copy(out=ob[:, 2048:3072], in_=acc[:, 2048:3072]), r=[acc], w=[ob])
            kb.dma("sp", C.PA[t * 128:(t + 1) * 128, 0:3072], ob[:, :], r=[ob], w=[kb.tag("PA", t)])


def phaseB_odd(C, l, n_ctx_tiles=2, lat_tiles=None, dirs=(0, 1)):
    kb = C.kb
    ps = C.ps
    if lat_tiles is None:
        lat_tiles = list(range(2, NT))
    with kb.phase():
        dc = kb.sb("dc", [128, 512], F32)
        kb.dma("sp", dc[:, :], C.dconst, w=[dc])
        MGE, MLE, SLT, SGT = [dc[:, q * 128:(q + 1) * 128] for q in range(4)]
        idf = kb.sb("idfb", [128, 128], F32)
        kb.op("dve", lambda e: e.tensor_copy(out=idf[:, :], in_=C.identb_s[:, :]), r=[C.identb_s], w=[idf])
        qkvb = [kb.sb("qkvd", [128, 3072], BF16) for _ in range(2)]
        gbb = [kb.sb("gbd", [128, 32], F32) for _ in range(2)]
        gcs = kb.sb("gcs", [128, 8], F32)
        egc = kb.sb("egc", [128, 8], F32)
        kbt = kb.sb("kbt", [128, 8, 128], BF16)
        vbt = kb.sb("vbt", [128, 8, 128], BF16)
        kbg = kb.sb("kbg", [128, 8, 128], BF16)
        qgt = kb.sb("qgt", [128, 8, 128], BF16)
        kdt = kb.sb("kdt", [128, 8, 128], BF16)
        kds = kb.sb("kds", [128, 8], F32)
        qT = kb.sb("qTd", [128, 8, 128], BF16)
        kT = kb.sb("kTd", [128, 8, 128], BF16)
        qgT = kb.sb("qgT", [128, 8, 128], BF16)
        wT = kb.sb("wTd", [128, 8, 128], BF16)
        attT = kb.sb("attTd", [128, 8, 128], BF16)
        uu = kb.sb("uu", [128, 8, 128], F32)
        glc = kb.sb("glc", [128, 8, 2], F32)
        grep_ = [kb.sb("grep", [128, 128], F32) for _ in range(8)]
        d1 = [kb.sb("d1", [128, 128], F32) for _ in range(8)]
        d2 = [kb.sb("d2", [128, 128], F32) for _ in range(8)]
        e1m = [kb.sb("e1m", [128, 128], F32) for _ in range(8)]
        e2m = [kb.sb("e2m", [128, 128], F32) for _ in range(8)]
        Lb = [[kb.sb("Lb", [128, 128], BF16) for _ in range(2)] for _ in range(8)]
        Ub = [[kb.sb("Ub", [128, 128], BF16) for _ in range(2)] for _ in range(8)]
        ILb = [kb.sb("ILb", [128, 128], BF16) for _ in range(8)]
        Mb = [[kb.sb("Mb", [128, 128], BF16) for _ in range(2)] for _ in range(8)]
        vn = kb.sb("vn", [128, 8, 128], BF16)
        S = kb.sb("Sd", [128, 8, 128], F32)
        Sb = kb.sb("Sbd", [128, 8, 128], BF16)
        osb = [kb.sb("osb", [128, D], F32) for _ in range(2)]
        n_ = 0
        for d_ in dirs:
            order = list(range(n_ctx_tiles)) + list(lat_tiles)
            if d_ == 1:
                order = list(range(n_ctx_tiles))[::-1] + list(lat_tiles)[::-1]
            CUM = MGE if d_ == 0 else MLE
            M_att = MGE if d_ == 0 else MLE
            M_lo = SLT if d_ == 0 else SGT
            chunks = (0, 1) if d_ == 0 else (1, 0)
            flast = (lambda c: c * 64 + 63) if d_ == 0 else (lambda c: c * 64)
            ODST = C.OF if d_ == 0 else C.OB
            otag = "OF" if d_ == 0 else "OB"
            kb.op("dve", lambda e: e.memset(S[:, :, :], 0.0), w=[S])
            kb.op("dve", lambda e: e.memset(Sb[:, :, :], 0.0), w=[Sb])
            for t in order:
                qkv = qkvb[n_ % 2]
                gbt = gbb[n_ % 2]
                ot = osb[n_ % 2]
                n_ += 1
                kb.dma("sp", qkv[:, :], C.PA[t * 128:(t + 1) * 128, 0:3072], r=[kb.tag("PA", t)], w=[qkv])
                kb.dma("sp", gbt[:, :], C.GB[t * 128:(t + 1) * 128, :], r=[kb.tag("GB", t)], w=[gbt])
                gcol = gbt[:, d_ * 8:(d_ + 1) * 8]
                bcol = gbt[:, 16 + d_ * 8:16 + (d_ + 1) * 8]
                pg = ps[6]
                kb.op("pe", lambda e: e.matmul(pg[:, 0:8], lhsT=CUM, rhs=gcol, start=True, stop=True), r=[dc, gbt], w=[pg])
                kb.op("dve", lambda e: e.tensor_copy(out=gcs[:, :], in_=pg[:, 0:8]), r=[pg], w=[gcs])
                kb.op("act", lambda e: e.activation(out=egc[:, :], in_=gcs[:, :], func=AF.Exp), r=[gcs], w=[egc])
                qv = qkv[:, 0:1024].rearrange("p (h d) -> p h d", h=8)
                kv = qkv[:, 1024:2048].rearrange("p (h d) -> p h d", h=8)
                vv = qkv[:, 2048:3072].rearrange("p (h d) -> p h d", h=8)
                bb = bcol.unsqueeze(2).to_broadcast([128, 8, 128])
                eb = egc[:, :].unsqueeze(2).to_broadcast([128, 8, 128])
                kb.op("dve", lambda e: e.tensor_tensor(out=kbt[:, :, :], in0=kv, in1=bb, op=ALU.mult), r=[qkv, gbt], w=[kbt])
                kb.op("pool", lambda e: e.tensor_tensor(out=vbt[:, :, :], in0=vv, in1=bb, op=ALU.mult), r=[qkv, gbt], w=[vbt])
                kb.op("dve", lambda e: e.tensor_tensor(out=kbg[:, :, :], in0=kbt[:, :, :], in1=eb, op=ALU.mult), r=[kbt, egc], w=[kbg])
                kb.op("pool", lambda e: e.tensor_tensor(out=qgt[:, :, :], in0=qv, in1=eb, op=ALU.mult), r=[qkv, egc], w=[qgt])
                for (src_ap, src_tl, dst) in ((qkv[:, 0:1024], qkv, qT), (qkv[:, 1024:2048], qkv, kT), (qgt[:, :, :].rearrange("p h d -> p (h d)"), qgt, qgT)):
                    pt = ps[7]
                    ptv = pt[:, :].bitcast(BF16)
                    for h in range(8):
                        kb.op("pe", lambda e: e.transpose(ptv[:, h * 128:(h + 1) * 128], src_ap[:, h * 128:(h + 1) * 128], C.identb_s[:, :]),
                              r=[src_tl, C.identb_s], w=[pt])
                    kb.op("act", lambda e: e.copy(out=dst[:, :, :].rearrange("p h t -> p (h t)"), in_=ptv[:, :]), r=[pt], w=[dst])
                def reg(h, j):
                    c = h * 3 + j
                    return ps[c // 4], slice((c % 4) * 128, (c % 4 + 1) * 128)
                for h in range(8):
                    kb.op("dve", lambda e: e.tensor_copy(out=grep_[h][:, :], in_=gcol[:, h:h + 1].to_broadcast([128, 128])), r=[gbt], w=[grep_[h]])
                for h in range(8):
                    (b0, c0), (b1, c1), (b2, c2) = reg(h, 0), reg(h, 1), reg(h, 2)
                    kb.op("pe", lambda e: e.matmul(b0[:, c0], lhsT=grep_[h][:, :], rhs=CUM, start=True, stop=True), r=[grep_[h], dc], w=[b0])
                    kb.op("pe", lambda e: e.matmul(b1[:, c1], lhsT=kT[:, h, :], rhs=kT[:, h, :], start=True, stop=True), r=[kT], w=[b1])
                    kb.op("pe", lambda e: e.matmul(b2[:, c2], lhsT=kT[:, h, :], rhs=qT[:, h, :], start=True, stop=True), r=[kT, qT], w=[b2])
                for h in range(8):
                    b0, c0 = reg(h, 0)
                    gch = gcs[:, h:h + 1]
                    kb.op("dve", lambda e: e.tensor_scalar(out=d1[h][:, :], in0=b0[:, c0], scalar1=gch, scalar2=0.0, op0=ALU.subtract, op1=ALU.min),
                          r=[b0, gcs], w=[d1[h]])
                    kb.op("dve", lambda e: e.tensor_scalar(out=d2[h][:, :], in0=b0[:, c0], scalar1=gch, scalar2=0.0, op0=ALU.subtract, op1=ALU.max),
                          r=[b0, gcs], w=[d2[h]])
                for h in range(8):
                    b0, c0 = reg(h, 0)
                    for c in (0, 1):
                        f = flast(c)
                        kb.op("act", lambda e: e.activation(out=glc[:, h, c:c + 1], in_=b0[:, c0.start + f:c0.start + f + 1], func=AF.Exp), r=[b0], w=[glc])
                    kb.op("act", lambda e: e.activation(out=d1[h][:, :], in_=d1[h][:, :], func=AF.Exp), r=[d1[h]], w=[d1[h]])
                    kb.op("act", lambda e: e.activation(out=d2[h][:, :], in_=d2[h][:, :], func=AF.Exp, scale=-1.0), r=[d2[h]], w=[d2[h]])
                f0, f1 = flast(0), flast(1)
                for h in range(8):
                    kb.op("pool", lambda e: e.tensor_tensor(out=e1m[h][:, :], in0=d1[h][:, :], in1=M_att, op=ALU.mult), r=[d1[h], dc], w=[e1m[h]])
                    kb.op("pool", lambda e: e.tensor_tensor(out=e2m[h][:, :], in0=d2[h][:, :], in1=M_lo, op=ALU.mult), r=[d2[h], dc], w=[e2m[h]])
                for h in range(8):
                    (b1, c1), (b2, c2) = reg(h, 1), reg(h, 2)
                    kb.op("dve", lambda e: e.tensor_tensor(out=kds[:, h:h + 1], in0=e1m[h][:, f0:f0 + 1], in1=e1m[h][:, f1:f1 + 1], op=ALU.add),
                          r=[e1m[h]], w=[kds])
                    kb.op("dve", lambda e: e.tensor_tensor(out=attT[:, h, :], in0=b2[:, c2], in1=e1m[h][:, :], op=ALU.mult), r=[b2, e1m[h]], w=[attT])
                    kb.op("dve", lambda e: e.scalar_tensor_tensor(out=Lb[h][0][:, :], in0=b1[:, c1], scalar=bcol[:, h:h + 1], in1=e2m[h][:, :],
                                                                  op0=ALU.mult, op1=ALU.mult), r=[b1, gbt, e2m[h]], w=[Lb[h][0]])
                for h in range(8):
                    b0, c0 = reg(h, 0)
                    utv = b0[:, c0].bitcast(BF16)
                    kb.op("pe", lambda e: e.transpose(utv[:, 0:128], Lb[h][0][:, :], C.identb_s[:, :]), r=[Lb[h][0], C.identb_s], w=[b0])
                for h in range(8):
                    b0, c0 = reg(h, 0)
                    utv = b0[:, c0].bitcast(BF16)
                    kb.op("act", lambda e: e.copy(out=Ub[h][0][:, :], in_=utv[:, 0:128]), r=[b0], w=[Ub[h][0]])
                    kb.op("pool", lambda e: e.tensor_tensor(out=Mb[h][0][:, :], in0=idf[:, :], in1=Ub[h][0][:, :], op=ALU.subtract),
                          r=[idf, Ub[h][0]], w=[Mb[h][0]])
                cur = 0
                for lev in range(5):
                    nxt = 1 - cur
                    for h in range(8):
                        (b0, c0), (b1, c1) = reg(h, 0), reg(h, 1)
                        kb.op("pe", lambda e: e.matmul(b0[:, c0], lhsT=Ub[h][cur][:, :], rhs=Lb[h][cur][:, :], start=True, stop=True),
                              r=[Ub[h][cur], Lb[h][cur]], w=[b0])
                        if lev < 4:
                            kb.op("pe", lambda e: e.matmul(b1[:, c1], lhsT=Lb[h][cur][:, :], rhs=Ub[h][cur][:, :], start=True, stop=True),
                                  r=[Ub[h][cur], Lb[h][cur]], w=[b1])
                    for h in range(8):
                        (b0, c0), (b1, c1) = reg(h, 0), reg(h, 1)
                        kb.op("dve", lambda e: e.tensor_tensor(out=ILb[h][:, :], in0=b0[:, c0], in1=idf[:, :], op=ALU.add), r=[b0, idf], w=[ILb[h]])
                        if lev < 4:
                            kb.op("act", lambda e: e.copy(out=Lb[h][nxt][:, :], in_=b0[:, c0]), r=[b0], w=[Lb[h][nxt]])
                            kb.op("act", lambda e: e.copy(out=Ub[h][nxt][:, :], in_=b1[:, c1]), r=[b1], w=[Ub[h][nxt]])
                    for h in range(8):
                        b2, c2 = reg(h, 2)
                        kb.op("pe", lambda e: e.matmul(b2[:, c2], lhsT=ILb[h][:, :], rhs=Mb[h][cur][:, :], start=True, stop=True),
                              r=[ILb[h], Mb[h][cur]], w=[b2])
                    for h in range(8):
                        b2, c2 = reg(h, 2)
                        kb.op("dve", lambda e: e.tensor_copy(out=Mb[h][nxt][:, :], in_=b2[:, c2]), r=[b2], w=[Mb[h][nxt]])
                    cur = nxt
                for h in range(8):
                    (b0, c0), (b1, c1) = reg(h, 0), reg(h, 1)
                    kb.op("pe", lambda e: e.matmul(b0[:, c0], lhsT=Mb[h][cur][:, :], rhs=vbt[:, h, :], start=True, stop=True), r=[Mb[h][cur], vbt], w=[b0])
                    kb.op("pe", lambda e: e.matmul(b1[:, c1], lhsT=kbg[:, h, :], rhs=Mb[h][cur][:, :], start=True, stop=True), r=[Mb[h][cur], kbg], w=[b1])
                for h in range(8):
                    (b0, c0), (b1, c1) = reg(h, 0), reg(h, 1)
                    kb.op("dve", lambda e: e.tensor_copy(out=uu[:, h, :], in_=b0[:, c0]), r=[b0], w=[uu])
                    kb.op("act", lambda e: e.copy(out=wT[:, h, :], in_=b1[:, c1]), r=[b1], w=[wT])
                kb.op("dve", lambda e: e.tensor_tensor(out=kdt[:, :, :], in0=kv, in1=kds[:, :].unsqueeze(2).to_broadcast([128, 8, 128]), op=ALU.mult),
                      r=[qkv, kds], w=[kdt])
                for c in chunks:
                    r0 = c * 64
                    pW = [ps[6], ps[7]]
                    for h in range(8):
                        kb.op("pe", lambda e: e.matmul(pW[h // 4][:, (h % 4) * 128:(h % 4 + 1) * 128], lhsT=wT[:, h, :], rhs=Sb[:, h, :],
                                                       start=True, stop=True), r=[wT, Sb], w=[pW[h // 4]])
                    for hb in range(2):
                        kb.op("dve", lambda e: e.tensor_tensor(out=vn[r0:r0 + 64, hb * 4:(hb + 1) * 4, :].rearrange("p h d -> p (h d)"),
                                                               in0=uu[r0:r0 + 64, hb * 4:(hb + 1) * 4, :].rearrange("p h d -> p (h d)"),
                                                               in1=pW[hb][r0:r0 + 64, :], op=ALU.subtract), r=[uu, pW[hb]], w=[vn])
                    pO = [ps[4], ps[5]]
                    pK = [ps[2], ps[3]]
                    for h in range(8):
                        oc = (h % 4) * 128
                        kb.op("pe", lambda e: e.matmul(pO[h // 4][:, oc:oc + 128], lhsT=qgT[:, h, :], rhs=Sb[:, h, :], start=True, stop=False),
                              r=[qgT, Sb], w=[pO[h // 4]])
                        kb.op("pe", lambda e: e.matmul(pO[h // 4][:, oc:oc + 128], lhsT=attT[r0:r0 + 64, h, :], rhs=vn[r0:r0 + 64, h, :],
                                                       start=False, stop=True), r=[attT, vn], w=[pO[h // 4]])
                        kb.op("pe", lambda e: e.matmul(pK[h // 4][:, oc:oc + 128], lhsT=kdt[r0:r0 + 64, h, :], rhs=vn[r0:r0 + 64, h, :],
                                                       start=True, stop=True), r=[kdt, vn], w=[pK[h // 4]])
                    for hb in range(2):
                        kb.op("act", lambda e: e.copy(out=ot[r0:r0 + 64, hb * 512:(hb + 1) * 512], in_=pO[hb][r0:r0 + 64, :]), r=[pO[hb]], w=[ot])
                    for h in range(8):
                        oc = (h % 4) * 128
                        kb.op("dve", lambda e: e.scalar_tensor_tensor(out=S[:, h, :], in0=S[:, h, :], scalar=glc[:, h, c:c + 1],
                                                                      in1=pK[h // 4][:, oc:oc + 128], op0=ALU.mult, op1=ALU.add),
                              r=[S, glc, pK[h // 4]], w=[S])
                    kb.op("act", lambda e: e.copy(out=Sb[:, :, :].rearrange("p h d -> p (h d)"), in_=S[:, :, :].rearrange("p h d -> p (h d)")),
                          r=[S], w=[Sb])
                kb.dma("sp", ODST[t * 128:(t + 1) * 128, :], ot[:, :], r=[ot], w=[kb.tag(otag, t)])


def odd_mix_src(C, l):
    kb = C.kb
    i = l // 2
    gn = kb.sb("ogain", [128, 128], F32)
    kb.dma("sp", gn[:, :], C.od_gain[i].partition_broadcast(128), w=[gn])
    ofb = [kb.sb("ofo", [128, D], F32) for _ in range(2)]
    obb = [kb.sb("obo", [128, D], F32) for _ in range(2)]
    zgb = [kb.sb("zgo", [128, D], BF16) for _ in range(2)]
    junk = kb.sb("junko", [128, D], F32)
    ssh = kb.sb("ssho", [128, 8], F32)
    rs8 = kb.sb("rs8o", [128, 8], F32)
    tmp8 = kb.sb("tmp8o", [128, 8], F32)
    mr = kb.sb("mro", [128, D], BF16)

    def src(t, mT, n_):
        of, ob, zg = ofb[n_ % 2], obb[n_ % 2], zgb[n_ % 2]
        kb.dma("sp", of[:, :], C.OF[t * 128:(t + 1) * 128, :], r=[kb.tag("OF", t)], w=[of])
        kb.dma("sp", ob[:, :], C.OB[t * 128:(t + 1) * 128, :], r=[kb.tag("OB", t)], w=[ob])
        kb.dma("sp", zg[:, :], C.PA[t * 128:(t + 1) * 128, 3072:4096], r=[kb.tag("PAz", t)], w=[zg])
        kb.op("pool", lambda e: e.tensor_tensor(out=of[:, :], in0=of[:, :], in1=ob[:, :], op=ALU.add), r=[of, ob], w=[of])
        kb.op("act", lambda e: e.activation(out=junk[:, :], in_=of[:, :], func=AF.Square), r=[of], w=[junk])
        kb.op("dve", lambda e: e.reduce_sum(out=ssh[:, :], in_=junk[:, :].rearrange("p (h d) -> p h d", h=8), axis=AX.X), r=[junk], w=[ssh])
        rsqrt_mean(C, rs8, ssh, 8, 1.0 / 128, tmp8)
        kb.op("dve", lambda e: e.tensor_tensor(out=of[:, :].rearrange("p (h d) -> p h d", h=8), in0=of[:, :].rearrange("p (h d) -> p h d", h=8),
                                               in1=rs8[:, :].unsqueeze(2).to_broadcast([128, 8, 128]), op=ALU.mult), r=[of, rs8], w=[of])
        kb.op("dve", lambda e: e.tensor_tensor(out=of[:, :].rearrange("p (h d) -> p h d", h=8), in0=of[:, :].rearrange("p (h d) -> p h d", h=8),
                                               in1=gn[:, :].unsqueeze(1).to_broadcast([128, 8, 128]), op=ALU.mult), r=[of, gn], w=[of])
        kb.op("pool", lambda e: e.tensor_tensor(out=mr[:, :], in0=of[:, :], in1=zg[:, :], op=ALU.mult), r=[of, zg], w=[mr])
        pT = C.ps[0]
        pv = pT[:, :].bitcast(BF16)
        for k in range(8):
            kb.op("pe", lambda e: e.transpose(pv[:, k * 128:(k + 1) * 128], mr[:, k * 128:(k + 1) * 128], C.identb_s[:, :]),
                  r=[mr, C.identb_s], w=[pT])
        kb.op("act", lambda e: e.copy(out=mT[:, 0:8, :].rearrange("p k t -> p (k t)"), in_=pv[:, :]), r=[pT], w=[mT])
    return src


def final_phase(C, tiles):
    kb = C.kb
    with kb.phase():
        src = combine_src(C, DEPTH - 1, store=False)
        fn = kb.sb("fnrep", [128, D], F32)
        kb.dma("sp", fn[:, :], C.norms[2 * DEPTH].partition_broadcast(128), w=[fn])
        xb = [kb.sb("xf", [128, D], F32) for _ in range(2)]
        ob = [kb.sb("of_", [128, D], F32) for _ in range(2)]
        junk = kb.sb("junkf", [128, D], BF16)
        ss = kb.sb("ssf", [128, 1], F32)
        rstd = kb.sb("rstdf", [128, 1], F32)
        tmp1 = kb.sb("tmp1f", [128, 8], F32)
        for n_, t in enumerate(tiles):
            xt, o = xb[n_ % 2], ob[n_ % 2]
            src(t, xt)
            norm_tile(C, xt, ss, rstd, tmp1, junk, o)
            kb.op("dve", lambda e: e.tensor_tensor(out=o[:, :], in0=o[:, :], in1=fn[:, :], op=ALU.mult), r=[o, fn], w=[o])
            kb.dma("sp", C.out[(t - 2) * 128:(t - 1) * 128, :], o[:, :], r=[o], w=[kb.tag("out", t)])


def build_program():
    nc = bass.Bass("TRN2", target_bir_lowering=False)
    C = Ctx()
    C.nc = nc
    C.kb = KB(nc)
    C.dbg_out = set()
    C.moe_skip = True
    kb = C.kb
    declare_io(nc, C)
    C.out = nc.dram_tensor("out", [L, D], F32, kind="ExternalOutput").ap()
    setup_globals(C)
    setup_consts(C)
    phase0(C)
    all_tiles = list(range(NT))
    for l in range(DEPTH):
        last = l == DEPTH - 1
        if l == 0:
            def x_src(t, xt):
                kb.dma("sp", xt[:, :], C.xin[t * 128:(t + 1) * 128, :], w=[xt])
                kb.dma("sp", C.X[t * 128:(t + 1) * 128, :], xt[:, :], r=[xt], w=[kb.tag("X", t)])
            holder = None
        else:
            holder = kb.phase()
            holder.__enter__()
            x_src = combine_src(C, l - 1)
        if l % 2 == 0:
            phaseA_even(C, l, all_tiles, x_src)
        else:
            phaseA_odd(C, l, all_tiles, x_src)
        if holder is not None:
            holder.__exit__(None, None, None)
        if l % 2 == 0:
            phaseB_ret(C, l)
            phaseC_att(C, l)
            phaseD(C, l, all_tiles, C.ev_w_out[l // 2], 12, lambda: even_mix_src(C), set(all_tiles))
        else:
            phaseB_odd(C, l)
            tiles_d = all_tiles if not last else all_tiles[2:]
            phaseD(C, l, tiles_d, C.od_w_out[l // 2], 8, lambda: odd_mix_src(C, l), set(tiles_d))
        phaseE(C, l)
    final_phase(C, all_tiles[2:])
    kb.barrier()
    return nc


_CACHE = {}


def kernel(**inputs):
    inp = {k: np.asarray(v) for k, v in inputs.items()}
    maps = host_prep(inp)[:NCORES]
    if "nc" not in _CACHE:
        _CACHE["nc"] = build_program()
    res = run_bass_kernel_spmd(_CACHE["nc"], maps, core_ids=list(range(NCORES)))
    out = np.stack([np.asarray(res.results[b]["out"], np.float32) for b in range(4)], axis=0)
    return out


NCORES = 4
```
